# Optimizing a Trainium2 kernel written in Bass

```python
import jax, jax.numpy as jnp
from jax import lax
import numpy as np

D_MODEL = 1024
BATCH = 8
SEQ = 2048
DEPTH = 1

PLE_DIM = 256
LRU_WIDTH = D_MODEL
LRU_BLOCKS = 8
LRU_BLOCK = LRU_WIDTH // LRU_BLOCKS
CONV_WIDTH = 4
LRU_C = 8.0
LRU_A_MIN = 0.9
LRU_A_MAX = 0.999
GLA_HEADS = 4
GLA_DK = D_MODEL // 2 // GLA_HEADS
GLA_DV = D_MODEL // GLA_HEADS
GLA_RANK = 16
GLA_TAU = 16.0
GLA_CHUNK = 64
N_EXPERTS = 32
TOP_K = 4
D_FF = D_MODEL
SWIGLU_LIMIT = 7.0
SWIGLU_ALPHA = 1.702
MOE_BLOCK = 128
LN_EPS = 1e-5
RMS_EPS = 1e-5
DEEPNORM_ALPHA = (2.0 * DEPTH) ** 0.25
DEEPNORM_BETA = (8.0 * DEPTH) ** -0.25

IN_WIDTHS = [LRU_WIDTH, LRU_WIDTH, GLA_HEADS * GLA_DK, GLA_HEADS * GLA_DK,
             GLA_HEADS * GLA_DV, GLA_HEADS * GLA_DV, GLA_RANK, D_MODEL, D_MODEL]
IN_TOTAL = int(sum(IN_WIDTHS))
IN_SPLITS = [int(s) for s in np.cumsum(IN_WIDTHS)[:-1]]

kernel_name = "hybrid_rglru_gla_moe_deepnorm_ple"


def layer_norm(x, g, b):
    xf = x.astype(jnp.float32)
    mu = jnp.mean(xf, axis=-1, keepdims=True)
    var = jnp.mean(jnp.square(xf - mu), axis=-1, keepdims=True)
    return ((xf - mu) * lax.rsqrt(var + LN_EPS) * g + b).astype(x.dtype)


def causal_depthwise_conv(x, w, b):
    s = x.shape[1]
    xp = jnp.pad(x, ((0, 0), (CONV_WIDTH - 1, 0), (0, 0)))
    out = xp[:, 0:s] * w[0]
    for k in range(1, CONV_WIDTH):
        out = out + xp[:, k:k + s] * w[k]
    return out + b


def rg_lru(x, w_r, b_r, w_i, b_i, lam):
    bsz, s, c = x.shape
    xb = x.reshape(bsz, s, LRU_BLOCKS, LRU_BLOCK)
    r = jax.nn.sigmoid(jnp.einsum('bsgc,gcd->bsgd', xb, w_r).reshape(bsz, s, c) + b_r)
    i = jax.nn.sigmoid(jnp.einsum('bsgc,gcd->bsgd', xb, w_i).reshape(bsz, s, c) + b_i)
    log_a = (-LRU_C * r * jax.nn.softplus(-lam)).astype(jnp.float32)
    a = jnp.exp(log_a)
    mult = jnp.sqrt(-jnp.expm1(2.0 * log_a))
    u = mult * (i * x).astype(jnp.float32)

    def combine(left, right):
        a1, b1 = left
        a2, b2 = right
        return a1 * a2, a2 * b1 + b2

    _, h = lax.associative_scan(combine, (a, u), axis=1)
    return h.astype(x.dtype)


def gla_chunked(q, k, v, log_g):
    bsz, s, h, dk = q.shape
    dv = v.shape[-1]
    n = s // GLA_CHUNK

    def to_chunks(t):
        t = t.astype(jnp.float32).reshape(bsz, n, GLA_CHUNK, h, t.shape[-1])
        return t.transpose(1, 0, 3, 2, 4)

    qc = to_chunks(q * (dk ** -0.5))
    kc, vc, gc = to_chunks(k), to_chunks(v), to_chunks(log_g)
    causal = jnp.tril(jnp.ones((GLA_CHUNK, GLA_CHUNK), dtype=bool))

    def step(state, inp):
        qn, kn, vn, gn = inp
        bcum = jnp.cumsum(gn, axis=2)
        o_inter = jnp.einsum('bhik,bhkv->bhiv', qn * jnp.exp(bcum), state)
        diff = bcum[:, :, :, None, :] - bcum[:, :, None, :, :]
        decay = jnp.exp(jnp.where(causal[:, :, None], diff, -jnp.inf))
        scores = jnp.einsum('bhijk,bhjk->bhij', qn[:, :, :, None, :] * decay, kn)
        o_intra = jnp.einsum('bhij,bhjv->bhiv', scores, vn)
        b_last = bcum[:, :, -1:, :]
        k_dec = kn * jnp.exp(b_last - bcum)
        state = (jnp.exp(b_last[:, :, 0, :])[..., None] * state
                 + jnp.einsum('bhjk,bhjv->bhkv', k_dec, vn))
        return state, o_inter + o_intra

    state0 = jnp.zeros((bsz, h, dk, dv), jnp.float32)
    _, o = lax.scan(step, state0, (qc, kc, vc, gc))
    o = o.transpose(1, 0, 3, 2, 4).reshape(bsz, s, h, dv)
    return o


def moe(x, w_router, b_router, w_up, b_up, w_down, b_down):
    bsz, s, d = x.shape
    t = bsz * s
    xt = x.reshape(t, d)
    logits = (xt @ w_router + b_router).astype(jnp.float32)
    top_val, top_idx = lax.top_k(logits, TOP_K)
    gates = jax.nn.softmax(top_val, axis=-1).astype(x.dtype)

    n_assign = t * TOP_K
    expert_flat = top_idx.reshape(n_assign)
    token_flat = (jnp.arange(n_assign, dtype=jnp.int32) // TOP_K).astype(jnp.int32)
    gate_flat = gates.reshape(n_assign)
    order = jnp.argsort(expert_flat)
    sorted_expert = expert_flat[order]
    counts = jnp.bincount(expert_flat, length=N_EXPERTS)
    start = jnp.cumsum(counts) - counts
    padded_counts = (counts + MOE_BLOCK - 1) // MOE_BLOCK * MOE_BLOCK
    padded_end = jnp.cumsum(padded_counts)
    padded_start = padded_end - padded_counts
    slot = padded_start[sorted_expert] + jnp.arange(n_assign) - start[sorted_expert]

    n_slots = n_assign + N_EXPERTS * MOE_BLOCK
    n_blocks = n_slots // MOE_BLOCK
    slot_token = jnp.full((n_slots,), t, jnp.int32).at[slot].set(token_flat[order])
    slot_gate = jnp.zeros((n_slots,), x.dtype).at[slot].set(gate_flat[order])
    block_expert = jnp.minimum(
        jnp.searchsorted(padded_end, jnp.arange(n_blocks) * MOE_BLOCK, side='right'),
        N_EXPERTS - 1)
    x_pad = jnp.concatenate([xt, jnp.zeros((1, d), xt.dtype)], axis=0)

    def expert_block(args):
        tok, g, e = args
        xb = x_pad[tok]
        hdn = xb @ w_up[e] + b_up[e]
        gate_h = jnp.minimum(hdn[:, :D_FF], SWIGLU_LIMIT)
        up_h = jnp.clip(hdn[:, D_FF:], -SWIGLU_LIMIT, SWIGLU_LIMIT)
        act = (up_h + 1.0) * gate_h * jax.nn.sigmoid(SWIGLU_ALPHA * gate_h)
        y = act @ w_down[e] + b_down[e]
        return y * g[:, None]

    y_slots = lax.map(expert_block, (slot_token.reshape(n_blocks, MOE_BLOCK),
                                     slot_gate.reshape(n_blocks, MOE_BLOCK),
                                     block_expert))
    out = jnp.zeros((t + 1, d), y_slots.dtype).at[slot_token].add(y_slots.reshape(n_slots, d))
    return out[:t].reshape(bsz, s, d)


def setup_inputs(seed: int = 0) -> dict:
    key = jax.random.key(seed)
    ks = jax.random.split(key, 27)
    f32 = jnp.float32
    L = DEPTH

    def nrm(k, shape, scale):
        return jax.random.normal(k, shape, f32) * scale

    u = jax.random.uniform(ks[9], (L, LRU_WIDTH), f32, LRU_A_MIN, LRU_A_MAX)
    s_base = u ** (1.0 / LRU_C)
    lru_lambda = jnp.log(s_base) - jnp.log1p(-s_base)
    return {
        "x": nrm(ks[0], (BATCH, SEQ, D_MODEL), 1.0),
        "p": nrm(ks[1], (DEPTH, BATCH, SEQ, PLE_DIM), 1.0),
        "w_in": nrm(ks[2], (L, D_MODEL, IN_TOTAL), D_MODEL ** -0.5),
        "conv_w": nrm(ks[3], (L, CONV_WIDTH, LRU_WIDTH), CONV_WIDTH ** -0.5),
        "conv_b": nrm(ks[4], (L, LRU_WIDTH), 0.01),
        "lru_w_r": nrm(ks[5], (L, LRU_BLOCKS, LRU_BLOCK, LRU_BLOCK), LRU_BLOCK ** -0.5),
        "lru_b_r": nrm(ks[6], (L, LRU_WIDTH), 0.01),
        "lru_w_i": nrm(ks[7], (L, LRU_BLOCKS, LRU_BLOCK, LRU_BLOCK), LRU_BLOCK ** -0.5),
        "lru_b_i": nrm(ks[8], (L, LRU_WIDTH), 0.01),
        "lru_lambda": lru_lambda,
        "gla_w_gate": nrm(ks[10], (L, GLA_RANK, GLA_HEADS * GLA_DK), GLA_RANK ** -0.5),
        "gla_b_gate": nrm(ks[11], (L, GLA_HEADS * GLA_DK), 0.01),
        "gla_norm_g": 1.0 + nrm(ks[12], (L, GLA_DV), 0.02),
        "w_out": nrm(ks[13], (L, D_MODEL, D_MODEL), DEEPNORM_BETA * D_MODEL ** -0.5),
        "ln1_g": 1.0 + nrm(ks[14], (L, D_MODEL), 0.02),
        "ln1_b": nrm(ks[15], (L, D_MODEL), 0.01),
        "w_router": nrm(ks[16], (L, D_MODEL, N_EXPERTS), D_MODEL ** -0.5),
        "b_router": nrm(ks[17], (L, N_EXPERTS), 0.01),
        "w_up": nrm(ks[18], (L, N_EXPERTS, D_MODEL, 2 * D_FF), D_MODEL ** -0.5),
        "b_up": nrm(ks[19], (L, N_EXPERTS, 2 * D_FF), 0.01),
        "w_down": nrm(ks[20], (L, N_EXPERTS, D_FF, D_MODEL), DEEPNORM_BETA * D_FF ** -0.5),
        "b_down": nrm(ks[21], (L, N_EXPERTS, D_MODEL), 0.01),
        "ln2_g": 1.0 + nrm(ks[22], (L, D_MODEL), 0.02),
        "ln2_b": nrm(ks[23], (L, D_MODEL), 0.01),
        "w_ple": nrm(ks[24], (L, PLE_DIM, D_MODEL), PLE_DIM ** -0.5),
        "w_ple_gate": nrm(ks[25], (L, D_MODEL, D_MODEL), D_MODEL ** -0.5),
        "b_ple_gate": nrm(ks[26], (L, D_MODEL), 0.01),
    }


def reference(x, p, w_in, conv_w, conv_b, lru_w_r, lru_b_r, lru_w_i, lru_b_i, lru_lambda,
              gla_w_gate, gla_b_gate, gla_norm_g, w_out, ln1_g, ln1_b,
              w_router, b_router, w_up, b_up, w_down, b_down, ln2_g, ln2_b,
              w_ple, w_ple_gate, b_ple_gate):
    bsz, s, d = x.shape
    h = x
    for i in range(DEPTH):
        proj = h @ w_in[i]
        xa, ga, q, k, v, go, glr, ma, mb = jnp.split(proj, IN_SPLITS, axis=-1)

        xa = causal_depthwise_conv(xa, conv_w[i], conv_b[i])
        ya = rg_lru(xa, lru_w_r[i], lru_b_r[i], lru_w_i[i], lru_b_i[i], lru_lambda[i])
        ya = ya * jax.nn.gelu(ga)

        log_g = jax.nn.log_sigmoid(glr @ gla_w_gate[i] + gla_b_gate[i]) / GLA_TAU
        o = gla_chunked(q.reshape(bsz, s, GLA_HEADS, GLA_DK),
                        k.reshape(bsz, s, GLA_HEADS, GLA_DK),
                        v.reshape(bsz, s, GLA_HEADS, GLA_DV),
                        log_g.reshape(bsz, s, GLA_HEADS, GLA_DK))
        o = o * lax.rsqrt(jnp.mean(jnp.square(o), axis=-1, keepdims=True) + RMS_EPS) * gla_norm_g[i]
        yb = o.reshape(bsz, s, GLA_HEADS * GLA_DV).astype(h.dtype) * jax.nn.silu(go)

        y = jax.nn.sigmoid(ma) * ya + jax.nn.sigmoid(mb) * yb
        h = layer_norm(DEEPNORM_ALPHA * h + y @ w_out[i], ln1_g[i], ln1_b[i])

        m = moe(h, w_router[i], b_router[i], w_up[i], b_up[i], w_down[i], b_down[i])
        h = layer_norm(DEEPNORM_ALPHA * h + m, ln2_g[i], ln2_b[i])

        h = h + jax.nn.sigmoid(h @ w_ple_gate[i] + b_ple_gate[i]) * (p[i] @ w_ple[i])
    return h
```

```python
import numpy as np
from contextlib import ExitStack
import concourse.bass as bass
import concourse.mybir as mybir
from concourse.bass_utils import run_bass_kernel_spmd

F32 = mybir.dt.float32
BF16 = mybir.dt.bfloat16
U32 = mybir.dt.uint32
AF = mybir.ActivationFunctionType
ALU = mybir.AluOpType
AX = mybir.AxisListType

T = 2048
D = 1024
NE = 32
CAP = 512
NSLOT = NE * CAP
ALPHA = 2.0 ** 0.25
N_IN = 7184
O_XA, O_GA, O_Q, O_K, O_V, O_GO, O_GLR, O_MA, O_MB = 0, 1024, 2048, 2560, 3072, 4096, 5120, 5136, 6160


class Buf:
    __slots__ = ("name", "w", "r")

    def __init__(self, name):
        self.name = name
        self.w = {}
        self.r = {}


class KB:
    def __init__(self, nc, es, n_dma_sems=24):
        self.nc = nc
        self.eng = dict(pe=nc.tensor, act=nc.scalar, dve=nc.vector, pool=nc.gpsimd, sp=nc.sync)
        self.esem = {}
        self.ecnt = {}
        self.seen = {}
        self.semobj = {}
        for n in self.eng:
            s = es.enter_context(nc.semaphore("es_" + n))
            self.esem[n] = s
            self.semobj[id(s)] = s
            self.ecnt[n] = 0
            self.seen[n] = {}
        self.dsem = []
        self.dcnt = []
        for i in range(2 * n_dma_sems):
            s = es.enter_context(nc.semaphore("ds_%d" % i))
            self.dsem.append(s)
            self.semobj[id(s)] = s
            self.dcnt.append(0)
        self.nds = n_dma_sems
        self.drr = {"pool": 0, "hw": 0}

    def _wait(self, en, toks):
        e = self.eng[en]
        seen = self.seen[en]
        for sid, val in toks.items():
            if en == "pe" and sid == id(self.esem["pe"]):
                continue
            if seen.get(sid, 0) >= val:
                continue
            e.wait_ge(self.semobj[sid], val)
            seen[sid] = val

    @staticmethod
    def _merge(dst, src):
        for k, v in src.items():
            if dst.get(k, 0) < v:
                dst[k] = v

    def _deps(self, reads, writes):
        toks = {}
        for b in reads:
            self._merge(toks, b.w)
        for b in writes:
            self._merge(toks, b.w)
            self._merge(toks, b.r)
        return toks

    def _commit(self, tok, reads, writes):
        for b in reads:
            self._merge(b.r, tok)
        for b in writes:
            b.w = dict(tok)
            b.r = {}

    disabled = False

    def op(self, en, fn, reads=(), writes=(), inc=True):
        if self.disabled:
            return None
        self._wait(en, self._deps(reads, writes))
        ins = fn(self.eng[en])
        s = self.esem[en]
        if inc:
            self.ecnt[en] += 1
            ins.then_inc(s, 1)
            tok = {id(s): self.ecnt[en]}
        else:
            tok = {id(s): self.ecnt[en] + 1}
        self._commit(tok, reads, writes)
        return ins

    def dma(self, en, fn, reads=(), writes=()):
        if self.disabled:
            return None
        kind = "pool" if en == "pool" else "hw"
        i = self.drr[kind] + (self.nds if kind == "pool" else 0)
        self.drr[kind] = (self.drr[kind] + 1) % self.nds
        s = self.dsem[i]
        toks = self._deps(reads, writes)
        if self.dcnt[i] > 0:
            self._merge(toks, {id(s): self.dcnt[i]})
        self._wait(en, toks)
        ins = fn(self.eng[en])
        self.dcnt[i] += 16
        ins.then_inc(s, 16)
        tok = {id(s): self.dcnt[i]}
        self._commit(tok, reads, writes)
        return ins

    def all_tokens(self):
        toks = {}
        for n in self.eng:
            if self.ecnt[n] > 0:
                toks[id(self.esem[n])] = self.ecnt[n]
        for i, s in enumerate(self.dsem):
            if self.dcnt[i] > 0:
                toks[id(s)] = self.dcnt[i]
        return toks

    def barrier(self, engines=None):
        toks = self.all_tokens()
        for n in (engines or list(self.eng)):
            own = id(self.esem[n])
            t = {k: v for k, v in toks.items() if not (n == "pe" and k == own)}
            self._wait(n, t)


def _consts():
    c = np.zeros((128, 5 * 128 + 64 + 4), np.float32)
    j = np.arange(128)
    same = (j[:, None] // 64) == (j[None, :] // 64)
    c[:, 0:128] = np.eye(128, dtype=np.float32)
    c[:, 128:256] = (same & (j[:, None] <= j[None, :]))
    c[:, 256:384] = (same & (j[:, None] > j[None, :]))
    c[:, 384:512] = (j[:, None] < j[None, :])
    c[:, 512:640] = 1.0
    c[:, 640:672] = np.arange(32)[None, :]
    c[:, 672:704] = (np.arange(32) * CAP)[None, :]
    c[:, 704] = (j < 64)
    c[:, 705] = (j >= 64)
    return c


def prep_shared(inp):
    f = lambda a: np.ascontiguousarray(a, dtype=np.float32)
    w_in = inp["w_in"][0]
    cols = np.concatenate([np.arange(0, O_GLR), np.arange(O_MA, N_IN)])
    wm = w_in[:, cols]
    sh = {}
    sh["w_in_t"] = f(wm.reshape(8, 128, 56, 128).transpose(2, 1, 0, 3).reshape(56, 128, 1024))
    sh["w_glr"] = f(w_in[:, O_GLR:O_GLR + 16].reshape(8, 128, 16).transpose(1, 0, 2).reshape(128, 128))
    chan = np.concatenate([inp["conv_w"][0], inp["conv_b"], inp["lru_b_r"], inp["lru_b_i"],
                           inp["lru_lambda"]], axis=0)
    sh["chanp"] = f(chan.reshape(8, 8, 128).transpose(2, 1, 0).reshape(128, 64))
    sh["lru_wr"] = f(inp["lru_w_r"][0].transpose(1, 0, 2).reshape(128, 1024))
    sh["lru_wi"] = f(inp["lru_w_i"][0].transpose(1, 0, 2).reshape(128, 1024))
    sh["gla_wg"] = f(inp["gla_w_gate"][0])
    sh["gla_bg"] = f(inp["gla_b_gate"])
    sh["gla_ng"] = f(inp["gla_norm_g"][0].reshape(2, 128).T)
    sh["w_out"] = f(inp["w_out"][0].reshape(8, 128, 1024).transpose(1, 0, 2).reshape(128, 8192))
    rows = np.concatenate([inp["ln1_g"], inp["ln1_b"], inp["ln2_g"], inp["ln2_b"],
                           inp["b_ple_gate"]], axis=0)
    sh["rowp"] = f(np.broadcast_to(rows.reshape(1, 5 * 1024), (128, 5 * 1024)))
    sh["w_router"] = f(inp["w_router"][0].reshape(8, 128, 32).transpose(1, 0, 2).reshape(128, 256))
    sh["b_router"] = f(inp["b_router"])
    sh["w_up"] = inp["w_up"][0]
    sh["b_up"] = f(inp["b_up"][0].reshape(32, 16, 128).transpose(2, 0, 1).reshape(128, 512))
    sh["w_down"] = inp["w_down"][0]
    sh["b_down"] = f(inp["b_down"][0])
    sh["w_ple"] = f(inp["w_ple"][0].reshape(2, 128, 1024).transpose(1, 0, 2).reshape(128, 2048))
    sh["w_pg"] = f(inp["w_ple_gate"][0].reshape(8, 128, 1024).transpose(1, 0, 2).reshape(128, 8192))
    sh["consts"] = _consts()
    return sh


def prep_core(inp, b):
    x = np.asarray(inp["x"][b], dtype=np.float32)
    p = np.asarray(inp["p"][0, b], dtype=np.float32)
    return {"x": np.ascontiguousarray(x), "xT": np.ascontiguousarray(x.T),
            "pT": np.ascontiguousarray(p.T)}


SHARED_SHAPES = {
    "w_in_t": [56, 128, 1024], "w_glr": [128, 128], "chanp": [128, 64], "lru_wr": [128, 1024],
    "lru_wi": [128, 1024], "gla_wg": [16, 512], "gla_bg": [1, 512], "gla_ng": [128, 2],
    "w_out": [128, 8192], "rowp": [128, 5120], "w_router": [128, 256], "b_router": [1, 32],
    "w_up": [32, 1024, 2048], "b_up": [128, 512], "w_down": [32, 1024, 1024], "b_down": [32, 1024],
    "w_ple": [128, 2048], "w_pg": [128, 8192], "consts": [128, 708],
}
CORE_SHAPES = {"x": [T, D], "xT": [D, T], "pT": [256, T]}


class StopBuild(Exception):
    pass


class Prog:
    ck_n = 0
    ck_stop = None

    def ck(self, label=""):
        self.ck_n += 1
        if self.ck_stop is not None and self.ck_n >= self.ck_stop:
            if not self.kb.disabled:
                print("STOP at checkpoint", self.ck_n, label)
            self.kb.disabled = True

    def __init__(self, dbg=(), stop_after=None):
        self.dbg = set(dbg)
        self.stop_after = stop_after
        self.nc = nc = bass.Bass("TRN2", target_bir_lowering=False)
        self.din = {}
        for n, s in list(SHARED_SHAPES.items()) + list(CORE_SHAPES.items()):
            self.din[n] = nc.dram_tensor(n, s, F32, kind="ExternalInput").ap()
        self.out = nc.dram_tensor("out", [T, D], F32, kind="ExternalOutput").ap()
        self.dbg_out = {}
        self.es = ExitStack()

    def dbg_tensor(self, name, shape):
        t = self.nc.dram_tensor("dbg_" + name, shape, F32, kind="ExternalOutput").ap()
        self.dbg_out[name] = t
        return t

    def sb(self, es, name, shape, dt=F32):
        return es.enter_context(self.nc.sbuf_tensor(name, shape, dt))

    def build(self):
        nc = self.nc
        with self.es as es:
            kb = self.kb = KB(nc, es)
            self.bound_reg = nc.gpsimd.to_reg(NSLOT - 1)
            self.cst = self.sb(es, "cst", [128, 708])
            self.b_cst = Buf("cst")
            kb.dma("sp", lambda e: e.dma_start(out=self.cst[:], in_=self.din["consts"][:, :]),
                   writes=[self.b_cst])
            self.cstb = self.sb(es, "cstb", [128, 708], BF16)
            self.b_cstb = Buf("cstb")
            kb.op("dve", lambda e: e.tensor_copy(self.cstb[:], self.cst[:]),
                  reads=[self.b_cst], writes=[self.b_cstb])
            self.GATES = self.sb(es, "GATES", [128, 16, 4])
            self.SLOTS = self.sb(es, "SLOTS", [128, 16, 4], U32)
            self.b_route = [Buf("route%d" % i) for i in range(16)]
            self.rowp = self.sb(es, "rowp_sb", [128, 5, 1024])
            self.b_rowp = Buf("rowp")
            kb.dma("sp", lambda e: e.dma_start(out=self.rowp[:], in_=self.din["rowp"].rearrange("p (r d) -> p r d", d=1024)),
                   writes=[self.b_rowp])
            self.Xg = nc.dram_tensor("Xg", [NSLOT, D], BF16).ap()
            self.Yg = nc.dram_tensor("Yg", [NSLOT, D], F32).ap()
            self.H1d = nc.dram_tensor("H1d", [T, D], F32).ap()
            self.b_Xg, self.b_Yg, self.b_H1d = Buf("Xg"), Buf("Yg"), Buf("H1d")
            self.zero_xg(es)
            with ExitStack() as es_y:
                self.yT = self.sb(es_y, "yT", [128, 8, T], BF16)
                self.b_yT = [Buf("yT%d" % g) for g in range(8)]
                with ExitStack() as es1:
                    self.alloc_psum(es1, 8, 0)
                    self.XT = self.sb(es1, "XT", [128, 8, T], BF16)
                    self.b_XT = Buf("XT")
                    xT = self.din["xT"].rearrange("(kc p) t -> p kc t", p=128)
                    for kc in range(8):
                        kb.dma("pool", lambda e, kc=kc: e.dma_start(out=self.XT[:, kc, :], in_=xT[:, kc, :]),
                               writes=[self.b_XT])
                    if not getattr(self, "skip_lru", False):
                        self.phase_lru(es1)
                    else:
                        kb.op("dve", lambda e: e.memset(self.yT[:], 0.0), writes=self.b_yT)
                    if self.stop_after == "lru":
                        return self.finish()
                    kb.barrier()
                    self.phase_gla(es1)
                    if self.stop_after == "gla":
                        return self.dump_yT()
                kb.barrier()
                self.phase_outproj()
                if self.stop_after == "outproj":
                    return self.finish()
            kb.barrier()
            self.phase_moe()
            if self.stop_after == "moe":
                return self.finish()
            kb.barrier()
            self.phase_final()
        return self.finish()

    def alloc_psum(self, es, n32, n16):
        nc = self.nc
        self.ps = [es.enter_context(nc.psum_tensor("ps%d_%d" % (i, self.ps_gen), [128, 512], F32)) for i in range(n32)]
        self.psb = [Buf("ps%d" % i) for i in range(n32)]
        self.pst = [es.enter_context(nc.psum_tensor("pst%d_%d" % (i, self.ps_gen), [128, 1024], BF16)) for i in range(n16)]
        self.pstb = [Buf("pst%d" % i) for i in range(n16)]
        self.ps_rr = 0
        self.pst_rr = 0
        self.ps_gen += 1

    def zero_xg(self, es):
        kb = self.kb
        z = self.sb(es, "zeros", [128, 4, 1024], BF16)
        bz = Buf("zeros")
        kb.op("dve", lambda e: e.memset(z[:], 0.0), writes=[bz])
        xg = self.Xg.rearrange("(n p) d -> p n d", p=128)
        for i in range(NSLOT // 128 // 4):
            kb.dma("sp", lambda e, i=i: e.dma_start(out=xg[:, i * 4:(i + 1) * 4, :], in_=z[:]), reads=[bz], writes=[self.b_Xg])

    ps_gen = 0

    def bank(self):
        i = self.ps_rr
        self.ps_rr = (self.ps_rr + 1) % len(self.ps)
        return self.ps[i], self.psb[i]

    def bank16(self):
        i = self.pst_rr
        self.pst_rr = (self.pst_rr + 1) % len(self.pst)
        return self.pst[i], self.pstb[i]

    def finish(self):
        kb = self.kb
        kb.disabled = False
        kb.barrier(["sp"])
        return self.nc

    def dump_yT(self):
        kb = self.kb
        kb.barrier()
        with ExitStack() as es:
            tmp = self.sb(es, "dump_tmp", [128, 8, T])
            b = Buf("dump_tmp")
            kb.op("dve", lambda e: e.tensor_copy(tmp[:], self.yT[:]), reads=self.b_yT, writes=[b])
            t = self.dbg_tensor("yT", [128, 8, T])
            kb.dma("sp", lambda e: e.dma_start(out=t, in_=tmp[:]), reads=[b])
            return self.finish()

    def dump(self, name, sb_ap, buf, shape):
        if name not in self.dbg:
            return
        t = self.dbg_tensor(name, shape)
        self.kb.dma("sp", lambda e: e.dma_start(out=t, in_=sb_ap), reads=[buf])

    def inproj_fm(self, wt, wb, ncols, tg, evac):
        kb = self.kb
        ps, pb = self.bank()
        for kc in range(8):
            kb.op("pe", lambda e, kc=kc: e.matmul(ps[0:ncols, :], wt[:, kc * ncols:(kc + 1) * ncols],
                                                   self.XT[:, kc, tg * 512:(tg + 1) * 512],
                                                   start=(kc == 0), stop=(kc == 7)),
                  reads=[wb, self.b_XT], writes=[pb], inc=(kc == 7))
        evac(ps, pb)

    def load_w(self, grp):
        i = self.w_rr
        self.w_rr = (self.w_rr + 1) % len(self.wring)
        wt, wb = self.wring[i], self.wringb[i]
        self.kb.dma("pool", lambda e: e.dma_start(out=wt[:], in_=self.din["w_in_t"][grp, :, :]), writes=[wb])
        return wt, wb

    def phase_lru(self, es1):
        nc, kb = self.nc, self.kb
        with ExitStack() as es:
            NW = 6
            self.wring = [self.sb(es, "wr%d" % i, [128, 1024], BF16) for i in range(NW)]
            self.wringb = [Buf("wr%d" % i) for i in range(NW)]
            self.w_rr = 0
            chan = self.sb(es, "chan", [128, 8, 8])
            b_chan = Buf("chan")
            kb.dma("sp", lambda e: e.dma_start(out=chan[:], in_=self.din["chanp"].rearrange("p (g k) -> p g k", k=8)),
                   writes=[b_chan])
            wr = self.sb(es, "lwr", [128, 8, 128], BF16)
            wi = self.sb(es, "lwi", [128, 8, 128], BF16)
            b_wr, b_wi = Buf("lwr"), Buf("lwi")
            kb.dma("pool", lambda e: e.dma_start(out=wr[:], in_=self.din["lru_wr"].rearrange("p (g d) -> p g d", d=128)), writes=[b_wr])
            kb.dma("pool", lambda e: e.dma_start(out=wi[:], in_=self.din["lru_wi"].rearrange("p (g d) -> p g d", d=128)), writes=[b_wi])
            sc = self.sb(es, "lsc", [128, 8, 4])
            b_sc = Buf("lsc")
            kb.op("act", lambda e: e.activation(out=sc[:, :, 0], in_=chan[:, :, 7], func=AF.Exp, scale=-1.0),
                  reads=[b_chan], writes=[b_sc])
            kb.op("act", lambda e: e.activation(out=sc[:, :, 1], in_=sc[:, :, 0], func=AF.Ln, bias=1.0),
                  reads=[b_sc], writes=[b_sc])
            kb.op("dve", lambda e: e.tensor_scalar_mul(sc[:, :, 2], sc[:, :, 1], -8.0), reads=[b_sc], writes=[b_sc])
            kb.op("dve", lambda e: e.tensor_scalar_mul(sc[:, :, 3], sc[:, :, 1], -16.0), reads=[b_sc], writes=[b_sc])

            names = ["xa", "xc", "r", "i", "a", "m", "u", "h", "ga", "t1", "t2"]
            A = {n: self.sb(es, "L_" + n, [128, T + (3 if n == "xa" else 0)]) for n in names}
            B = {n: Buf("L_" + n) for n in names}
            xcb = self.sb(es, "L_xcb", [128, T], BF16)
            b_xcb = Buf("L_xcb")
            kb.op("dve", lambda e: e.memset(A["xa"][:, 0:3], 0.0), writes=[B["xa"]])

            for g in range(8):
                w_xa, wb_xa = self.load_w(g)
                w_ga, wb_ga = self.load_w(8 + g)
                w_ma, wb_ma = self.load_w(40 + g)
                for tg in range(4):
                    self.inproj_fm(w_xa, wb_xa, 128, tg, lambda ps, pb, tg=tg: kb.op(
                        "act", lambda e: e.activation(out=A["xa"][:, 3 + tg * 512:3 + (tg + 1) * 512], in_=ps[:, :], func=AF.Copy),
                        reads=[pb], writes=[B["xa"]]))
                kb.op("dve", lambda e: e.tensor_scalar(A["xc"][:, :], A["xa"][:, 0:T], chan[:, g, 0:1], chan[:, g, 4:5],
                                                       ALU.mult, ALU.add),
                      reads=[B["xa"], b_chan], writes=[B["xc"]])
                for k in range(1, 4):
                    kb.op("dve", lambda e, k=k: e.scalar_tensor_tensor(A["xc"][:, :], A["xa"][:, k:k + T], chan[:, g, k:k + 1],
                                                                      A["xc"][:, :], ALU.mult, ALU.add),
                          reads=[B["xa"], B["xc"], b_chan], writes=[B["xc"]])
                kb.op("act", lambda e: e.activation(out=xcb[:, :], in_=A["xc"][:, :], func=AF.Copy),
                      reads=[B["xc"]], writes=[b_xcb])
                self.dump("xc%d" % g, A["xc"][:, :], B["xc"], [128, T])
                for (wg, bwg, dst, bi) in ((wr, b_wr, "r", 5), (wi, b_wi, "i", 6)):
                    for tg in range(4):
                        ps, pb = self.bank()
                        kb.op("pe", lambda e, tg=tg, ps=ps, wg=wg: e.matmul(ps[:, :], wg[:, g, :], xcb[:, tg * 512:(tg + 1) * 512],
                                                                          start=True, stop=True),
                              reads=[bwg, b_xcb], writes=[pb])
                        kb.op("act", lambda e, tg=tg, ps=ps, dst=dst, bi=bi: e.activation(
                            out=A[dst][:, tg * 512:(tg + 1) * 512], in_=ps[:, :], func=AF.Sigmoid, bias=chan[:, g, bi:bi + 1]),
                            reads=[pb, b_chan], writes=[B[dst]])
                kb.op("act", lambda e: e.activation(out=A["a"][:, :], in_=A["r"][:, :], func=AF.Exp, scale=sc[:, g, 2:3]),
                      reads=[B["r"], b_sc], writes=[B["a"]])
                kb.op("act", lambda e: e.activation(out=A["m"][:, :], in_=A["r"][:, :], func=AF.Exp, scale=sc[:, g, 3:4]),
                      reads=[B["r"], b_sc], writes=[B["m"]])
                kb.op("act", lambda e: e.activation(out=A["m"][:, :], in_=A["m"][:, :], func=AF.Sqrt, scale=-1.0, bias=1.0),
                      reads=[B["m"]], writes=[B["m"]])
                kb.op("dve", lambda e: e.tensor_tensor(A["u"][:, :], A["m"][:, :], A["i"][:, :], ALU.mult),
                      reads=[B["m"], B["i"]], writes=[B["u"]])
                kb.op("dve", lambda e: e.tensor_tensor(A["u"][:, :], A["u"][:, :], A["xc"][:, :], ALU.mult),
                      reads=[B["u"], B["xc"]], writes=[B["u"]])
                kb.op("dve", lambda e: e.tensor_tensor_scan(A["h"][:, :], A["a"][:, :], A["u"][:, :], 0.0, ALU.mult, ALU.add),
                      reads=[B["a"], B["u"]], writes=[B["h"]])
                self.dump("h%d" % g, A["h"][:, :], B["h"], [128, T])
                for tg in range(4):
                    self.inproj_fm(w_ga, wb_ga, 128, tg, lambda ps, pb, tg=tg: kb.op(
                        "act", lambda e: e.activation(out=A["ga"][:, tg * 512:(tg + 1) * 512], in_=ps[:, :], func=AF.Copy),
                        reads=[pb], writes=[B["ga"]]))
                kb.op("dve", lambda e: e.tensor_tensor(A["t1"][:, :], A["ga"][:, :], A["ga"][:, :], ALU.mult),
                      reads=[B["ga"]], writes=[B["t1"]])
                kb.op("dve", lambda e: e.tensor_scalar(A["t1"][:, :], A["t1"][:, :], 0.044715, 1.0, ALU.mult, ALU.add),
                      reads=[B["t1"]], writes=[B["t1"]])
                kb.op("dve", lambda e: e.tensor_tensor(A["t1"][:, :], A["t1"][:, :], A["ga"][:, :], ALU.mult),
                      reads=[B["t1"], B["ga"]], writes=[B["t1"]])
                kb.op("act", lambda e: e.activation(out=A["t1"][:, :], in_=A["t1"][:, :], func=AF.Sigmoid, scale=1.5957691216057308),
                      reads=[B["t1"]], writes=[B["t1"]])
                kb.op("dve", lambda e: e.tensor_tensor(A["t1"][:, :], A["t1"][:, :], A["ga"][:, :], ALU.mult),
                      reads=[B["t1"], B["ga"]], writes=[B["t1"]])
                kb.op("dve", lambda e: e.tensor_tensor(A["t1"][:, :], A["t1"][:, :], A["h"][:, :], ALU.mult),
                      reads=[B["t1"], B["h"]], writes=[B["t1"]])
                for tg in range(4):
                    self.inproj_fm(w_ma, wb_ma, 128, tg, lambda ps, pb, tg=tg: kb.op(
                        "act", lambda e: e.activation(out=A["t2"][:, tg * 512:(tg + 1) * 512], in_=ps[:, :], func=AF.Sigmoid),
                        reads=[pb], writes=[B["t2"]]))
                kb.op("dve", lambda e: e.tensor_tensor(self.yT[:, g, :], A["t1"][:, :], A["t2"][:, :], ALU.mult),
                      reads=[B["t1"], B["t2"]], writes=[self.b_yT[g]])
                self.dump("ya%d" % g, A["t1"][:, :], B["t1"], [128, T])

    def phase_gla(self, es1):
        nc, kb = self.nc, self.kb
        cst = self.cstb
        TRI, UU, ONES = cst[:, 128:256], cst[:, 256:384], cst[:, 512:640]
        TRI32 = self.cst[:, 128:256]
        bc = self.b_cstb
        with ExitStack() as es:
            NW = 8
            self.wring = [self.sb(es, "gw%d" % i, [128, 1024], BF16) for i in range(NW)]
            self.wringb = [Buf("gw%d" % i) for i in range(NW)]
            self.w_rr = 0
            wglr = self.sb(es, "wglr", [128, 128], BF16)
            b_wglr = Buf("wglr")
            kb.dma("pool", lambda e: e.dma_start(out=wglr[:], in_=self.din["w_glr"][:, :]), writes=[b_wglr])
            wg = self.sb(es, "wg", [16, 512], BF16)
            bg = self.sb(es, "bg", [1, 512], BF16)
            ng = self.sb(es, "ng", [128, 2])
            b_wg, b_bg, b_ng = Buf("wg"), Buf("bg"), Buf("ng")
            kb.dma("pool", lambda e: e.dma_start(out=wg[:], in_=self.din["gla_wg"][:, :]), writes=[b_wg])
            kb.dma("pool", lambda e: e.dma_start(out=bg[:], in_=self.din["gla_bg"][:, :]), writes=[b_bg])
            kb.dma("sp", lambda e: e.dma_start(out=ng[:], in_=self.din["gla_ng"][:, :]), writes=[b_ng])
            glrT = self.sb(es, "glrT", [16, T], BF16)
            b_glrT = Buf("glrT")
            for tg in range(4):
                self.inproj_fm(wglr, b_wglr, 16, tg, lambda ps, pb, tg=tg: kb.op(
                    "act", lambda e: e.activation(out=glrT[:, tg * 512:(tg + 1) * 512], in_=ps[0:16, :], func=AF.Copy),
                    reads=[pb], writes=[b_glrT]))

            self.ck("glrT")

            def mk(name, shape, dt=F32):
                return self.sb(es, "G_" + name, shape, dt), Buf("G_" + name)
            QT, b_QT = mk("QT", [128, T], BF16)
            KT, b_KT = mk("KT", [128, T], BF16)
            KD0, b_KD0 = mk("KD0", [128, 16, 128], BF16)
            KD1, b_KD1 = mk("KD1", [128, 16, 128], BF16)
            V, b_V = mk("V", [128, 16, 256], BF16)
            OT, b_OT = mk("OT", [128, 2, T])
            EB, b_EB = mk("EB", [128, 32])
            Gsp, b_Gsp = mk("Gsp", [128, 4, 128], BF16)
            Gz, b_Gz = mk("Gz", [128, 4, 128])
            EQ, b_EQ = mk("EQ", [128, 512])
            EK, b_EK = mk("EK", [128, 512])
            ED, b_ED = mk("ED", [128, 4, 128])
            STs = [mk("ST%d" % i, [128, 128], BF16) for i in range(2)]
            Sb = [mk("S%d" % i, [128, 256]) for i in range(4)]
            Sbb = [mk("Sb%d" % i, [128, 256], BF16) for i in range(4)]
            SQ = [mk("SQ%d" % i, [128, 512], BF16) for i in range(2)]
            RI, b_RI = mk("RI", [128, 512])
            SG, b_SG = mk("SG", [128, 512])
            SM, b_SM = mk("SM", [128, 512])
            TT, b_TT = mk("TT", [128, 512])

            for hd in range(4):
                w_q, wb_q = self.load_w(16 + hd)
                w_k, wb_k = self.load_w(20 + hd)
                w_v = [self.load_w(24 + 2 * hd + j) for j in range(2)]
                for tg in range(4):
                    ps, pb = self.bank()
                    for j in range(4):
                        tt = tg * 4 + j
                        kb.op("pe", lambda e, j=j, tt=tt, ps=ps: e.matmul(ps[:, j * 128:(j + 1) * 128], glrT[0:16, tt * 128:(tt + 1) * 128],
                                                                      wg[0:16, hd * 128:(hd + 1) * 128], start=True, stop=False),
                              reads=[b_glrT, b_wg], writes=[pb], inc=False)
                        kb.op("pe", lambda e, j=j, ps=ps: e.matmul(ps[:, j * 128:(j + 1) * 128], cst[0:1, 512:640],
                                                               bg[0:1, hd * 128:(hd + 1) * 128], start=False, stop=True),
                              reads=[bc, b_bg], writes=[pb], inc=(j == 3))
                    kb.op("act", lambda e, ps=ps: e.activation(out=Gz[:, :, :], in_=ps[:, :].rearrange("p (j k) -> p j k", k=128),
                                                              func=AF.Exp, scale=-1.0), reads=[pb], writes=[b_Gz])
                    kb.op("act", lambda e: e.activation(out=Gsp[:, :, :], in_=Gz[:, :, :], func=AF.Ln, bias=1.0),
                          reads=[b_Gz], writes=[b_Gsp])
                    self.ck("z/Gsp")
                    ps_c, pb_c = self.bank()
                    ps_r, pb_r = self.bank()
                    for j in range(4):
                        kb.op("pe", lambda e, j=j, ps_c=ps_c: e.matmul(ps_c[:, j * 128:(j + 1) * 128], Gsp[:, j, :], TRI, start=True, stop=True),
                              reads=[b_Gsp, bc], writes=[pb_c], inc=False)
                        kb.op("pe", lambda e, j=j, ps_r=ps_r: e.matmul(ps_r[:, j * 128:(j + 1) * 128], UU, Gsp[:, j, :], start=True, stop=True),
                              reads=[b_Gsp, bc], writes=[pb_r], inc=(j == 3))
                    kb.op("act", lambda e, ps_c=ps_c: e.activation(out=EQ[:, :], in_=ps_c[:, :], func=AF.Exp, scale=-1.0 / 16), reads=[pb_c], writes=[b_EQ])
                    kb.op("act", lambda e, ps_c=ps_c: e.activation(out=EK[:, :], in_=ps_c[:, :], func=AF.Exp, scale=1.0 / 16), reads=[pb_c], writes=[b_EK])
                    kb.op("act", lambda e, ps_r=ps_r: e.activation(out=ED[:, :, :], in_=ps_r[:, :].rearrange("p (j k) -> p j k", k=128),
                                                                func=AF.Exp, scale=-1.0 / 16), reads=[pb_r], writes=[b_ED])
                    kb.op("dve", lambda e, tg=tg: e.tensor_copy(EB[:, tg * 8:(tg + 1) * 8], EQ[:, 63:512:64]), reads=[b_EQ], writes=[b_EB])
                    self.ck("cs/rev/E")
                    self.inproj_fm(w_q, wb_q, 128, tg, lambda ps, pb, tg=tg: kb.op(
                        "dve", lambda e: e.scalar_tensor_tensor(QT[:, tg * 512:(tg + 1) * 512], ps[:, :], 128.0 ** -0.5, EQ[:, :], ALU.mult, ALU.mult),
                        reads=[pb, b_EQ], writes=[b_QT]))
                    self.inproj_fm(w_k, wb_k, 128, tg, lambda ps, pb, tg=tg: kb.op(
                        "dve", lambda e: e.tensor_tensor(KT[:, tg * 512:(tg + 1) * 512], ps[:, :], EK[:, :], ALU.mult),
                        reads=[pb, b_EK], writes=[b_KT]))
                    self.ck("qk fm")
                    ps, pb = self.bank()
                    for j in range(4):
                        tt = tg * 4 + j
                        for kc in range(8):
                            kb.op("pe", lambda e, j=j, tt=tt, kc=kc, ps=ps: e.matmul(ps[:, j * 128:(j + 1) * 128], self.XT[:, kc, tt * 128:(tt + 1) * 128],
                                                                                 w_k[:, kc * 128:(kc + 1) * 128], start=(kc == 0), stop=(kc == 7)),
                                  reads=[self.b_XT, wb_k], writes=[pb], inc=(j == 3 and kc == 7))
                    for KDm, b_KDm, mcol in ((KD0, b_KD0, 704), (KD1, b_KD1, 705)):
                        kb.op("dve", lambda e, ps=ps, tg=tg, KDm=KDm, mcol=mcol: e.scalar_tensor_tensor(
                            KDm[:, tg * 4:(tg + 1) * 4, :], ps[:, :].rearrange("p (j k) -> p j k", k=128), self.cst[:, mcol:mcol + 1],
                            ED[:, :, :], ALU.mult, ALU.mult), reads=[pb, b_ED, self.b_cst], writes=[b_KDm])
                    self.ck("kd")
                    for jj in range(2):
                        ps, pb = self.bank()
                        for j2 in range(2):
                            tt = tg * 4 + jj * 2 + j2
                            for half in range(2):
                                wv, wbv = w_v[half]
                                for kc in range(8):
                                    kb.op("pe", lambda e, j2=j2, tt=tt, kc=kc, ps=ps, half=half, wv=wv: e.matmul(
                                        ps[:, j2 * 256 + half * 128:j2 * 256 + (half + 1) * 128], self.XT[:, kc, tt * 128:(tt + 1) * 128],
                                        wv[:, kc * 128:(kc + 1) * 128], start=(kc == 0), stop=(kc == 7)),
                                        reads=[self.b_XT, wbv], writes=[pb], inc=(j2 == 1 and half == 1 and kc == 7))
                        t0 = tg * 4 + jj * 2
                        kb.op("dve", lambda e, ps=ps, t0=t0: e.tensor_copy(V[:, t0:t0 + 2, :], ps[:, :].rearrange("p (j v) -> p j v", v=256)),
                              reads=[pb], writes=[b_V])
                    self.ck("v")
                kb.op("dve", lambda e: e.memset(Sb[0][0][:, :], 0.0), writes=[Sb[0][1]])
                kb.op("dve", lambda e: e.memset(Sbb[0][0][:, :], 0.0), writes=[Sbb[0][1]])
                for tt in range(16):
                    c0, c1 = 2 * tt, 2 * tt + 1
                    ps_st, pb_st = self.ps[tt % 2], self.psb[tt % 2]
                    st, b_st = STs[tt % 2]
                    kb.op("pe", lambda e: e.matmul(ps_st[:, 0:128], KT[:, tt * 128:(tt + 1) * 128], QT[:, tt * 128:(tt + 1) * 128], start=True, stop=True),
                          reads=[b_KT, b_QT], writes=[pb_st])
                    kb.op("dve", lambda e: e.tensor_tensor(st[:, :], ps_st[:, 0:128], TRI32, ALU.mult), reads=[pb_st, self.b_cst], writes=[b_st])
                    ps_kv, pb_kv = self.ps[2 + tt % 2], self.psb[2 + tt % 2]
                    kb.op("pe", lambda e: e.matmul(ps_kv[:, 0:256], KD0[:, tt, :], V[:, tt, :], start=True, stop=True),
                          reads=[b_KD0, b_V], writes=[pb_kv], inc=False)
                    kb.op("pe", lambda e: e.matmul(ps_kv[:, 256:512], KD1[:, tt, :], V[:, tt, :], start=True, stop=True),
                          reads=[b_KD1, b_V], writes=[pb_kv])
                    self.ck("st/kv")
                    grp = (tt // 4) % 2
                    col = (tt % 4) * 128
                    for vc in range(2):
                        pso, pbo = self.ps[4 + 2 * grp + vc], self.psb[4 + 2 * grp + vc]
                        kb.op("pe", lambda e, vc=vc, pso=pso: e.matmul(pso[:, col:col + 128], V[:, tt, vc * 128:(vc + 1) * 128], st[:, :], start=True, stop=False),
                              reads=[b_V, b_st], writes=[pbo], inc=False)
                        kb.op("pe", lambda e, vc=vc, pso=pso: e.matmul(pso[:, col:col + 64], Sbb[c0 % 4][0][:, vc * 128:(vc + 1) * 128], QT[:, c0 * 64:(c0 + 1) * 64],
                                                                    start=False, stop=False), reads=[Sbb[c0 % 4][1], b_QT], writes=[pbo], inc=False)
                        if vc == 0:
                            kb.op("dve", lambda e: e.scalar_tensor_tensor(Sb[c1 % 4][0][:, :], Sb[c0 % 4][0][:, :], EB[:, c0:c0 + 1], ps_kv[:, 0:256], ALU.mult, ALU.add),
                                  reads=[Sb[c0 % 4][1], b_EB, pb_kv], writes=[Sb[c1 % 4][1]])
                            kb.op("act", lambda e: e.activation(out=Sbb[c1 % 4][0][:, :], in_=Sb[c1 % 4][0][:, :], func=AF.Copy),
                                  reads=[Sb[c1 % 4][1]], writes=[Sbb[c1 % 4][1]])
                        kb.op("pe", lambda e, vc=vc, pso=pso: e.matmul(pso[:, col + 64:col + 128], Sbb[c1 % 4][0][:, vc * 128:(vc + 1) * 128], QT[:, c1 * 64:(c1 + 1) * 64],
                                                                    start=False, stop=True), reads=[Sbb[c1 % 4][1], b_QT], writes=[pbo], inc=True)
                    kb.op("dve", lambda e: e.scalar_tensor_tensor(Sb[(c1 + 1) % 4][0][:, :], Sb[c1 % 4][0][:, :], EB[:, c1:c1 + 1], ps_kv[:, 256:512], ALU.mult, ALU.add),
                          reads=[Sb[c1 % 4][1], b_EB, pb_kv], writes=[Sb[(c1 + 1) % 4][1]])
                    kb.op("act", lambda e: e.activation(out=Sbb[(c1 + 1) % 4][0][:, :], in_=Sb[(c1 + 1) % 4][0][:, :], func=AF.Copy),
                          reads=[Sb[(c1 + 1) % 4][1]], writes=[Sbb[(c1 + 1) % 4][1]])
                    self.ck("o tile")
                    if tt % 4 == 3:
                        tg = tt // 4
                        for vc in range(2):
                            pso, pbo = self.ps[4 + 2 * grp + vc], self.psb[4 + 2 * grp + vc]
                            kb.op("act", lambda e, vc=vc, pso=pso, tg=tg: e.activation(out=OT[:, vc, tg * 512:(tg + 1) * 512], in_=pso[:, :], func=AF.Copy),
                                  reads=[pbo], writes=[b_OT])
                if hd == 0:
                    self.dump("o_raw0", OT[:, 0, :], b_OT, [128, T])
                w_go = [self.load_w(32 + 2 * hd + j) for j in range(2)]
                w_mb = [self.load_w(48 + 2 * hd + j) for j in range(2)]
                for tg in range(4):
                    sl = slice(tg * 512, (tg + 1) * 512)
                    for vc in range(2):
                        kb.op("act", lambda e, vc=vc: e.activation(out=SQ[vc][0][:, :], in_=OT[:, vc, sl], func=AF.Square), reads=[b_OT], writes=[SQ[vc][1]])
                    ps, pb = self.bank()
                    for vc in range(2):
                        kb.op("pe", lambda e, vc=vc, ps=ps: e.matmul(ps[:, :], ONES, SQ[vc][0][:, :], start=(vc == 0), stop=(vc == 1)),
                              reads=[bc, SQ[vc][1]], writes=[pb], inc=(vc == 1))
                    kb.op("act", lambda e, ps=ps: e.activation(out=RI[:, :], in_=ps[:, :], func=AF.Sqrt, scale=1.0 / 256, bias=1e-5), reads=[pb], writes=[b_RI])
                    kb.op("dve", lambda e: e.reciprocal(RI[:, :], RI[:, :]), reads=[b_RI], writes=[b_RI])
                    for vc in range(2):
                        self.inproj_fm(w_go[vc][0], w_go[vc][1], 128, tg, lambda ps, pb: kb.op(
                            "act", lambda e: e.activation(out=SG[:, :], in_=ps[:, :], func=AF.Silu), reads=[pb], writes=[b_SG]))
                        self.inproj_fm(w_mb[vc][0], w_mb[vc][1], 128, tg, lambda ps, pb: kb.op(
                            "act", lambda e: e.activation(out=SM[:, :], in_=ps[:, :], func=AF.Sigmoid), reads=[pb], writes=[b_SM]))
                        kb.op("dve", lambda e, vc=vc: e.scalar_tensor_tensor(TT[:, :], OT[:, vc, sl], ng[:, vc:vc + 1], RI[:, :], ALU.mult, ALU.mult),
                              reads=[b_OT, b_ng, b_RI], writes=[b_TT])
                        kb.op("dve", lambda e: e.tensor_tensor(TT[:, :], TT[:, :], SG[:, :], ALU.mult), reads=[b_TT, b_SG], writes=[b_TT])
                        kb.op("dve", lambda e: e.tensor_tensor(TT[:, :], TT[:, :], SM[:, :], ALU.mult), reads=[b_TT, b_SM], writes=[b_TT])
                        g = hd * 2 + vc
                        kb.op("dve", lambda e, g=g: e.tensor_tensor(self.yT[:, g, sl], TT[:, :], self.yT[:, g, sl], ALU.add),
                              reads=[b_TT, self.b_yT[g]], writes=[self.b_yT[g]])

    def layer_norm(self, es_tmp, R, b_R, grow, brow, OUT, b_OUT, tag):
        kb = self.kb
        st = self.ln_st
        kb.op("dve", lambda e: e.bn_stats(st["stats"][:, 0, :], R[:, 0:512]), reads=[b_R], writes=[st["b"]])
        kb.op("dve", lambda e: e.bn_stats(st["stats"][:, 1, :], R[:, 512:1024]), reads=[b_R], writes=[st["b"]])
        kb.op("dve", lambda e: e.bn_aggr(st["mv"][:, :], st["stats"][:, :, :].rearrange("p a b -> p (a b)")), reads=[st["b"]], writes=[st["b"]])
        kb.op("act", lambda e: e.activation(out=st["rs"][:, 0:1], in_=st["mv"][:, 1:2], func=AF.Sqrt, bias=1e-5), reads=[st["b"]], writes=[st["b2"]])
        kb.op("dve", lambda e: e.reciprocal(st["rs"][:, 0:1], st["rs"][:, 0:1]), reads=[st["b2"]], writes=[st["b2"]])
        kb.op("dve", lambda e: e.scalar_tensor_tensor(st["rs"][:, 1:2], st["mv"][:, 0:1], -1.0, st["rs"][:, 0:1], ALU.mult, ALU.mult),
              reads=[st["b"], st["b2"]], writes=[st["b2"]])
        kb.op("act", lambda e: e.activation(out=OUT[:, :], in_=R[:, :], func=AF.Identity, scale=st["rs"][:, 0:1], bias=st["rs"][:, 1:2]),
              reads=[b_R, st["b2"]], writes=[b_OUT])
        kb.op("dve", lambda e: e.tensor_tensor(OUT[:, :], OUT[:, :], self.rowp[:, grow, :], ALU.mult), reads=[b_OUT, self.b_rowp], writes=[b_OUT])
        kb.op("dve", lambda e: e.tensor_tensor(OUT[:, :], OUT[:, :], self.rowp[:, brow, :], ALU.add), reads=[b_OUT, self.b_rowp], writes=[b_OUT])

    def alloc_ln(self, es):
        g = self.ps_gen
        self.ln_st = {"stats": self.sb(es, "ln_stats%d" % g, [128, 2, 6]), "mv": self.sb(es, "ln_mv%d" % g, [128, 2]),
                      "rs": self.sb(es, "ln_rs%d" % g, [128, 2]), "b": Buf("ln_b"), "b2": Buf("ln_b2")}

    def phase_outproj(self):
        nc, kb = self.nc, self.kb
        cstb, bcb = self.cstb, self.b_cstb
        IDb, LTb, ONEb = cstb[:, 0:128], cstb[:, 384:512], cstb[:, 512:640]
        with ExitStack() as es:
            self.alloc_psum(es, 6, 2)
            self.alloc_ln(es)

            def mk(name, shape, dt=F32):
                return self.sb(es, "P2_" + name, shape, dt), Buf("P2_" + name)
            Wout, b_Wout = mk("Wout", [128, 8, 1024], BF16)
            kb.dma("pool", lambda e: e.dma_start(out=Wout[:, 0:4, :], in_=self.din["w_out"].rearrange("p (k n) -> p k n", n=1024)[:, 0:4, :]), writes=[b_Wout])
            kb.dma("pool", lambda e: e.dma_start(out=Wout[:, 4:8, :], in_=self.din["w_out"].rearrange("p (k n) -> p k n", n=1024)[:, 4:8, :]), writes=[b_Wout])
            wr32, b_wr32 = mk("wr32", [128, 8, 32])
            wrh, b_wrh = mk("wrh", [128, 8, 32], BF16)
            wrl, b_wrl = mk("wrl", [128, 8, 32], BF16)
            kb.dma("sp", lambda e: e.dma_start(out=wr32[:], in_=self.din["w_router"].rearrange("p (k n) -> p k n", n=32)), writes=[b_wr32])
            kb.op("dve", lambda e: e.tensor_copy(wrh[:], wr32[:]), reads=[b_wr32], writes=[b_wrh])
            kb.op("dve", lambda e: e.tensor_tensor(wrl[:], wr32[:], wrh[:], ALU.subtract), reads=[b_wr32, b_wrh], writes=[b_wrl])
            brt, b_brt = mk("brt", [128, 32])
            kb.dma("sp", lambda e: e.dma_start(out=brt[:], in_=self.din["b_router"][0:1, :].partition_broadcast(128)), writes=[b_brt])
            carry, b_carry = mk("carry", [128, 32])
            kb.op("dve", lambda e: e.memset(carry[:], 0.0), writes=[b_carry])
            Xt = [mk("x%d" % i, [128, 1024]) for i in range(2)]
            R, b_R = mk("R", [128, 1024])
            H1 = [mk("H1_%d" % i, [128, 1024]) for i in range(2)]
            H1b = [mk("H1b_%d" % i, [128, 1024], BF16) for i in range(2)]
            H1l = [mk("H1l_%d" % i, [128, 1024], BF16) for i in range(2)]
            HT = [mk("HT_%d" % i, [128, 8, 128], BF16) for i in range(2)]
            lg, b_lg = mk("lg", [128, 32])
            v8, b_v8 = mk("v8", [128, 8])
            i8, b_i8 = mk("i8", [128, 8], U32)
            i8f, b_i8f = mk("i8f", [128, 8])
            sm, b_sm = mk("sm", [128, 8])
            mask, b_mask = mk("mask", [128, 32], BF16)
            sc, b_sc = mk("sc", [128, 32])
            ov, b_ov = mk("ov", [128, 32])
            junk, b_junk = mk("junk", [128, 32])
            slf, b_slf = mk("slf", [128, 4])

            for tt in range(16):
                tsl = slice(tt * 128, (tt + 1) * 128)
                xt, b_xt = Xt[tt % 2]
                kb.dma("sp", lambda e: e.dma_start(out=xt[:], in_=self.din["x"][tsl, :]), writes=[b_xt])
                for half in range(2):
                    ps, pb = self.bank()
                    for kc in range(8):
                        kb.op("pe", lambda e, kc=kc, ps=ps, half=half: e.matmul(ps[:, :], self.yT[:, kc, tsl], Wout[:, kc, half * 512:(half + 1) * 512],
                                                                             start=(kc == 0), stop=(kc == 7)),
                              reads=[self.b_yT[kc], b_Wout], writes=[pb], inc=(kc == 7))
                    kb.op("dve", lambda e, ps=ps, half=half: e.scalar_tensor_tensor(R[:, half * 512:(half + 1) * 512], xt[:, half * 512:(half + 1) * 512], ALPHA,
                                                                                  ps[:, :], ALU.mult, ALU.add), reads=[b_xt, pb], writes=[b_R])
                h1, b_h1 = H1[tt % 2]
                self.layer_norm(es, R, b_R, 0, 1, h1, b_h1, "ln1")
                kb.dma("sp", lambda e: e.dma_start(out=self.H1d[tsl, :], in_=h1[:]), reads=[b_h1], writes=[self.b_H1d])
                if tt == 0:
                    self.dump("h1_0", h1[:], b_h1, [128, 1024])
                hb, b_hb = H1b[tt % 2]
                hl, b_hl = H1l[tt % 2]
                kb.op("act", lambda e: e.activation(out=hb[:, :], in_=h1[:, :], func=AF.Identity), reads=[b_h1], writes=[b_hb])
                kb.op("dve", lambda e: e.tensor_tensor(hl[:, :], h1[:, :], hb[:, :], ALU.subtract), reads=[b_h1, b_hb], writes=[b_hl])
                for (src, b_src, (dst, b_dst)) in ((hb, b_hb, HT[0]), (hl, b_hl, HT[1])):
                    pt, ptb = self.bank16()
                    for kc in range(8):
                        kb.op("pe", lambda e, kc=kc, pt=pt, src=src: e.transpose(pt[:, kc * 128:(kc + 1) * 128], src[:, kc * 128:(kc + 1) * 128], IDb),
                              reads=[b_src, bcb], writes=[ptb], inc=(kc == 7))
                    kb.op("dve", lambda e, pt=pt, dst=dst: e.tensor_copy(dst[:, :, :], pt[:, :].rearrange("p (k t) -> p k t", t=128)),
                          reads=[ptb], writes=[b_dst])
                ps, pb = self.bank()
                combos = [(HT[0], wrh, b_wrh), (HT[0], wrl, b_wrl), (HT[1], wrh, b_wrh)]
                n = 0
                for (ht, b_ht), w, b_w in combos:
                    for kc in range(8):
                        n += 1
                        kb.op("pe", lambda e, kc=kc, ps=ps, ht=ht, w=w, n=n: e.matmul(ps[:, 0:32], ht[:, kc, :], w[:, kc, :], start=(n == 1), stop=(n == 24)),
                              reads=[b_ht, b_w], writes=[pb], inc=(n == 24))
                kb.op("dve", lambda e, ps=ps: e.tensor_tensor(lg[:, :], ps[:, 0:32], brt[:, :], ALU.add), reads=[pb, b_brt], writes=[b_lg])
                if tt == 0:
                    self.dump("lg_0", lg[:], b_lg, [128, 32])
                kb.op("dve", lambda e: e.max(out=v8[:, :], in_=lg[:, :]), reads=[b_lg], writes=[b_v8])
                kb.op("dve", lambda e: e.max_index(out=i8[:, :], in_max=v8[:, :], in_values=lg[:, :]), reads=[b_lg, b_v8], writes=[b_i8])
                kb.op("dve", lambda e: e.tensor_copy(i8f[:, :], i8[:, :]), reads=[b_i8], writes=[b_i8f])
                kb.op("dve", lambda e: e.tensor_scalar_mul(sm[:, 0:1], v8[:, 0:1], -1.0), reads=[b_v8], writes=[b_sm])
                kb.op("act", lambda e: e.activation(out=sm[:, 4:8], in_=v8[:, 0:4], func=AF.Exp, bias=sm[:, 0:1], accum_out=sm[:, 1:2]),
                      reads=[b_v8, b_sm], writes=[b_sm])
                kb.op("dve", lambda e: e.reciprocal(sm[:, 2:3], sm[:, 1:2]), reads=[b_sm], writes=[b_sm])
                kb.op("dve", lambda e: e.tensor_scalar_mul(self.GATES[:, tt, :], sm[:, 4:8], sm[:, 2:3]), reads=[b_sm], writes=[self.b_route[tt]])
                kb.op("dve", lambda e: e.tensor_scalar(mask[:, :], lg[:, :], v8[:, 3:4], None, ALU.is_ge), reads=[b_lg, b_v8], writes=[b_mask])
                ps, pb = self.bank()
                kb.op("pe", lambda e, ps=ps: e.matmul(ps[:, 0:32], LTb, mask[:, :], start=True, stop=True), reads=[bcb, b_mask], writes=[pb], inc=False)
                kb.op("pe", lambda e, ps=ps: e.matmul(ps[:, 32:64], ONEb, mask[:, :], start=True, stop=True), reads=[bcb, b_mask], writes=[pb])
                kb.op("dve", lambda e, ps=ps: e.tensor_tensor(sc[:, :], ps[:, 0:32], carry[:, :], ALU.add), reads=[pb, b_carry], writes=[b_sc])
                kb.op("dve", lambda e, ps=ps: e.tensor_tensor(carry[:, :], ps[:, 32:64], carry[:, :], ALU.add), reads=[pb, b_carry, b_sc], writes=[b_carry])
                kb.op("dve", lambda e: e.tensor_scalar(ov[:, :], sc[:, :], float(CAP), float(4 * NSLOT), ALU.is_ge, ALU.mult), reads=[b_sc], writes=[b_ov])
                kb.op("dve", lambda e: e.tensor_tensor(sc[:, :], sc[:, :], self.cst[:, 672:704], ALU.add), reads=[b_sc, self.b_cst], writes=[b_sc])
                kb.op("dve", lambda e: e.tensor_tensor(sc[:, :], sc[:, :], ov[:, :], ALU.add), reads=[b_sc, b_ov], writes=[b_sc])
                for k in range(4):
                    kb.op("dve", lambda e, k=k: e.scalar_tensor_tensor(junk[:, :], self.cst[:, 640:672], i8f[:, k:k + 1], sc[:, :], ALU.is_equal, ALU.mult,
                                                                      accum_out=slf[:, k:k + 1]), reads=[self.b_cst, b_i8f, b_sc], writes=[b_junk, b_slf])
                kb.op("dve", lambda e: e.tensor_copy(self.SLOTS[:, tt, :], slf[:, :]), reads=[b_slf], writes=[self.b_route[tt]])
                for k in range(4):
                    kb.dma("pool", lambda e, k=k: e.indirect_dma_start(
                        out=self.Xg, out_offset=bass.IndirectOffsetOnAxis(ap=self.SLOTS[:, tt, k:k + 1], axis=0),
                        in_=hb[:, :], in_offset=None, bounds_check=self.bound_reg, oob_is_err=False),
                        reads=[b_hb, self.b_route[tt]], writes=[self.b_Xg])
            self.dump("gates", self.GATES[:].rearrange("p a b -> p (a b)"), self.b_route[15], [128, 64])
            if "slots" in self.dbg:
                sf, b_sf = mk("slots_f", [128, 64])
                kb.op("dve", lambda e: e.tensor_copy(sf[:, :], self.SLOTS[:].rearrange("p a b -> p (a b)")), reads=self.b_route, writes=[b_sf])
                self.dump("slots", sf[:], b_sf, [128, 64])

    def phase_moe(self):
        nc, kb = self.nc, self.kb
        IDb, bcb = self.cstb[:, 0:128], self.b_cstb
        NST = CAP // 128
        with ExitStack() as es:
            self.alloc_psum(es, 6, 2)

            def mk(name, shape, dt=F32):
                return self.sb(es, "M_" + name, shape, dt), Buf("M_" + name)
            WU = [mk("wu%d" % i, [128, 8, 2048], BF16) for i in range(2)]
            WD = [mk("wd%d" % i, [128, 8, 1024], BF16) for i in range(2)]
            BD = [mk("bd%d" % i, [128, 1024]) for i in range(2)]
            XG = [mk("xg%d" % i, [128, NST, 1024], BF16) for i in range(2)]
            XGT = [mk("xgt%d" % i, [128, 8, CAP], BF16) for i in range(2)]
            ACTT, b_ACTT = mk("actt", [128, 8, CAP], BF16)
            Gt = [mk("g%d" % i, [128, CAP]) for i in range(2)]
            St = [mk("s%d" % i, [128, CAP]) for i in range(2)]
            Ut = [mk("u%d" % i, [128, CAP]) for i in range(2)]
            Ysb = [mk("y%d" % i, [128, 1024]) for i in range(2)]
            bup, b_bup = mk("bup", [128, 32, 16])
            kb.dma("sp", lambda e: e.dma_start(out=bup[:], in_=self.din["b_up"].rearrange("p (e f) -> p e f", f=16)), writes=[b_bup])

            def load(ex):
                sl = ex % 2
                wu = self.din["w_up"][ex].rearrange("(kc p) f -> p kc f", p=128)
                wd = self.din["w_down"][ex].rearrange("(kc p) f -> p kc f", p=128)
                kb.dma("sp", lambda e: e.dma_start(out=XG[sl][0][:], in_=self.Xg[ex * CAP:(ex + 1) * CAP, :].rearrange("(st p) d -> p st d", p=128)),
                       reads=[self.b_Xg], writes=[XG[sl][1]])
                kb.dma("sp", lambda e: e.dma_start(out=BD[sl][0][:], in_=self.din["b_down"][ex:ex + 1, :].partition_broadcast(128)), writes=[BD[sl][1]])
                for q in range(4):
                    kb.dma("pool", lambda e, q=q: e.dma_start(out=WU[sl][0][:, 2 * q:2 * q + 2, :], in_=wu[:, 2 * q:2 * q + 2, :]), writes=[WU[sl][1]])
                for q in range(2):
                    kb.dma("pool", lambda e, q=q: e.dma_start(out=WD[sl][0][:, 4 * q:4 * q + 4, :], in_=wd[:, 4 * q:4 * q + 4, :]), writes=[WD[sl][1]])

            def compute(ex):
                sl = ex % 2
                wu, b_wu = WU[sl]
                wd, b_wd = WD[sl]
                xg, b_xg = XG[sl]
                xgt, b_xgt = XGT[sl]
                bd, b_bd = BD[sl]
                for st in range(NST):
                    pt, ptb = self.bank16()
                    for kc in range(8):
                        kb.op("pe", lambda e, kc=kc, pt=pt, st=st: e.transpose(pt[:, kc * 128:(kc + 1) * 128], xg[:, st, kc * 128:(kc + 1) * 128], IDb),
                              reads=[b_xg, bcb], writes=[ptb], inc=(kc == 7))
                    kb.op("dve", lambda e, pt=pt, st=st: e.tensor_copy(xgt[:, :, st * 128:(st + 1) * 128], pt[:, :].rearrange("p (k s) -> p k s", s=128)),
                          reads=[ptb], writes=[b_xgt])
                for c in range(8):
                    g, b_g = Gt[c % 2]
                    s_, b_s = St[c % 2]
                    u, b_u = Ut[c % 2]
                    ps_g, pb_g = self.bank()
                    for kc in range(8):
                        kb.op("pe", lambda e, kc=kc, ps_g=ps_g, c=c: e.matmul(ps_g[:, 0:CAP], wu[:, kc, c * 128:(c + 1) * 128], xgt[:, kc, :], start=(kc == 0), stop=(kc == 7)),
                              reads=[b_wu, b_xgt], writes=[pb_g], inc=(kc == 7))
                    ps_u, pb_u = self.bank()
                    for kc in range(8):
                        kb.op("pe", lambda e, kc=kc, ps_u=ps_u, c=c: e.matmul(ps_u[:, 0:CAP], wu[:, kc, 1024 + c * 128:1024 + (c + 1) * 128], xgt[:, kc, :],
                                                                           start=(kc == 0), stop=(kc == 7)),
                              reads=[b_wu, b_xgt], writes=[pb_u], inc=(kc == 7))
                    kb.op("dve", lambda e, ps_g=ps_g, c=c: e.tensor_scalar(g[:, :], ps_g[:, 0:CAP], bup[:, ex, c:c + 1], 7.0, ALU.add, ALU.min),
                          reads=[pb_g, b_bup], writes=[b_g])
                    kb.op("act", lambda e: e.activation(out=s_[:, :], in_=g[:, :], func=AF.Sigmoid, scale=1.702), reads=[b_g], writes=[b_s])
                    kb.op("dve", lambda e, ps_u=ps_u, c=c: e.tensor_scalar(u[:, :], ps_u[:, 0:CAP], bup[:, ex, 8 + c:9 + c], 7.0, ALU.add, ALU.min),
                          reads=[pb_u, b_bup], writes=[b_u])
                    kb.op("dve", lambda e: e.tensor_scalar(u[:, :], u[:, :], -7.0, 1.0, ALU.max, ALU.add), reads=[b_u], writes=[b_u])
                    kb.op("dve", lambda e: e.tensor_tensor(g[:, :], g[:, :], s_[:, :], ALU.mult), reads=[b_g, b_s], writes=[b_g])
                    kb.op("dve", lambda e, c=c: e.tensor_tensor(ACTT[:, c, :], g[:, :], u[:, :], ALU.mult), reads=[b_g, b_u], writes=[b_ACTT])
                for st in range(NST):
                    y, b_y = Ysb[st % 2]
                    for half in range(2):
                        ps, pb = self.bank()
                        for fc in range(8):
                            kb.op("pe", lambda e, fc=fc, ps=ps, half=half, st=st: e.matmul(ps[:, :], ACTT[:, fc, st * 128:(st + 1) * 128],
                                                                                        wd[:, fc, half * 512:(half + 1) * 512], start=(fc == 0), stop=(fc == 7)),
                                  reads=[b_ACTT, b_wd], writes=[pb], inc=(fc == 7))
                        kb.op("dve", lambda e, ps=ps, half=half: e.tensor_tensor(y[:, half * 512:(half + 1) * 512], ps[:, :], bd[:, half * 512:(half + 1) * 512], ALU.add),
                              reads=[pb, b_bd], writes=[b_y])
                    r0 = ex * CAP + st * 128
                    kb.dma("sp", lambda e, r0=r0: e.dma_start(out=self.Yg[r0:r0 + 128, :], in_=y[:]), reads=[b_y], writes=[self.b_Yg])

            ne = getattr(self, "n_experts", NE)
            load(0)
            if ne > 1:
                load(1)
            for ex in range(ne):
                compute(ex)
                if ex + 2 < ne:
                    load(ex + 2)

    def phase_final(self):
        nc, kb = self.nc, self.kb
        IDb, bcb = self.cstb[:, 0:128], self.b_cstb
        with ExitStack() as es:
            self.alloc_psum(es, 6, 2)
            self.alloc_ln(es)

            def mk(name, shape, dt=F32):
                return self.sb(es, "F_" + name, shape, dt), Buf("F_" + name)
            Wpg, b_Wpg = mk("Wpg", [128, 8, 1024], BF16)
            for q in range(2):
                kb.dma("pool", lambda e, q=q: e.dma_start(out=Wpg[:, 4 * q:4 * q + 4, :], in_=self.din["w_pg"].rearrange("p (k n) -> p k n", n=1024)[:, 4 * q:4 * q + 4, :]),
                       writes=[b_Wpg])
            Wple, b_Wple = mk("Wple", [128, 2, 1024], BF16)
            kb.dma("pool", lambda e: e.dma_start(out=Wple[:], in_=self.din["w_ple"].rearrange("p (k n) -> p k n", n=1024)), writes=[b_Wple])
            PT, b_PT = mk("PT", [128, 2, T], BF16)
            pT = self.din["pT"].rearrange("(kc p) t -> p kc t", p=128)
            for kc in range(2):
                kb.dma("pool", lambda e, kc=kc: e.dma_start(out=PT[:, kc, :], in_=pT[:, kc, :]), writes=[b_PT])
            YG = [mk("yg%d" % i, [128, 4, 1024]) for i in range(2)]
            H1t = [mk("h1_%d" % i, [128, 1024]) for i in range(2)]
            ACC, b_ACC = mk("acc", [128, 1024])
            H2, b_H2 = mk("h2", [128, 1024])
            H2b, b_H2b = mk("h2b", [128, 1024], BF16)
            H2T, b_H2T = mk("h2T", [128, 8, 128], BF16)
            SGT, b_SGT = mk("sgt", [128, 1024])
            OUT = [mk("out%d" % i, [128, 1024]) for i in range(2)]
            for tt in range(16):
                tsl = slice(tt * 128, (tt + 1) * 128)
                yg, b_yg = YG[tt % 2]
                h1, b_h1 = H1t[tt % 2]
                kb.op("dve", lambda e: e.memset(yg[:], 0.0), writes=[b_yg])
                for k in range(4):
                    kb.dma("pool", lambda e, k=k: e.indirect_dma_start(
                        out=yg[:, k, :], out_offset=None, in_=self.Yg,
                        in_offset=bass.IndirectOffsetOnAxis(ap=self.SLOTS[:, tt, k:k + 1], axis=0),
                        bounds_check=self.bound_reg, oob_is_err=False), reads=[self.b_Yg, self.b_route[tt]], writes=[b_yg])
                kb.dma("sp", lambda e: e.dma_start(out=h1[:], in_=self.H1d[tsl, :]), reads=[self.b_H1d], writes=[b_h1])
                kb.op("act", lambda e: e.activation(out=ACC[:, :], in_=h1[:, :], func=AF.Identity, scale=ALPHA), reads=[b_h1], writes=[b_ACC])
                for k in range(4):
                    kb.op("dve", lambda e, k=k: e.scalar_tensor_tensor(ACC[:, :], yg[:, k, :], self.GATES[:, tt, k:k + 1], ACC[:, :], ALU.mult, ALU.add),
                          reads=[b_yg, self.b_route[tt], b_ACC], writes=[b_ACC])
                self.layer_norm(es, ACC, b_ACC, 2, 3, H2, b_H2, "ln2")
                if tt == 0:
                    self.dump("h2_0", H2[:], b_H2, [128, 1024])
                kb.op("act", lambda e: e.activation(out=H2b[:, :], in_=H2[:, :], func=AF.Identity), reads=[b_H2], writes=[b_H2b])
                pt, ptb = self.bank16()
                for kc in range(8):
                    kb.op("pe", lambda e, kc=kc, pt=pt: e.transpose(pt[:, kc * 128:(kc + 1) * 128], H2b[:, kc * 128:(kc + 1) * 128], IDb),
                          reads=[b_H2b, bcb], writes=[ptb], inc=(kc == 7))
                kb.op("dve", lambda e, pt=pt: e.tensor_copy(H2T[:, :, :], pt[:, :].rearrange("p (k t) -> p k t", t=128)), reads=[ptb], writes=[b_H2T])
                o, b_o = OUT[tt % 2]
                for half in range(2):
                    hs = slice(half * 512, (half + 1) * 512)
                    ps, pb = self.bank()
                    for kc in range(8):
                        kb.op("pe", lambda e, kc=kc, ps=ps, hs=hs: e.matmul(ps[:, :], H2T[:, kc, :], Wpg[:, kc, hs], start=(kc == 0), stop=(kc == 7)),
                              reads=[b_H2T, b_Wpg], writes=[pb], inc=(kc == 7))
                    kb.op("dve", lambda e, ps=ps, hs=hs: e.tensor_tensor(SGT[:, hs], ps[:, :], self.rowp[:, 4, hs], ALU.add), reads=[pb, self.b_rowp], writes=[b_SGT])
                    kb.op("act", lambda e, hs=hs: e.activation(out=SGT[:, hs], in_=SGT[:, hs], func=AF.Sigmoid), reads=[b_SGT], writes=[b_SGT])
                    ps2, pb2 = self.bank()
                    for kc in range(2):
                        kb.op("pe", lambda e, kc=kc, ps2=ps2, hs=hs: e.matmul(ps2[:, :], PT[:, kc, tsl], Wple[:, kc, hs], start=(kc == 0), stop=(kc == 1)),
                              reads=[b_PT, b_Wple], writes=[pb2], inc=(kc == 1))
                    kb.op("dve", lambda e, ps2=ps2, hs=hs: e.tensor_tensor(o[:, hs], SGT[:, hs], ps2[:, :], ALU.mult), reads=[b_SGT, pb2], writes=[b_o])
                    kb.op("dve", lambda e, hs=hs: e.tensor_tensor(o[:, hs], o[:, hs], H2[:, hs], ALU.add), reads=[b_o, b_H2], writes=[b_o])
                kb.dma("sp", lambda e: e.dma_start(out=self.out[tsl, :], in_=o[:]), reads=[b_o])


_PROG_CACHE = {}


def kernel(**inputs):
    inp = {k: np.asarray(v) for k, v in inputs.items()}
    sh = prep_shared(inp)
    in_maps = [dict(sh, **prep_core(inp, b)) for b in range(8)]
    if "nc" not in _PROG_CACHE:
        _PROG_CACHE["nc"] = Prog().build()
    nc = _PROG_CACHE["nc"]
    res = run_bass_kernel_spmd(nc, in_maps, core_ids=list(range(8)))
    out = np.stack([np.asarray(r["out"], dtype=np.float32) for r in res.results], axis=0)
    return out
```

```python
import numpy as np
from contextlib import ExitStack
import concourse.bass as bass
import concourse.mybir as mybir
from concourse.bass_utils import run_bass_kernel_spmd

F32 = mybir.dt.float32
BF16 = mybir.dt.bfloat16
U32 = mybir.dt.uint32
AF = mybir.ActivationFunctionType
ALU = mybir.AluOpType
AX = mybir.AxisListType

T = 2048
D = 1024
NE = 32
CAP = 384
NSLOT = NE * CAP
ALPHA = 2.0 ** 0.25
N_IN = 7184
O_XA, O_GA, O_Q, O_K, O_V, O_GO, O_GLR, O_MA, O_MB = 0, 1024, 2048, 2560, 3072, 4096, 5120, 5136, 6160


class Buf:
    __slots__ = ("name", "w", "r", "c")

    def __init__(self, name):
        self.name = name
        self.w = {}
        self.r = {}
        self.c = {}


class KB:
    def __init__(self, nc, es, n_dma_sems=24):
        self.nc = nc
        self.eng = dict(pe=nc.tensor, act=nc.scalar, dve=nc.vector, pool=nc.gpsimd, sp=nc.sync)
        self.esem = {}
        self.ecnt = {}
        self.seen = {}
        self.semobj = {}
        for n in self.eng:
            s = es.enter_context(nc.semaphore("es_" + n))
            self.esem[n] = s
            self.semobj[id(s)] = s
            self.ecnt[n] = 0
            self.seen[n] = {}
        self.dsem = []
        self.dcnt = []
        for i in range(2 * n_dma_sems):
            s = es.enter_context(nc.semaphore("ds_%d" % i))
            self.dsem.append(s)
            self.semobj[id(s)] = s
            self.dcnt.append(0)
        self.nds = n_dma_sems
        self.drr = {"pool": 0, "hw": 0}

    def _wait(self, en, toks):
        e = self.eng[en]
        seen = self.seen[en]
        for sid, val in toks.items():
            if en == "pe" and sid == id(self.esem["pe"]):
                continue
            if seen.get(sid, 0) >= val:
                continue
            e.wait_ge(self.semobj[sid], val)
            seen[sid] = val

    @staticmethod
    def _merge(dst, src):
        for k, v in src.items():
            if dst.get(k, 0) < v:
                dst[k] = v

    def _deps(self, reads, writes, cw=()):
        toks = {}
        for b in reads:
            self._merge(toks, b.w)
            self._merge(toks, b.c)
        for b in writes:
            self._merge(toks, b.w)
            self._merge(toks, b.c)
            self._merge(toks, b.r)
        for b in cw:
            self._merge(toks, b.w)
            self._merge(toks, b.r)
        return toks

    def _commit(self, tok, reads, writes, cw=()):
        for b in reads:
            self._merge(b.r, tok)
        for b in writes:
            b.w = dict(tok)
            b.r = {}
            b.c = {}
        for b in cw:
            self._merge(b.c, tok)

    disabled = False

    def op(self, en, fn, reads=(), writes=(), inc=True, cw=()):
        if self.disabled:
            return None
        self._wait(en, self._deps(reads, writes, cw))
        ins = fn(self.eng[en])
        s = self.esem[en]
        if inc:
            self.ecnt[en] += 1
            ins.then_inc(s, 1)
            tok = {id(s): self.ecnt[en]}
        else:
            tok = {id(s): self.ecnt[en] + 1}
        self._commit(tok, reads, writes, cw)
        return ins

    def dma(self, en, fn, reads=(), writes=(), cw=()):
        if self.disabled:
            return None
        kind = "pool" if en == "pool" else "hw"
        i = self.drr[kind] + (self.nds if kind == "pool" else 0)
        self.drr[kind] = (self.drr[kind] + 1) % self.nds
        s = self.dsem[i]
        toks = self._deps(reads, writes, cw)
        if self.dcnt[i] > 0:
            self._merge(toks, {id(s): self.dcnt[i]})
        self._wait(en, toks)
        ins = fn(self.eng[en])
        self.dcnt[i] += 16
        ins.then_inc(s, 16)
        tok = {id(s): self.dcnt[i]}
        self._commit(tok, reads, writes, cw)
        return ins

    def all_tokens(self):
        toks = {}
        for n in self.eng:
            if self.ecnt[n] > 0:
                toks[id(self.esem[n])] = self.ecnt[n]
        for i, s in enumerate(self.dsem):
            if self.dcnt[i] > 0:
                toks[id(s)] = self.dcnt[i]
        return toks

    def barrier(self, engines=None):
        toks = self.all_tokens()
        for n in (engines or list(self.eng)):
            own = id(self.esem[n])
            t = {k: v for k, v in toks.items() if not (n == "pe" and k == own)}
            self._wait(n, t)


def _consts():
    c = np.zeros((128, 5 * 128 + 64 + 4), np.float32)
    j = np.arange(128)
    same = (j[:, None] // 64) == (j[None, :] // 64)
    c[:, 0:128] = np.eye(128, dtype=np.float32)
    c[:, 128:256] = (same & (j[:, None] <= j[None, :]))
    c[:, 256:384] = (same & (j[:, None] > j[None, :]))
    c[:, 384:512] = (j[:, None] < j[None, :])
    c[:, 512:640] = 1.0
    c[:, 640:672] = np.arange(32)[None, :]
    c[:, 672:704] = (np.arange(32) * CAP)[None, :]
    c[:, 704] = (j < 64)
    c[:, 705] = (j >= 64)
    return c


def prep_shared(inp):
    f = lambda a: np.ascontiguousarray(a, dtype=np.float32)
    w_in = inp["w_in"][0]
    cols = np.concatenate([np.arange(0, O_GLR), np.arange(O_MA, N_IN)])
    wm = w_in[:, cols]
    sh = {}
    sh["w_in_t"] = f(wm.reshape(8, 128, 56, 128).transpose(2, 1, 0, 3).reshape(56, 128, 1024))
    sh["w_glr"] = f(w_in[:, O_GLR:O_GLR + 16].reshape(8, 128, 16).transpose(1, 0, 2).reshape(128, 128))
    chan = np.concatenate([inp["conv_w"][0], inp["conv_b"], inp["lru_b_r"], inp["lru_b_i"],
                           inp["lru_lambda"]], axis=0)
    sh["chanp"] = f(chan.reshape(8, 8, 128).transpose(2, 1, 0).reshape(128, 64))
    sh["lru_wr"] = f(inp["lru_w_r"][0].transpose(1, 0, 2).reshape(128, 1024))
    sh["lru_wi"] = f(inp["lru_w_i"][0].transpose(1, 0, 2).reshape(128, 1024))
    sh["gla_wg"] = f(inp["gla_w_gate"][0])
    sh["gla_bg"] = f(inp["gla_b_gate"])
    sh["gla_ng"] = f(inp["gla_norm_g"][0].reshape(2, 128).T)
    sh["w_out"] = f(inp["w_out"][0].reshape(8, 128, 1024).transpose(1, 0, 2).reshape(128, 8192))
    rows = np.concatenate([inp["ln1_g"], inp["ln1_b"], inp["ln2_g"], inp["ln2_b"],
                           inp["b_ple_gate"]], axis=0)
    sh["rowp"] = f(np.broadcast_to(rows.reshape(1, 5 * 1024), (128, 5 * 1024)))
    sh["w_router"] = f(inp["w_router"][0].reshape(8, 128, 32).transpose(1, 0, 2).reshape(128, 256))
    sh["b_router"] = f(inp["b_router"])
    sh["w_up"] = inp["w_up"][0]
    sh["b_up"] = f(inp["b_up"][0].reshape(32, 16, 128).transpose(2, 0, 1).reshape(128, 512))
    sh["w_down"] = inp["w_down"][0]
    sh["b_down"] = f(inp["b_down"][0])
    sh["w_ple"] = f(inp["w_ple"][0].reshape(2, 128, 1024).transpose(1, 0, 2).reshape(128, 2048))
    sh["w_pg"] = f(inp["w_ple_gate"][0].reshape(8, 128, 1024).transpose(1, 0, 2).reshape(128, 8192))
    sh["consts"] = _consts()
    return sh


def prep_core(inp, b):
    x = np.asarray(inp["x"][b], dtype=np.float32)
    p = np.asarray(inp["p"][0, b], dtype=np.float32)
    return {"x": np.ascontiguousarray(x), "xT": np.ascontiguousarray(x.T),
            "pT": np.ascontiguousarray(p.T)}


SHARED_SHAPES = {
    "w_in_t": [56, 128, 1024], "w_glr": [128, 128], "chanp": [128, 64], "lru_wr": [128, 1024],
    "lru_wi": [128, 1024], "gla_wg": [16, 512], "gla_bg": [1, 512], "gla_ng": [128, 2],
    "w_out": [128, 8192], "rowp": [128, 5120], "w_router": [128, 256], "b_router": [1, 32],
    "w_up": [32, 1024, 2048], "b_up": [128, 512], "w_down": [32, 1024, 1024], "b_down": [32, 1024],
    "w_ple": [128, 2048], "w_pg": [128, 8192], "consts": [128, 708],
}
CORE_SHAPES = {"x": [T, D], "xT": [D, T], "pT": [256, T]}


class StopBuild(Exception):
    pass


class Prog:
    ck_n = 0
    ck_stop = None

    def ck(self, label=""):
        self.ck_n += 1
        if self.ck_stop is not None and self.ck_n >= self.ck_stop:
            if not self.kb.disabled:
                print("STOP at checkpoint", self.ck_n, label)
            self.kb.disabled = True

    def __init__(self, dbg=(), stop_after=None):
        self.dbg = set(dbg)
        self.stop_after = stop_after
        self.nc = nc = bass.Bass("TRN2", target_bir_lowering=False)
        self.din = {}
        for n, s in list(SHARED_SHAPES.items()) + list(CORE_SHAPES.items()):
            self.din[n] = nc.dram_tensor(n, s, F32, kind="ExternalInput").ap()
        self.out = nc.dram_tensor("out", [T, D], F32, kind="ExternalOutput").ap()
        self.dbg_out = {}
        self.es = ExitStack()

    def dbg_tensor(self, name, shape):
        t = self.nc.dram_tensor("dbg_" + name, shape, F32, kind="ExternalOutput").ap()
        self.dbg_out[name] = t
        return t

    def sb(self, es, name, shape, dt=F32):
        return es.enter_context(self.nc.sbuf_tensor(name, shape, dt))

    def build(self):
        nc = self.nc
        with self.es as es:
            kb = self.kb = KB(nc, es)
            self.bound_reg = nc.gpsimd.to_reg(NSLOT - 1)
            self.cst = self.sb(es, "cst", [128, 708])
            self.b_cst = Buf("cst")
            kb.dma("sp", lambda e: e.dma_start(out=self.cst[:], in_=self.din["consts"][:, :]),
                   writes=[self.b_cst])
            self.cstb = self.sb(es, "cstb", [128, 708], BF16)
            self.b_cstb = Buf("cstb")
            kb.op("dve", lambda e: e.tensor_copy(self.cstb[:], self.cst[:]),
                  reads=[self.b_cst], writes=[self.b_cstb])
            self.GATES = self.sb(es, "GATES", [128, 16, 4])
            self.SLOTS = self.sb(es, "SLOTS", [128, 16, 4], U32)
            self.b_route = [Buf("route%d" % i) for i in range(16)]
            self.rowp = self.sb(es, "rowp_sb", [128, 5, 1024])
            self.b_rowp = Buf("rowp")
            kb.dma("sp", lambda e: e.dma_start(out=self.rowp[:], in_=self.din["rowp"].rearrange("p (r d) -> p r d", d=1024)),
                   writes=[self.b_rowp])
            self.Xg = nc.dram_tensor("Xg", [NSLOT, D], BF16).ap()
            self.Yg = nc.dram_tensor("Yg", [NSLOT, D], F32).ap()
            self.H1d = nc.dram_tensor("H1d", [T, D], F32).ap()
            self.b_Xg, self.b_Yg, self.b_H1d = Buf("Xg"), Buf("Yg"), Buf("H1d")
            self.b_Xgz = Buf("Xgz")
            self.zero_xg(es)
            with ExitStack() as es_y:
                self.yT = self.sb(es_y, "yT", [128, 8, T], BF16)
                self.b_yT = [Buf("yT%d" % g) for g in range(8)]
                with ExitStack() as es1:
                    self.alloc_psum(es1, 8, 0)
                    self.XT = self.sb(es1, "XT", [128, 8, T], BF16)
                    self.b_XT = Buf("XT")
                    xT = self.din["xT"].rearrange("(kc p) t -> p kc t", p=128)
                    for kc in range(8):
                        kb.dma("pool", lambda e, kc=kc: e.dma_start(out=self.XT[:, kc, :], in_=xT[:, kc, :]),
                               cw=[self.b_XT])
                    if not getattr(self, "skip_lru", False):
                        self.phase_lru(es1)
                    else:
                        kb.op("dve", lambda e: e.memset(self.yT[:], 0.0), writes=self.b_yT)
                    if self.stop_after == "lru":
                        return self.finish()
                    kb.barrier()
                    self.phase_gla(es1)
                    if self.stop_after == "gla":
                        return self.dump_yT()
                kb.barrier()
                self.phase_outproj()
                if self.stop_after == "outproj":
                    return self.finish()
            kb.barrier()
            self.phase_moe()
            if self.stop_after == "moe":
                return self.finish()
            kb.barrier()
            self.phase_final()
        return self.finish()

    def alloc_psum(self, es, n32, n16):
        nc = self.nc
        self.ps = [es.enter_context(nc.psum_tensor("ps%d_%d" % (i, self.ps_gen), [128, 512], F32)) for i in range(n32)]
        self.psb = [Buf("ps%d" % i) for i in range(n32)]
        self.pst = [es.enter_context(nc.psum_tensor("pst%d_%d" % (i, self.ps_gen), [128, 1024], BF16)) for i in range(n16)]
        self.pstb = [Buf("pst%d" % i) for i in range(n16)]
        self.ps_rr = 0
        self.pst_rr = 0
        self.ps_gen += 1

    def zero_xg(self, es):
        kb = self.kb
        z = self.sb(es, "zeros", [128, 4, 1024], BF16)
        bz = Buf("zeros")
        kb.op("dve", lambda e: e.memset(z[:], 0.0), writes=[bz])
        xg = self.Xg.rearrange("(n p) d -> p n d", p=128)
        for i in range(NSLOT // 128 // 4):
            kb.dma("sp", lambda e, i=i: e.dma_start(out=xg[:, i * 4:(i + 1) * 4, :], in_=z[:]), reads=[bz], cw=[self.b_Xgz])

    ps_gen = 0

    def bank(self):
        i = self.ps_rr
        self.ps_rr = (self.ps_rr + 1) % len(self.ps)
        return self.ps[i], self.psb[i]

    def bank16(self):
        i = self.pst_rr
        self.pst_rr = (self.pst_rr + 1) % len(self.pst)
        return self.pst[i], self.pstb[i]

    def finish(self):
        kb = self.kb
        kb.disabled = False
        kb.barrier(["sp"])
        return self.nc

    def dump_yT(self):
        kb = self.kb
        kb.barrier()
        with ExitStack() as es:
            tmp = self.sb(es, "dump_tmp", [128, 8, T])
            b = Buf("dump_tmp")
            kb.op("dve", lambda e: e.tensor_copy(tmp[:], self.yT[:]), reads=self.b_yT, writes=[b])
            t = self.dbg_tensor("yT", [128, 8, T])
            kb.dma("sp", lambda e: e.dma_start(out=t, in_=tmp[:]), reads=[b])
            return self.finish()

    def dump(self, name, sb_ap, buf, shape):
        if name not in self.dbg:
            return
        t = self.dbg_tensor(name, shape)
        self.kb.dma("sp", lambda e: e.dma_start(out=t, in_=sb_ap), reads=[buf])

    def inproj_fm(self, wt, wb, ncols, tg, evac):
        kb = self.kb
        ps, pb = self.bank()
        for kc in range(8):
            kb.op("pe", lambda e, kc=kc: e.matmul(ps[0:ncols, :], wt[:, kc * ncols:(kc + 1) * ncols],
                                                   self.XT[:, kc, tg * 512:(tg + 1) * 512],
                                                   start=(kc == 0), stop=(kc == 7)),
                  reads=[wb, self.b_XT], writes=[pb], inc=(kc == 7))
        evac(ps, pb)

    def load_w(self, grp):
        i = self.w_rr
        self.w_rr = (self.w_rr + 1) % len(self.wring)
        wt, wb = self.wring[i], self.wringb[i]
        self.kb.dma("pool", lambda e: e.dma_start(out=wt[:], in_=self.din["w_in_t"][grp, :, :]), writes=[wb])
        return wt, wb

    def phase_lru(self, es1):
        nc, kb = self.nc, self.kb
        with ExitStack() as es:
            NW = 6
            self.wring = [self.sb(es, "wr%d" % i, [128, 1024], BF16) for i in range(NW)]
            self.wringb = [Buf("wr%d" % i) for i in range(NW)]
            self.w_rr = 0
            chan = self.sb(es, "chan", [128, 8, 8])
            b_chan = Buf("chan")
            kb.dma("sp", lambda e: e.dma_start(out=chan[:], in_=self.din["chanp"].rearrange("p (g k) -> p g k", k=8)),
                   writes=[b_chan])
            wr = self.sb(es, "lwr", [128, 8, 128], BF16)
            wi = self.sb(es, "lwi", [128, 8, 128], BF16)
            b_wr, b_wi = Buf("lwr"), Buf("lwi")
            kb.dma("pool", lambda e: e.dma_start(out=wr[:], in_=self.din["lru_wr"].rearrange("p (g d) -> p g d", d=128)), writes=[b_wr])
            kb.dma("pool", lambda e: e.dma_start(out=wi[:], in_=self.din["lru_wi"].rearrange("p (g d) -> p g d", d=128)), writes=[b_wi])
            sc = self.sb(es, "lsc", [128, 8, 4])
            b_sc = Buf("lsc")
            kb.op("act", lambda e: e.activation(out=sc[:, :, 0], in_=chan[:, :, 7], func=AF.Exp, scale=-1.0),
                  reads=[b_chan], writes=[b_sc])
            kb.op("act", lambda e: e.activation(out=sc[:, :, 1], in_=sc[:, :, 0], func=AF.Ln, bias=1.0),
                  reads=[b_sc], writes=[b_sc])
            kb.op("dve", lambda e: e.tensor_scalar_mul(sc[:, :, 2], sc[:, :, 1], -8.0), reads=[b_sc], writes=[b_sc])
            kb.op("dve", lambda e: e.tensor_scalar_mul(sc[:, :, 3], sc[:, :, 1], -16.0), reads=[b_sc], writes=[b_sc])

            names = ["xa", "xc", "r", "i", "a", "m", "u", "h", "ga", "t1", "t2"]
            A = {n: self.sb(es, "L_" + n, [128, T + (3 if n == "xa" else 0)]) for n in names}
            B = {n: Buf("L_" + n) for n in names}
            xcb = self.sb(es, "L_xcb", [128, T], BF16)
            b_xcb = Buf("L_xcb")
            kb.op("dve", lambda e: e.memset(A["xa"][:, 0:3], 0.0), writes=[B["xa"]])

            for g in range(8):
                w_xa, wb_xa = self.load_w(g)
                w_ga, wb_ga = self.load_w(8 + g)
                w_ma, wb_ma = self.load_w(40 + g)
                for tg in range(4):
                    self.inproj_fm(w_xa, wb_xa, 128, tg, lambda ps, pb, tg=tg: kb.op(
                        "act", lambda e: e.activation(out=A["xa"][:, 3 + tg * 512:3 + (tg + 1) * 512], in_=ps[:, :], func=AF.Copy),
                        reads=[pb], cw=[B["xa"]]))
                kb.op("dve", lambda e: e.tensor_scalar(A["xc"][:, :], A["xa"][:, 0:T], chan[:, g, 0:1], chan[:, g, 4:5],
                                                       ALU.mult, ALU.add),
                      reads=[B["xa"], b_chan], writes=[B["xc"]])
                for k in range(1, 4):
                    kb.op("dve", lambda e, k=k: e.scalar_tensor_tensor(A["xc"][:, :], A["xa"][:, k:k + T], chan[:, g, k:k + 1],
                                                                      A["xc"][:, :], ALU.mult, ALU.add),
                          reads=[B["xa"], B["xc"], b_chan], writes=[B["xc"]])
                kb.op("act", lambda e: e.activation(out=xcb[:, :], in_=A["xc"][:, :], func=AF.Copy),
                      reads=[B["xc"]], writes=[b_xcb])
                self.dump("xc%d" % g, A["xc"][:, :], B["xc"], [128, T])
                for (wg, bwg, dst, bi) in ((wr, b_wr, "r", 5), (wi, b_wi, "i", 6)):
                    for tg in range(4):
                        ps, pb = self.bank()
                        kb.op("pe", lambda e, tg=tg, ps=ps, wg=wg: e.matmul(ps[:, :], wg[:, g, :], xcb[:, tg * 512:(tg + 1) * 512],
                                                                          start=True, stop=True),
                              reads=[bwg, b_xcb], writes=[pb])
                        kb.op("act", lambda e, tg=tg, ps=ps, dst=dst, bi=bi: e.activation(
                            out=A[dst][:, tg * 512:(tg + 1) * 512], in_=ps[:, :], func=AF.Sigmoid, bias=chan[:, g, bi:bi + 1]),
                            reads=[pb, b_chan], cw=[B[dst]])
                kb.op("act", lambda e: e.activation(out=A["a"][:, :], in_=A["r"][:, :], func=AF.Exp, scale=sc[:, g, 2:3]),
                      reads=[B["r"], b_sc], writes=[B["a"]])
                kb.op("act", lambda e: e.activation(out=A["m"][:, :], in_=A["r"][:, :], func=AF.Exp, scale=sc[:, g, 3:4]),
                      reads=[B["r"], b_sc], writes=[B["m"]])
                kb.op("act", lambda e: e.activation(out=A["m"][:, :], in_=A["m"][:, :], func=AF.Sqrt, scale=-1.0, bias=1.0),
                      reads=[B["m"]], writes=[B["m"]])
                kb.op("dve", lambda e: e.tensor_tensor(A["u"][:, :], A["m"][:, :], A["i"][:, :], ALU.mult),
                      reads=[B["m"], B["i"]], writes=[B["u"]])
                kb.op("dve", lambda e: e.tensor_tensor(A["u"][:, :], A["u"][:, :], A["xc"][:, :], ALU.mult),
                      reads=[B["u"], B["xc"]], writes=[B["u"]])
                kb.op("dve", lambda e: e.tensor_tensor_scan(A["h"][:, :], A["a"][:, :], A["u"][:, :], 0.0, ALU.mult, ALU.add),
                      reads=[B["a"], B["u"]], writes=[B["h"]])
                self.dump("h%d" % g, A["h"][:, :], B["h"], [128, T])
                for tg in range(4):
                    self.inproj_fm(w_ga, wb_ga, 128, tg, lambda ps, pb, tg=tg: kb.op(
                        "act", lambda e: e.activation(out=A["ga"][:, tg * 512:(tg + 1) * 512], in_=ps[:, :], func=AF.Copy),
                        reads=[pb], cw=[B["ga"]]))
                kb.op("dve", lambda e: e.tensor_tensor(A["t1"][:, :], A["ga"][:, :], A["ga"][:, :], ALU.mult),
                      reads=[B["ga"]], writes=[B["t1"]])
                kb.op("dve", lambda e: e.tensor_scalar(A["t1"][:, :], A["t1"][:, :], 0.044715, 1.0, ALU.mult, ALU.add),
                      reads=[B["t1"]], writes=[B["t1"]])
                kb.op("dve", lambda e: e.tensor_tensor(A["t1"][:, :], A["t1"][:, :], A["ga"][:, :], ALU.mult),
                      reads=[B["t1"], B["ga"]], writes=[B["t1"]])
                kb.op("act", lambda e: e.activation(out=A["t1"][:, :], in_=A["t1"][:, :], func=AF.Sigmoid, scale=1.5957691216057308),
                      reads=[B["t1"]], writes=[B["t1"]])
                kb.op("dve", lambda e: e.tensor_tensor(A["t1"][:, :], A["t1"][:, :], A["ga"][:, :], ALU.mult),
                      reads=[B["t1"], B["ga"]], writes=[B["t1"]])
                kb.op("dve", lambda e: e.tensor_tensor(A["t1"][:, :], A["t1"][:, :], A["h"][:, :], ALU.mult),
                      reads=[B["t1"], B["h"]], writes=[B["t1"]])
                for tg in range(4):
                    self.inproj_fm(w_ma, wb_ma, 128, tg, lambda ps, pb, tg=tg: kb.op(
                        "act", lambda e: e.activation(out=A["t2"][:, tg * 512:(tg + 1) * 512], in_=ps[:, :], func=AF.Sigmoid),
                        reads=[pb], cw=[B["t2"]]))
                kb.op("dve", lambda e: e.tensor_tensor(self.yT[:, g, :], A["t1"][:, :], A["t2"][:, :], ALU.mult),
                      reads=[B["t1"], B["t2"]], writes=[self.b_yT[g]])
                self.dump("ya%d" % g, A["t1"][:, :], B["t1"], [128, T])

    def phase_gla(self, es1):
        nc, kb = self.nc, self.kb
        cst = self.cstb
        TRI, UU, ONES = cst[:, 128:256], cst[:, 256:384], cst[:, 512:640]
        TRI32 = self.cst[:, 128:256]
        bc = self.b_cstb
        with ExitStack() as es:
            NW = 8
            self.wring = [self.sb(es, "gw%d" % i, [128, 1024], BF16) for i in range(NW)]
            self.wringb = [Buf("gw%d" % i) for i in range(NW)]
            self.w_rr = 0
            wglr = self.sb(es, "wglr", [128, 128], BF16)
            b_wglr = Buf("wglr")
            kb.dma("pool", lambda e: e.dma_start(out=wglr[:], in_=self.din["w_glr"][:, :]), writes=[b_wglr])
            wg = self.sb(es, "wg", [16, 512], BF16)
            bg = self.sb(es, "bg", [1, 512], BF16)
            ng = self.sb(es, "ng", [128, 2])
            b_wg, b_bg, b_ng = Buf("wg"), Buf("bg"), Buf("ng")
            kb.dma("pool", lambda e: e.dma_start(out=wg[:], in_=self.din["gla_wg"][:, :]), writes=[b_wg])
            kb.dma("pool", lambda e: e.dma_start(out=bg[:], in_=self.din["gla_bg"][:, :]), writes=[b_bg])
            kb.dma("sp", lambda e: e.dma_start(out=ng[:], in_=self.din["gla_ng"][:, :]), writes=[b_ng])
            glrT = self.sb(es, "glrT", [16, T], BF16)
            b_glrT = Buf("glrT")
            for tg in range(4):
                self.inproj_fm(wglr, b_wglr, 16, tg, lambda ps, pb, tg=tg: kb.op(
                    "act", lambda e: e.activation(out=glrT[:, tg * 512:(tg + 1) * 512], in_=ps[0:16, :], func=AF.Copy),
                    reads=[pb], cw=[b_glrT]))

            self.ck("glrT")

            def mk(name, shape, dt=F32):
                return self.sb(es, "G_" + name, shape, dt), Buf("G_" + name)
            QT, b_QT = mk("QT", [128, T], BF16)
            KT, b_KT = mk("KT", [128, T], BF16)
            KD0, b_KD0 = mk("KD0", [128, 16, 128], BF16)
            KD1, b_KD1 = mk("KD1", [128, 16, 128], BF16)
            V, b_V = mk("V", [128, 16, 256], BF16)
            OT, b_OT = mk("OT", [128, 2, T])
            EB, b_EB = mk("EB", [128, 32])
            Gsp, b_Gsp = mk("Gsp", [128, 4, 128], BF16)
            Gz, b_Gz = mk("Gz", [128, 4, 128])
            EQ, b_EQ = mk("EQ", [128, 512])
            EK, b_EK = mk("EK", [128, 512])
            ED, b_ED = mk("ED", [128, 4, 128])
            STs = [mk("ST%d" % i, [128, 128], BF16) for i in range(2)]
            Sb = [mk("S%d" % i, [128, 256]) for i in range(4)]
            Sbb = [mk("Sb%d" % i, [128, 256], BF16) for i in range(4)]
            SQ = [mk("SQ%d" % i, [128, 512], BF16) for i in range(2)]
            RI, b_RI = mk("RI", [128, 512])
            SG, b_SG = mk("SG", [128, 512])
            SM, b_SM = mk("SM", [128, 512])
            TT, b_TT = mk("TT", [128, 512])

            for hd in range(4):
                w_q, wb_q = self.load_w(16 + hd)
                w_k, wb_k = self.load_w(20 + hd)
                w_v = [self.load_w(24 + 2 * hd + j) for j in range(2)]
                for tg in range(4):
                    ps, pb = self.bank()
                    for j in range(4):
                        tt = tg * 4 + j
                        kb.op("pe", lambda e, j=j, tt=tt, ps=ps: e.matmul(ps[:, j * 128:(j + 1) * 128], glrT[0:16, tt * 128:(tt + 1) * 128],
                                                                      wg[0:16, hd * 128:(hd + 1) * 128], start=True, stop=False),
                              reads=[b_glrT, b_wg], writes=[pb], inc=False)
                        kb.op("pe", lambda e, j=j, ps=ps: e.matmul(ps[:, j * 128:(j + 1) * 128], cst[0:1, 512:640],
                                                               bg[0:1, hd * 128:(hd + 1) * 128], start=False, stop=True),
                              reads=[bc, b_bg], writes=[pb], inc=(j == 3))
                    kb.op("act", lambda e, ps=ps: e.activation(out=Gz[:, :, :], in_=ps[:, :].rearrange("p (j k) -> p j k", k=128),
                                                              func=AF.Exp, scale=-1.0), reads=[pb], writes=[b_Gz])
                    kb.op("act", lambda e: e.activation(out=Gsp[:, :, :], in_=Gz[:, :, :], func=AF.Ln, bias=1.0),
                          reads=[b_Gz], writes=[b_Gsp])
                    self.ck("z/Gsp")
                    ps_c, pb_c = self.bank()
                    ps_r, pb_r = self.bank()
                    for j in range(4):
                        kb.op("pe", lambda e, j=j, ps_c=ps_c: e.matmul(ps_c[:, j * 128:(j + 1) * 128], Gsp[:, j, :], TRI, start=True, stop=True),
                              reads=[b_Gsp, bc], writes=[pb_c], inc=False)
                        kb.op("pe", lambda e, j=j, ps_r=ps_r: e.matmul(ps_r[:, j * 128:(j + 1) * 128], UU, Gsp[:, j, :], start=True, stop=True),
                              reads=[b_Gsp, bc], writes=[pb_r], inc=(j == 3))
                    kb.op("act", lambda e, ps_c=ps_c: e.activation(out=EQ[:, :], in_=ps_c[:, :], func=AF.Exp, scale=-1.0 / 16), reads=[pb_c], writes=[b_EQ])
                    kb.op("act", lambda e, ps_c=ps_c: e.activation(out=EK[:, :], in_=ps_c[:, :], func=AF.Exp, scale=1.0 / 16), reads=[pb_c], writes=[b_EK])
                    kb.op("act", lambda e, ps_r=ps_r: e.activation(out=ED[:, :, :], in_=ps_r[:, :].rearrange("p (j k) -> p j k", k=128),
                                                                func=AF.Exp, scale=-1.0 / 16), reads=[pb_r], writes=[b_ED])
                    kb.op("dve", lambda e, tg=tg: e.tensor_copy(EB[:, tg * 8:(tg + 1) * 8], EQ[:, 63:512:64]), reads=[b_EQ], cw=[b_EB])
                    self.ck("cs/rev/E")
                    self.inproj_fm(w_q, wb_q, 128, tg, lambda ps, pb, tg=tg: kb.op(
                        "dve", lambda e: e.scalar_tensor_tensor(QT[:, tg * 512:(tg + 1) * 512], ps[:, :], 128.0 ** -0.5, EQ[:, :], ALU.mult, ALU.mult),
                        reads=[pb, b_EQ], cw=[b_QT]))
                    self.inproj_fm(w_k, wb_k, 128, tg, lambda ps, pb, tg=tg: kb.op(
                        "dve", lambda e: e.tensor_tensor(KT[:, tg * 512:(tg + 1) * 512], ps[:, :], EK[:, :], ALU.mult),
                        reads=[pb, b_EK], cw=[b_KT]))
                    self.ck("qk fm")
                    ps, pb = self.bank()
                    for j in range(4):
                        tt = tg * 4 + j
                        for kc in range(8):
                            kb.op("pe", lambda e, j=j, tt=tt, kc=kc, ps=ps: e.matmul(ps[:, j * 128:(j + 1) * 128], self.XT[:, kc, tt * 128:(tt + 1) * 128],
                                                                                 w_k[:, kc * 128:(kc + 1) * 128], start=(kc == 0), stop=(kc == 7)),
                                  reads=[self.b_XT, wb_k], writes=[pb], inc=(j == 3 and kc == 7))
                    for KDm, b_KDm, mcol in ((KD0, b_KD0, 704), (KD1, b_KD1, 705)):
                        kb.op("dve", lambda e, ps=ps, tg=tg, KDm=KDm, mcol=mcol: e.scalar_tensor_tensor(
                            KDm[:, tg * 4:(tg + 1) * 4, :], ps[:, :].rearrange("p (j k) -> p j k", k=128), self.cst[:, mcol:mcol + 1],
                            ED[:, :, :], ALU.mult, ALU.mult), reads=[pb, b_ED, self.b_cst], cw=[b_KDm])
                    self.ck("kd")
                    for jj in range(2):
                        ps, pb = self.bank()
                        for j2 in range(2):
                            tt = tg * 4 + jj * 2 + j2
                            for half in range(2):
                                wv, wbv = w_v[half]
                                for kc in range(8):
                                    kb.op("pe", lambda e, j2=j2, tt=tt, kc=kc, ps=ps, half=half, wv=wv: e.matmul(
                                        ps[:, j2 * 256 + half * 128:j2 * 256 + (half + 1) * 128], self.XT[:, kc, tt * 128:(tt + 1) * 128],
                                        wv[:, kc * 128:(kc + 1) * 128], start=(kc == 0), stop=(kc == 7)),
                                        reads=[self.b_XT, wbv], writes=[pb], inc=(j2 == 1 and half == 1 and kc == 7))
                        t0 = tg * 4 + jj * 2
                        kb.op("dve", lambda e, ps=ps, t0=t0: e.tensor_copy(V[:, t0:t0 + 2, :], ps[:, :].rearrange("p (j v) -> p j v", v=256)),
                              reads=[pb], cw=[b_V])
                    self.ck("v")
                kb.op("dve", lambda e: e.memset(Sb[0][0][:, :], 0.0), writes=[Sb[0][1]])
                kb.op("dve", lambda e: e.memset(Sbb[0][0][:, :], 0.0), writes=[Sbb[0][1]])
                for tt in range(16):
                    c0, c1 = 2 * tt, 2 * tt + 1
                    ps_st, pb_st = self.ps[tt % 2], self.psb[tt % 2]
                    st, b_st = STs[tt % 2]
                    kb.op("pe", lambda e: e.matmul(ps_st[:, 0:128], KT[:, tt * 128:(tt + 1) * 128], QT[:, tt * 128:(tt + 1) * 128], start=True, stop=True),
                          reads=[b_KT, b_QT], writes=[pb_st])
                    kb.op("dve", lambda e: e.tensor_tensor(st[:, :], ps_st[:, 0:128], TRI32, ALU.mult), reads=[pb_st, self.b_cst], writes=[b_st])
                    ps_kv, pb_kv = self.ps[2 + tt % 2], self.psb[2 + tt % 2]
                    kb.op("pe", lambda e: e.matmul(ps_kv[:, 0:256], KD0[:, tt, :], V[:, tt, :], start=True, stop=True),
                          reads=[b_KD0, b_V], writes=[pb_kv], inc=False)
                    kb.op("pe", lambda e: e.matmul(ps_kv[:, 256:512], KD1[:, tt, :], V[:, tt, :], start=True, stop=True),
                          reads=[b_KD1, b_V], writes=[pb_kv])
                    self.ck("st/kv")
                    grp = (tt // 4) % 2
                    col = (tt % 4) * 128
                    for vc in range(2):
                        pso, pbo = self.ps[4 + 2 * grp + vc], self.psb[4 + 2 * grp + vc]
                        kb.op("pe", lambda e, vc=vc, pso=pso: e.matmul(pso[:, col:col + 128], V[:, tt, vc * 128:(vc + 1) * 128], st[:, :], start=True, stop=False),
                              reads=[b_V, b_st], writes=[pbo], inc=False)
                        kb.op("pe", lambda e, vc=vc, pso=pso: e.matmul(pso[:, col:col + 64], Sbb[c0 % 4][0][:, vc * 128:(vc + 1) * 128], QT[:, c0 * 64:(c0 + 1) * 64],
                                                                    start=False, stop=False), reads=[Sbb[c0 % 4][1], b_QT], writes=[pbo], inc=False)
                        if vc == 0:
                            kb.op("dve", lambda e: e.scalar_tensor_tensor(Sb[c1 % 4][0][:, :], Sb[c0 % 4][0][:, :], EB[:, c0:c0 + 1], ps_kv[:, 0:256], ALU.mult, ALU.add),
                                  reads=[Sb[c0 % 4][1], b_EB, pb_kv], writes=[Sb[c1 % 4][1]])
                            kb.op("act", lambda e: e.activation(out=Sbb[c1 % 4][0][:, :], in_=Sb[c1 % 4][0][:, :], func=AF.Copy),
                                  reads=[Sb[c1 % 4][1]], writes=[Sbb[c1 % 4][1]])
                        kb.op("pe", lambda e, vc=vc, pso=pso: e.matmul(pso[:, col + 64:col + 128], Sbb[c1 % 4][0][:, vc * 128:(vc + 1) * 128], QT[:, c1 * 64:(c1 + 1) * 64],
                                                                    start=False, stop=True), reads=[Sbb[c1 % 4][1], b_QT], writes=[pbo], inc=True)
                    kb.op("dve", lambda e: e.scalar_tensor_tensor(Sb[(c1 + 1) % 4][0][:, :], Sb[c1 % 4][0][:, :], EB[:, c1:c1 + 1], ps_kv[:, 256:512], ALU.mult, ALU.add),
                          reads=[Sb[c1 % 4][1], b_EB, pb_kv], writes=[Sb[(c1 + 1) % 4][1]])
                    kb.op("act", lambda e: e.activation(out=Sbb[(c1 + 1) % 4][0][:, :], in_=Sb[(c1 + 1) % 4][0][:, :], func=AF.Copy),
                          reads=[Sb[(c1 + 1) % 4][1]], writes=[Sbb[(c1 + 1) % 4][1]])
                    self.ck("o tile")
                    if tt % 4 == 3:
                        tg = tt // 4
                        for vc in range(2):
                            pso, pbo = self.ps[4 + 2 * grp + vc], self.psb[4 + 2 * grp + vc]
                            kb.op("act", lambda e, vc=vc, pso=pso, tg=tg: e.activation(out=OT[:, vc, tg * 512:(tg + 1) * 512], in_=pso[:, :], func=AF.Copy),
                                  reads=[pbo], cw=[b_OT])
                if hd == 0:
                    self.dump("o_raw0", OT[:, 0, :], b_OT, [128, T])
                w_go = [self.load_w(32 + 2 * hd + j) for j in range(2)]
                w_mb = [self.load_w(48 + 2 * hd + j) for j in range(2)]
                for tg in range(4):
                    sl = slice(tg * 512, (tg + 1) * 512)
                    for vc in range(2):
                        kb.op("act", lambda e, vc=vc: e.activation(out=SQ[vc][0][:, :], in_=OT[:, vc, sl], func=AF.Square), reads=[b_OT], writes=[SQ[vc][1]])
                    ps, pb = self.bank()
                    for vc in range(2):
                        kb.op("pe", lambda e, vc=vc, ps=ps: e.matmul(ps[:, :], ONES, SQ[vc][0][:, :], start=(vc == 0), stop=(vc == 1)),
                              reads=[bc, SQ[vc][1]], writes=[pb], inc=(vc == 1))
                    kb.op("act", lambda e, ps=ps: e.activation(out=RI[:, :], in_=ps[:, :], func=AF.Sqrt, scale=1.0 / 256, bias=1e-5), reads=[pb], writes=[b_RI])
                    kb.op("dve", lambda e: e.reciprocal(RI[:, :], RI[:, :]), reads=[b_RI], writes=[b_RI])
                    for vc in range(2):
                        self.inproj_fm(w_go[vc][0], w_go[vc][1], 128, tg, lambda ps, pb: kb.op(
                            "act", lambda e: e.activation(out=SG[:, :], in_=ps[:, :], func=AF.Silu), reads=[pb], writes=[b_SG]))
                        self.inproj_fm(w_mb[vc][0], w_mb[vc][1], 128, tg, lambda ps, pb: kb.op(
                            "act", lambda e: e.activation(out=SM[:, :], in_=ps[:, :], func=AF.Sigmoid), reads=[pb], writes=[b_SM]))
                        kb.op("dve", lambda e, vc=vc: e.scalar_tensor_tensor(TT[:, :], OT[:, vc, sl], ng[:, vc:vc + 1], RI[:, :], ALU.mult, ALU.mult),
                              reads=[b_OT, b_ng, b_RI], writes=[b_TT])
                        kb.op("dve", lambda e: e.tensor_tensor(TT[:, :], TT[:, :], SG[:, :], ALU.mult), reads=[b_TT, b_SG], writes=[b_TT])
                        kb.op("dve", lambda e: e.tensor_tensor(TT[:, :], TT[:, :], SM[:, :], ALU.mult), reads=[b_TT, b_SM], writes=[b_TT])
                        g = hd * 2 + vc
                        kb.op("dve", lambda e, g=g: e.tensor_tensor(self.yT[:, g, sl], TT[:, :], self.yT[:, g, sl], ALU.add),
                              reads=[b_TT, self.b_yT[g]], writes=[self.b_yT[g]])

    def layer_norm(self, es_tmp, R, b_R, grow, brow, OUT, b_OUT, tag):
        kb = self.kb
        st = self.ln_st
        kb.op("dve", lambda e: e.bn_stats(st["stats"][:, 0, :], R[:, 0:512]), reads=[b_R], writes=[st["b"]])
        kb.op("dve", lambda e: e.bn_stats(st["stats"][:, 1, :], R[:, 512:1024]), reads=[b_R], writes=[st["b"]])
        kb.op("dve", lambda e: e.bn_aggr(st["mv"][:, :], st["stats"][:, :, :].rearrange("p a b -> p (a b)")), reads=[st["b"]], writes=[st["b"]])
        kb.op("act", lambda e: e.activation(out=st["rs"][:, 0:1], in_=st["mv"][:, 1:2], func=AF.Sqrt, bias=1e-5), reads=[st["b"]], writes=[st["b2"]])
        kb.op("dve", lambda e: e.reciprocal(st["rs"][:, 0:1], st["rs"][:, 0:1]), reads=[st["b2"]], writes=[st["b2"]])
        kb.op("dve", lambda e: e.scalar_tensor_tensor(st["rs"][:, 1:2], st["mv"][:, 0:1], -1.0, st["rs"][:, 0:1], ALU.mult, ALU.mult),
              reads=[st["b"], st["b2"]], writes=[st["b2"]])
        kb.op("act", lambda e: e.activation(out=OUT[:, :], in_=R[:, :], func=AF.Identity, scale=st["rs"][:, 0:1], bias=st["rs"][:, 1:2]),
              reads=[b_R, st["b2"]], writes=[b_OUT])
        kb.op("dve", lambda e: e.tensor_tensor(OUT[:, :], OUT[:, :], self.rowp[:, grow, :], ALU.mult), reads=[b_OUT, self.b_rowp], writes=[b_OUT])
        kb.op("dve", lambda e: e.tensor_tensor(OUT[:, :], OUT[:, :], self.rowp[:, brow, :], ALU.add), reads=[b_OUT, self.b_rowp], writes=[b_OUT])

    def alloc_ln(self, es):
        g = self.ps_gen
        self.ln_st = {"stats": self.sb(es, "ln_stats%d" % g, [128, 2, 6]), "mv": self.sb(es, "ln_mv%d" % g, [128, 2]),
                      "rs": self.sb(es, "ln_rs%d" % g, [128, 2]), "b": Buf("ln_b"), "b2": Buf("ln_b2")}

    def phase_outproj(self):
        nc, kb = self.nc, self.kb
        cstb, bcb = self.cstb, self.b_cstb
        IDb, LTb, ONEb = cstb[:, 0:128], cstb[:, 384:512], cstb[:, 512:640]
        with ExitStack() as es:
            self.alloc_psum(es, 6, 2)
            self.alloc_ln(es)

            def mk(name, shape, dt=F32):
                return self.sb(es, "P2_" + name, shape, dt), Buf("P2_" + name)
            Wout, b_Wout = mk("Wout", [128, 8, 1024], BF16)
            kb.dma("pool", lambda e: e.dma_start(out=Wout[:, 0:4, :], in_=self.din["w_out"].rearrange("p (k n) -> p k n", n=1024)[:, 0:4, :]), cw=[b_Wout])
            kb.dma("pool", lambda e: e.dma_start(out=Wout[:, 4:8, :], in_=self.din["w_out"].rearrange("p (k n) -> p k n", n=1024)[:, 4:8, :]), cw=[b_Wout])
            wr32, b_wr32 = mk("wr32", [128, 8, 32])
            wrh, b_wrh = mk("wrh", [128, 8, 32], BF16)
            wrl, b_wrl = mk("wrl", [128, 8, 32], BF16)
            kb.dma("sp", lambda e: e.dma_start(out=wr32[:], in_=self.din["w_router"].rearrange("p (k n) -> p k n", n=32)), writes=[b_wr32])
            kb.op("dve", lambda e: e.tensor_copy(wrh[:], wr32[:]), reads=[b_wr32], writes=[b_wrh])
            kb.op("dve", lambda e: e.tensor_tensor(wrl[:], wr32[:], wrh[:], ALU.subtract), reads=[b_wr32, b_wrh], writes=[b_wrl])
            brt, b_brt = mk("brt", [128, 32])
            kb.dma("sp", lambda e: e.dma_start(out=brt[:], in_=self.din["b_router"][0:1, :].partition_broadcast(128)), writes=[b_brt])
            carry, b_carry = mk("carry", [128, 32])
            kb.op("dve", lambda e: e.memset(carry[:], 0.0), writes=[b_carry])
            Xt = [mk("x%d" % i, [128, 1024]) for i in range(2)]
            R, b_R = mk("R", [128, 1024])
            H1 = [mk("H1_%d" % i, [128, 1024]) for i in range(2)]
            H1b = [mk("H1b_%d" % i, [128, 1024], BF16) for i in range(2)]
            H1l = [mk("H1l_%d" % i, [128, 1024], BF16) for i in range(2)]
            HT = [mk("HT_%d" % i, [128, 8, 128], BF16) for i in range(2)]
            lg, b_lg = mk("lg", [128, 32])
            v8, b_v8 = mk("v8", [128, 8])
            i8, b_i8 = mk("i8", [128, 8], U32)
            i8f, b_i8f = mk("i8f", [128, 8])
            sm, b_sm = mk("sm", [128, 8])
            mask, b_mask = mk("mask", [128, 32], BF16)
            sc, b_sc = mk("sc", [128, 32])
            ov, b_ov = mk("ov", [128, 32])
            junk, b_junk = mk("junk", [128, 32])
            slf, b_slf = mk("slf", [128, 4])

            for tt in range(16):
                tsl = slice(tt * 128, (tt + 1) * 128)
                xt, b_xt = Xt[tt % 2]
                kb.dma("sp", lambda e: e.dma_start(out=xt[:], in_=self.din["x"][tsl, :]), writes=[b_xt])
                for half in range(2):
                    ps, pb = self.bank()
                    for kc in range(8):
                        kb.op("pe", lambda e, kc=kc, ps=ps, half=half: e.matmul(ps[:, :], self.yT[:, kc, tsl], Wout[:, kc, half * 512:(half + 1) * 512],
                                                                             start=(kc == 0), stop=(kc == 7)),
                              reads=[self.b_yT[kc], b_Wout], writes=[pb], inc=(kc == 7))
                    kb.op("dve", lambda e, ps=ps, half=half: e.scalar_tensor_tensor(R[:, half * 512:(half + 1) * 512], xt[:, half * 512:(half + 1) * 512], ALPHA,
                                                                                  ps[:, :], ALU.mult, ALU.add), reads=[b_xt, pb], cw=[b_R])
                h1, b_h1 = H1[tt % 2]
                self.layer_norm(es, R, b_R, 0, 1, h1, b_h1, "ln1")
                kb.dma("sp", lambda e: e.dma_start(out=self.H1d[tsl, :], in_=h1[:]), reads=[b_h1], cw=[self.b_H1d])
                if tt == 0:
                    self.dump("h1_0", h1[:], b_h1, [128, 1024])
                hb, b_hb = H1b[tt % 2]
                hl, b_hl = H1l[tt % 2]
                kb.op("act", lambda e: e.activation(out=hb[:, :], in_=h1[:, :], func=AF.Identity), reads=[b_h1], writes=[b_hb])
                kb.op("dve", lambda e: e.tensor_tensor(hl[:, :], h1[:, :], hb[:, :], ALU.subtract), reads=[b_h1, b_hb], writes=[b_hl])
                for (src, b_src, (dst, b_dst)) in ((hb, b_hb, HT[0]), (hl, b_hl, HT[1])):
                    pt, ptb = self.bank16()
                    for kc in range(8):
                        kb.op("pe", lambda e, kc=kc, pt=pt, src=src: e.transpose(pt[:, kc * 128:(kc + 1) * 128], src[:, kc * 128:(kc + 1) * 128], IDb),
                              reads=[b_src, bcb], writes=[ptb], inc=(kc == 7))
                    kb.op("dve", lambda e, pt=pt, dst=dst: e.tensor_copy(dst[:, :, :], pt[:, :].rearrange("p (k t) -> p k t", t=128)),
                          reads=[ptb], writes=[b_dst])
                ps, pb = self.bank()
                combos = [(HT[0], wrh, b_wrh), (HT[0], wrl, b_wrl), (HT[1], wrh, b_wrh)]
                n = 0
                for (ht, b_ht), w, b_w in combos:
                    for kc in range(8):
                        n += 1
                        kb.op("pe", lambda e, kc=kc, ps=ps, ht=ht, w=w, n=n: e.matmul(ps[:, 0:32], ht[:, kc, :], w[:, kc, :], start=(n == 1), stop=(n == 24)),
                              reads=[b_ht, b_w], writes=[pb], inc=(n == 24))
                kb.op("dve", lambda e, ps=ps: e.tensor_tensor(lg[:, :], ps[:, 0:32], brt[:, :], ALU.add), reads=[pb, b_brt], writes=[b_lg])
                if tt == 0:
                    self.dump("lg_0", lg[:], b_lg, [128, 32])
                kb.op("dve", lambda e: e.max(out=v8[:, :], in_=lg[:, :]), reads=[b_lg], writes=[b_v8])
                kb.op("dve", lambda e: e.max_index(out=i8[:, :], in_max=v8[:, :], in_values=lg[:, :]), reads=[b_lg, b_v8], writes=[b_i8])
                kb.op("dve", lambda e: e.tensor_copy(i8f[:, :], i8[:, :]), reads=[b_i8], writes=[b_i8f])
                kb.op("dve", lambda e: e.tensor_scalar_mul(sm[:, 0:1], v8[:, 0:1], -1.0), reads=[b_v8], writes=[b_sm])
                kb.op("act", lambda e: e.activation(out=sm[:, 4:8], in_=v8[:, 0:4], func=AF.Exp, bias=sm[:, 0:1], accum_out=sm[:, 1:2]),
                      reads=[b_v8, b_sm], writes=[b_sm])
                kb.op("dve", lambda e: e.reciprocal(sm[:, 2:3], sm[:, 1:2]), reads=[b_sm], writes=[b_sm])
                kb.op("dve", lambda e: e.tensor_scalar_mul(self.GATES[:, tt, :], sm[:, 4:8], sm[:, 2:3]), reads=[b_sm], writes=[self.b_route[tt]])
                kb.op("dve", lambda e: e.tensor_scalar(mask[:, :], lg[:, :], v8[:, 3:4], None, ALU.is_ge), reads=[b_lg, b_v8], writes=[b_mask])
                ps, pb = self.bank()
                kb.op("pe", lambda e, ps=ps: e.matmul(ps[:, 0:32], LTb, mask[:, :], start=True, stop=True), reads=[bcb, b_mask], writes=[pb], inc=False)
                kb.op("pe", lambda e, ps=ps: e.matmul(ps[:, 32:64], ONEb, mask[:, :], start=True, stop=True), reads=[bcb, b_mask], writes=[pb])
                kb.op("dve", lambda e, ps=ps: e.tensor_tensor(sc[:, :], ps[:, 0:32], carry[:, :], ALU.add), reads=[pb, b_carry], writes=[b_sc])
                kb.op("dve", lambda e, ps=ps: e.tensor_tensor(carry[:, :], ps[:, 32:64], carry[:, :], ALU.add), reads=[pb, b_carry, b_sc], writes=[b_carry])
                kb.op("dve", lambda e: e.tensor_scalar(ov[:, :], sc[:, :], float(CAP), float(4 * NSLOT), ALU.is_ge, ALU.mult), reads=[b_sc], writes=[b_ov])
                kb.op("dve", lambda e: e.tensor_tensor(sc[:, :], sc[:, :], self.cst[:, 672:704], ALU.add), reads=[b_sc, self.b_cst], writes=[b_sc])
                kb.op("dve", lambda e: e.tensor_tensor(sc[:, :], sc[:, :], ov[:, :], ALU.add), reads=[b_sc, b_ov], writes=[b_sc])
                for k in range(4):
                    kb.op("dve", lambda e, k=k: e.scalar_tensor_tensor(junk[:, :], self.cst[:, 640:672], i8f[:, k:k + 1], sc[:, :], ALU.is_equal, ALU.mult,
                                                                      accum_out=slf[:, k:k + 1]), reads=[self.b_cst, b_i8f, b_sc], writes=[b_junk, b_slf])
                kb.op("dve", lambda e: e.tensor_copy(self.SLOTS[:, tt, :], slf[:, :]), reads=[b_slf], writes=[self.b_route[tt]])
                for k in range(4):
                    kb.dma("pool", lambda e, k=k: e.indirect_dma_start(
                        out=self.Xg, out_offset=bass.IndirectOffsetOnAxis(ap=self.SLOTS[:, tt, k:k + 1], axis=0),
                        in_=hb[:, :], in_offset=None, bounds_check=self.bound_reg, oob_is_err=False),
                        reads=[b_hb, self.b_route[tt], self.b_Xgz], cw=[self.b_Xg])
            self.dump("gates", self.GATES[:].rearrange("p a b -> p (a b)"), self.b_route[15], [128, 64])
            if "slots" in self.dbg:
                sf, b_sf = mk("slots_f", [128, 64])
                kb.op("dve", lambda e: e.tensor_copy(sf[:, :], self.SLOTS[:].rearrange("p a b -> p (a b)")), reads=self.b_route, writes=[b_sf])
                self.dump("slots", sf[:], b_sf, [128, 64])

    def phase_moe(self):
        nc, kb = self.nc, self.kb
        IDb, bcb = self.cstb[:, 0:128], self.b_cstb
        NST = CAP // 128
        with ExitStack() as es:
            self.alloc_psum(es, 6, 2)

            def mk(name, shape, dt=F32):
                return self.sb(es, "M_" + name, shape, dt), Buf("M_" + name)
            WU = [mk("wu%d" % i, [128, 8, 2048], BF16) for i in range(2)]
            WD = [mk("wd%d" % i, [128, 8, 1024], BF16) for i in range(2)]
            BD = [mk("bd%d" % i, [128, 1024]) for i in range(2)]
            XG = [mk("xg%d" % i, [128, NST, 1024], BF16) for i in range(2)]
            XGT = [mk("xgt%d" % i, [128, 8, CAP], BF16) for i in range(2)]
            ACTT, b_ACTT = mk("actt", [128, 8, CAP], BF16)
            Gt = [mk("g%d" % i, [128, CAP]) for i in range(2)]
            St = [mk("s%d" % i, [128, CAP]) for i in range(2)]
            Ut = [mk("u%d" % i, [128, CAP]) for i in range(2)]
            Ysb = [mk("y%d" % i, [128, 1024]) for i in range(2)]
            bup, b_bup = mk("bup", [128, 32, 16])
            kb.dma("sp", lambda e: e.dma_start(out=bup[:], in_=self.din["b_up"].rearrange("p (e f) -> p e f", f=16)), writes=[b_bup])

            def load(ex):
                sl = ex % 2
                wu = self.din["w_up"][ex].rearrange("(kc p) f -> p kc f", p=128)
                wd = self.din["w_down"][ex].rearrange("(kc p) f -> p kc f", p=128)
                kb.dma("sp", lambda e: e.dma_start(out=XG[sl][0][:], in_=self.Xg[ex * CAP:(ex + 1) * CAP, :].rearrange("(st p) d -> p st d", p=128)),
                       reads=[self.b_Xg], writes=[XG[sl][1]])
                kb.dma("sp", lambda e: e.dma_start(out=BD[sl][0][:], in_=self.din["b_down"][ex:ex + 1, :].partition_broadcast(128)), writes=[BD[sl][1]])
                for q in range(4):
                    kb.dma("pool", lambda e, q=q: e.dma_start(out=WU[sl][0][:, 2 * q:2 * q + 2, :], in_=wu[:, 2 * q:2 * q + 2, :]), cw=[WU[sl][1]])
                for q in range(2):
                    kb.dma("pool", lambda e, q=q: e.dma_start(out=WD[sl][0][:, 4 * q:4 * q + 4, :], in_=wd[:, 4 * q:4 * q + 4, :]), cw=[WD[sl][1]])

            def compute(ex):
                sl = ex % 2
                wu, b_wu = WU[sl]
                wd, b_wd = WD[sl]
                xg, b_xg = XG[sl]
                xgt, b_xgt = XGT[sl]
                bd, b_bd = BD[sl]
                for st in range(NST):
                    pt, ptb = self.bank16()
                    for kc in range(8):
                        kb.op("pe", lambda e, kc=kc, pt=pt, st=st: e.transpose(pt[:, kc * 128:(kc + 1) * 128], xg[:, st, kc * 128:(kc + 1) * 128], IDb),
                              reads=[b_xg, bcb], writes=[ptb], inc=(kc == 7))
                    kb.op("dve", lambda e, pt=pt, st=st: e.tensor_copy(xgt[:, :, st * 128:(st + 1) * 128], pt[:, :].rearrange("p (k s) -> p k s", s=128)),
                          reads=[ptb], cw=[b_xgt])
                for c in range(8):
                    g, b_g = Gt[c % 2]
                    s_, b_s = St[c % 2]
                    u, b_u = Ut[c % 2]
                    ps_g, pb_g = self.bank()
                    for kc in range(8):
                        kb.op("pe", lambda e, kc=kc, ps_g=ps_g, c=c: e.matmul(ps_g[:, 0:CAP], wu[:, kc, c * 128:(c + 1) * 128], xgt[:, kc, :], start=(kc == 0), stop=(kc == 7)),
                              reads=[b_wu, b_xgt], writes=[pb_g], inc=(kc == 7))
                    ps_u, pb_u = self.bank()
                    for kc in range(8):
                        kb.op("pe", lambda e, kc=kc, ps_u=ps_u, c=c: e.matmul(ps_u[:, 0:CAP], wu[:, kc, 1024 + c * 128:1024 + (c + 1) * 128], xgt[:, kc, :],
                                                                           start=(kc == 0), stop=(kc == 7)),
                              reads=[b_wu, b_xgt], writes=[pb_u], inc=(kc == 7))
                    kb.op("dve", lambda e, ps_g=ps_g, c=c: e.tensor_scalar(g[:, :], ps_g[:, 0:CAP], bup[:, ex, c:c + 1], 7.0, ALU.add, ALU.min),
                          reads=[pb_g, b_bup], writes=[b_g])
                    kb.op("act", lambda e: e.activation(out=s_[:, :], in_=g[:, :], func=AF.Sigmoid, scale=1.702), reads=[b_g], writes=[b_s])
                    kb.op("dve", lambda e, ps_u=ps_u, c=c: e.tensor_scalar(u[:, :], ps_u[:, 0:CAP], bup[:, ex, 8 + c:9 + c], 7.0, ALU.add, ALU.min),
                          reads=[pb_u, b_bup], writes=[b_u])
                    kb.op("dve", lambda e: e.tensor_scalar(u[:, :], u[:, :], -7.0, 1.0, ALU.max, ALU.add), reads=[b_u], writes=[b_u])
                    kb.op("dve", lambda e: e.tensor_tensor(g[:, :], g[:, :], s_[:, :], ALU.mult), reads=[b_g, b_s], writes=[b_g])
                    kb.op("dve", lambda e, c=c: e.tensor_tensor(ACTT[:, c, :], g[:, :], u[:, :], ALU.mult), reads=[b_g, b_u], cw=[b_ACTT])
                for st in range(NST):
                    y, b_y = Ysb[st % 2]
                    for half in range(2):
                        ps, pb = self.bank()
                        for fc in range(8):
                            kb.op("pe", lambda e, fc=fc, ps=ps, half=half, st=st: e.matmul(ps[:, :], ACTT[:, fc, st * 128:(st + 1) * 128],
                                                                                        wd[:, fc, half * 512:(half + 1) * 512], start=(fc == 0), stop=(fc == 7)),
                                  reads=[b_ACTT, b_wd], writes=[pb], inc=(fc == 7))
                        kb.op("dve", lambda e, ps=ps, half=half: e.tensor_tensor(y[:, half * 512:(half + 1) * 512], ps[:, :], bd[:, half * 512:(half + 1) * 512], ALU.add),
                              reads=[pb, b_bd], cw=[b_y])
                    r0 = ex * CAP + st * 128
                    kb.dma("sp", lambda e, r0=r0: e.dma_start(out=self.Yg[r0:r0 + 128, :], in_=y[:]), reads=[b_y], cw=[self.b_Yg])

            ne = getattr(self, "n_experts", NE)
            load(0)
            if ne > 1:
                load(1)
            for ex in range(ne):
                compute(ex)
                if ex + 2 < ne:
                    load(ex + 2)

    def phase_final(self):
        nc, kb = self.nc, self.kb
        IDb, bcb = self.cstb[:, 0:128], self.b_cstb
        with ExitStack() as es:
            self.alloc_psum(es, 6, 2)
            self.alloc_ln(es)

            def mk(name, shape, dt=F32):
                return self.sb(es, "F_" + name, shape, dt), Buf("F_" + name)
            Wpg, b_Wpg = mk("Wpg", [128, 8, 1024], BF16)
            for q in range(2):
                kb.dma("pool", lambda e, q=q: e.dma_start(out=Wpg[:, 4 * q:4 * q + 4, :], in_=self.din["w_pg"].rearrange("p (k n) -> p k n", n=1024)[:, 4 * q:4 * q + 4, :]),
                       cw=[b_Wpg])
            Wple, b_Wple = mk("Wple", [128, 2, 1024], BF16)
            kb.dma("pool", lambda e: e.dma_start(out=Wple[:], in_=self.din["w_ple"].rearrange("p (k n) -> p k n", n=1024)), writes=[b_Wple])
            PT, b_PT = mk("PT", [128, 2, T], BF16)
            pT = self.din["pT"].rearrange("(kc p) t -> p kc t", p=128)
            for kc in range(2):
                kb.dma("pool", lambda e, kc=kc: e.dma_start(out=PT[:, kc, :], in_=pT[:, kc, :]), cw=[b_PT])
            YG = [mk("yg%d" % i, [128, 4, 1024]) for i in range(2)]
            H1t = [mk("h1_%d" % i, [128, 1024]) for i in range(2)]
            ACC, b_ACC = mk("acc", [128, 1024])
            H2, b_H2 = mk("h2", [128, 1024])
            H2b, b_H2b = mk("h2b", [128, 1024], BF16)
            H2T, b_H2T = mk("h2T", [128, 8, 128], BF16)
            SGT, b_SGT = mk("sgt", [128, 1024])
            OUT = [mk("out%d" % i, [128, 1024]) for i in range(2)]
            def prefetch(tt):
                tsl = slice(tt * 128, (tt + 1) * 128)
                yg, b_yg = YG[tt % 2]
                h1, b_h1 = H1t[tt % 2]
                kb.op("pool", lambda e: e.memset(yg[:], 0.0), writes=[b_yg])
                for k in range(4):
                    kb.dma("pool", lambda e, k=k: e.indirect_dma_start(
                        out=yg[:, k, :], out_offset=None, in_=self.Yg,
                        in_offset=bass.IndirectOffsetOnAxis(ap=self.SLOTS[:, tt, k:k + 1], axis=0),
                        bounds_check=self.bound_reg, oob_is_err=False), reads=[self.b_Yg, self.b_route[tt]], cw=[b_yg])
                kb.dma("sp", lambda e: e.dma_start(out=h1[:], in_=self.H1d[tsl, :]), reads=[self.b_H1d], writes=[b_h1])

            prefetch(0)
            for tt in range(16):
                tsl = slice(tt * 128, (tt + 1) * 128)
                yg, b_yg = YG[tt % 2]
                h1, b_h1 = H1t[tt % 2]
                if tt + 1 < 16:
                    prefetch(tt + 1)
                kb.op("act", lambda e: e.activation(out=ACC[:, :], in_=h1[:, :], func=AF.Identity, scale=ALPHA), reads=[b_h1], writes=[b_ACC])
                for k in range(4):
                    kb.op("dve", lambda e, k=k: e.scalar_tensor_tensor(ACC[:, :], yg[:, k, :], self.GATES[:, tt, k:k + 1], ACC[:, :], ALU.mult, ALU.add),
                          reads=[b_yg, self.b_route[tt], b_ACC], writes=[b_ACC])
                self.layer_norm(es, ACC, b_ACC, 2, 3, H2, b_H2, "ln2")
                if tt == 0:
                    self.dump("h2_0", H2[:], b_H2, [128, 1024])
                kb.op("act", lambda e: e.activation(out=H2b[:, :], in_=H2[:, :], func=AF.Identity), reads=[b_H2], writes=[b_H2b])
                pt, ptb = self.bank16()
                for kc in range(8):
                    kb.op("pe", lambda e, kc=kc, pt=pt: e.transpose(pt[:, kc * 128:(kc + 1) * 128], H2b[:, kc * 128:(kc + 1) * 128], IDb),
                          reads=[b_H2b, bcb], writes=[ptb], inc=(kc == 7))
                kb.op("dve", lambda e, pt=pt: e.tensor_copy(H2T[:, :, :], pt[:, :].rearrange("p (k t) -> p k t", t=128)), reads=[ptb], writes=[b_H2T])
                o, b_o = OUT[tt % 2]
                for half in range(2):
                    hs = slice(half * 512, (half + 1) * 512)
                    ps, pb = self.bank()
                    for kc in range(8):
                        kb.op("pe", lambda e, kc=kc, ps=ps, hs=hs: e.matmul(ps[:, :], H2T[:, kc, :], Wpg[:, kc, hs], start=(kc == 0), stop=(kc == 7)),
                              reads=[b_H2T, b_Wpg], writes=[pb], inc=(kc == 7))
                    kb.op("dve", lambda e, ps=ps, hs=hs: e.tensor_tensor(SGT[:, hs], ps[:, :], self.rowp[:, 4, hs], ALU.add), reads=[pb, self.b_rowp], writes=[b_SGT])
                    kb.op("act", lambda e, hs=hs: e.activation(out=SGT[:, hs], in_=SGT[:, hs], func=AF.Sigmoid), reads=[b_SGT], writes=[b_SGT])
                    ps2, pb2 = self.bank()
                    for kc in range(2):
                        kb.op("pe", lambda e, kc=kc, ps2=ps2, hs=hs: e.matmul(ps2[:, :], PT[:, kc, tsl], Wple[:, kc, hs], start=(kc == 0), stop=(kc == 1)),
                              reads=[b_PT, b_Wple], writes=[pb2], inc=(kc == 1))
                    kb.op("dve", lambda e, ps2=ps2, hs=hs: e.tensor_tensor(o[:, hs], SGT[:, hs], ps2[:, :], ALU.mult), reads=[b_SGT, pb2], writes=[b_o])
                    kb.op("dve", lambda e, hs=hs: e.tensor_tensor(o[:, hs], o[:, hs], H2[:, hs], ALU.add), reads=[b_o, b_H2], writes=[b_o])
                kb.dma("sp", lambda e: e.dma_start(out=self.out[tsl, :], in_=o[:]), reads=[b_o])


_PROG_CACHE = {}


def kernel(**inputs):
    inp = {k: np.asarray(v) for k, v in inputs.items()}
    sh = prep_shared(inp)
    in_maps = [dict(sh, **prep_core(inp, b)) for b in range(8)]
    if "nc" not in _PROG_CACHE:
        _PROG_CACHE["nc"] = Prog().build()
    nc = _PROG_CACHE["nc"]
    res = run_bass_kernel_spmd(nc, in_maps, core_ids=list(range(8)))
    out = np.stack([np.asarray(r["out"], dtype=np.float32) for r in res.results], axis=0)
    return out
```

```python
import numpy as np
from contextlib import ExitStack
import concourse.bass as bass
import concourse.mybir as mybir
from concourse.bass_utils import run_bass_kernel_spmd

F32 = mybir.dt.float32
BF16 = mybir.dt.bfloat16
U32 = mybir.dt.uint32
AF = mybir.ActivationFunctionType
ALU = mybir.AluOpType
AX = mybir.AxisListType

T = 2048
D = 1024
NE = 32
CAP = 384
NSLOT = NE * CAP
ALPHA = 2.0 ** 0.25
N_IN = 7184
O_XA, O_GA, O_Q, O_K, O_V, O_GO, O_GLR, O_MA, O_MB = 0, 1024, 2048, 2560, 3072, 4096, 5120, 5136, 6160


class Buf:
    __slots__ = ("name", "w", "r", "c")

    def __init__(self, name):
        self.name = name
        self.w = {}
        self.r = {}
        self.c = {}


class KB:
    def __init__(self, nc, es, n_dma_sems=24):
        self.nc = nc
        self.eng = dict(pe=nc.tensor, act=nc.scalar, dve=nc.vector, pool=nc.gpsimd, sp=nc.sync)
        self.esem = {}
        self.ecnt = {}
        self.seen = {}
        self.semobj = {}
        for n in self.eng:
            s = es.enter_context(nc.semaphore("es_" + n))
            self.esem[n] = s
            self.semobj[id(s)] = s
            self.ecnt[n] = 0
            self.seen[n] = {}
        self.dsem = []
        self.dcnt = []
        for i in range(2 * n_dma_sems):
            s = es.enter_context(nc.semaphore("ds_%d" % i))
            self.dsem.append(s)
            self.semobj[id(s)] = s
            self.dcnt.append(0)
        self.nds = n_dma_sems
        self.drr = {"pool": 0, "hw": 0}

    def _wait(self, en, toks):
        e = self.eng[en]
        seen = self.seen[en]
        for sid, val in toks.items():
            if en == "pe" and sid == id(self.esem["pe"]):
                continue
            if seen.get(sid, 0) >= val:
                continue
            e.wait_ge(self.semobj[sid], val)
            seen[sid] = val

    @staticmethod
    def _merge(dst, src):
        for k, v in src.items():
            if dst.get(k, 0) < v:
                dst[k] = v

    def _deps(self, reads, writes, cw=()):
        toks = {}
        for b in reads:
            self._merge(toks, b.w)
            self._merge(toks, b.c)
        for b in writes:
            self._merge(toks, b.w)
            self._merge(toks, b.c)
            self._merge(toks, b.r)
        for b in cw:
            self._merge(toks, b.w)
            self._merge(toks, b.r)
        return toks

    def _commit(self, tok, reads, writes, cw=()):
        for b in reads:
            self._merge(b.r, tok)
        for b in writes:
            b.w = dict(tok)
            b.r = {}
            b.c = {}
        for b in cw:
            self._merge(b.c, tok)

    disabled = False

    def op(self, en, fn, reads=(), writes=(), inc=True, cw=()):
        if self.disabled:
            return None
        self._wait(en, self._deps(reads, writes, cw))
        ins = fn(self.eng[en])
        s = self.esem[en]
        if inc:
            self.ecnt[en] += 1
            ins.then_inc(s, 1)
            tok = {id(s): self.ecnt[en]}
        else:
            tok = {id(s): self.ecnt[en] + 1}
        self._commit(tok, reads, writes, cw)
        return ins

    def dma(self, en, fn, reads=(), writes=(), cw=()):
        if self.disabled:
            return None
        kind = "pool" if en == "pool" else "hw"
        i = self.drr[kind] + (self.nds if kind == "pool" else 0)
        self.drr[kind] = (self.drr[kind] + 1) % self.nds
        s = self.dsem[i]
        toks = self._deps(reads, writes, cw)
        if self.dcnt[i] > 0:
            self._merge(toks, {id(s): self.dcnt[i]})
        self._wait(en, toks)
        ins = fn(self.eng[en])
        self.dcnt[i] += 16
        ins.then_inc(s, 16)
        tok = {id(s): self.dcnt[i]}
        self._commit(tok, reads, writes, cw)
        return ins

    def all_tokens(self):
        toks = {}
        for n in self.eng:
            if self.ecnt[n] > 0:
                toks[id(self.esem[n])] = self.ecnt[n]
        for i, s in enumerate(self.dsem):
            if self.dcnt[i] > 0:
                toks[id(s)] = self.dcnt[i]
        return toks

    def barrier(self, engines=None):
        toks = self.all_tokens()
        for n in (engines or list(self.eng)):
            own = id(self.esem[n])
            t = {k: v for k, v in toks.items() if not (n == "pe" and k == own)}
            self._wait(n, t)


def _consts():
    c = np.zeros((128, 5 * 128 + 64 + 4), np.float32)
    j = np.arange(128)
    same = (j[:, None] // 64) == (j[None, :] // 64)
    c[:, 0:128] = np.eye(128, dtype=np.float32)
    c[:, 128:256] = (same & (j[:, None] <= j[None, :]))
    c[:, 256:384] = (same & (j[:, None] > j[None, :]))
    c[:, 384:512] = (j[:, None] < j[None, :])
    c[:, 512:640] = 1.0
    c[:, 640:672] = np.arange(32)[None, :]
    c[:, 672:704] = (np.arange(32) * CAP)[None, :]
    c[:, 704] = (j < 64)
    c[:, 705] = (j >= 64)
    return c


def prep_shared(inp):
    f = lambda a: np.ascontiguousarray(a, dtype=np.float32)
    w_in = inp["w_in"][0]
    cols = np.concatenate([np.arange(0, O_GLR), np.arange(O_MA, N_IN)])
    wm = w_in[:, cols]
    sh = {}
    sh["w_in_t"] = f(wm.reshape(8, 128, 56, 128).transpose(2, 1, 0, 3).reshape(56, 128, 1024))
    sh["w_glr"] = f(w_in[:, O_GLR:O_GLR + 16].reshape(8, 128, 16).transpose(1, 0, 2).reshape(128, 128))
    chan = np.concatenate([inp["conv_w"][0], inp["conv_b"], inp["lru_b_r"], inp["lru_b_i"],
                           inp["lru_lambda"]], axis=0)
    sh["chanp"] = f(chan.reshape(8, 8, 128).transpose(2, 1, 0).reshape(128, 64))
    sh["lru_wr"] = f(inp["lru_w_r"][0].transpose(1, 0, 2).reshape(128, 1024))
    sh["lru_wi"] = f(inp["lru_w_i"][0].transpose(1, 0, 2).reshape(128, 1024))
    sh["gla_wg"] = f(inp["gla_w_gate"][0])
    sh["gla_bg"] = f(inp["gla_b_gate"])
    sh["gla_ng"] = f(inp["gla_norm_g"][0].reshape(2, 128).T)
    sh["w_out"] = f(inp["w_out"][0].reshape(8, 128, 1024).transpose(1, 0, 2).reshape(128, 8192))
    rows = np.concatenate([inp["ln1_g"], inp["ln1_b"], inp["ln2_g"], inp["ln2_b"],
                           inp["b_ple_gate"]], axis=0)
    sh["rowp"] = f(np.broadcast_to(rows.reshape(1, 5 * 1024), (128, 5 * 1024)))
    sh["w_router"] = f(inp["w_router"][0].reshape(8, 128, 32).transpose(1, 0, 2).reshape(128, 256))
    sh["b_router"] = f(inp["b_router"])
    sh["w_up"] = inp["w_up"][0]
    sh["b_up"] = f(inp["b_up"][0].reshape(32, 16, 128).transpose(2, 0, 1).reshape(128, 512))
    sh["w_down"] = inp["w_down"][0]
    sh["b_down"] = f(inp["b_down"][0])
    sh["w_ple"] = f(inp["w_ple"][0].reshape(2, 128, 1024).transpose(1, 0, 2).reshape(128, 2048))
    sh["w_pg"] = f(inp["w_ple_gate"][0].reshape(8, 128, 1024).transpose(1, 0, 2).reshape(128, 8192))
    sh["consts"] = _consts()
    return sh


def prep_core(inp, b):
    x = np.asarray(inp["x"][b], dtype=np.float32)
    p = np.asarray(inp["p"][0, b], dtype=np.float32)
    return {"x": np.ascontiguousarray(x), "xT": np.ascontiguousarray(x.T),
            "pT": np.ascontiguousarray(p.T)}


SHARED_SHAPES = {
    "w_in_t": [56, 128, 1024], "w_glr": [128, 128], "chanp": [128, 64], "lru_wr": [128, 1024],
    "lru_wi": [128, 1024], "gla_wg": [16, 512], "gla_bg": [1, 512], "gla_ng": [128, 2],
    "w_out": [128, 8192], "rowp": [128, 5120], "w_router": [128, 256], "b_router": [1, 32],
    "w_up": [32, 1024, 2048], "b_up": [128, 512], "w_down": [32, 1024, 1024], "b_down": [32, 1024],
    "w_ple": [128, 2048], "w_pg": [128, 8192], "consts": [128, 708],
}
CORE_SHAPES = {"x": [T, D], "xT": [D, T], "pT": [256, T]}


class StopBuild(Exception):
    pass


class Prog:
    ck_n = 0
    ck_stop = None

    def ck(self, label=""):
        self.ck_n += 1
        if self.ck_stop is not None and self.ck_n >= self.ck_stop:
            if not self.kb.disabled:
                print("STOP at checkpoint", self.ck_n, label)
            self.kb.disabled = True

    def __init__(self, dbg=(), stop_after=None):
        self.dbg = set(dbg)
        self.stop_after = stop_after
        self.nc = nc = bass.Bass("TRN2", target_bir_lowering=False)
        self.din = {}
        for n, s in list(SHARED_SHAPES.items()) + list(CORE_SHAPES.items()):
            self.din[n] = nc.dram_tensor(n, s, F32, kind="ExternalInput").ap()
        self.out = nc.dram_tensor("out", [T, D], F32, kind="ExternalOutput").ap()
        self.dbg_out = {}
        self.es = ExitStack()

    def dbg_tensor(self, name, shape):
        t = self.nc.dram_tensor("dbg_" + name, shape, F32, kind="ExternalOutput").ap()
        self.dbg_out[name] = t
        return t

    def sb(self, es, name, shape, dt=F32):
        return es.enter_context(self.nc.sbuf_tensor(name, shape, dt))

    def build(self):
        nc = self.nc
        with self.es as es:
            kb = self.kb = KB(nc, es)
            self.bound_reg = nc.gpsimd.to_reg(NSLOT - 1)
            self.cst = self.sb(es, "cst", [128, 708])
            self.b_cst = Buf("cst")
            kb.dma("sp", lambda e: e.dma_start(out=self.cst[:], in_=self.din["consts"][:, :]),
                   writes=[self.b_cst])
            self.cstb = self.sb(es, "cstb", [128, 708], BF16)
            self.b_cstb = Buf("cstb")
            kb.op("dve", lambda e: e.tensor_copy(self.cstb[:], self.cst[:]),
                  reads=[self.b_cst], writes=[self.b_cstb])
            self.GATES = self.sb(es, "GATES", [128, 16, 4])
            self.SLOTS = self.sb(es, "SLOTS", [128, 16, 4], U32)
            self.b_route = [Buf("route%d" % i) for i in range(16)]
            self.rowp = self.sb(es, "rowp_sb", [128, 5, 1024])
            self.b_rowp = Buf("rowp")
            kb.dma("sp", lambda e: e.dma_start(out=self.rowp[:], in_=self.din["rowp"].rearrange("p (r d) -> p r d", d=1024)),
                   writes=[self.b_rowp])
            self.Xg = nc.dram_tensor("Xg", [NSLOT, D], BF16).ap()
            self.Yg = nc.dram_tensor("Yg", [NSLOT, D], F32).ap()
            self.H1d = nc.dram_tensor("H1d", [T, D], F32).ap()
            self.b_Xg, self.b_Yg, self.b_H1d = Buf("Xg"), Buf("Yg"), Buf("H1d")
            self.b_Xgz = Buf("Xgz")
            self.zero_xg(es)
            with ExitStack() as es_y:
                self.yT = self.sb(es_y, "yT", [128, 8, T], BF16)
                self.b_yT = [Buf("yT%d" % g) for g in range(8)]
                with ExitStack() as es1:
                    self.alloc_psum(es1, 8, 0)
                    self.XT = self.sb(es1, "XT", [128, 8, T], BF16)
                    self.b_XT = Buf("XT")
                    xT = self.din["xT"].rearrange("(kc p) t -> p kc t", p=128)
                    for kc in range(8):
                        kb.dma("pool", lambda e, kc=kc: e.dma_start(out=self.XT[:, kc, :], in_=xT[:, kc, :]),
                               cw=[self.b_XT])
                    if not getattr(self, "skip_lru", False):
                        self.phase_lru(es1)
                    else:
                        kb.op("dve", lambda e: e.memset(self.yT[:], 0.0), writes=self.b_yT)
                    if self.stop_after == "lru":
                        return self.finish()
                    kb.barrier()
                    self.phase_gla(es1)
                    if self.stop_after == "gla":
                        return self.dump_yT()
                kb.barrier()
                self.phase_outproj()
                if self.stop_after == "outproj":
                    return self.finish()
            kb.barrier()
            self.phase_moe()
            if self.stop_after == "moe":
                return self.finish()
            kb.barrier()
            self.phase_final()
        return self.finish()

    def alloc_psum(self, es, n32, n16):
        nc = self.nc
        self.ps = [es.enter_context(nc.psum_tensor("ps%d_%d" % (i, self.ps_gen), [128, 512], F32)) for i in range(n32)]
        self.psb = [Buf("ps%d" % i) for i in range(n32)]
        self.pst = [es.enter_context(nc.psum_tensor("pst%d_%d" % (i, self.ps_gen), [128, 1024], BF16)) for i in range(n16)]
        self.pstb = [Buf("pst%d" % i) for i in range(n16)]
        self.ps_rr = 0
        self.pst_rr = 0
        self.ps_gen += 1

    def zero_xg(self, es):
        kb = self.kb
        z = self.sb(es, "zeros", [128, 4, 1024], BF16)
        bz = Buf("zeros")
        kb.op("dve", lambda e: e.memset(z[:], 0.0), writes=[bz])
        xg = self.Xg.rearrange("(n p) d -> p n d", p=128)
        for i in range(NSLOT // 128 // 4):
            kb.dma("sp", lambda e, i=i: e.dma_start(out=xg[:, i * 4:(i + 1) * 4, :], in_=z[:]), reads=[bz], cw=[self.b_Xgz])

    ps_gen = 0

    def bank(self):
        i = self.ps_rr
        self.ps_rr = (self.ps_rr + 1) % len(self.ps)
        return self.ps[i], self.psb[i]

    def bank16(self):
        i = self.pst_rr
        self.pst_rr = (self.pst_rr + 1) % len(self.pst)
        return self.pst[i], self.pstb[i]

    def finish(self):
        kb = self.kb
        kb.disabled = False
        kb.barrier(["sp"])
        return self.nc

    def dump_yT(self):
        kb = self.kb
        kb.barrier()
        with ExitStack() as es:
            tmp = self.sb(es, "dump_tmp", [128, 8, T])
            b = Buf("dump_tmp")
            kb.op("dve", lambda e: e.tensor_copy(tmp[:], self.yT[:]), reads=self.b_yT, writes=[b])
            t = self.dbg_tensor("yT", [128, 8, T])
            kb.dma("sp", lambda e: e.dma_start(out=t, in_=tmp[:]), reads=[b])
            return self.finish()

    def dump(self, name, sb_ap, buf, shape):
        if name not in self.dbg:
            return
        t = self.dbg_tensor(name, shape)
        self.kb.dma("sp", lambda e: e.dma_start(out=t, in_=sb_ap), reads=[buf])

    def inproj_fm(self, wt, wb, ncols, tg, evac):
        kb = self.kb
        ps, pb = self.bank()
        for kc in range(8):
            kb.op("pe", lambda e, kc=kc: e.matmul(ps[0:ncols, :], wt[:, kc * ncols:(kc + 1) * ncols],
                                                   self.XT[:, kc, tg * 512:(tg + 1) * 512],
                                                   start=(kc == 0), stop=(kc == 7)),
                  reads=[wb, self.b_XT], writes=[pb], inc=(kc == 7))
        evac(ps, pb)

    def load_w(self, grp):
        i = self.w_rr
        self.w_rr = (self.w_rr + 1) % len(self.wring)
        wt, wb = self.wring[i], self.wringb[i]
        self.kb.dma("pool", lambda e: e.dma_start(out=wt[:], in_=self.din["w_in_t"][grp, :, :]), writes=[wb])
        return wt, wb

    def phase_lru(self, es1):
        nc, kb = self.nc, self.kb
        with ExitStack() as es:
            NW = 6
            self.wring = [self.sb(es, "wr%d" % i, [128, 1024], BF16) for i in range(NW)]
            self.wringb = [Buf("wr%d" % i) for i in range(NW)]
            self.w_rr = 0
            chan = self.sb(es, "chan", [128, 8, 8])
            b_chan = Buf("chan")
            kb.dma("sp", lambda e: e.dma_start(out=chan[:], in_=self.din["chanp"].rearrange("p (g k) -> p g k", k=8)),
                   writes=[b_chan])
            wr = self.sb(es, "lwr", [128, 8, 128], BF16)
            wi = self.sb(es, "lwi", [128, 8, 128], BF16)
            b_wr, b_wi = Buf("lwr"), Buf("lwi")
            kb.dma("pool", lambda e: e.dma_start(out=wr[:], in_=self.din["lru_wr"].rearrange("p (g d) -> p g d", d=128)), writes=[b_wr])
            kb.dma("pool", lambda e: e.dma_start(out=wi[:], in_=self.din["lru_wi"].rearrange("p (g d) -> p g d", d=128)), writes=[b_wi])
            sc = self.sb(es, "lsc", [128, 8, 4])
            b_sc = Buf("lsc")
            kb.op("act", lambda e: e.activation(out=sc[:, :, 0], in_=chan[:, :, 7], func=AF.Exp, scale=-1.0),
                  reads=[b_chan], writes=[b_sc])
            kb.op("act", lambda e: e.activation(out=sc[:, :, 1], in_=sc[:, :, 0], func=AF.Ln, bias=1.0),
                  reads=[b_sc], writes=[b_sc])
            kb.op("dve", lambda e: e.tensor_scalar_mul(sc[:, :, 2], sc[:, :, 1], -8.0), reads=[b_sc], writes=[b_sc])
            kb.op("dve", lambda e: e.tensor_scalar_mul(sc[:, :, 3], sc[:, :, 1], -16.0), reads=[b_sc], writes=[b_sc])

            names = ["xa", "xc", "r", "i", "a", "m", "u", "h", "ga", "t1", "t2"]
            A = {n: self.sb(es, "L_" + n, [128, T + (3 if n == "xa" else 0)]) for n in names}
            B = {n: Buf("L_" + n) for n in names}
            xcb = self.sb(es, "L_xcb", [128, T], BF16)
            b_xcb = Buf("L_xcb")
            kb.op("dve", lambda e: e.memset(A["xa"][:, 0:3], 0.0), writes=[B["xa"]])

            for g in range(8):
                w_xa, wb_xa = self.load_w(g)
                w_ga, wb_ga = self.load_w(8 + g)
                w_ma, wb_ma = self.load_w(40 + g)
                for tg in range(4):
                    self.inproj_fm(w_xa, wb_xa, 128, tg, lambda ps, pb, tg=tg: kb.op(
                        "act", lambda e: e.activation(out=A["xa"][:, 3 + tg * 512:3 + (tg + 1) * 512], in_=ps[:, :], func=AF.Copy),
                        reads=[pb], cw=[B["xa"]]))
                kb.op("dve", lambda e: e.tensor_scalar(A["xc"][:, :], A["xa"][:, 0:T], chan[:, g, 0:1], chan[:, g, 4:5],
                                                       ALU.mult, ALU.add),
                      reads=[B["xa"], b_chan], writes=[B["xc"]])
                for k in range(1, 4):
                    kb.op("dve", lambda e, k=k: e.scalar_tensor_tensor(A["xc"][:, :], A["xa"][:, k:k + T], chan[:, g, k:k + 1],
                                                                      A["xc"][:, :], ALU.mult, ALU.add),
                          reads=[B["xa"], B["xc"], b_chan], writes=[B["xc"]])
                kb.op("act", lambda e: e.activation(out=xcb[:, :], in_=A["xc"][:, :], func=AF.Copy),
                      reads=[B["xc"]], writes=[b_xcb])
                self.dump("xc%d" % g, A["xc"][:, :], B["xc"], [128, T])
                for (wg, bwg, dst, bi) in ((wr, b_wr, "r", 5), (wi, b_wi, "i", 6)):
                    for tg in range(4):
                        ps, pb = self.bank()
                        kb.op("pe", lambda e, tg=tg, ps=ps, wg=wg: e.matmul(ps[:, :], wg[:, g, :], xcb[:, tg * 512:(tg + 1) * 512],
                                                                          start=True, stop=True),
                              reads=[bwg, b_xcb], writes=[pb])
                        kb.op("act", lambda e, tg=tg, ps=ps, dst=dst, bi=bi: e.activation(
                            out=A[dst][:, tg * 512:(tg + 1) * 512], in_=ps[:, :], func=AF.Sigmoid, bias=chan[:, g, bi:bi + 1]),
                            reads=[pb, b_chan], cw=[B[dst]])
                kb.op("act", lambda e: e.activation(out=A["a"][:, :], in_=A["r"][:, :], func=AF.Exp, scale=sc[:, g, 2:3]),
                      reads=[B["r"], b_sc], writes=[B["a"]])
                kb.op("act", lambda e: e.activation(out=A["m"][:, :], in_=A["r"][:, :], func=AF.Exp, scale=sc[:, g, 3:4]),
                      reads=[B["r"], b_sc], writes=[B["m"]])
                kb.op("act", lambda e: e.activation(out=A["m"][:, :], in_=A["m"][:, :], func=AF.Sqrt, scale=-1.0, bias=1.0),
                      reads=[B["m"]], writes=[B["m"]])
                kb.op("dve", lambda e: e.tensor_tensor(A["u"][:, :], A["m"][:, :], A["i"][:, :], ALU.mult),
                      reads=[B["m"], B["i"]], writes=[B["u"]])
                kb.op("dve", lambda e: e.tensor_tensor(A["u"][:, :], A["u"][:, :], A["xc"][:, :], ALU.mult),
                      reads=[B["u"], B["xc"]], writes=[B["u"]])
                kb.op("dve", lambda e: e.tensor_tensor_scan(A["h"][:, :], A["a"][:, :], A["u"][:, :], 0.0, ALU.mult, ALU.add),
                      reads=[B["a"], B["u"]], writes=[B["h"]])
                self.dump("h%d" % g, A["h"][:, :], B["h"], [128, T])
                for tg in range(4):
                    self.inproj_fm(w_ga, wb_ga, 128, tg, lambda ps, pb, tg=tg: kb.op(
                        "act", lambda e: e.activation(out=A["ga"][:, tg * 512:(tg + 1) * 512], in_=ps[:, :], func=AF.Copy),
                        reads=[pb], cw=[B["ga"]]))
                kb.op("dve", lambda e: e.tensor_tensor(A["t1"][:, :], A["ga"][:, :], A["ga"][:, :], ALU.mult),
                      reads=[B["ga"]], writes=[B["t1"]])
                kb.op("dve", lambda e: e.tensor_scalar(A["t1"][:, :], A["t1"][:, :], 0.044715, 1.0, ALU.mult, ALU.add),
                      reads=[B["t1"]], writes=[B["t1"]])
                kb.op("dve", lambda e: e.tensor_tensor(A["t1"][:, :], A["t1"][:, :], A["ga"][:, :], ALU.mult),
                      reads=[B["t1"], B["ga"]], writes=[B["t1"]])
                kb.op("act", lambda e: e.activation(out=A["t1"][:, :], in_=A["t1"][:, :], func=AF.Sigmoid, scale=1.5957691216057308),
                      reads=[B["t1"]], writes=[B["t1"]])
                kb.op("dve", lambda e: e.tensor_tensor(A["t1"][:, :], A["t1"][:, :], A["ga"][:, :], ALU.mult),
                      reads=[B["t1"], B["ga"]], writes=[B["t1"]])
                kb.op("dve", lambda e: e.tensor_tensor(A["t1"][:, :], A["t1"][:, :], A["h"][:, :], ALU.mult),
                      reads=[B["t1"], B["h"]], writes=[B["t1"]])
                for tg in range(4):
                    self.inproj_fm(w_ma, wb_ma, 128, tg, lambda ps, pb, tg=tg: kb.op(
                        "act", lambda e: e.activation(out=A["t2"][:, tg * 512:(tg + 1) * 512], in_=ps[:, :], func=AF.Sigmoid),
                        reads=[pb], cw=[B["t2"]]))
                kb.op("dve", lambda e: e.tensor_tensor(self.yT[:, g, :], A["t1"][:, :], A["t2"][:, :], ALU.mult),
                      reads=[B["t1"], B["t2"]], writes=[self.b_yT[g]])
                self.dump("ya%d" % g, A["t1"][:, :], B["t1"], [128, T])

    def phase_gla(self, es1):
        nc, kb = self.nc, self.kb
        cst = self.cstb
        TRI, UU, ONES = cst[:, 128:256], cst[:, 256:384], cst[:, 512:640]
        TRI32 = self.cst[:, 128:256]
        bc = self.b_cstb
        with ExitStack() as es:
            NW = 8
            self.wring = [self.sb(es, "gw%d" % i, [128, 1024], BF16) for i in range(NW)]
            self.wringb = [Buf("gw%d" % i) for i in range(NW)]
            self.w_rr = 0
            wglr = self.sb(es, "wglr", [128, 128], BF16)
            b_wglr = Buf("wglr")
            kb.dma("pool", lambda e: e.dma_start(out=wglr[:], in_=self.din["w_glr"][:, :]), writes=[b_wglr])
            wg = self.sb(es, "wg", [16, 512], BF16)
            bg = self.sb(es, "bg", [1, 512], BF16)
            ng = self.sb(es, "ng", [128, 2])
            b_wg, b_bg, b_ng = Buf("wg"), Buf("bg"), Buf("ng")
            kb.dma("pool", lambda e: e.dma_start(out=wg[:], in_=self.din["gla_wg"][:, :]), writes=[b_wg])
            kb.dma("pool", lambda e: e.dma_start(out=bg[:], in_=self.din["gla_bg"][:, :]), writes=[b_bg])
            kb.dma("sp", lambda e: e.dma_start(out=ng[:], in_=self.din["gla_ng"][:, :]), writes=[b_ng])
            glrT = self.sb(es, "glrT", [16, T], BF16)
            b_glrT = Buf("glrT")
            for tg in range(4):
                self.inproj_fm(wglr, b_wglr, 16, tg, lambda ps, pb, tg=tg: kb.op(
                    "act", lambda e: e.activation(out=glrT[:, tg * 512:(tg + 1) * 512], in_=ps[0:16, :], func=AF.Copy),
                    reads=[pb], cw=[b_glrT]))

            self.ck("glrT")

            def mk(name, shape, dt=F32):
                return self.sb(es, "G_" + name, shape, dt), Buf("G_" + name)
            QT, b_QT = mk("QT", [128, T], BF16)
            KT, b_KT = mk("KT", [128, T], BF16)
            KD0, b_KD0 = mk("KD0", [128, 16, 128], BF16)
            KD1, b_KD1 = mk("KD1", [128, 16, 128], BF16)
            V, b_V = mk("V", [128, 16, 256], BF16)
            OT, b_OT = mk("OT", [128, 2, T])
            EB, b_EB = mk("EB", [128, 32])
            Gsp, b_Gsp = mk("Gsp", [128, 4, 128], BF16)
            Gz, b_Gz = mk("Gz", [128, 4, 128])
            EQ, b_EQ = mk("EQ", [128, 512])
            EK, b_EK = mk("EK", [128, 512])
            ED, b_ED = mk("ED", [128, 4, 128])
            STs = [mk("ST%d" % i, [128, 128], BF16) for i in range(2)]
            Sb = [mk("S%d" % i, [128, 256]) for i in range(4)]
            Sbb = [mk("Sb%d" % i, [128, 256], BF16) for i in range(4)]
            SQ = [mk("SQ%d" % i, [128, 512], BF16) for i in range(2)]
            RI, b_RI = mk("RI", [128, 512])
            SG, b_SG = mk("SG", [128, 512])
            SM, b_SM = mk("SM", [128, 512])
            TT, b_TT = mk("TT", [128, 512])

            for hd in range(4):
                w_q, wb_q = self.load_w(16 + hd)
                w_k, wb_k = self.load_w(20 + hd)
                w_v = [self.load_w(24 + 2 * hd + j) for j in range(2)]
                for tg in range(4):
                    ps, pb = self.bank()
                    for j in range(4):
                        tt = tg * 4 + j
                        kb.op("pe", lambda e, j=j, tt=tt, ps=ps: e.matmul(ps[:, j * 128:(j + 1) * 128], glrT[0:16, tt * 128:(tt + 1) * 128],
                                                                      wg[0:16, hd * 128:(hd + 1) * 128], start=True, stop=False),
                              reads=[b_glrT, b_wg], writes=[pb], inc=False)
                        kb.op("pe", lambda e, j=j, ps=ps: e.matmul(ps[:, j * 128:(j + 1) * 128], cst[0:1, 512:640],
                                                               bg[0:1, hd * 128:(hd + 1) * 128], start=False, stop=True),
                              reads=[bc, b_bg], writes=[pb], inc=(j == 3))
                    kb.op("act", lambda e, ps=ps: e.activation(out=Gz[:, :, :], in_=ps[:, :].rearrange("p (j k) -> p j k", k=128),
                                                              func=AF.Exp, scale=-1.0), reads=[pb], writes=[b_Gz])
                    kb.op("act", lambda e: e.activation(out=Gsp[:, :, :], in_=Gz[:, :, :], func=AF.Ln, bias=1.0),
                          reads=[b_Gz], writes=[b_Gsp])
                    self.ck("z/Gsp")
                    ps_c, pb_c = self.bank()
                    ps_r, pb_r = self.bank()
                    for j in range(4):
                        kb.op("pe", lambda e, j=j, ps_c=ps_c: e.matmul(ps_c[:, j * 128:(j + 1) * 128], Gsp[:, j, :], TRI, start=True, stop=True),
                              reads=[b_Gsp, bc], writes=[pb_c], inc=False)
                        kb.op("pe", lambda e, j=j, ps_r=ps_r: e.matmul(ps_r[:, j * 128:(j + 1) * 128], UU, Gsp[:, j, :], start=True, stop=True),
                              reads=[b_Gsp, bc], writes=[pb_r], inc=(j == 3))
                    kb.op("act", lambda e, ps_c=ps_c: e.activation(out=EQ[:, :], in_=ps_c[:, :], func=AF.Exp, scale=-1.0 / 16), reads=[pb_c], writes=[b_EQ])
                    kb.op("act", lambda e, ps_c=ps_c: e.activation(out=EK[:, :], in_=ps_c[:, :], func=AF.Exp, scale=1.0 / 16), reads=[pb_c], writes=[b_EK])
                    kb.op("act", lambda e, ps_r=ps_r: e.activation(out=ED[:, :, :], in_=ps_r[:, :].rearrange("p (j k) -> p j k", k=128),
                                                                func=AF.Exp, scale=-1.0 / 16), reads=[pb_r], writes=[b_ED])
                    kb.op("dve", lambda e, tg=tg: e.tensor_copy(EB[:, tg * 8:(tg + 1) * 8], EQ[:, 63:512:64]), reads=[b_EQ], cw=[b_EB])
                    self.ck("cs/rev/E")
                    self.inproj_fm(w_q, wb_q, 128, tg, lambda ps, pb, tg=tg: kb.op(
                        "dve", lambda e: e.scalar_tensor_tensor(QT[:, tg * 512:(tg + 1) * 512], ps[:, :], 128.0 ** -0.5, EQ[:, :], ALU.mult, ALU.mult),
                        reads=[pb, b_EQ], cw=[b_QT]))
                    self.inproj_fm(w_k, wb_k, 128, tg, lambda ps, pb, tg=tg: kb.op(
                        "dve", lambda e: e.tensor_tensor(KT[:, tg * 512:(tg + 1) * 512], ps[:, :], EK[:, :], ALU.mult),
                        reads=[pb, b_EK], cw=[b_KT]))
                    self.ck("qk fm")
                    ps, pb = self.bank()
                    for j in range(4):
                        tt = tg * 4 + j
                        for kc in range(8):
                            kb.op("pe", lambda e, j=j, tt=tt, kc=kc, ps=ps: e.matmul(ps[:, j * 128:(j + 1) * 128], self.XT[:, kc, tt * 128:(tt + 1) * 128],
                                                                                 w_k[:, kc * 128:(kc + 1) * 128], start=(kc == 0), stop=(kc == 7)),
                                  reads=[self.b_XT, wb_k], writes=[pb], inc=(j == 3 and kc == 7))
                    for KDm, b_KDm, mcol in ((KD0, b_KD0, 704), (KD1, b_KD1, 705)):
                        kb.op("dve", lambda e, ps=ps, tg=tg, KDm=KDm, mcol=mcol: e.scalar_tensor_tensor(
                            KDm[:, tg * 4:(tg + 1) * 4, :], ps[:, :].rearrange("p (j k) -> p j k", k=128), self.cst[:, mcol:mcol + 1],
                            ED[:, :, :], ALU.mult, ALU.mult), reads=[pb, b_ED, self.b_cst], cw=[b_KDm])
                    self.ck("kd")
                    for jj in range(2):
                        ps, pb = self.bank()
                        for j2 in range(2):
                            tt = tg * 4 + jj * 2 + j2
                            for half in range(2):
                                wv, wbv = w_v[half]
                                for kc in range(8):
                                    kb.op("pe", lambda e, j2=j2, tt=tt, kc=kc, ps=ps, half=half, wv=wv: e.matmul(
                                        ps[:, j2 * 256 + half * 128:j2 * 256 + (half + 1) * 128], self.XT[:, kc, tt * 128:(tt + 1) * 128],
                                        wv[:, kc * 128:(kc + 1) * 128], start=(kc == 0), stop=(kc == 7)),
                                        reads=[self.b_XT, wbv], writes=[pb], inc=(j2 == 1 and half == 1 and kc == 7))
                        t0 = tg * 4 + jj * 2
                        kb.op("dve", lambda e, ps=ps, t0=t0: e.tensor_copy(V[:, t0:t0 + 2, :], ps[:, :].rearrange("p (j v) -> p j v", v=256)),
                              reads=[pb], cw=[b_V])
                    self.ck("v")
                kb.op("dve", lambda e: e.memset(Sb[0][0][:, :], 0.0), writes=[Sb[0][1]])
                kb.op("dve", lambda e: e.memset(Sbb[0][0][:, :], 0.0), writes=[Sbb[0][1]])
                for tt in range(16):
                    c0, c1 = 2 * tt, 2 * tt + 1
                    ps_st, pb_st = self.ps[tt % 2], self.psb[tt % 2]
                    st, b_st = STs[tt % 2]
                    kb.op("pe", lambda e: e.matmul(ps_st[:, 0:128], KT[:, tt * 128:(tt + 1) * 128], QT[:, tt * 128:(tt + 1) * 128], start=True, stop=True),
                          reads=[b_KT, b_QT], writes=[pb_st])
                    kb.op("dve", lambda e: e.tensor_tensor(st[:, :], ps_st[:, 0:128], TRI32, ALU.mult), reads=[pb_st, self.b_cst], writes=[b_st])
                    ps_kv, pb_kv = self.ps[2 + tt % 2], self.psb[2 + tt % 2]
                    kb.op("pe", lambda e: e.matmul(ps_kv[:, 0:256], KD0[:, tt, :], V[:, tt, :], start=True, stop=True),
                          reads=[b_KD0, b_V], writes=[pb_kv], inc=False)
                    kb.op("pe", lambda e: e.matmul(ps_kv[:, 256:512], KD1[:, tt, :], V[:, tt, :], start=True, stop=True),
                          reads=[b_KD1, b_V], writes=[pb_kv])
                    self.ck("st/kv")
                    grp = (tt // 4) % 2
                    col = (tt % 4) * 128
                    for vc in range(2):
                        pso, pbo = self.ps[4 + 2 * grp + vc], self.psb[4 + 2 * grp + vc]
                        kb.op("pe", lambda e, vc=vc, pso=pso: e.matmul(pso[:, col:col + 128], V[:, tt, vc * 128:(vc + 1) * 128], st[:, :], start=True, stop=False),
                              reads=[b_V, b_st], writes=[pbo], inc=False)
                        kb.op("pe", lambda e, vc=vc, pso=pso: e.matmul(pso[:, col:col + 64], Sbb[c0 % 4][0][:, vc * 128:(vc + 1) * 128], QT[:, c0 * 64:(c0 + 1) * 64],
                                                                    start=False, stop=False), reads=[Sbb[c0 % 4][1], b_QT], writes=[pbo], inc=False)
                        if vc == 0:
                            kb.op("dve", lambda e: e.scalar_tensor_tensor(Sb[c1 % 4][0][:, :], Sb[c0 % 4][0][:, :], EB[:, c0:c0 + 1], ps_kv[:, 0:256], ALU.mult, ALU.add),
                                  reads=[Sb[c0 % 4][1], b_EB, pb_kv], writes=[Sb[c1 % 4][1]])
                            kb.op("act", lambda e: e.activation(out=Sbb[c1 % 4][0][:, :], in_=Sb[c1 % 4][0][:, :], func=AF.Copy),
                                  reads=[Sb[c1 % 4][1]], writes=[Sbb[c1 % 4][1]])
                        kb.op("pe", lambda e, vc=vc, pso=pso: e.matmul(pso[:, col + 64:col + 128], Sbb[c1 % 4][0][:, vc * 128:(vc + 1) * 128], QT[:, c1 * 64:(c1 + 1) * 64],
                                                                    start=False, stop=True), reads=[Sbb[c1 % 4][1], b_QT], writes=[pbo], inc=True)
                    kb.op("dve", lambda e: e.scalar_tensor_tensor(Sb[(c1 + 1) % 4][0][:, :], Sb[c1 % 4][0][:, :], EB[:, c1:c1 + 1], ps_kv[:, 256:512], ALU.mult, ALU.add),
                          reads=[Sb[c1 % 4][1], b_EB, pb_kv], writes=[Sb[(c1 + 1) % 4][1]])
                    kb.op("act", lambda e: e.activation(out=Sbb[(c1 + 1) % 4][0][:, :], in_=Sb[(c1 + 1) % 4][0][:, :], func=AF.Copy),
                          reads=[Sb[(c1 + 1) % 4][1]], writes=[Sbb[(c1 + 1) % 4][1]])
                    self.ck("o tile")
                    if tt % 4 == 3:
                        tg = tt // 4
                        for vc in range(2):
                            pso, pbo = self.ps[4 + 2 * grp + vc], self.psb[4 + 2 * grp + vc]
                            kb.op("act", lambda e, vc=vc, pso=pso, tg=tg: e.activation(out=OT[:, vc, tg * 512:(tg + 1) * 512], in_=pso[:, :], func=AF.Copy),
                                  reads=[pbo], cw=[b_OT])
                if hd == 0:
                    self.dump("o_raw0", OT[:, 0, :], b_OT, [128, T])
                w_go = [self.load_w(32 + 2 * hd + j) for j in range(2)]
                w_mb = [self.load_w(48 + 2 * hd + j) for j in range(2)]
                for tg in range(4):
                    sl = slice(tg * 512, (tg + 1) * 512)
                    for vc in range(2):
                        kb.op("act", lambda e, vc=vc: e.activation(out=SQ[vc][0][:, :], in_=OT[:, vc, sl], func=AF.Square), reads=[b_OT], writes=[SQ[vc][1]])
                    ps, pb = self.bank()
                    for vc in range(2):
                        kb.op("pe", lambda e, vc=vc, ps=ps: e.matmul(ps[:, :], ONES, SQ[vc][0][:, :], start=(vc == 0), stop=(vc == 1)),
                              reads=[bc, SQ[vc][1]], writes=[pb], inc=(vc == 1))
                    kb.op("act", lambda e, ps=ps: e.activation(out=RI[:, :], in_=ps[:, :], func=AF.Sqrt, scale=1.0 / 256, bias=1e-5), reads=[pb], writes=[b_RI])
                    kb.op("dve", lambda e: e.reciprocal(RI[:, :], RI[:, :]), reads=[b_RI], writes=[b_RI])
                    for vc in range(2):
                        self.inproj_fm(w_go[vc][0], w_go[vc][1], 128, tg, lambda ps, pb: kb.op(
                            "act", lambda e: e.activation(out=SG[:, :], in_=ps[:, :], func=AF.Silu), reads=[pb], writes=[b_SG]))
                        self.inproj_fm(w_mb[vc][0], w_mb[vc][1], 128, tg, lambda ps, pb: kb.op(
                            "act", lambda e: e.activation(out=SM[:, :], in_=ps[:, :], func=AF.Sigmoid), reads=[pb], writes=[b_SM]))
                        kb.op("dve", lambda e, vc=vc: e.scalar_tensor_tensor(TT[:, :], OT[:, vc, sl], ng[:, vc:vc + 1], RI[:, :], ALU.mult, ALU.mult),
                              reads=[b_OT, b_ng, b_RI], writes=[b_TT])
                        kb.op("dve", lambda e: e.tensor_tensor(TT[:, :], TT[:, :], SG[:, :], ALU.mult), reads=[b_TT, b_SG], writes=[b_TT])
                        kb.op("dve", lambda e: e.tensor_tensor(TT[:, :], TT[:, :], SM[:, :], ALU.mult), reads=[b_TT, b_SM], writes=[b_TT])
                        g = hd * 2 + vc
                        kb.op("dve", lambda e, g=g: e.tensor_tensor(self.yT[:, g, sl], TT[:, :], self.yT[:, g, sl], ALU.add),
                              reads=[b_TT, self.b_yT[g]], writes=[self.b_yT[g]])

    def layer_norm(self, es_tmp, R, b_R, grow, brow, OUT, b_OUT, tag):
        kb = self.kb
        st = self.ln_st
        kb.op("dve", lambda e: e.bn_stats(st["stats"][:, 0, :], R[:, 0:512]), reads=[b_R], writes=[st["b"]])
        kb.op("dve", lambda e: e.bn_stats(st["stats"][:, 1, :], R[:, 512:1024]), reads=[b_R], writes=[st["b"]])
        kb.op("dve", lambda e: e.bn_aggr(st["mv"][:, :], st["stats"][:, :, :].rearrange("p a b -> p (a b)")), reads=[st["b"]], writes=[st["b"]])
        kb.op("act", lambda e: e.activation(out=st["rs"][:, 0:1], in_=st["mv"][:, 1:2], func=AF.Sqrt, bias=1e-5), reads=[st["b"]], writes=[st["b2"]])
        kb.op("dve", lambda e: e.reciprocal(st["rs"][:, 0:1], st["rs"][:, 0:1]), reads=[st["b2"]], writes=[st["b2"]])
        kb.op("dve", lambda e: e.scalar_tensor_tensor(st["rs"][:, 1:2], st["mv"][:, 0:1], -1.0, st["rs"][:, 0:1], ALU.mult, ALU.mult),
              reads=[st["b"], st["b2"]], writes=[st["b2"]])
        kb.op("act", lambda e: e.activation(out=OUT[:, :], in_=R[:, :], func=AF.Identity, scale=st["rs"][:, 0:1], bias=st["rs"][:, 1:2]),
              reads=[b_R, st["b2"]], writes=[b_OUT])
        kb.op("dve", lambda e: e.tensor_tensor(OUT[:, :], OUT[:, :], self.rowp[:, grow, :], ALU.mult), reads=[b_OUT, self.b_rowp], writes=[b_OUT])
        kb.op("dve", lambda e: e.tensor_tensor(OUT[:, :], OUT[:, :], self.rowp[:, brow, :], ALU.add), reads=[b_OUT, self.b_rowp], writes=[b_OUT])

    def alloc_ln(self, es):
        g = self.ps_gen
        self.ln_st = {"stats": self.sb(es, "ln_stats%d" % g, [128, 2, 6]), "mv": self.sb(es, "ln_mv%d" % g, [128, 2]),
                      "rs": self.sb(es, "ln_rs%d" % g, [128, 2]), "b": Buf("ln_b"), "b2": Buf("ln_b2")}

    def phase_outproj(self):
        nc, kb = self.nc, self.kb
        cstb, bcb = self.cstb, self.b_cstb
        IDb, LTb, ONEb = cstb[:, 0:128], cstb[:, 384:512], cstb[:, 512:640]
        with ExitStack() as es:
            self.alloc_psum(es, 6, 2)
            self.alloc_ln(es)

            def mk(name, shape, dt=F32):
                return self.sb(es, "P2_" + name, shape, dt), Buf("P2_" + name)
            Wout, b_Wout = mk("Wout", [128, 8, 1024], BF16)
            kb.dma("pool", lambda e: e.dma_start(out=Wout[:, 0:4, :], in_=self.din["w_out"].rearrange("p (k n) -> p k n", n=1024)[:, 0:4, :]), cw=[b_Wout])
            kb.dma("pool", lambda e: e.dma_start(out=Wout[:, 4:8, :], in_=self.din["w_out"].rearrange("p (k n) -> p k n", n=1024)[:, 4:8, :]), cw=[b_Wout])
            wr32, b_wr32 = mk("wr32", [128, 8, 32])
            wrh, b_wrh = mk("wrh", [128, 8, 32], BF16)
            wrl, b_wrl = mk("wrl", [128, 8, 32], BF16)
            kb.dma("sp", lambda e: e.dma_start(out=wr32[:], in_=self.din["w_router"].rearrange("p (k n) -> p k n", n=32)), writes=[b_wr32])
            kb.op("dve", lambda e: e.tensor_copy(wrh[:], wr32[:]), reads=[b_wr32], writes=[b_wrh])
            kb.op("dve", lambda e: e.tensor_tensor(wrl[:], wr32[:], wrh[:], ALU.subtract), reads=[b_wr32, b_wrh], writes=[b_wrl])
            brt, b_brt = mk("brt", [128, 32])
            kb.dma("sp", lambda e: e.dma_start(out=brt[:], in_=self.din["b_router"][0:1, :].partition_broadcast(128)), writes=[b_brt])
            carry, b_carry = mk("carry", [128, 32])
            kb.op("dve", lambda e: e.memset(carry[:], 0.0), writes=[b_carry])
            Xt = [mk("x%d" % i, [128, 1024]) for i in range(2)]
            R, b_R = mk("R", [128, 1024])
            H1 = [mk("H1_%d" % i, [128, 1024]) for i in range(2)]
            H1b = [mk("H1b_%d" % i, [128, 1024], BF16) for i in range(2)]
            H1l = [mk("H1l_%d" % i, [128, 1024], BF16) for i in range(2)]
            HT = [mk("HT_%d" % i, [128, 8, 128], BF16) for i in range(2)]
            lg, b_lg = mk("lg", [128, 32])
            v8, b_v8 = mk("v8", [128, 8])
            i8, b_i8 = mk("i8", [128, 8], U32)
            i8f, b_i8f = mk("i8f", [128, 8])
            sm, b_sm = mk("sm", [128, 8])
            mask, b_mask = mk("mask", [128, 32], BF16)
            sc, b_sc = mk("sc", [128, 32])
            ov, b_ov = mk("ov", [128, 32])
            junk, b_junk = mk("junk", [128, 32])
            slf, b_slf = mk("slf", [128, 4])

            for tt in range(16):
                tsl = slice(tt * 128, (tt + 1) * 128)
                xt, b_xt = Xt[tt % 2]
                kb.dma("sp", lambda e: e.dma_start(out=xt[:], in_=self.din["x"][tsl, :]), writes=[b_xt])
                for half in range(2):
                    ps, pb = self.bank()
                    for kc in range(8):
                        kb.op("pe", lambda e, kc=kc, ps=ps, half=half: e.matmul(ps[:, :], self.yT[:, kc, tsl], Wout[:, kc, half * 512:(half + 1) * 512],
                                                                             start=(kc == 0), stop=(kc == 7)),
                              reads=[self.b_yT[kc], b_Wout], writes=[pb], inc=(kc == 7))
                    kb.op("dve", lambda e, ps=ps, half=half: e.scalar_tensor_tensor(R[:, half * 512:(half + 1) * 512], xt[:, half * 512:(half + 1) * 512], ALPHA,
                                                                                  ps[:, :], ALU.mult, ALU.add), reads=[b_xt, pb], cw=[b_R])
                h1, b_h1 = H1[tt % 2]
                self.layer_norm(es, R, b_R, 0, 1, h1, b_h1, "ln1")
                kb.dma("sp", lambda e: e.dma_start(out=self.H1d[tsl, :], in_=h1[:]), reads=[b_h1], cw=[self.b_H1d])
                if tt == 0:
                    self.dump("h1_0", h1[:], b_h1, [128, 1024])
                hb, b_hb = H1b[tt % 2]
                hl, b_hl = H1l[tt % 2]
                kb.op("act", lambda e: e.activation(out=hb[:, :], in_=h1[:, :], func=AF.Identity), reads=[b_h1], writes=[b_hb])
                kb.op("dve", lambda e: e.tensor_tensor(hl[:, :], h1[:, :], hb[:, :], ALU.subtract), reads=[b_h1, b_hb], writes=[b_hl])
                for (src, b_src, (dst, b_dst)) in ((hb, b_hb, HT[0]), (hl, b_hl, HT[1])):
                    pt, ptb = self.bank16()
                    for kc in range(8):
                        kb.op("pe", lambda e, kc=kc, pt=pt, src=src: e.transpose(pt[:, kc * 128:(kc + 1) * 128], src[:, kc * 128:(kc + 1) * 128], IDb),
                              reads=[b_src, bcb], writes=[ptb], inc=(kc == 7))
                    kb.op("dve", lambda e, pt=pt, dst=dst: e.tensor_copy(dst[:, :, :], pt[:, :].rearrange("p (k t) -> p k t", t=128)),
                          reads=[ptb], writes=[b_dst])
                ps, pb = self.bank()
                combos = [(HT[0], wrh, b_wrh), (HT[0], wrl, b_wrl), (HT[1], wrh, b_wrh)]
                n = 0
                for (ht, b_ht), w, b_w in combos:
                    for kc in range(8):
                        n += 1
                        kb.op("pe", lambda e, kc=kc, ps=ps, ht=ht, w=w, n=n: e.matmul(ps[:, 0:32], ht[:, kc, :], w[:, kc, :], start=(n == 1), stop=(n == 24)),
                              reads=[b_ht, b_w], writes=[pb], inc=(n == 24))
                kb.op("dve", lambda e, ps=ps: e.tensor_tensor(lg[:, :], ps[:, 0:32], brt[:, :], ALU.add), reads=[pb, b_brt], writes=[b_lg])
                if tt == 0:
                    self.dump("lg_0", lg[:], b_lg, [128, 32])
                kb.op("dve", lambda e: e.max(out=v8[:, :], in_=lg[:, :]), reads=[b_lg], writes=[b_v8])
                kb.op("dve", lambda e: e.max_index(out=i8[:, :], in_max=v8[:, :], in_values=lg[:, :]), reads=[b_lg, b_v8], writes=[b_i8])
                kb.op("dve", lambda e: e.tensor_copy(i8f[:, :], i8[:, :]), reads=[b_i8], writes=[b_i8f])
                kb.op("dve", lambda e: e.tensor_scalar_mul(sm[:, 0:1], v8[:, 0:1], -1.0), reads=[b_v8], writes=[b_sm])
                kb.op("act", lambda e: e.activation(out=sm[:, 4:8], in_=v8[:, 0:4], func=AF.Exp, bias=sm[:, 0:1], accum_out=sm[:, 1:2]),
                      reads=[b_v8, b_sm], writes=[b_sm])
                kb.op("dve", lambda e: e.reciprocal(sm[:, 2:3], sm[:, 1:2]), reads=[b_sm], writes=[b_sm])
                kb.op("dve", lambda e: e.tensor_scalar_mul(self.GATES[:, tt, :], sm[:, 4:8], sm[:, 2:3]), reads=[b_sm], writes=[self.b_route[tt]])
                kb.op("dve", lambda e: e.tensor_scalar(mask[:, :], lg[:, :], v8[:, 3:4], None, ALU.is_ge), reads=[b_lg, b_v8], writes=[b_mask])
                ps, pb = self.bank()
                kb.op("pe", lambda e, ps=ps: e.matmul(ps[:, 0:32], LTb, mask[:, :], start=True, stop=True), reads=[bcb, b_mask], writes=[pb], inc=False)
                kb.op("pe", lambda e, ps=ps: e.matmul(ps[:, 32:64], ONEb, mask[:, :], start=True, stop=True), reads=[bcb, b_mask], writes=[pb])
                kb.op("dve", lambda e, ps=ps: e.tensor_tensor(sc[:, :], ps[:, 0:32], carry[:, :], ALU.add), reads=[pb, b_carry], writes=[b_sc])
                kb.op("dve", lambda e, ps=ps: e.tensor_tensor(carry[:, :], ps[:, 32:64], carry[:, :], ALU.add), reads=[pb, b_carry, b_sc], writes=[b_carry])
                kb.op("dve", lambda e: e.tensor_scalar(ov[:, :], sc[:, :], float(CAP), float(4 * NSLOT), ALU.is_ge, ALU.mult), reads=[b_sc], writes=[b_ov])
                kb.op("dve", lambda e: e.tensor_tensor(sc[:, :], sc[:, :], self.cst[:, 672:704], ALU.add), reads=[b_sc, self.b_cst], writes=[b_sc])
                kb.op("dve", lambda e: e.tensor_tensor(sc[:, :], sc[:, :], ov[:, :], ALU.add), reads=[b_sc, b_ov], writes=[b_sc])
                for k in range(4):
                    kb.op("dve", lambda e, k=k: e.scalar_tensor_tensor(junk[:, :], self.cst[:, 640:672], i8f[:, k:k + 1], sc[:, :], ALU.is_equal, ALU.mult,
                                                                      accum_out=slf[:, k:k + 1]), reads=[self.b_cst, b_i8f, b_sc], writes=[b_junk, b_slf])
                kb.op("dve", lambda e: e.tensor_copy(self.SLOTS[:, tt, :], slf[:, :]), reads=[b_slf], writes=[self.b_route[tt]])
                for k in range(4):
                    kb.dma("pool", lambda e, k=k: e.indirect_dma_start(
                        out=self.Xg, out_offset=bass.IndirectOffsetOnAxis(ap=self.SLOTS[:, tt, k:k + 1], axis=0),
                        in_=hb[:, :], in_offset=None, bounds_check=self.bound_reg, oob_is_err=False),
                        reads=[b_hb, self.b_route[tt], self.b_Xgz], cw=[self.b_Xg])
            self.dump("gates", self.GATES[:].rearrange("p a b -> p (a b)"), self.b_route[15], [128, 64])
            if "slots" in self.dbg:
                sf, b_sf = mk("slots_f", [128, 64])
                kb.op("dve", lambda e: e.tensor_copy(sf[:, :], self.SLOTS[:].rearrange("p a b -> p (a b)")), reads=self.b_route, writes=[b_sf])
                self.dump("slots", sf[:], b_sf, [128, 64])

    def phase_moe(self):
        nc, kb = self.nc, self.kb
        IDb, bcb = self.cstb[:, 0:128], self.b_cstb
        NST = CAP // 128
        with ExitStack() as es:
            self.alloc_psum(es, 6, 2)

            def mk(name, shape, dt=F32):
                return self.sb(es, "M_" + name, shape, dt), Buf("M_" + name)
            WU = [mk("wu%d" % i, [128, 8, 2048], BF16) for i in range(2)]
            WD = [mk("wd%d" % i, [128, 8, 1024], BF16) for i in range(2)]
            BD = [mk("bd%d" % i, [128, 1024]) for i in range(2)]
            XG = [mk("xg%d" % i, [128, NST, 1024], BF16) for i in range(2)]
            XGT = [mk("xgt%d" % i, [128, 8, CAP], BF16) for i in range(2)]
            ACTT, b_ACTT = mk("actt", [128, 8, CAP], BF16)
            Gt = [mk("g%d" % i, [128, CAP]) for i in range(2)]
            St = [mk("s%d" % i, [128, CAP]) for i in range(2)]
            Ut = [mk("u%d" % i, [128, CAP]) for i in range(2)]
            Ysb = [mk("y%d" % i, [128, 1024]) for i in range(2)]
            bup, b_bup = mk("bup", [128, 32, 16])
            kb.dma("sp", lambda e: e.dma_start(out=bup[:], in_=self.din["b_up"].rearrange("p (e f) -> p e f", f=16)), writes=[b_bup])

            def load(ex):
                sl = ex % 2
                wu = self.din["w_up"][ex].rearrange("(kc p) f -> p kc f", p=128)
                wd = self.din["w_down"][ex].rearrange("(kc p) f -> p kc f", p=128)
                kb.dma("sp", lambda e: e.dma_start(out=XG[sl][0][:], in_=self.Xg[ex * CAP:(ex + 1) * CAP, :].rearrange("(st p) d -> p st d", p=128)),
                       reads=[self.b_Xg], writes=[XG[sl][1]])
                kb.dma("sp", lambda e: e.dma_start(out=BD[sl][0][:], in_=self.din["b_down"][ex:ex + 1, :].partition_broadcast(128)), writes=[BD[sl][1]])
                for q in range(4):
                    kb.dma("pool", lambda e, q=q: e.dma_start(out=WU[sl][0][:, 2 * q:2 * q + 2, :], in_=wu[:, 2 * q:2 * q + 2, :]), cw=[WU[sl][1]])
                for q in range(2):
                    kb.dma("pool", lambda e, q=q: e.dma_start(out=WD[sl][0][:, 4 * q:4 * q + 4, :], in_=wd[:, 4 * q:4 * q + 4, :]), cw=[WD[sl][1]])

            def compute(ex):
                sl = ex % 2
                wu, b_wu = WU[sl]
                wd, b_wd = WD[sl]
                xg, b_xg = XG[sl]
                xgt, b_xgt = XGT[sl]
                bd, b_bd = BD[sl]
                for st in range(NST):
                    pt, ptb = self.bank16()
                    for kc in range(8):
                        kb.op("pe", lambda e, kc=kc, pt=pt, st=st: e.transpose(pt[:, kc * 128:(kc + 1) * 128], xg[:, st, kc * 128:(kc + 1) * 128], IDb),
                              reads=[b_xg, bcb], writes=[ptb], inc=(kc == 7))
                    kb.op("dve", lambda e, pt=pt, st=st: e.tensor_copy(xgt[:, :, st * 128:(st + 1) * 128], pt[:, :].rearrange("p (k s) -> p k s", s=128)),
                          reads=[ptb], cw=[b_xgt])
                for c in range(8):
                    g, b_g = Gt[c % 2]
                    s_, b_s = St[c % 2]
                    u, b_u = Ut[c % 2]
                    ps_g, pb_g = self.bank()
                    for kc in range(8):
                        kb.op("pe", lambda e, kc=kc, ps_g=ps_g, c=c: e.matmul(ps_g[:, 0:CAP], wu[:, kc, c * 128:(c + 1) * 128], xgt[:, kc, :], start=(kc == 0), stop=(kc == 7)),
                              reads=[b_wu, b_xgt], writes=[pb_g], inc=(kc == 7))
                    ps_u, pb_u = self.bank()
                    for kc in range(8):
                        kb.op("pe", lambda e, kc=kc, ps_u=ps_u, c=c: e.matmul(ps_u[:, 0:CAP], wu[:, kc, 1024 + c * 128:1024 + (c + 1) * 128], xgt[:, kc, :],
                                                                           start=(kc == 0), stop=(kc == 7)),
                              reads=[b_wu, b_xgt], writes=[pb_u], inc=(kc == 7))
                    kb.op("dve", lambda e, ps_g=ps_g, c=c: e.tensor_scalar(g[:, :], ps_g[:, 0:CAP], bup[:, ex, c:c + 1], 7.0, ALU.add, ALU.min),
                          reads=[pb_g, b_bup], writes=[b_g])
                    kb.op("act", lambda e: e.activation(out=s_[:, :], in_=g[:, :], func=AF.Sigmoid, scale=1.702), reads=[b_g], writes=[b_s])
                    kb.op("dve", lambda e, ps_u=ps_u, c=c: e.tensor_scalar(u[:, :], ps_u[:, 0:CAP], bup[:, ex, 8 + c:9 + c], 7.0, ALU.add, ALU.min),
                          reads=[pb_u, b_bup], writes=[b_u])
                    kb.op("dve", lambda e: e.tensor_scalar(u[:, :], u[:, :], -7.0, 1.0, ALU.max, ALU.add), reads=[b_u], writes=[b_u])
                    kb.op("dve", lambda e: e.tensor_tensor(g[:, :], g[:, :], s_[:, :], ALU.mult), reads=[b_g, b_s], writes=[b_g])
                    kb.op("dve", lambda e, c=c: e.tensor_tensor(ACTT[:, c, :], g[:, :], u[:, :], ALU.mult), reads=[b_g, b_u], cw=[b_ACTT])
                for st in range(NST):
                    y, b_y = Ysb[st % 2]
                    for half in range(2):
                        ps, pb = self.bank()
                        for fc in range(8):
                            kb.op("pe", lambda e, fc=fc, ps=ps, half=half, st=st: e.matmul(ps[:, :], ACTT[:, fc, st * 128:(st + 1) * 128],
                                                                                        wd[:, fc, half * 512:(half + 1) * 512], start=(fc == 0), stop=(fc == 7)),
                                  reads=[b_ACTT, b_wd], writes=[pb], inc=(fc == 7))
                        kb.op("dve", lambda e, ps=ps, half=half: e.tensor_tensor(y[:, half * 512:(half + 1) * 512], ps[:, :], bd[:, half * 512:(half + 1) * 512], ALU.add),
                              reads=[pb, b_bd], cw=[b_y])
                    r0 = ex * CAP + st * 128
                    kb.dma("sp", lambda e, r0=r0: e.dma_start(out=self.Yg[r0:r0 + 128, :], in_=y[:]), reads=[b_y], cw=[self.b_Yg])

            ne = getattr(self, "n_experts", NE)
            load(0)
            if ne > 1:
                load(1)
            for ex in range(ne):
                compute(ex)
                if ex + 2 < ne:
                    load(ex + 2)

    def phase_final(self):
        nc, kb = self.nc, self.kb
        IDb, bcb = self.cstb[:, 0:128], self.b_cstb
        with ExitStack() as es:
            self.alloc_psum(es, 6, 2)
            self.alloc_ln(es)

            def mk(name, shape, dt=F32):
                return self.sb(es, "F_" + name, shape, dt), Buf("F_" + name)
            Wpg, b_Wpg = mk("Wpg", [128, 8, 1024], BF16)
            for q in range(2):
                kb.dma("pool", lambda e, q=q: e.dma_start(out=Wpg[:, 4 * q:4 * q + 4, :], in_=self.din["w_pg"].rearrange("p (k n) -> p k n", n=1024)[:, 4 * q:4 * q + 4, :]),
                       cw=[b_Wpg])
            Wple, b_Wple = mk("Wple", [128, 2, 1024], BF16)
            kb.dma("pool", lambda e: e.dma_start(out=Wple[:], in_=self.din["w_ple"].rearrange("p (k n) -> p k n", n=1024)), writes=[b_Wple])
            PT, b_PT = mk("PT", [128, 2, T], BF16)
            pT = self.din["pT"].rearrange("(kc p) t -> p kc t", p=128)
            for kc in range(2):
                kb.dma("pool", lambda e, kc=kc: e.dma_start(out=PT[:, kc, :], in_=pT[:, kc, :]), cw=[b_PT])
            YG = [mk("yg%d" % i, [128, 4, 1024]) for i in range(3)]
            H1t = [mk("h1_%d" % i, [128, 1024]) for i in range(3)]
            ACC, b_ACC = mk("acc", [128, 1024])
            H2T, b_H2T = mk("h2T", [128, 8, 128], BF16)
            SGT, b_SGT = mk("sgt", [128, 1024])
            OUT = [mk("out%d" % i, [128, 1024]) for i in range(2)]
            def prefetch(tt):
                tsl = slice(tt * 128, (tt + 1) * 128)
                yg, b_yg = YG[tt % 3]
                h1, b_h1 = H1t[tt % 3]
                kb.op("pool", lambda e: e.memset(yg[:], 0.0), writes=[b_yg])
                for k in range(4):
                    kb.dma("pool", lambda e, k=k: e.indirect_dma_start(
                        out=yg[:, k, :], out_offset=None, in_=self.Yg,
                        in_offset=bass.IndirectOffsetOnAxis(ap=self.SLOTS[:, tt, k:k + 1], axis=0),
                        bounds_check=self.bound_reg, oob_is_err=False), reads=[self.b_Yg, self.b_route[tt]], cw=[b_yg])
                kb.dma("sp", lambda e: e.dma_start(out=h1[:], in_=self.H1d[tsl, :]), reads=[self.b_H1d], writes=[b_h1])

            H2s = [mk("h2_%d" % i, [128, 1024]) for i in range(2)]
            H2bs = [mk("h2b_%d" % i, [128, 1024], BF16) for i in range(2)]

            def stage_a(tt):
                yg, b_yg = YG[tt % 3]
                h1, b_h1 = H1t[tt % 3]
                H2, b_H2 = H2s[tt % 2]
                H2b, b_H2b = H2bs[tt % 2]
                kb.op("act", lambda e: e.activation(out=ACC[:, :], in_=h1[:, :], func=AF.Identity, scale=ALPHA), reads=[b_h1], writes=[b_ACC])
                for k in range(4):
                    kb.op("dve", lambda e, k=k: e.scalar_tensor_tensor(ACC[:, :], yg[:, k, :], self.GATES[:, tt, k:k + 1], ACC[:, :], ALU.mult, ALU.add),
                          reads=[b_yg, self.b_route[tt], b_ACC], writes=[b_ACC])
                self.layer_norm(es, ACC, b_ACC, 2, 3, H2, b_H2, "ln2")
                if tt == 0:
                    self.dump("h2_0", H2[:], b_H2, [128, 1024])
                kb.op("act", lambda e: e.activation(out=H2b[:, :], in_=H2[:, :], func=AF.Identity), reads=[b_H2], writes=[b_H2b])

            def stage_b(tt):
                tsl = slice(tt * 128, (tt + 1) * 128)
                H2, b_H2 = H2s[tt % 2]
                H2b, b_H2b = H2bs[tt % 2]
                pt, ptb = self.bank16()
                for kc in range(8):
                    kb.op("pe", lambda e, kc=kc, pt=pt: e.transpose(pt[:, kc * 128:(kc + 1) * 128], H2b[:, kc * 128:(kc + 1) * 128], IDb),
                          reads=[b_H2b, bcb], writes=[ptb], inc=(kc == 7))
                kb.op("dve", lambda e, pt=pt: e.tensor_copy(H2T[:, :, :], pt[:, :].rearrange("p (k t) -> p k t", t=128)), reads=[ptb], writes=[b_H2T])
                o, b_o = OUT[tt % 2]
                for half in range(2):
                    hs = slice(half * 512, (half + 1) * 512)
                    ps, pb = self.bank()
                    for kc in range(8):
                        kb.op("pe", lambda e, kc=kc, ps=ps, hs=hs: e.matmul(ps[:, :], H2T[:, kc, :], Wpg[:, kc, hs], start=(kc == 0), stop=(kc == 7)),
                              reads=[b_H2T, b_Wpg], writes=[pb], inc=(kc == 7))
                    kb.op("dve", lambda e, ps=ps, hs=hs: e.tensor_tensor(SGT[:, hs], ps[:, :], self.rowp[:, 4, hs], ALU.add), reads=[pb, self.b_rowp], cw=[b_SGT])
                    kb.op("act", lambda e, hs=hs: e.activation(out=SGT[:, hs], in_=SGT[:, hs], func=AF.Sigmoid), reads=[b_SGT], cw=[b_SGT])
                    ps2, pb2 = self.bank()
                    for kc in range(2):
                        kb.op("pe", lambda e, kc=kc, ps2=ps2, hs=hs: e.matmul(ps2[:, :], PT[:, kc, tsl], Wple[:, kc, hs], start=(kc == 0), stop=(kc == 1)),
                              reads=[b_PT, b_Wple], writes=[pb2], inc=(kc == 1))
                    kb.op("dve", lambda e, ps2=ps2, hs=hs: e.tensor_tensor(o[:, hs], SGT[:, hs], ps2[:, :], ALU.mult), reads=[b_SGT, pb2], cw=[b_o])
                    kb.op("dve", lambda e, hs=hs: e.tensor_tensor(o[:, hs], o[:, hs], H2[:, hs], ALU.add), reads=[b_o, b_H2], cw=[b_o])
                kb.dma("sp", lambda e: e.dma_start(out=self.out[tsl, :], in_=o[:]), reads=[b_o])

            prefetch(0)
            prefetch(1)
            stage_a(0)
            for tt in range(16):
                if tt + 2 < 16:
                    prefetch(tt + 2)
                if tt + 1 < 16:
                    stage_a(tt + 1)
                stage_b(tt)


_PROG_CACHE = {}


def kernel(**inputs):
    inp = {k: np.asarray(v) for k, v in inputs.items()}
    sh = prep_shared(inp)
    in_maps = [dict(sh, **prep_core(inp, b)) for b in range(8)]
    if "nc" not in _PROG_CACHE:
        _PROG_CACHE["nc"] = Prog().build()
    nc = _PROG_CACHE["nc"]
    res = run_bass_kernel_spmd(nc, in_maps, core_ids=list(range(8)))
    out = np.stack([np.asarray(r["out"], dtype=np.float32) for r in res.results], axis=0)
    return out
```

```python
import numpy as np
from contextlib import ExitStack
import concourse.bass as bass
import concourse.mybir as mybir
from concourse.bass_utils import run_bass_kernel_spmd

F32 = mybir.dt.float32
BF16 = mybir.dt.bfloat16
U32 = mybir.dt.uint32
AF = mybir.ActivationFunctionType
ALU = mybir.AluOpType
AX = mybir.AxisListType

T = 2048
D = 1024
NE = 32
CAP = 384
NSLOT = NE * CAP
ALPHA = 2.0 ** 0.25
N_IN = 7184
O_XA, O_GA, O_Q, O_K, O_V, O_GO, O_GLR, O_MA, O_MB = 0, 1024, 2048, 2560, 3072, 4096, 5120, 5136, 6160


class Buf:
    __slots__ = ("name", "w", "r", "c")

    def __init__(self, name):
        self.name = name
        self.w = {}
        self.r = {}
        self.c = {}


class KB:
    def __init__(self, nc, es, n_dma_sems=24):
        self.nc = nc
        self.eng = dict(pe=nc.tensor, act=nc.scalar, dve=nc.vector, pool=nc.gpsimd, sp=nc.sync)
        self.esem = {}
        self.ecnt = {}
        self.seen = {}
        self.semobj = {}
        for n in self.eng:
            s = es.enter_context(nc.semaphore("es_" + n))
            self.esem[n] = s
            self.semobj[id(s)] = s
            self.ecnt[n] = 0
            self.seen[n] = {}
        self.dsem = []
        self.dcnt = []
        for i in range(2 * n_dma_sems):
            s = es.enter_context(nc.semaphore("ds_%d" % i))
            self.dsem.append(s)
            self.semobj[id(s)] = s
            self.dcnt.append(0)
        self.nds = n_dma_sems
        self.drr = {"pool": 0, "hw": 0}

    def _wait(self, en, toks):
        e = self.eng[en]
        seen = self.seen[en]
        for sid, val in toks.items():
            if en == "pe" and sid == id(self.esem["pe"]):
                continue
            if seen.get(sid, 0) >= val:
                continue
            e.wait_ge(self.semobj[sid], val)
            seen[sid] = val

    @staticmethod
    def _merge(dst, src):
        for k, v in src.items():
            if dst.get(k, 0) < v:
                dst[k] = v

    def _deps(self, reads, writes, cw=()):
        toks = {}
        for b in reads:
            self._merge(toks, b.w)
            self._merge(toks, b.c)
        for b in writes:
            self._merge(toks, b.w)
            self._merge(toks, b.c)
            self._merge(toks, b.r)
        for b in cw:
            self._merge(toks, b.w)
            self._merge(toks, b.r)
        return toks

    def _commit(self, tok, reads, writes, cw=()):
        for b in reads:
            self._merge(b.r, tok)
        for b in writes:
            b.w = dict(tok)
            b.r = {}
            b.c = {}
        for b in cw:
            self._merge(b.c, tok)

    disabled = False

    def op(self, en, fn, reads=(), writes=(), inc=True, cw=()):
        if self.disabled:
            return None
        self._wait(en, self._deps(reads, writes, cw))
        ins = fn(self.eng[en])
        s = self.esem[en]
        if inc:
            self.ecnt[en] += 1
            ins.then_inc(s, 1)
            tok = {id(s): self.ecnt[en]}
        else:
            tok = {id(s): self.ecnt[en] + 1}
        self._commit(tok, reads, writes, cw)
        return ins

    def dma(self, en, fn, reads=(), writes=(), cw=()):
        if self.disabled:
            return None
        kind = "pool" if en == "pool" else "hw"
        i = self.drr[kind] + (self.nds if kind == "pool" else 0)
        self.drr[kind] = (self.drr[kind] + 1) % self.nds
        s = self.dsem[i]
        toks = self._deps(reads, writes, cw)
        if self.dcnt[i] > 0:
            self._merge(toks, {id(s): self.dcnt[i]})
        self._wait(en, toks)
        ins = fn(self.eng[en])
        self.dcnt[i] += 16
        ins.then_inc(s, 16)
        tok = {id(s): self.dcnt[i]}
        self._commit(tok, reads, writes, cw)
        return ins

    def all_tokens(self):
        toks = {}
        for n in self.eng:
            if self.ecnt[n] > 0:
                toks[id(self.esem[n])] = self.ecnt[n]
        for i, s in enumerate(self.dsem):
            if self.dcnt[i] > 0:
                toks[id(s)] = self.dcnt[i]
        return toks

    def barrier(self, engines=None):
        toks = self.all_tokens()
        for n in (engines or list(self.eng)):
            own = id(self.esem[n])
            t = {k: v for k, v in toks.items() if not (n == "pe" and k == own)}
            self._wait(n, t)


def _consts():
    c = np.zeros((128, 5 * 128 + 64 + 4), np.float32)
    j = np.arange(128)
    same = (j[:, None] // 64) == (j[None, :] // 64)
    c[:, 0:128] = np.eye(128, dtype=np.float32)
    c[:, 128:256] = (same & (j[:, None] <= j[None, :]))
    c[:, 256:384] = (same & (j[:, None] > j[None, :]))
    c[:, 384:512] = (j[:, None] < j[None, :])
    c[:, 512:640] = 1.0
    c[:, 640:672] = np.arange(32)[None, :]
    c[:, 672:704] = (np.arange(32) * CAP)[None, :]
    c[:, 704] = (j < 64)
    c[:, 705] = (j >= 64)
    return c


def prep_shared(inp):
    f = lambda a: np.ascontiguousarray(a, dtype=np.float32)
    w_in = inp["w_in"][0]
    cols = np.concatenate([np.arange(0, O_GLR), np.arange(O_MA, N_IN)])
    wm = w_in[:, cols]
    sh = {}
    sh["w_in_t"] = f(wm.reshape(8, 128, 56, 128).transpose(2, 1, 0, 3).reshape(56, 128, 1024))
    sh["w_glr"] = f(w_in[:, O_GLR:O_GLR + 16].reshape(8, 128, 16).transpose(1, 0, 2).reshape(128, 128))
    chan = np.concatenate([inp["conv_w"][0], inp["conv_b"], inp["lru_b_r"], inp["lru_b_i"],
                           inp["lru_lambda"]], axis=0)
    sh["chanp"] = f(chan.reshape(8, 8, 128).transpose(2, 1, 0).reshape(128, 64))
    sh["lru_wr"] = f(inp["lru_w_r"][0].transpose(1, 0, 2).reshape(128, 1024))
    sh["lru_wi"] = f(inp["lru_w_i"][0].transpose(1, 0, 2).reshape(128, 1024))
    sh["gla_wg"] = f(inp["gla_w_gate"][0])
    sh["gla_bg"] = f(inp["gla_b_gate"])
    sh["gla_ng"] = f(inp["gla_norm_g"][0].reshape(2, 128).T)
    sh["w_out"] = f(inp["w_out"][0].reshape(8, 128, 1024).transpose(1, 0, 2).reshape(128, 8192))
    rows = np.concatenate([inp["ln1_g"], inp["ln1_b"], inp["ln2_g"], inp["ln2_b"],
                           inp["b_ple_gate"]], axis=0)
    sh["rowp"] = f(np.broadcast_to(rows.reshape(1, 5 * 1024), (128, 5 * 1024)))
    sh["w_router"] = f(inp["w_router"][0].reshape(8, 128, 32).transpose(1, 0, 2).reshape(128, 256))
    sh["b_router"] = f(inp["b_router"])
    sh["w_up"] = inp["w_up"][0]
    sh["b_up"] = f(inp["b_up"][0].reshape(32, 16, 128).transpose(2, 0, 1).reshape(128, 512))
    sh["w_down"] = inp["w_down"][0]
    sh["b_down"] = f(inp["b_down"][0])
    sh["w_ple"] = f(inp["w_ple"][0].reshape(2, 128, 1024).transpose(1, 0, 2).reshape(128, 2048))
    sh["w_pg"] = f(inp["w_ple_gate"][0].reshape(8, 128, 1024).transpose(1, 0, 2).reshape(128, 8192))
    sh["consts"] = _consts()
    return sh


def prep_core(inp, b):
    x = np.asarray(inp["x"][b], dtype=np.float32)
    p = np.asarray(inp["p"][0, b], dtype=np.float32)
    return {"x": np.ascontiguousarray(x), "xT": np.ascontiguousarray(x.T),
            "pT": np.ascontiguousarray(p.T)}


SHARED_SHAPES = {
    "w_in_t": [56, 128, 1024], "w_glr": [128, 128], "chanp": [128, 64], "lru_wr": [128, 1024],
    "lru_wi": [128, 1024], "gla_wg": [16, 512], "gla_bg": [1, 512], "gla_ng": [128, 2],
    "w_out": [128, 8192], "rowp": [128, 5120], "w_router": [128, 256], "b_router": [1, 32],
    "w_up": [32, 1024, 2048], "b_up": [128, 512], "w_down": [32, 1024, 1024], "b_down": [32, 1024],
    "w_ple": [128, 2048], "w_pg": [128, 8192], "consts": [128, 708],
}
CORE_SHAPES = {"x": [T, D], "xT": [D, T], "pT": [256, T]}


class StopBuild(Exception):
    pass


class Prog:
    ck_n = 0
    ck_stop = None

    def ck(self, label=""):
        self.ck_n += 1
        if self.ck_stop is not None and self.ck_n >= self.ck_stop:
            if not self.kb.disabled:
                print("STOP at checkpoint", self.ck_n, label)
            self.kb.disabled = True

    def __init__(self, dbg=(), stop_after=None):
        self.dbg = set(dbg)
        self.stop_after = stop_after
        self.nc = nc = bass.Bass("TRN2", target_bir_lowering=False)
        self.din = {}
        for n, s in list(SHARED_SHAPES.items()) + list(CORE_SHAPES.items()):
            self.din[n] = nc.dram_tensor(n, s, F32, kind="ExternalInput").ap()
        self.out = nc.dram_tensor("out", [T, D], F32, kind="ExternalOutput").ap()
        self.dbg_out = {}
        self.es = ExitStack()

    def dbg_tensor(self, name, shape):
        t = self.nc.dram_tensor("dbg_" + name, shape, F32, kind="ExternalOutput").ap()
        self.dbg_out[name] = t
        return t

    def sb(self, es, name, shape, dt=F32):
        return es.enter_context(self.nc.sbuf_tensor(name, shape, dt))

    def build(self):
        nc = self.nc
        with self.es as es:
            kb = self.kb = KB(nc, es)
            self.bound_reg = nc.gpsimd.to_reg(NSLOT - 1)
            self.cst = self.sb(es, "cst", [128, 708])
            self.b_cst = Buf("cst")
            kb.dma("sp", lambda e: e.dma_start(out=self.cst[:], in_=self.din["consts"][:, :]),
                   writes=[self.b_cst])
            self.cstb = self.sb(es, "cstb", [128, 708], BF16)
            self.b_cstb = Buf("cstb")
            kb.op("dve", lambda e: e.tensor_copy(self.cstb[:], self.cst[:]),
                  reads=[self.b_cst], writes=[self.b_cstb])
            self.GATES = self.sb(es, "GATES", [128, 16, 4])
            self.SLOTS = self.sb(es, "SLOTS", [128, 16, 4], U32)
            self.b_route = [Buf("route%d" % i) for i in range(16)]
            self.Xg = nc.dram_tensor("Xg", [NSLOT, D], BF16).ap()
            self.Yg = nc.dram_tensor("Yg", [NSLOT, D], F32).ap()
            self.H1d = nc.dram_tensor("H1d", [T, D], F32).ap()
            self.b_Xg, self.b_Yg, self.b_H1d = Buf("Xg"), Buf("Yg"), Buf("H1d")
            self.b_Xgz = Buf("Xgz")
            self.zero_xg(es)
            with ExitStack() as es_y:
                self.yT = self.sb(es_y, "yT", [128, 8, T], BF16)
                self.b_yT = [Buf("yT%d" % g) for g in range(8)]
                with ExitStack() as es1:
                    self.alloc_psum(es1, 8, 0)
                    self.XT = self.sb(es1, "XT", [128, 8, T], BF16)
                    self.b_XT = Buf("XT")
                    xT = self.din["xT"].rearrange("(kc p) t -> p kc t", p=128)
                    for kc in range(8):
                        kb.dma("pool", lambda e, kc=kc: e.dma_start(out=self.XT[:, kc, :], in_=xT[:, kc, :]),
                               cw=[self.b_XT])
                    if not getattr(self, "skip_lru", False):
                        self.phase_lru(es1)
                    else:
                        kb.op("dve", lambda e: e.memset(self.yT[:], 0.0), writes=self.b_yT)
                    if self.stop_after == "lru":
                        return self.finish()
                    kb.barrier()
                    self.phase_gla(es1)
                    if self.stop_after == "gla":
                        return self.dump_yT()
                kb.barrier()
                self.phase_outproj()
                if self.stop_after == "outproj":
                    return self.finish()
            kb.barrier()
            self.phase_moe()
            if self.stop_after == "moe":
                return self.finish()
            kb.barrier()
            self.phase_final()
        return self.finish()

    def alloc_psum(self, es, n32, n16):
        nc = self.nc
        self.ps = [es.enter_context(nc.psum_tensor("ps%d_%d" % (i, self.ps_gen), [128, 512], F32)) for i in range(n32)]
        self.psb = [Buf("ps%d" % i) for i in range(n32)]
        self.pst = [es.enter_context(nc.psum_tensor("pst%d_%d" % (i, self.ps_gen), [128, 1024], BF16)) for i in range(n16)]
        self.pstb = [Buf("pst%d" % i) for i in range(n16)]
        self.ps_rr = 0
        self.pst_rr = 0
        self.ps_gen += 1

    def zero_xg(self, es):
        kb = self.kb
        z = self.sb(es, "zeros", [128, 2, 1024], BF16)
        bz = Buf("zeros")
        kb.op("dve", lambda e: e.memset(z[:], 0.0), writes=[bz])
        xg = self.Xg.rearrange("(n p) d -> p n d", p=128)
        for i in range(NSLOT // 128 // 2):
            kb.dma("sp", lambda e, i=i: e.dma_start(out=xg[:, i * 2:(i + 1) * 2, :], in_=z[:]), reads=[bz], cw=[self.b_Xgz])

    ps_gen = 0

    def bank(self):
        i = self.ps_rr
        self.ps_rr = (self.ps_rr + 1) % len(self.ps)
        return self.ps[i], self.psb[i]

    def bank16(self):
        i = self.pst_rr
        self.pst_rr = (self.pst_rr + 1) % len(self.pst)
        return self.pst[i], self.pstb[i]

    def finish(self):
        kb = self.kb
        kb.disabled = False
        kb.barrier(["sp"])
        return self.nc

    def dump_yT(self):
        kb = self.kb
        kb.barrier()
        with ExitStack() as es:
            tmp = self.sb(es, "dump_tmp", [128, 8, T])
            b = Buf("dump_tmp")
            kb.op("dve", lambda e: e.tensor_copy(tmp[:], self.yT[:]), reads=self.b_yT, writes=[b])
            t = self.dbg_tensor("yT", [128, 8, T])
            kb.dma("sp", lambda e: e.dma_start(out=t, in_=tmp[:]), reads=[b])
            return self.finish()

    def dump(self, name, sb_ap, buf, shape):
        if name not in self.dbg:
            return
        t = self.dbg_tensor(name, shape)
        self.kb.dma("sp", lambda e: e.dma_start(out=t, in_=sb_ap), reads=[buf])

    def inproj_fm(self, wt, wb, ncols, tg, evac):
        kb = self.kb
        ps, pb = self.bank()
        for kc in range(8):
            kb.op("pe", lambda e, kc=kc: e.matmul(ps[0:ncols, :], wt[:, kc * ncols:(kc + 1) * ncols],
                                                   self.XT[:, kc, tg * 512:(tg + 1) * 512],
                                                   start=(kc == 0), stop=(kc == 7)),
                  reads=[wb, self.b_XT], writes=[pb], inc=(kc == 7))
        evac(ps, pb)

    def load_w(self, grp):
        i = self.w_rr
        self.w_rr = (self.w_rr + 1) % len(self.wring)
        wt, wb = self.wring[i], self.wringb[i]
        self.kb.dma("pool", lambda e: e.dma_start(out=wt[:], in_=self.din["w_in_t"][grp, :, :]), writes=[wb])
        return wt, wb

    def phase_lru(self, es1):
        nc, kb = self.nc, self.kb
        with ExitStack() as es:
            NW = 6
            self.wring = [self.sb(es, "wr%d" % i, [128, 1024], BF16) for i in range(NW)]
            self.wringb = [Buf("wr%d" % i) for i in range(NW)]
            self.w_rr = 0
            chan = self.sb(es, "chan", [128, 8, 8])
            b_chan = Buf("chan")
            kb.dma("sp", lambda e: e.dma_start(out=chan[:], in_=self.din["chanp"].rearrange("p (g k) -> p g k", k=8)),
                   writes=[b_chan])
            wr = self.sb(es, "lwr", [128, 8, 128], BF16)
            wi = self.sb(es, "lwi", [128, 8, 128], BF16)
            b_wr, b_wi = Buf("lwr"), Buf("lwi")
            kb.dma("pool", lambda e: e.dma_start(out=wr[:], in_=self.din["lru_wr"].rearrange("p (g d) -> p g d", d=128)), writes=[b_wr])
            kb.dma("pool", lambda e: e.dma_start(out=wi[:], in_=self.din["lru_wi"].rearrange("p (g d) -> p g d", d=128)), writes=[b_wi])
            sc = self.sb(es, "lsc", [128, 8, 4])
            b_sc = Buf("lsc")
            kb.op("act", lambda e: e.activation(out=sc[:, :, 0], in_=chan[:, :, 7], func=AF.Exp, scale=-1.0),
                  reads=[b_chan], writes=[b_sc])
            kb.op("act", lambda e: e.activation(out=sc[:, :, 1], in_=sc[:, :, 0], func=AF.Ln, bias=1.0),
                  reads=[b_sc], writes=[b_sc])
            kb.op("dve", lambda e: e.tensor_scalar_mul(sc[:, :, 2], sc[:, :, 1], -8.0), reads=[b_sc], writes=[b_sc])
            kb.op("dve", lambda e: e.tensor_scalar_mul(sc[:, :, 3], sc[:, :, 1], -16.0), reads=[b_sc], writes=[b_sc])

            def mk(name, shape, dt=F32):
                return self.sb(es, "L_" + name, shape, dt), [Buf("L_%s_%d" % (name, i)) for i in range(4)]
            xa, b_xa = mk("xa", [128, T + 3], BF16)
            DG = self.sb(es, "L_dg", [128, 8, 4, 128], BF16)
            b_DG = Buf("L_dg")
            for g_ in range(8):
                for k_ in range(4):
                    kb.op("dve", lambda e, g_=g_, k_=k_: e.tensor_scalar_mul(DG[:, g_, k_, :], self.cstb[:, 0:128], chan[:, g_, k_:k_ + 1]),
                          reads=[self.b_cstb, b_chan], cw=[b_DG])
            xcb, b_xcb = mk("xcb", [128, T], BF16)
            xc, b_xc = mk("xc", [128, T])
            r, b_r = mk("r", [128, T])
            ii, b_ii = mk("i", [128, T])
            aa, b_aa = mk("a", [128, T])
            mm, b_mm = mk("m", [128, T])
            h, b_h = mk("h", [128, T])
            ga, b_ga = mk("ga", [128, T])
            t1, b_t1 = mk("t1", [128, T])
            t2, b_t2 = mk("t2", [128, T])
            b_pad = Buf("xa_pad")
            kb.op("dve", lambda e: e.memset(xa[:, 0:3], 0.0), writes=[b_pad])
            TG = range(4)

            def cs(tg, off=0):
                return slice(off + tg * 512, off + (tg + 1) * 512)

            for g in range(8):
                w_xa, wb_xa = self.load_w(g)
                w_ga, wb_ga = self.load_w(8 + g)
                w_ma, wb_ma = self.load_w(40 + g)
                for tg in TG:
                    self.inproj_fm(w_xa, wb_xa, 128, tg, lambda ps, pb, tg=tg: kb.op(
                        "act", lambda e: e.activation(out=xa[:, cs(tg, 3)], in_=ps[:, :], func=AF.Copy), reads=[pb], writes=[b_xa[tg]]))
                for tg in TG:
                    self.inproj_fm(w_ga, wb_ga, 128, tg, lambda ps, pb, tg=tg: kb.op(
                        "act", lambda e: e.activation(out=ga[:, cs(tg)], in_=ps[:, :], func=AF.Copy), reads=[pb], writes=[b_ga[tg]]))
                for tg in TG:
                    self.inproj_fm(w_ma, wb_ma, 128, tg, lambda ps, pb, tg=tg: kb.op(
                        "act", lambda e: e.activation(out=t2[:, cs(tg)], in_=ps[:, :], func=AF.Sigmoid), reads=[pb], writes=[b_t2[tg]]))
                for tg in TG:
                    kb.op("dve", lambda e, tg=tg: e.tensor_tensor(t1[:, cs(tg)], ga[:, cs(tg)], ga[:, cs(tg)], ALU.mult), reads=[b_ga[tg]], writes=[b_t1[tg]])
                    kb.op("dve", lambda e, tg=tg: e.tensor_scalar(t1[:, cs(tg)], t1[:, cs(tg)], 0.044715, 1.0, ALU.mult, ALU.add), reads=[b_t1[tg]], writes=[b_t1[tg]])
                    kb.op("dve", lambda e, tg=tg: e.tensor_tensor(t1[:, cs(tg)], t1[:, cs(tg)], ga[:, cs(tg)], ALU.mult), reads=[b_t1[tg], b_ga[tg]], writes=[b_t1[tg]])
                for tg in TG:
                    prev = [b_xa[tg - 1]] if tg > 0 else [b_pad]
                    ps, pb = self.bank()
                    for k in range(4):
                        kb.op("pe", lambda e, k=k, tg=tg, ps=ps: e.matmul(ps[:, :], DG[:, g, k, :], xa[:, cs(tg, k)], start=(k == 0), stop=(k == 3)),
                              reads=[b_DG, b_xa[tg]] + prev, writes=[pb], inc=(k == 3))
                    kb.op("act", lambda e, tg=tg, ps=ps: e.activation(out=xc[:, cs(tg)], in_=ps[:, :], func=AF.Identity, bias=chan[:, g, 4:5]),
                          reads=[pb, b_chan], writes=[b_xc[tg]])
                    kb.op("dve", lambda e, tg=tg: e.tensor_copy(xcb[:, cs(tg)], xc[:, cs(tg)]), reads=[b_xc[tg]], writes=[b_xcb[tg]])
                if g == 0:
                    self.dump("xc0", xc[:, :], b_xc[3], [128, T])
                for (wg, bwg, dst, b_dst, bi) in ((wr, b_wr, r, b_r, 5), (wi, b_wi, ii, b_ii, 6)):
                    for tg in TG:
                        ps, pb = self.bank()
                        kb.op("pe", lambda e, tg=tg, ps=ps, wg=wg: e.matmul(ps[:, :], wg[:, g, :], xcb[:, cs(tg)], start=True, stop=True),
                              reads=[bwg, b_xcb[tg]], writes=[pb])
                        kb.op("act", lambda e, tg=tg, ps=ps, dst=dst, bi=bi: e.activation(
                            out=dst[:, cs(tg)], in_=ps[:, :], func=AF.Sigmoid, bias=chan[:, g, bi:bi + 1]),
                            reads=[pb, b_chan], writes=[b_dst[tg]])
                for tg in TG:
                    kb.op("act", lambda e, tg=tg: e.activation(out=aa[:, cs(tg)], in_=r[:, cs(tg)], func=AF.Exp, scale=sc[:, g, 2:3]),
                          reads=[b_r[tg], b_sc], writes=[b_aa[tg]])
                for tg in TG:
                    kb.op("act", lambda e, tg=tg: e.activation(out=mm[:, cs(tg)], in_=r[:, cs(tg)], func=AF.Exp, scale=sc[:, g, 3:4]),
                          reads=[b_r[tg], b_sc], writes=[b_mm[tg]])
                for tg in TG:
                    kb.op("act", lambda e, tg=tg: e.activation(out=mm[:, cs(tg)], in_=mm[:, cs(tg)], func=AF.Ln, scale=-1.0, bias=1.0),
                          reads=[b_mm[tg]], writes=[b_mm[tg]])
                for tg in TG:
                    kb.op("act", lambda e, tg=tg: e.activation(out=mm[:, cs(tg)], in_=mm[:, cs(tg)], func=AF.Exp, scale=0.5),
                          reads=[b_mm[tg]], writes=[b_mm[tg]])
                for tg in TG:
                    kb.op("dve", lambda e, tg=tg: e.tensor_tensor(mm[:, cs(tg)], mm[:, cs(tg)], ii[:, cs(tg)], ALU.mult), reads=[b_mm[tg], b_ii[tg]], writes=[b_mm[tg]])
                    kb.op("dve", lambda e, tg=tg: e.tensor_tensor(mm[:, cs(tg)], mm[:, cs(tg)], xc[:, cs(tg)], ALU.mult), reads=[b_mm[tg], b_xc[tg]], writes=[b_mm[tg]])
                for tg in TG:
                    init = 0.0 if tg == 0 else h[:, tg * 512 - 1:tg * 512]
                    kb.op("dve", lambda e, tg=tg, init=init: e.tensor_tensor_scan(h[:, cs(tg)], aa[:, cs(tg)], mm[:, cs(tg)], init, ALU.mult, ALU.add),
                          reads=[b_aa[tg], b_mm[tg]] + ([b_h[tg - 1]] if tg > 0 else []), writes=[b_h[tg]])
                if g == 0:
                    self.dump("h0", h[:, :], b_h[3], [128, T])
                for tg in TG:
                    kb.op("act", lambda e, tg=tg: e.activation(out=t1[:, cs(tg)], in_=t1[:, cs(tg)], func=AF.Sigmoid, scale=1.5957691216057308),
                          reads=[b_t1[tg]], writes=[b_t1[tg]])
                for tg in TG:
                    kb.op("dve", lambda e, tg=tg: e.tensor_tensor(t1[:, cs(tg)], t1[:, cs(tg)], ga[:, cs(tg)], ALU.mult), reads=[b_t1[tg], b_ga[tg]], writes=[b_t1[tg]])
                    kb.op("dve", lambda e, tg=tg: e.tensor_tensor(t1[:, cs(tg)], t1[:, cs(tg)], h[:, cs(tg)], ALU.mult), reads=[b_t1[tg], b_h[tg]], writes=[b_t1[tg]])
                    kb.op("dve", lambda e, tg=tg: e.tensor_tensor(self.yT[:, g, cs(tg)], t1[:, cs(tg)], t2[:, cs(tg)], ALU.mult),
                          reads=[b_t1[tg], b_t2[tg]], cw=[self.b_yT[g]])

    def phase_gla(self, es1):
        nc, kb = self.nc, self.kb
        cst = self.cstb
        TRI, UU, ONES = cst[:, 128:256], cst[:, 256:384], cst[:, 512:640]
        TRI32 = self.cst[:, 128:256]
        bc = self.b_cstb
        with ExitStack() as es:
            NW = 8
            self.wring = [self.sb(es, "gw%d" % i, [128, 1024], BF16) for i in range(NW)]
            self.wringb = [Buf("gw%d" % i) for i in range(NW)]
            self.w_rr = 0
            wglr = self.sb(es, "wglr", [128, 128], BF16)
            b_wglr = Buf("wglr")
            kb.dma("pool", lambda e: e.dma_start(out=wglr[:], in_=self.din["w_glr"][:, :]), writes=[b_wglr])
            wg = self.sb(es, "wg", [16, 512], BF16)
            bg = self.sb(es, "bg", [1, 512], BF16)
            ng = self.sb(es, "ng", [128, 2])
            b_wg, b_bg, b_ng = Buf("wg"), Buf("bg"), Buf("ng")
            kb.dma("pool", lambda e: e.dma_start(out=wg[:], in_=self.din["gla_wg"][:, :]), writes=[b_wg])
            kb.dma("pool", lambda e: e.dma_start(out=bg[:], in_=self.din["gla_bg"][:, :]), writes=[b_bg])
            kb.dma("sp", lambda e: e.dma_start(out=ng[:], in_=self.din["gla_ng"][:, :]), writes=[b_ng])
            glrT = self.sb(es, "glrT", [16, T], BF16)
            b_glrT = Buf("glrT")
            for tg in range(4):
                self.inproj_fm(wglr, b_wglr, 16, tg, lambda ps, pb, tg=tg: kb.op(
                    "act", lambda e: e.activation(out=glrT[:, tg * 512:(tg + 1) * 512], in_=ps[0:16, :], func=AF.Copy),
                    reads=[pb], cw=[b_glrT]))

            self.ck("glrT")

            def mk(name, shape, dt=F32):
                return self.sb(es, "G_" + name, shape, dt), Buf("G_" + name)
            QT, b_QT = mk("QT", [128, T], BF16)
            KT, b_KT = mk("KT", [128, T], BF16)
            KD0, b_KD0 = mk("KD0", [128, 16, 128], BF16)
            KD1, b_KD1 = mk("KD1", [128, 16, 128], BF16)
            V, b_V = mk("V", [128, 16, 256], BF16)
            OT, b_OT = mk("OT", [128, 2, T])
            EB, b_EB = mk("EB", [128, 32])
            Gsp, b_Gsp = mk("Gsp", [128, 4, 128], BF16)
            Gz, b_Gz = mk("Gz", [128, 4, 128])
            EQ, b_EQ = mk("EQ", [128, 512])
            EK, b_EK = mk("EK", [128, 512])
            ED, b_ED = mk("ED", [128, 4, 128])
            STs = [mk("ST%d" % i, [128, 128], BF16) for i in range(2)]
            Sb = [mk("S%d" % i, [128, 256]) for i in range(4)]
            Sbb = [mk("Sb%d" % i, [128, 256], BF16) for i in range(4)]
            SQ = [mk("SQ%d" % i, [128, 512], BF16) for i in range(2)]
            RIf, b_RIf = mk("RIf", [128, T])
            SG, b_SG = mk("SG", [128, 512])
            SM, b_SM = mk("SM", [128, 512])
            TT, b_TT = mk("TT", [128, 512])

            HW = {}

            def h1(hd, tg):
                if tg == 0:
                    HW[hd] = dict(q=self.load_w(16 + hd), k=self.load_w(20 + hd), v=[self.load_w(24 + 2 * hd + j) for j in range(2)])
                w_q, wb_q = HW[hd]['q']
                w_k, wb_k = HW[hd]['k']
                w_v = HW[hd]['v']
                ps, pb = self.bank()
                for j in range(4):
                    tt = tg * 4 + j
                    kb.op("pe", lambda e, j=j, tt=tt, ps=ps: e.matmul(ps[:, j * 128:(j + 1) * 128], glrT[0:16, tt * 128:(tt + 1) * 128],
                                                                  wg[0:16, hd * 128:(hd + 1) * 128], start=True, stop=False),
                          reads=[b_glrT, b_wg], writes=[pb], inc=False)
                    kb.op("pe", lambda e, j=j, ps=ps: e.matmul(ps[:, j * 128:(j + 1) * 128], cst[0:1, 512:640],
                                                           bg[0:1, hd * 128:(hd + 1) * 128], start=False, stop=True),
                          reads=[bc, b_bg], writes=[pb], inc=(j == 3))
                kb.op("act", lambda e, ps=ps: e.activation(out=Gz[:, :, :], in_=ps[:, :].rearrange("p (j k) -> p j k", k=128),
                                                          func=AF.Exp, scale=-1.0), reads=[pb], writes=[b_Gz])
                kb.op("act", lambda e: e.activation(out=Gsp[:, :, :], in_=Gz[:, :, :], func=AF.Ln, bias=1.0),
                      reads=[b_Gz], writes=[b_Gsp])
                self.ck("z/Gsp")
                ps_c, pb_c = self.bank()
                ps_r, pb_r = self.bank()
                for j in range(4):
                    kb.op("pe", lambda e, j=j, ps_c=ps_c: e.matmul(ps_c[:, j * 128:(j + 1) * 128], Gsp[:, j, :], TRI, start=True, stop=True),
                          reads=[b_Gsp, bc], writes=[pb_c], inc=False)
                    kb.op("pe", lambda e, j=j, ps_r=ps_r: e.matmul(ps_r[:, j * 128:(j + 1) * 128], UU, Gsp[:, j, :], start=True, stop=True),
                          reads=[b_Gsp, bc], writes=[pb_r], inc=(j == 3))
                kb.op("act", lambda e, ps_c=ps_c: e.activation(out=EQ[:, :], in_=ps_c[:, :], func=AF.Exp, scale=-1.0 / 16), reads=[pb_c], writes=[b_EQ])
                kb.op("act", lambda e, ps_c=ps_c: e.activation(out=EK[:, :], in_=ps_c[:, :], func=AF.Exp, scale=1.0 / 16), reads=[pb_c], writes=[b_EK])
                kb.op("act", lambda e, ps_r=ps_r: e.activation(out=ED[:, :, :], in_=ps_r[:, :].rearrange("p (j k) -> p j k", k=128),
                                                            func=AF.Exp, scale=-1.0 / 16), reads=[pb_r], writes=[b_ED])
                kb.op("dve", lambda e, tg=tg: e.tensor_copy(EB[:, tg * 8:(tg + 1) * 8], EQ[:, 63:512:64]), reads=[b_EQ], cw=[b_EB])
                self.ck("cs/rev/E")
                self.inproj_fm(w_q, wb_q, 128, tg, lambda ps, pb, tg=tg: kb.op(
                    "dve", lambda e: e.scalar_tensor_tensor(QT[:, tg * 512:(tg + 1) * 512], ps[:, :], 128.0 ** -0.5, EQ[:, :], ALU.mult, ALU.mult),
                    reads=[pb, b_EQ], cw=[b_QT]))
                self.inproj_fm(w_k, wb_k, 128, tg, lambda ps, pb, tg=tg: kb.op(
                    "dve", lambda e: e.tensor_tensor(KT[:, tg * 512:(tg + 1) * 512], ps[:, :], EK[:, :], ALU.mult),
                    reads=[pb, b_EK], cw=[b_KT]))
                self.ck("qk fm")
                ps, pb = self.bank()
                for j in range(4):
                    tt = tg * 4 + j
                    for kc in range(8):
                        kb.op("pe", lambda e, j=j, tt=tt, kc=kc, ps=ps: e.matmul(ps[:, j * 128:(j + 1) * 128], self.XT[:, kc, tt * 128:(tt + 1) * 128],
                                                                             w_k[:, kc * 128:(kc + 1) * 128], start=(kc == 0), stop=(kc == 7)),
                              reads=[self.b_XT, wb_k], writes=[pb], inc=(j == 3 and kc == 7))
                for KDm, b_KDm, mcol in ((KD0, b_KD0, 704), (KD1, b_KD1, 705)):
                    kb.op("dve", lambda e, ps=ps, tg=tg, KDm=KDm, mcol=mcol: e.scalar_tensor_tensor(
                        KDm[:, tg * 4:(tg + 1) * 4, :], ps[:, :].rearrange("p (j k) -> p j k", k=128), self.cst[:, mcol:mcol + 1],
                        ED[:, :, :], ALU.mult, ALU.mult), reads=[pb, b_ED, self.b_cst], cw=[b_KDm])
                self.ck("kd")
                for jj in range(2):
                    ps, pb = self.bank()
                    for j2 in range(2):
                        tt = tg * 4 + jj * 2 + j2
                        for half in range(2):
                            wv, wbv = w_v[half]
                            for kc in range(8):
                                kb.op("pe", lambda e, j2=j2, tt=tt, kc=kc, ps=ps, half=half, wv=wv: e.matmul(
                                    ps[:, j2 * 256 + half * 128:j2 * 256 + (half + 1) * 128], self.XT[:, kc, tt * 128:(tt + 1) * 128],
                                    wv[:, kc * 128:(kc + 1) * 128], start=(kc == 0), stop=(kc == 7)),
                                    reads=[self.b_XT, wbv], writes=[pb], inc=(j2 == 1 and half == 1 and kc == 7))
                    t0 = tg * 4 + jj * 2
                    kb.op("dve", lambda e, ps=ps, t0=t0: e.tensor_copy(V[:, t0:t0 + 2, :], ps[:, :].rearrange("p (j v) -> p j v", v=256)),
                          reads=[pb], cw=[b_V])
                self.ck("v")

            def h2(hd):
                kb.op("dve", lambda e: e.memset(Sb[0][0][:, :], 0.0), writes=[Sb[0][1]])
                kb.op("dve", lambda e: e.memset(Sbb[0][0][:, :], 0.0), writes=[Sbb[0][1]])
                for tt in range(16):
                    c0, c1 = 2 * tt, 2 * tt + 1
                    ps_st, pb_st = self.ps[tt % 2], self.psb[tt % 2]
                    st, b_st = STs[tt % 2]
                    kb.op("pe", lambda e: e.matmul(ps_st[:, 0:128], KT[:, tt * 128:(tt + 1) * 128], QT[:, tt * 128:(tt + 1) * 128], start=True, stop=True),
                          reads=[b_KT, b_QT], writes=[pb_st])
                    kb.op("dve", lambda e: e.tensor_tensor(st[:, :], ps_st[:, 0:128], TRI32, ALU.mult), reads=[pb_st, self.b_cst], writes=[b_st])
                    ps_kv, pb_kv = self.ps[2 + tt % 2], self.psb[2 + tt % 2]
                    kb.op("pe", lambda e: e.matmul(ps_kv[:, 0:256], KD0[:, tt, :], V[:, tt, :], start=True, stop=True),
                          reads=[b_KD0, b_V], writes=[pb_kv], inc=False)
                    kb.op("pe", lambda e: e.matmul(ps_kv[:, 256:512], KD1[:, tt, :], V[:, tt, :], start=True, stop=True),
                          reads=[b_KD1, b_V], writes=[pb_kv])
                    self.ck("st/kv")
                    grp = (tt // 4) % 2
                    col = (tt % 4) * 128
                    for vc in range(2):
                        pso, pbo = self.ps[4 + 2 * grp + vc], self.psb[4 + 2 * grp + vc]
                        kb.op("pe", lambda e, vc=vc, pso=pso: e.matmul(pso[:, col:col + 128], V[:, tt, vc * 128:(vc + 1) * 128], st[:, :], start=True, stop=False),
                              reads=[b_V, b_st], writes=[pbo], inc=False)
                        kb.op("pe", lambda e, vc=vc, pso=pso: e.matmul(pso[:, col:col + 64], Sbb[c0 % 4][0][:, vc * 128:(vc + 1) * 128], QT[:, c0 * 64:(c0 + 1) * 64],
                                                                    start=False, stop=False), reads=[Sbb[c0 % 4][1], b_QT], writes=[pbo], inc=False)
                        if vc == 0:
                            kb.op("dve", lambda e: e.scalar_tensor_tensor(Sb[c1 % 4][0][:, :], Sb[c0 % 4][0][:, :], EB[:, c0:c0 + 1], ps_kv[:, 0:256], ALU.mult, ALU.add),
                                  reads=[Sb[c0 % 4][1], b_EB, pb_kv], writes=[Sb[c1 % 4][1]])
                            kb.op("act", lambda e: e.activation(out=Sbb[c1 % 4][0][:, :], in_=Sb[c1 % 4][0][:, :], func=AF.Copy),
                                  reads=[Sb[c1 % 4][1]], writes=[Sbb[c1 % 4][1]])
                        kb.op("pe", lambda e, vc=vc, pso=pso: e.matmul(pso[:, col + 64:col + 128], Sbb[c1 % 4][0][:, vc * 128:(vc + 1) * 128], QT[:, c1 * 64:(c1 + 1) * 64],
                                                                    start=False, stop=True), reads=[Sbb[c1 % 4][1], b_QT], writes=[pbo], inc=True)
                    kb.op("dve", lambda e: e.scalar_tensor_tensor(Sb[(c1 + 1) % 4][0][:, :], Sb[c1 % 4][0][:, :], EB[:, c1:c1 + 1], ps_kv[:, 256:512], ALU.mult, ALU.add),
                          reads=[Sb[c1 % 4][1], b_EB, pb_kv], writes=[Sb[(c1 + 1) % 4][1]])
                    kb.op("act", lambda e: e.activation(out=Sbb[(c1 + 1) % 4][0][:, :], in_=Sb[(c1 + 1) % 4][0][:, :], func=AF.Copy),
                          reads=[Sb[(c1 + 1) % 4][1]], writes=[Sbb[(c1 + 1) % 4][1]])
                    self.ck("o tile")
                    if tt % 4 == 3:
                        tg = tt // 4
                        for vc in range(2):
                            pso, pbo = self.ps[4 + 2 * grp + vc], self.psb[4 + 2 * grp + vc]
                            kb.op("act", lambda e, vc=vc, pso=pso, tg=tg: e.activation(out=OT[:, vc, tg * 512:(tg + 1) * 512], in_=pso[:, :], func=AF.Copy),
                                  reads=[pbo], cw=[b_OT])
                if hd == 0:
                    self.dump("o_raw0", OT[:, 0, :], b_OT, [128, T])
                for tg in range(4):
                    sl = slice(tg * 512, (tg + 1) * 512)
                    for vc in range(2):
                        kb.op("act", lambda e, vc=vc, sl=sl: e.activation(out=SQ[vc][0][:, :], in_=OT[:, vc, sl], func=AF.Square), reads=[b_OT], writes=[SQ[vc][1]])
                    ps, pb = self.bank()
                    for vc in range(2):
                        kb.op("pe", lambda e, vc=vc, ps=ps: e.matmul(ps[:, :], ONES, SQ[vc][0][:, :], start=(vc == 0), stop=(vc == 1)),
                              reads=[bc, SQ[vc][1]], writes=[pb], inc=(vc == 1))
                    kb.op("act", lambda e, ps=ps, sl=sl: e.activation(out=RIf[:, sl], in_=ps[:, :], func=AF.Sqrt, scale=1.0 / 256, bias=1e-5), reads=[pb], cw=[b_RIf])
                kb.op("dve", lambda e: e.reciprocal(RIf[:, :], RIf[:, :]), reads=[b_RIf], writes=[b_RIf])

            def h3(hd, tg):
                if tg == 0:
                    HW[hd]['go'] = [self.load_w(32 + 2 * hd + j) for j in range(2)]
                    HW[hd]['mb'] = [self.load_w(48 + 2 * hd + j) for j in range(2)]
                w_go, w_mb = HW[hd]['go'], HW[hd]['mb']
                sl = slice(tg * 512, (tg + 1) * 512)
                for vc in range(2):
                    g = hd * 2 + vc
                    go_ps = {}
                    self.inproj_fm(w_go[vc][0], w_go[vc][1], 128, tg, lambda ps, pb: (go_ps.update(ps=ps, pb=pb), kb.op(
                        "act", lambda e: e.activation(out=SG[:, :], in_=ps[:, :], func=AF.Sigmoid), reads=[pb], writes=[b_SG])))
                    self.inproj_fm(w_mb[vc][0], w_mb[vc][1], 128, tg, lambda ps, pb: kb.op(
                        "act", lambda e: e.activation(out=SM[:, :], in_=ps[:, :], func=AF.Sigmoid), reads=[pb], writes=[b_SM]))
                    kb.op("dve", lambda e, vc=vc, sl=sl: e.scalar_tensor_tensor(TT[:, :], OT[:, vc, sl], ng[:, vc:vc + 1], RIf[:, sl], ALU.mult, ALU.mult),
                          reads=[b_OT, b_ng, b_RIf], writes=[b_TT])
                    kb.op("dve", lambda e: e.tensor_tensor(TT[:, :], TT[:, :], SG[:, :], ALU.mult), reads=[b_TT, b_SG], writes=[b_TT])
                    kb.op("dve", lambda e: e.tensor_tensor(TT[:, :], TT[:, :], go_ps["ps"][:, :], ALU.mult), reads=[b_TT, b_SG, go_ps["pb"]], writes=[b_TT])
                    kb.op("dve", lambda e: e.tensor_tensor(TT[:, :], TT[:, :], SM[:, :], ALU.mult), reads=[b_TT, b_SM], writes=[b_TT])
                    kb.op("dve", lambda e, g=g, sl=sl: e.tensor_tensor(self.yT[:, g, sl], TT[:, :], self.yT[:, g, sl], ALU.add),
                          reads=[b_TT, self.b_yT[g]], writes=[self.b_yT[g]])


            for hd in range(4):
                for tg in range(4):
                    h1(hd, tg)
                    if hd > 0:
                        h3(hd - 1, tg)
                h2(hd)
            for tg in range(4):
                h3(3, tg)

    def layer_norm(self, es_tmp, R, b_R, grow, brow, OUT, b_OUT, tag):
        kb = self.kb
        st = self.ln_st
        kb.op("dve", lambda e: e.bn_stats(st["stats"][:, 0, :], R[:, 0:512]), reads=[b_R], writes=[st["b"]])
        kb.op("dve", lambda e: e.bn_stats(st["stats"][:, 1, :], R[:, 512:1024]), reads=[b_R], writes=[st["b"]])
        kb.op("dve", lambda e: e.bn_aggr(st["mv"][:, :], st["stats"][:, :, :].rearrange("p a b -> p (a b)")), reads=[st["b"]], writes=[st["b"]])
        kb.op("act", lambda e: e.activation(out=st["rs"][:, 0:1], in_=st["mv"][:, 1:2], func=AF.Ln, bias=1e-5), reads=[st["b"]], writes=[st["b2"]])
        kb.op("act", lambda e: e.activation(out=st["rs"][:, 0:1], in_=st["rs"][:, 0:1], func=AF.Exp, scale=-0.5), reads=[st["b2"]], writes=[st["b2"]])
        kb.op("dve", lambda e: e.scalar_tensor_tensor(st["rs"][:, 1:2], st["mv"][:, 0:1], -1.0, st["rs"][:, 0:1], ALU.mult, ALU.mult),
              reads=[st["b"], st["b2"]], writes=[st["b2"]])
        kb.op("act", lambda e: e.activation(out=OUT[:, :], in_=R[:, :], func=AF.Identity, scale=st["rs"][:, 0:1], bias=st["rs"][:, 1:2]),
              reads=[b_R, st["b2"]], writes=[b_OUT])
        kb.op("dve", lambda e: e.tensor_tensor(OUT[:, :], OUT[:, :], self.rowp[:, grow, :], ALU.mult), reads=[b_OUT, self.b_rowp], writes=[b_OUT])
        kb.op("dve", lambda e: e.tensor_tensor(OUT[:, :], OUT[:, :], self.rowp[:, brow, :], ALU.add), reads=[b_OUT, self.b_rowp], writes=[b_OUT])

    def alloc_ln(self, es):
        g = self.ps_gen
        self.rowp = self.sb(es, "rowp_sb%d" % g, [128, 5, 1024])
        self.b_rowp = Buf("rowp")
        self.kb.dma("sp", lambda e: e.dma_start(out=self.rowp[:], in_=self.din["rowp"].rearrange("p (r d) -> p r d", d=1024)),
                    writes=[self.b_rowp])
        self.ln_st = {"stats": self.sb(es, "ln_stats%d" % g, [128, 2, 6]), "mv": self.sb(es, "ln_mv%d" % g, [128, 2]),
                      "rs": self.sb(es, "ln_rs%d" % g, [128, 2]), "b": Buf("ln_b"), "b2": Buf("ln_b2")}

    def phase_outproj(self):
        nc, kb = self.nc, self.kb
        cstb, bcb = self.cstb, self.b_cstb
        IDb, LTb, ONEb = cstb[:, 0:128], cstb[:, 384:512], cstb[:, 512:640]
        with ExitStack() as es:
            self.alloc_psum(es, 6, 2)
            self.alloc_ln(es)

            def mk(name, shape, dt=F32):
                return self.sb(es, "P2_" + name, shape, dt), Buf("P2_" + name)
            Wout, b_Wout = mk("Wout", [128, 8, 1024], BF16)
            kb.dma("pool", lambda e: e.dma_start(out=Wout[:, 0:4, :], in_=self.din["w_out"].rearrange("p (k n) -> p k n", n=1024)[:, 0:4, :]), cw=[b_Wout])
            kb.dma("pool", lambda e: e.dma_start(out=Wout[:, 4:8, :], in_=self.din["w_out"].rearrange("p (k n) -> p k n", n=1024)[:, 4:8, :]), cw=[b_Wout])
            wr32, b_wr32 = mk("wr32", [128, 8, 32])
            wrh, b_wrh = mk("wrh", [128, 8, 32], BF16)
            wrl, b_wrl = mk("wrl", [128, 8, 32], BF16)
            kb.dma("sp", lambda e: e.dma_start(out=wr32[:], in_=self.din["w_router"].rearrange("p (k n) -> p k n", n=32)), writes=[b_wr32])
            kb.op("dve", lambda e: e.tensor_copy(wrh[:], wr32[:]), reads=[b_wr32], writes=[b_wrh])
            kb.op("dve", lambda e: e.tensor_tensor(wrl[:], wr32[:], wrh[:], ALU.subtract), reads=[b_wr32, b_wrh], writes=[b_wrl])
            brt, b_brt = mk("brt", [128, 32])
            kb.dma("sp", lambda e: e.dma_start(out=brt[:], in_=self.din["b_router"][0:1, :].partition_broadcast(128)), writes=[b_brt])
            carry, b_carry = mk("carry", [128, 32])
            kb.op("dve", lambda e: e.memset(carry[:], 0.0), writes=[b_carry])
            Xt = [mk("x%d" % i, [128, 1024]) for i in range(2)]
            R, b_R = mk("R", [128, 1024])
            H1 = [mk("H1_%d" % i, [128, 1024]) for i in range(2)]
            H1b = [mk("H1b_%d" % i, [128, 1024], BF16) for i in range(2)]
            H1l = [mk("H1l_%d" % i, [128, 1024], BF16) for i in range(2)]
            HT = [mk("HT_%d" % i, [128, 8, 128], BF16) for i in range(2)]
            lg, b_lg = mk("lg", [128, 32])
            v8, b_v8 = mk("v8", [128, 8])
            i8, b_i8 = mk("i8", [128, 8], U32)
            i8f, b_i8f = mk("i8f", [128, 8])
            sm, b_sm = mk("sm", [128, 8])
            mask, b_mask = mk("mask", [128, 32], BF16)
            sc, b_sc = mk("sc", [128, 32])
            ov, b_ov = mk("ov", [128, 32])
            junk, b_junk = mk("junk", [128, 32])
            slf, b_slf = mk("slf", [128, 4])

            for tt in range(16):
                tsl = slice(tt * 128, (tt + 1) * 128)
                xt, b_xt = Xt[tt % 2]
                kb.dma("sp", lambda e: e.dma_start(out=xt[:], in_=self.din["x"][tsl, :]), writes=[b_xt])
                for half in range(2):
                    ps, pb = self.bank()
                    for kc in range(8):
                        kb.op("pe", lambda e, kc=kc, ps=ps, half=half: e.matmul(ps[:, :], self.yT[:, kc, tsl], Wout[:, kc, half * 512:(half + 1) * 512],
                                                                             start=(kc == 0), stop=(kc == 7)),
                              reads=[self.b_yT[kc], b_Wout], writes=[pb], inc=(kc == 7))
                    kb.op("dve", lambda e, ps=ps, half=half: e.scalar_tensor_tensor(R[:, half * 512:(half + 1) * 512], xt[:, half * 512:(half + 1) * 512], ALPHA,
                                                                                  ps[:, :], ALU.mult, ALU.add), reads=[b_xt, pb], cw=[b_R])
                h1, b_h1 = H1[tt % 2]
                self.layer_norm(es, R, b_R, 0, 1, h1, b_h1, "ln1")
                kb.dma("sp", lambda e: e.dma_start(out=self.H1d[tsl, :], in_=h1[:]), reads=[b_h1], cw=[self.b_H1d])
                if tt == 0:
                    self.dump("h1_0", h1[:], b_h1, [128, 1024])
                hb, b_hb = H1b[tt % 2]
                hl, b_hl = H1l[tt % 2]
                kb.op("act", lambda e: e.activation(out=hb[:, :], in_=h1[:, :], func=AF.Identity), reads=[b_h1], writes=[b_hb])
                kb.op("dve", lambda e: e.tensor_tensor(hl[:, :], h1[:, :], hb[:, :], ALU.subtract), reads=[b_h1, b_hb], writes=[b_hl])
                for (src, b_src, (dst, b_dst)) in ((hb, b_hb, HT[0]), (hl, b_hl, HT[1])):
                    pt, ptb = self.bank16()
                    for kc in range(8):
                        kb.op("pe", lambda e, kc=kc, pt=pt, src=src: e.transpose(pt[:, kc * 128:(kc + 1) * 128], src[:, kc * 128:(kc + 1) * 128], IDb),
                              reads=[b_src, bcb], writes=[ptb], inc=(kc == 7))
                    kb.op("dve", lambda e, pt=pt, dst=dst: e.tensor_copy(dst[:, :, :], pt[:, :].rearrange("p (k t) -> p k t", t=128)),
                          reads=[ptb], writes=[b_dst])
                ps, pb = self.bank()
                combos = [(HT[0], wrh, b_wrh), (HT[0], wrl, b_wrl), (HT[1], wrh, b_wrh)]
                n = 0
                for (ht, b_ht), w, b_w in combos:
                    for kc in range(8):
                        n += 1
                        kb.op("pe", lambda e, kc=kc, ps=ps, ht=ht, w=w, n=n: e.matmul(ps[:, 0:32], ht[:, kc, :], w[:, kc, :], start=(n == 1), stop=(n == 24)),
                              reads=[b_ht, b_w], writes=[pb], inc=(n == 24))
                kb.op("dve", lambda e, ps=ps: e.tensor_tensor(lg[:, :], ps[:, 0:32], brt[:, :], ALU.add), reads=[pb, b_brt], writes=[b_lg])
                if tt == 0:
                    self.dump("lg_0", lg[:], b_lg, [128, 32])
                kb.op("dve", lambda e: e.max(out=v8[:, :], in_=lg[:, :]), reads=[b_lg], writes=[b_v8])
                kb.op("dve", lambda e: e.max_index(out=i8[:, :], in_max=v8[:, :], in_values=lg[:, :]), reads=[b_lg, b_v8], writes=[b_i8])
                kb.op("dve", lambda e: e.tensor_copy(i8f[:, :], i8[:, :]), reads=[b_i8], writes=[b_i8f])
                kb.op("dve", lambda e: e.tensor_scalar_mul(sm[:, 0:1], v8[:, 0:1], -1.0), reads=[b_v8], writes=[b_sm])
                kb.op("act", lambda e: e.activation(out=sm[:, 4:8], in_=v8[:, 0:4], func=AF.Exp, bias=sm[:, 0:1], accum_out=sm[:, 1:2]),
                      reads=[b_v8, b_sm], writes=[b_sm])
                kb.op("dve", lambda e: e.reciprocal(sm[:, 2:3], sm[:, 1:2]), reads=[b_sm], writes=[b_sm])
                kb.op("dve", lambda e: e.tensor_scalar_mul(self.GATES[:, tt, :], sm[:, 4:8], sm[:, 2:3]), reads=[b_sm], writes=[self.b_route[tt]])
                kb.op("dve", lambda e: e.tensor_scalar(mask[:, :], lg[:, :], v8[:, 3:4], None, ALU.is_ge), reads=[b_lg, b_v8], writes=[b_mask])
                ps, pb = self.bank()
                kb.op("pe", lambda e, ps=ps: e.matmul(ps[:, 0:32], LTb, mask[:, :], start=True, stop=True), reads=[bcb, b_mask], writes=[pb], inc=False)
                kb.op("pe", lambda e, ps=ps: e.matmul(ps[:, 32:64], ONEb, mask[:, :], start=True, stop=True), reads=[bcb, b_mask], writes=[pb])
                kb.op("dve", lambda e, ps=ps: e.tensor_tensor(sc[:, :], ps[:, 0:32], carry[:, :], ALU.add), reads=[pb, b_carry], writes=[b_sc])
                kb.op("dve", lambda e, ps=ps: e.tensor_tensor(carry[:, :], ps[:, 32:64], carry[:, :], ALU.add), reads=[pb, b_carry, b_sc], writes=[b_carry])
                kb.op("dve", lambda e: e.tensor_scalar(ov[:, :], sc[:, :], float(CAP), float(4 * NSLOT), ALU.is_ge, ALU.mult), reads=[b_sc], writes=[b_ov])
                kb.op("dve", lambda e: e.tensor_tensor(sc[:, :], sc[:, :], self.cst[:, 672:704], ALU.add), reads=[b_sc, self.b_cst], writes=[b_sc])
                kb.op("dve", lambda e: e.tensor_tensor(sc[:, :], sc[:, :], ov[:, :], ALU.add), reads=[b_sc, b_ov], writes=[b_sc])
                for k in range(4):
                    kb.op("dve", lambda e, k=k: e.scalar_tensor_tensor(junk[:, :], self.cst[:, 640:672], i8f[:, k:k + 1], sc[:, :], ALU.is_equal, ALU.mult,
                                                                      accum_out=slf[:, k:k + 1]), reads=[self.b_cst, b_i8f, b_sc], writes=[b_junk, b_slf])
                kb.op("dve", lambda e: e.tensor_copy(self.SLOTS[:, tt, :], slf[:, :]), reads=[b_slf], writes=[self.b_route[tt]])
                for k in range(4):
                    kb.dma("pool", lambda e, k=k: e.indirect_dma_start(
                        out=self.Xg, out_offset=bass.IndirectOffsetOnAxis(ap=self.SLOTS[:, tt, k:k + 1], axis=0),
                        in_=hb[:, :], in_offset=None, bounds_check=self.bound_reg, oob_is_err=False),
                        reads=[b_hb, self.b_route[tt], self.b_Xgz], cw=[self.b_Xg])
            self.dump("gates", self.GATES[:].rearrange("p a b -> p (a b)"), self.b_route[15], [128, 64])
            if "slots" in self.dbg:
                sf, b_sf = mk("slots_f", [128, 64])
                kb.op("dve", lambda e: e.tensor_copy(sf[:, :], self.SLOTS[:].rearrange("p a b -> p (a b)")), reads=self.b_route, writes=[b_sf])
                self.dump("slots", sf[:], b_sf, [128, 64])

    def phase_moe(self):
        nc, kb = self.nc, self.kb
        IDb, bcb = self.cstb[:, 0:128], self.b_cstb
        NST = CAP // 128
        with ExitStack() as es:
            self.alloc_psum(es, 6, 2)

            def mk(name, shape, dt=F32):
                return self.sb(es, "M_" + name, shape, dt), Buf("M_" + name)
            WU = [mk("wu%d" % i, [128, 8, 2048], BF16) for i in range(2)]
            WD = [mk("wd%d" % i, [128, 8, 1024], BF16) for i in range(2)]
            BD = [mk("bd%d" % i, [128, 1024]) for i in range(2)]
            XG = [mk("xg%d" % i, [128, NST, 1024], BF16) for i in range(2)]
            XGT = [mk("xgt%d" % i, [128, 8, CAP], BF16) for i in range(2)]
            ACTT, b_ACTT = mk("actt", [128, 8, CAP], BF16)
            Gt = [mk("g%d" % i, [128, CAP]) for i in range(2)]
            St = [mk("s%d" % i, [128, CAP]) for i in range(2)]
            Ut = [mk("u%d" % i, [128, CAP]) for i in range(2)]
            Ysb = [mk("y%d" % i, [128, 1024]) for i in range(2)]
            bup, b_bup = mk("bup", [128, 32, 16])
            kb.dma("sp", lambda e: e.dma_start(out=bup[:], in_=self.din["b_up"].rearrange("p (e f) -> p e f", f=16)), writes=[b_bup])

            def load(ex):
                sl = ex % 2
                wu = self.din["w_up"][ex].rearrange("(kc p) f -> p kc f", p=128)
                wd = self.din["w_down"][ex].rearrange("(kc p) f -> p kc f", p=128)
                kb.dma("sp", lambda e: e.dma_start(out=XG[sl][0][:], in_=self.Xg[ex * CAP:(ex + 1) * CAP, :].rearrange("(st p) d -> p st d", p=128)),
                       reads=[self.b_Xg], writes=[XG[sl][1]])
                kb.dma("sp", lambda e: e.dma_start(out=BD[sl][0][:], in_=self.din["b_down"][ex:ex + 1, :].partition_broadcast(128)), writes=[BD[sl][1]])
                for q in range(4):
                    kb.dma("pool", lambda e, q=q: e.dma_start(out=WU[sl][0][:, 2 * q:2 * q + 2, :], in_=wu[:, 2 * q:2 * q + 2, :]), cw=[WU[sl][1]])
                for q in range(2):
                    kb.dma("pool", lambda e, q=q: e.dma_start(out=WD[sl][0][:, 4 * q:4 * q + 4, :], in_=wd[:, 4 * q:4 * q + 4, :]), cw=[WD[sl][1]])

            def compute(ex):
                sl = ex % 2
                wu, b_wu = WU[sl]
                wd, b_wd = WD[sl]
                xg, b_xg = XG[sl]
                xgt, b_xgt = XGT[sl]
                bd, b_bd = BD[sl]
                for st in range(NST):
                    pt, ptb = self.bank16()
                    for kc in range(8):
                        kb.op("pe", lambda e, kc=kc, pt=pt, st=st: e.transpose(pt[:, kc * 128:(kc + 1) * 128], xg[:, st, kc * 128:(kc + 1) * 128], IDb),
                              reads=[b_xg, bcb], writes=[ptb], inc=(kc == 7))
                    kb.op("dve", lambda e, pt=pt, st=st: e.tensor_copy(xgt[:, :, st * 128:(st + 1) * 128], pt[:, :].rearrange("p (k s) -> p k s", s=128)),
                          reads=[ptb], cw=[b_xgt])
                for c in range(8):
                    g, b_g = Gt[c % 2]
                    s_, b_s = St[c % 2]
                    u, b_u = Ut[c % 2]
                    ps_g, pb_g = self.bank()
                    for kc in range(8):
                        kb.op("pe", lambda e, kc=kc, ps_g=ps_g, c=c: e.matmul(ps_g[:, 0:CAP], wu[:, kc, c * 128:(c + 1) * 128], xgt[:, kc, :], start=(kc == 0), stop=(kc == 7)),
                              reads=[b_wu, b_xgt], writes=[pb_g], inc=(kc == 7))
                    ps_u, pb_u = self.bank()
                    for kc in range(8):
                        kb.op("pe", lambda e, kc=kc, ps_u=ps_u, c=c: e.matmul(ps_u[:, 0:CAP], wu[:, kc, 1024 + c * 128:1024 + (c + 1) * 128], xgt[:, kc, :],
                                                                           start=(kc == 0), stop=(kc == 7)),
                              reads=[b_wu, b_xgt], writes=[pb_u], inc=(kc == 7))
                    kb.op("dve", lambda e, ps_g=ps_g, c=c: e.tensor_scalar(g[:, :], ps_g[:, 0:CAP], bup[:, ex, c:c + 1], 7.0, ALU.add, ALU.min),
                          reads=[pb_g, b_bup], writes=[b_g])
                    kb.op("act", lambda e: e.activation(out=s_[:, :], in_=g[:, :], func=AF.Sigmoid, scale=1.702), reads=[b_g], writes=[b_s])
                    kb.op("dve", lambda e, ps_u=ps_u, c=c: e.tensor_scalar(u[:, :], ps_u[:, 0:CAP], bup[:, ex, 8 + c:9 + c], 7.0, ALU.add, ALU.min),
                          reads=[pb_u, b_bup], writes=[b_u])
                    kb.op("dve", lambda e: e.tensor_scalar(u[:, :], u[:, :], -7.0, 1.0, ALU.max, ALU.add), reads=[b_u], writes=[b_u])
                    kb.op("dve", lambda e: e.tensor_tensor(g[:, :], g[:, :], s_[:, :], ALU.mult), reads=[b_g, b_s], writes=[b_g])
                    kb.op("dve", lambda e, c=c: e.tensor_tensor(ACTT[:, c, :], g[:, :], u[:, :], ALU.mult), reads=[b_g, b_u], cw=[b_ACTT])
                for st in range(NST):
                    y, b_y = Ysb[st % 2]
                    for half in range(2):
                        ps, pb = self.bank()
                        for fc in range(8):
                            kb.op("pe", lambda e, fc=fc, ps=ps, half=half, st=st: e.matmul(ps[:, :], ACTT[:, fc, st * 128:(st + 1) * 128],
                                                                                        wd[:, fc, half * 512:(half + 1) * 512], start=(fc == 0), stop=(fc == 7)),
                                  reads=[b_ACTT, b_wd], writes=[pb], inc=(fc == 7))
                        kb.op("dve", lambda e, ps=ps, half=half: e.tensor_tensor(y[:, half * 512:(half + 1) * 512], ps[:, :], bd[:, half * 512:(half + 1) * 512], ALU.add),
                              reads=[pb, b_bd], cw=[b_y])
                    r0 = ex * CAP + st * 128
                    kb.dma("sp", lambda e, r0=r0: e.dma_start(out=self.Yg[r0:r0 + 128, :], in_=y[:]), reads=[b_y], cw=[self.b_Yg])

            ne = getattr(self, "n_experts", NE)
            load(0)
            if ne > 1:
                load(1)
            for ex in range(ne):
                compute(ex)
                if ex + 2 < ne:
                    load(ex + 2)

    def phase_final(self):
        nc, kb = self.nc, self.kb
        IDb, bcb = self.cstb[:, 0:128], self.b_cstb
        with ExitStack() as es:
            self.alloc_psum(es, 6, 2)
            self.alloc_ln(es)

            def mk(name, shape, dt=F32):
                return self.sb(es, "F_" + name, shape, dt), Buf("F_" + name)
            Wpg, b_Wpg = mk("Wpg", [128, 8, 1024], BF16)
            for q in range(2):
                kb.dma("pool", lambda e, q=q: e.dma_start(out=Wpg[:, 4 * q:4 * q + 4, :], in_=self.din["w_pg"].rearrange("p (k n) -> p k n", n=1024)[:, 4 * q:4 * q + 4, :]),
                       cw=[b_Wpg])
            Wple, b_Wple = mk("Wple", [128, 2, 1024], BF16)
            kb.dma("pool", lambda e: e.dma_start(out=Wple[:], in_=self.din["w_ple"].rearrange("p (k n) -> p k n", n=1024)), writes=[b_Wple])
            PT, b_PT = mk("PT", [128, 2, T], BF16)
            pT = self.din["pT"].rearrange("(kc p) t -> p kc t", p=128)
            for kc in range(2):
                kb.dma("pool", lambda e, kc=kc: e.dma_start(out=PT[:, kc, :], in_=pT[:, kc, :]), cw=[b_PT])
            YG = [mk("yg%d" % i, [128, 4, 1024]) for i in range(3)]
            H1t = [mk("h1_%d" % i, [128, 1024]) for i in range(3)]
            ACC, b_ACC = mk("acc", [128, 1024])
            H2T, b_H2T = mk("h2T", [128, 8, 128], BF16)
            SGT, b_SGT = mk("sgt", [128, 1024])
            OUT = [mk("out%d" % i, [128, 1024]) for i in range(2)]
            def prefetch(tt):
                tsl = slice(tt * 128, (tt + 1) * 128)
                yg, b_yg = YG[tt % 3]
                h1, b_h1 = H1t[tt % 3]
                kb.op("pool", lambda e: e.memset(yg[:], 0.0), writes=[b_yg])
                for k in range(4):
                    kb.dma("pool", lambda e, k=k: e.indirect_dma_start(
                        out=yg[:, k, :], out_offset=None, in_=self.Yg,
                        in_offset=bass.IndirectOffsetOnAxis(ap=self.SLOTS[:, tt, k:k + 1], axis=0),
                        bounds_check=self.bound_reg, oob_is_err=False), reads=[self.b_Yg, self.b_route[tt]], cw=[b_yg])
                kb.dma("sp", lambda e: e.dma_start(out=h1[:], in_=self.H1d[tsl, :]), reads=[self.b_H1d], writes=[b_h1])

            H2s = [mk("h2_%d" % i, [128, 1024]) for i in range(2)]
            H2bs = [mk("h2b_%d" % i, [128, 1024], BF16) for i in range(2)]

            def stage_a(tt):
                yg, b_yg = YG[tt % 3]
                h1, b_h1 = H1t[tt % 3]
                H2, b_H2 = H2s[tt % 2]
                H2b, b_H2b = H2bs[tt % 2]
                kb.op("act", lambda e: e.activation(out=ACC[:, :], in_=h1[:, :], func=AF.Identity, scale=ALPHA), reads=[b_h1], writes=[b_ACC])
                for k in range(4):
                    kb.op("dve", lambda e, k=k: e.scalar_tensor_tensor(ACC[:, :], yg[:, k, :], self.GATES[:, tt, k:k + 1], ACC[:, :], ALU.mult, ALU.add),
                          reads=[b_yg, self.b_route[tt], b_ACC], writes=[b_ACC])
                self.layer_norm(es, ACC, b_ACC, 2, 3, H2, b_H2, "ln2")
                if tt == 0:
                    self.dump("h2_0", H2[:], b_H2, [128, 1024])
                kb.op("act", lambda e: e.activation(out=H2b[:, :], in_=H2[:, :], func=AF.Identity), reads=[b_H2], writes=[b_H2b])

            def stage_b(tt):
                tsl = slice(tt * 128, (tt + 1) * 128)
                H2, b_H2 = H2s[tt % 2]
                H2b, b_H2b = H2bs[tt % 2]
                pt, ptb = self.bank16()
                for kc in range(8):
                    kb.op("pe", lambda e, kc=kc, pt=pt: e.transpose(pt[:, kc * 128:(kc + 1) * 128], H2b[:, kc * 128:(kc + 1) * 128], IDb),
                          reads=[b_H2b, bcb], writes=[ptb], inc=(kc == 7))
                kb.op("dve", lambda e, pt=pt: e.tensor_copy(H2T[:, :, :], pt[:, :].rearrange("p (k t) -> p k t", t=128)), reads=[ptb], writes=[b_H2T])
                o, b_o = OUT[tt % 2]
                for half in range(2):
                    hs = slice(half * 512, (half + 1) * 512)
                    ps, pb = self.bank()
                    for kc in range(8):
                        kb.op("pe", lambda e, kc=kc, ps=ps, hs=hs: e.matmul(ps[:, :], H2T[:, kc, :], Wpg[:, kc, hs], start=(kc == 0), stop=(kc == 7)),
                              reads=[b_H2T, b_Wpg], writes=[pb], inc=(kc == 7))
                    kb.op("dve", lambda e, ps=ps, hs=hs: e.tensor_tensor(SGT[:, hs], ps[:, :], self.rowp[:, 4, hs], ALU.add), reads=[pb, self.b_rowp], cw=[b_SGT])
                    kb.op("act", lambda e, hs=hs: e.activation(out=SGT[:, hs], in_=SGT[:, hs], func=AF.Sigmoid), reads=[b_SGT], cw=[b_SGT])
                    ps2, pb2 = self.bank()
                    for kc in range(2):
                        kb.op("pe", lambda e, kc=kc, ps2=ps2, hs=hs: e.matmul(ps2[:, :], PT[:, kc, tsl], Wple[:, kc, hs], start=(kc == 0), stop=(kc == 1)),
                              reads=[b_PT, b_Wple], writes=[pb2], inc=(kc == 1))
                    kb.op("dve", lambda e, ps2=ps2, hs=hs: e.tensor_tensor(o[:, hs], SGT[:, hs], ps2[:, :], ALU.mult), reads=[b_SGT, pb2], cw=[b_o])
                    kb.op("dve", lambda e, hs=hs: e.tensor_tensor(o[:, hs], o[:, hs], H2[:, hs], ALU.add), reads=[b_o, b_H2], cw=[b_o])
                kb.dma("sp", lambda e: e.dma_start(out=self.out[tsl, :], in_=o[:]), reads=[b_o])

            prefetch(0)
            prefetch(1)
            stage_a(0)
            for tt in range(16):
                if tt + 2 < 16:
                    prefetch(tt + 2)
                if tt + 1 < 16:
                    stage_a(tt + 1)
                stage_b(tt)


_PROG_CACHE = {}


def kernel(**inputs):
    inp = {k: np.asarray(v) for k, v in inputs.items()}
    sh = prep_shared(inp)
    in_maps = [dict(sh, **prep_core(inp, b)) for b in range(8)]
    if "nc" not in _PROG_CACHE:
        _PROG_CACHE["nc"] = Prog().build()
    nc = _PROG_CACHE["nc"]
    res = run_bass_kernel_spmd(nc, in_maps, core_ids=list(range(8)))
    out = np.stack([np.asarray(r["out"], dtype=np.float32) for r in res.results], axis=0)
    return out
```

```python
import numpy as np
from contextlib import ExitStack
import concourse.bass as bass
import concourse.mybir as mybir
from concourse.bass_utils import run_bass_kernel_spmd

F32 = mybir.dt.float32
BF16 = mybir.dt.bfloat16
U32 = mybir.dt.uint32
AF = mybir.ActivationFunctionType
ALU = mybir.AluOpType
AX = mybir.AxisListType

T = 2048
D = 1024
NE = 32
CAP = 384
NSLOT = NE * CAP
ALPHA = 2.0 ** 0.25
N_IN = 7184
O_XA, O_GA, O_Q, O_K, O_V, O_GO, O_GLR, O_MA, O_MB = 0, 1024, 2048, 2560, 3072, 4096, 5120, 5136, 6160


class Buf:
    __slots__ = ("name", "w", "r", "c")

    def __init__(self, name):
        self.name = name
        self.w = {}
        self.r = {}
        self.c = {}


class KB:
    def __init__(self, nc, es, n_dma_sems=24):
        self.nc = nc
        self.eng = dict(pe=nc.tensor, act=nc.scalar, dve=nc.vector, pool=nc.gpsimd, sp=nc.sync)
        self.esem = {}
        self.ecnt = {}
        self.seen = {}
        self.semobj = {}
        for n in self.eng:
            s = es.enter_context(nc.semaphore("es_" + n))
            self.esem[n] = s
            self.semobj[id(s)] = s
            self.ecnt[n] = 0
            self.seen[n] = {}
        self.dsem = []
        self.dcnt = []
        for i in range(2 * n_dma_sems):
            s = es.enter_context(nc.semaphore("ds_%d" % i))
            self.dsem.append(s)
            self.semobj[id(s)] = s
            self.dcnt.append(0)
        self.nds = n_dma_sems
        self.drr = {"pool": 0, "hw": 0}

    def _wait(self, en, toks):
        e = self.eng[en]
        seen = self.seen[en]
        for sid, val in toks.items():
            if en == "pe" and sid == id(self.esem["pe"]):
                continue
            if seen.get(sid, 0) >= val:
                continue
            e.wait_ge(self.semobj[sid], val)
            seen[sid] = val

    @staticmethod
    def _merge(dst, src):
        for k, v in src.items():
            if dst.get(k, 0) < v:
                dst[k] = v

    def _deps(self, reads, writes, cw=()):
        toks = {}
        for b in reads:
            self._merge(toks, b.w)
            self._merge(toks, b.c)
        for b in writes:
            self._merge(toks, b.w)
            self._merge(toks, b.c)
            self._merge(toks, b.r)
        for b in cw:
            self._merge(toks, b.w)
            self._merge(toks, b.r)
        return toks

    def _commit(self, tok, reads, writes, cw=()):
        for b in reads:
            self._merge(b.r, tok)
        for b in writes:
            b.w = dict(tok)
            b.r = {}
            b.c = {}
        for b in cw:
            self._merge(b.c, tok)

    disabled = False

    def op(self, en, fn, reads=(), writes=(), inc=True, cw=()):
        if self.disabled:
            return None
        self._wait(en, self._deps(reads, writes, cw))
        ins = fn(self.eng[en])
        s = self.esem[en]
        if inc:
            self.ecnt[en] += 1
            ins.then_inc(s, 1)
            tok = {id(s): self.ecnt[en]}
        else:
            tok = {id(s): self.ecnt[en] + 1}
        self._commit(tok, reads, writes, cw)
        return ins

    def dma(self, en, fn, reads=(), writes=(), cw=()):
        if self.disabled:
            return None
        kind = "pool" if en == "pool" else "hw"
        i = self.drr[kind] + (self.nds if kind == "pool" else 0)
        self.drr[kind] = (self.drr[kind] + 1) % self.nds
        s = self.dsem[i]
        toks = self._deps(reads, writes, cw)
        if self.dcnt[i] > 0:
            self._merge(toks, {id(s): self.dcnt[i]})
        self._wait(en, toks)
        ins = fn(self.eng[en])
        self.dcnt[i] += 16
        ins.then_inc(s, 16)
        tok = {id(s): self.dcnt[i]}
        self._commit(tok, reads, writes, cw)
        return ins

    def all_tokens(self):
        toks = {}
        for n in self.eng:
            if self.ecnt[n] > 0:
                toks[id(self.esem[n])] = self.ecnt[n]
        for i, s in enumerate(self.dsem):
            if self.dcnt[i] > 0:
                toks[id(s)] = self.dcnt[i]
        return toks

    def barrier(self, engines=None):
        toks = self.all_tokens()
        for n in (engines or list(self.eng)):
            own = id(self.esem[n])
            t = {k: v for k, v in toks.items() if not (n == "pe" and k == own)}
            self._wait(n, t)


def _consts():
    c = np.zeros((128, 5 * 128 + 64 + 4), np.float32)
    j = np.arange(128)
    same = (j[:, None] // 64) == (j[None, :] // 64)
    c[:, 0:128] = np.eye(128, dtype=np.float32)
    c[:, 128:256] = (same & (j[:, None] <= j[None, :]))
    c[:, 256:384] = (same & (j[:, None] > j[None, :]))
    c[:, 384:512] = (j[:, None] < j[None, :])
    c[:, 512:640] = 1.0
    c[:, 640:672] = np.arange(32)[None, :]
    c[:, 672:704] = (np.arange(32) * CAP)[None, :]
    c[:, 704] = (j < 64)
    c[:, 705] = (j >= 64)
    return c


def prep_shared(inp):
    f = lambda a: np.ascontiguousarray(a, dtype=np.float32)
    w_in = inp["w_in"][0]
    cols = np.concatenate([np.arange(0, O_GLR), np.arange(O_MA, N_IN)])
    wm = w_in[:, cols]
    sh = {}
    sh["w_in_t"] = f(wm.reshape(8, 128, 56, 128).transpose(2, 1, 0, 3).reshape(56, 128, 1024))
    sh["w_glr"] = f(w_in[:, O_GLR:O_GLR + 16].reshape(8, 128, 16).transpose(1, 0, 2).reshape(128, 128))
    chan = np.concatenate([inp["conv_w"][0], inp["conv_b"], inp["lru_b_r"], inp["lru_b_i"],
                           inp["lru_lambda"]], axis=0)
    sh["chanp"] = f(chan.reshape(8, 8, 128).transpose(2, 1, 0).reshape(128, 64))
    sh["lru_wr"] = f(inp["lru_w_r"][0].transpose(1, 0, 2).reshape(128, 1024))
    sh["lru_wi"] = f(inp["lru_w_i"][0].transpose(1, 0, 2).reshape(128, 1024))
    sh["gla_wg"] = f(inp["gla_w_gate"][0])
    sh["gla_bg"] = f(inp["gla_b_gate"])
    sh["gla_ng"] = f(inp["gla_norm_g"][0].reshape(2, 128).T)
    sh["w_out"] = f(inp["w_out"][0].reshape(8, 128, 1024).transpose(1, 0, 2).reshape(128, 8192))
    rows = np.concatenate([inp["ln1_g"], inp["ln1_b"], inp["ln2_g"], inp["ln2_b"],
                           inp["b_ple_gate"]], axis=0)
    sh["rowp"] = f(np.broadcast_to(rows.reshape(1, 5 * 1024), (128, 5 * 1024)))
    sh["w_router"] = f(inp["w_router"][0].reshape(8, 128, 32).transpose(1, 0, 2).reshape(128, 256))
    sh["b_router"] = f(inp["b_router"])
    sh["w_up"] = inp["w_up"][0]
    sh["b_up"] = f(inp["b_up"][0].reshape(32, 16, 128).transpose(2, 0, 1).reshape(128, 512))
    sh["w_down"] = inp["w_down"][0]
    sh["b_down"] = f(inp["b_down"][0])
    sh["w_ple"] = f(inp["w_ple"][0].reshape(2, 128, 1024).transpose(1, 0, 2).reshape(128, 2048))
    sh["w_pg"] = f(inp["w_ple_gate"][0].reshape(8, 128, 1024).transpose(1, 0, 2).reshape(128, 8192))
    sh["consts"] = _consts()
    return sh


def prep_core(inp, b):
    x = np.asarray(inp["x"][b], dtype=np.float32)
    p = np.asarray(inp["p"][0, b], dtype=np.float32)
    return {"x": np.ascontiguousarray(x), "xT": np.ascontiguousarray(x.T),
            "pT": np.ascontiguousarray(p.T)}


SHARED_SHAPES = {
    "w_in_t": [56, 128, 1024], "w_glr": [128, 128], "chanp": [128, 64], "lru_wr": [128, 1024],
    "lru_wi": [128, 1024], "gla_wg": [16, 512], "gla_bg": [1, 512], "gla_ng": [128, 2],
    "w_out": [128, 8192], "rowp": [128, 5120], "w_router": [128, 256], "b_router": [1, 32],
    "w_up": [32, 1024, 2048], "b_up": [128, 512], "w_down": [32, 1024, 1024], "b_down": [32, 1024],
    "w_ple": [128, 2048], "w_pg": [128, 8192], "consts": [128, 708],
}
CORE_SHAPES = {"x": [T, D], "xT": [D, T], "pT": [256, T]}


class StopBuild(Exception):
    pass


class Prog:
    ck_n = 0
    ck_stop = None

    def ck(self, label=""):
        self.ck_n += 1
        if self.ck_stop is not None and self.ck_n >= self.ck_stop:
            if not self.kb.disabled:
                print("STOP at checkpoint", self.ck_n, label)
            self.kb.disabled = True

    def __init__(self, dbg=(), stop_after=None):
        self.dbg = set(dbg)
        self.stop_after = stop_after
        self.nc = nc = bass.Bass("TRN2", target_bir_lowering=False)
        self.din = {}
        for n, s in list(SHARED_SHAPES.items()) + list(CORE_SHAPES.items()):
            self.din[n] = nc.dram_tensor(n, s, F32, kind="ExternalInput").ap()
        self.out = nc.dram_tensor("out", [T, D], F32, kind="ExternalOutput").ap()
        self.dbg_out = {}
        self.es = ExitStack()

    def dbg_tensor(self, name, shape):
        t = self.nc.dram_tensor("dbg_" + name, shape, F32, kind="ExternalOutput").ap()
        self.dbg_out[name] = t
        return t

    def sb(self, es, name, shape, dt=F32):
        return es.enter_context(self.nc.sbuf_tensor(name, shape, dt))

    def build(self):
        nc = self.nc
        with self.es as es:
            kb = self.kb = KB(nc, es)
            self.bound_reg = nc.gpsimd.to_reg(NSLOT - 1)
            self.cst = self.sb(es, "cst", [128, 708])
            self.b_cst = Buf("cst")
            kb.dma("sp", lambda e: e.dma_start(out=self.cst[:], in_=self.din["consts"][:, :]),
                   writes=[self.b_cst])
            self.cstb = self.sb(es, "cstb", [128, 708], BF16)
            self.b_cstb = Buf("cstb")
            kb.op("dve", lambda e: e.tensor_copy(self.cstb[:], self.cst[:]),
                  reads=[self.b_cst], writes=[self.b_cstb])
            self.GATES = self.sb(es, "GATES", [128, 16, 4])
            self.SLOTS = self.sb(es, "SLOTS", [128, 16, 4], U32)
            self.b_route = [Buf("route%d" % i) for i in range(16)]
            self.Xg = nc.dram_tensor("Xg", [NSLOT, D], BF16).ap()
            self.Yg = nc.dram_tensor("Yg", [NSLOT, D], F32).ap()
            self.H1d = nc.dram_tensor("H1d", [T, D], F32).ap()
            self.b_Xg, self.b_Yg, self.b_H1d = Buf("Xg"), Buf("Yg"), Buf("H1d")
            self.b_Xgz = Buf("Xgz")
            self.zero_xg(es)
            with ExitStack() as es_y:
                self.yT = self.sb(es_y, "yT", [128, 8, T], BF16)
                self.b_yT = [Buf("yT%d" % g) for g in range(8)]
                with ExitStack() as es1:
                    self.alloc_psum(es1, 8, 0)
                    self.XT = self.sb(es1, "XT", [128, 8, T], BF16)
                    self.b_XT = Buf("XT")
                    xT = self.din["xT"].rearrange("(kc p) t -> p kc t", p=128)
                    for kc in range(8):
                        kb.dma("pool", lambda e, kc=kc: e.dma_start(out=self.XT[:, kc, :], in_=xT[:, kc, :]),
                               cw=[self.b_XT])
                    if not getattr(self, "skip_lru", False):
                        self.phase_lru(es1)
                    else:
                        kb.op("dve", lambda e: e.memset(self.yT[:], 0.0), writes=self.b_yT)
                    if self.stop_after == "lru":
                        return self.finish()
                    kb.barrier()
                    self.phase_gla(es1)
                    if self.stop_after == "gla":
                        return self.dump_yT()
                kb.barrier()
                self.phase_outproj()
                if self.stop_after == "outproj":
                    return self.finish()
            kb.barrier()
            self.phase_moe()
            if self.stop_after == "moe":
                return self.finish()
            kb.barrier()
            self.phase_final()
        return self.finish()

    def alloc_psum(self, es, n32, n16):
        nc = self.nc
        self.ps = [es.enter_context(nc.psum_tensor("ps%d_%d" % (i, self.ps_gen), [128, 512], F32)) for i in range(n32)]
        self.psb = [Buf("ps%d" % i) for i in range(n32)]
        self.pst = [es.enter_context(nc.psum_tensor("pst%d_%d" % (i, self.ps_gen), [128, 1024], BF16)) for i in range(n16)]
        self.pstb = [Buf("pst%d" % i) for i in range(n16)]
        self.ps_rr = 0
        self.pst_rr = 0
        self.ps_gen += 1

    def zero_xg(self, es):
        kb = self.kb
        z = self.sb(es, "zeros", [128, 2, 1024], BF16)
        bz = Buf("zeros")
        kb.op("dve", lambda e: e.memset(z[:], 0.0), writes=[bz])
        xg = self.Xg.rearrange("(n p) d -> p n d", p=128)
        for i in range(NSLOT // 128 // 2):
            kb.dma("sp", lambda e, i=i: e.dma_start(out=xg[:, i * 2:(i + 1) * 2, :], in_=z[:]), reads=[bz], cw=[self.b_Xgz])

    ps_gen = 0

    def bank(self):
        i = self.ps_rr
        self.ps_rr = (self.ps_rr + 1) % len(self.ps)
        return self.ps[i], self.psb[i]

    def bank16(self):
        i = self.pst_rr
        self.pst_rr = (self.pst_rr + 1) % len(self.pst)
        return self.pst[i], self.pstb[i]

    def finish(self):
        kb = self.kb
        kb.disabled = False
        kb.barrier(["sp"])
        return self.nc

    def dump_yT(self):
        kb = self.kb
        kb.barrier()
        with ExitStack() as es:
            tmp = self.sb(es, "dump_tmp", [128, 8, T])
            b = Buf("dump_tmp")
            kb.op("dve", lambda e: e.tensor_copy(tmp[:], self.yT[:]), reads=self.b_yT, writes=[b])
            t = self.dbg_tensor("yT", [128, 8, T])
            kb.dma("sp", lambda e: e.dma_start(out=t, in_=tmp[:]), reads=[b])
            return self.finish()

    def dump(self, name, sb_ap, buf, shape):
        if name not in self.dbg:
            return
        t = self.dbg_tensor(name, shape)
        self.kb.dma("sp", lambda e: e.dma_start(out=t, in_=sb_ap), reads=[buf])

    def inproj_fm(self, wt, wb, ncols, tg, evac):
        kb = self.kb
        ps, pb = self.bank()
        for kc in range(8):
            kb.op("pe", lambda e, kc=kc: e.matmul(ps[0:ncols, :], wt[:, kc * ncols:(kc + 1) * ncols],
                                                   self.XT[:, kc, tg * 512:(tg + 1) * 512],
                                                   start=(kc == 0), stop=(kc == 7)),
                  reads=[wb, self.b_XT], writes=[pb], inc=(kc == 7))
        evac(ps, pb)

    def load_w(self, grp):
        i = self.w_rr
        self.w_rr = (self.w_rr + 1) % len(self.wring)
        wt, wb = self.wring[i], self.wringb[i]
        self.kb.dma("pool", lambda e: e.dma_start(out=wt[:], in_=self.din["w_in_t"][grp, :, :]), writes=[wb])
        return wt, wb

    def phase_lru(self, es1):
        nc, kb = self.nc, self.kb
        with ExitStack() as es:
            NW = 6
            self.wring = [self.sb(es, "wr%d" % i, [128, 1024], BF16) for i in range(NW)]
            self.wringb = [Buf("wr%d" % i) for i in range(NW)]
            self.w_rr = 0
            chan = self.sb(es, "chan", [128, 8, 8])
            b_chan = Buf("chan")
            kb.dma("sp", lambda e: e.dma_start(out=chan[:], in_=self.din["chanp"].rearrange("p (g k) -> p g k", k=8)),
                   writes=[b_chan])
            wr = self.sb(es, "lwr", [128, 8, 128], BF16)
            wi = self.sb(es, "lwi", [128, 8, 128], BF16)
            b_wr, b_wi = Buf("lwr"), Buf("lwi")
            kb.dma("pool", lambda e: e.dma_start(out=wr[:], in_=self.din["lru_wr"].rearrange("p (g d) -> p g d", d=128)), writes=[b_wr])
            kb.dma("pool", lambda e: e.dma_start(out=wi[:], in_=self.din["lru_wi"].rearrange("p (g d) -> p g d", d=128)), writes=[b_wi])
            sc = self.sb(es, "lsc", [128, 8, 4])
            b_sc = Buf("lsc")
            kb.op("act", lambda e: e.activation(out=sc[:, :, 0], in_=chan[:, :, 7], func=AF.Exp, scale=-1.0),
                  reads=[b_chan], writes=[b_sc])
            kb.op("act", lambda e: e.activation(out=sc[:, :, 1], in_=sc[:, :, 0], func=AF.Ln, bias=1.0),
                  reads=[b_sc], writes=[b_sc])
            kb.op("dve", lambda e: e.tensor_scalar_mul(sc[:, :, 2], sc[:, :, 1], -8.0), reads=[b_sc], writes=[b_sc])
            kb.op("dve", lambda e: e.tensor_scalar_mul(sc[:, :, 3], sc[:, :, 1], -16.0), reads=[b_sc], writes=[b_sc])

            def mk(name, shape, dt=F32):
                return self.sb(es, "L_" + name, shape, dt), [Buf("L_%s_%d" % (name, i)) for i in range(4)]
            xa, b_xa = mk("xa", [128, T + 3], BF16)
            DG = self.sb(es, "L_dg", [128, 8, 4, 128], BF16)
            b_DG = Buf("L_dg")
            for g_ in range(8):
                for k_ in range(4):
                    kb.op("dve", lambda e, g_=g_, k_=k_: e.tensor_scalar_mul(DG[:, g_, k_, :], self.cstb[:, 0:128], chan[:, g_, k_:k_ + 1]),
                          reads=[self.b_cstb, b_chan], cw=[b_DG])
            xcb, b_xcb = mk("xcb", [128, T], BF16)
            xc, b_xc = mk("xc", [128, T])
            r, b_r = mk("r", [128, T])
            ii, b_ii = mk("i", [128, T])
            aa, b_aa = mk("a", [128, T])
            mm, b_mm = mk("m", [128, T])
            h, b_h = mk("h", [128, T])
            ga, b_ga = mk("ga", [128, T])
            t1, b_t1 = mk("t1", [128, T])
            t2, b_t2 = mk("t2", [128, T])
            b_pad = Buf("xa_pad")
            kb.op("dve", lambda e: e.memset(xa[:, 0:3], 0.0), writes=[b_pad])
            TG = range(4)

            def cs(tg, off=0):
                return slice(off + tg * 512, off + (tg + 1) * 512)

            for g in range(8):
                w_xa, wb_xa = self.load_w(g)
                w_ga, wb_ga = self.load_w(8 + g)
                w_ma, wb_ma = self.load_w(40 + g)
                for tg in TG:
                    self.inproj_fm(w_xa, wb_xa, 128, tg, lambda ps, pb, tg=tg: kb.op(
                        "act", lambda e: e.activation(out=xa[:, cs(tg, 3)], in_=ps[:, :], func=AF.Copy), reads=[pb], writes=[b_xa[tg]]))
                for tg in TG:
                    self.inproj_fm(w_ga, wb_ga, 128, tg, lambda ps, pb, tg=tg: kb.op(
                        "act", lambda e: e.activation(out=ga[:, cs(tg)], in_=ps[:, :], func=AF.Copy), reads=[pb], writes=[b_ga[tg]]))
                for tg in TG:
                    self.inproj_fm(w_ma, wb_ma, 128, tg, lambda ps, pb, tg=tg: kb.op(
                        "act", lambda e: e.activation(out=t2[:, cs(tg)], in_=ps[:, :], func=AF.Sigmoid), reads=[pb], writes=[b_t2[tg]]))
                for tg in TG:
                    kb.op("dve", lambda e, tg=tg: e.tensor_tensor(t1[:, cs(tg)], ga[:, cs(tg)], ga[:, cs(tg)], ALU.mult), reads=[b_ga[tg]], writes=[b_t1[tg]])
                    kb.op("dve", lambda e, tg=tg: e.tensor_scalar(t1[:, cs(tg)], t1[:, cs(tg)], 0.044715, 1.0, ALU.mult, ALU.add), reads=[b_t1[tg]], writes=[b_t1[tg]])
                    kb.op("dve", lambda e, tg=tg: e.tensor_tensor(t1[:, cs(tg)], t1[:, cs(tg)], ga[:, cs(tg)], ALU.mult), reads=[b_t1[tg], b_ga[tg]], writes=[b_t1[tg]])
                for tg in TG:
                    prev = [b_xa[tg - 1]] if tg > 0 else [b_pad]
                    ps, pb = self.bank()
                    for k in range(4):
                        kb.op("pe", lambda e, k=k, tg=tg, ps=ps: e.matmul(ps[:, :], DG[:, g, k, :], xa[:, cs(tg, k)], start=(k == 0), stop=(k == 3)),
                              reads=[b_DG, b_xa[tg]] + prev, writes=[pb], inc=(k == 3))
                    kb.op("act", lambda e, tg=tg, ps=ps: e.activation(out=xc[:, cs(tg)], in_=ps[:, :], func=AF.Identity, bias=chan[:, g, 4:5]),
                          reads=[pb, b_chan], writes=[b_xc[tg]])
                    kb.op("dve", lambda e, tg=tg: e.tensor_copy(xcb[:, cs(tg)], xc[:, cs(tg)]), reads=[b_xc[tg]], writes=[b_xcb[tg]])
                if g == 0:
                    self.dump("xc0", xc[:, :], b_xc[3], [128, T])
                for (wg, bwg, dst, b_dst, bi) in ((wr, b_wr, r, b_r, 5), (wi, b_wi, ii, b_ii, 6)):
                    for tg in TG:
                        ps, pb = self.bank()
                        kb.op("pe", lambda e, tg=tg, ps=ps, wg=wg: e.matmul(ps[:, :], wg[:, g, :], xcb[:, cs(tg)], start=True, stop=True),
                              reads=[bwg, b_xcb[tg]], writes=[pb])
                        kb.op("act", lambda e, tg=tg, ps=ps, dst=dst, bi=bi: e.activation(
                            out=dst[:, cs(tg)], in_=ps[:, :], func=AF.Sigmoid, bias=chan[:, g, bi:bi + 1]),
                            reads=[pb, b_chan], writes=[b_dst[tg]])
                for tg in TG:
                    kb.op("act", lambda e, tg=tg: e.activation(out=aa[:, cs(tg)], in_=r[:, cs(tg)], func=AF.Exp, scale=sc[:, g, 2:3]),
                          reads=[b_r[tg], b_sc], writes=[b_aa[tg]])
                for tg in TG:
                    kb.op("act", lambda e, tg=tg: e.activation(out=mm[:, cs(tg)], in_=r[:, cs(tg)], func=AF.Exp, scale=sc[:, g, 3:4]),
                          reads=[b_r[tg], b_sc], writes=[b_mm[tg]])
                for tg in TG:
                    kb.op("act", lambda e, tg=tg: e.activation(out=mm[:, cs(tg)], in_=mm[:, cs(tg)], func=AF.Ln, scale=-1.0, bias=1.0),
                          reads=[b_mm[tg]], writes=[b_mm[tg]])
                for tg in TG:
                    kb.op("act", lambda e, tg=tg: e.activation(out=mm[:, cs(tg)], in_=mm[:, cs(tg)], func=AF.Exp, scale=0.5),
                          reads=[b_mm[tg]], writes=[b_mm[tg]])
                for tg in TG:
                    kb.op("dve", lambda e, tg=tg: e.tensor_tensor(mm[:, cs(tg)], mm[:, cs(tg)], ii[:, cs(tg)], ALU.mult), reads=[b_mm[tg], b_ii[tg]], writes=[b_mm[tg]])
                    kb.op("dve", lambda e, tg=tg: e.tensor_tensor(mm[:, cs(tg)], mm[:, cs(tg)], xc[:, cs(tg)], ALU.mult), reads=[b_mm[tg], b_xc[tg]], writes=[b_mm[tg]])
                for tg in TG:
                    init = 0.0 if tg == 0 else h[:, tg * 512 - 1:tg * 512]
                    kb.op("dve", lambda e, tg=tg, init=init: e.tensor_tensor_scan(h[:, cs(tg)], aa[:, cs(tg)], mm[:, cs(tg)], init, ALU.mult, ALU.add),
                          reads=[b_aa[tg], b_mm[tg]] + ([b_h[tg - 1]] if tg > 0 else []), writes=[b_h[tg]])
                if g == 0:
                    self.dump("h0", h[:, :], b_h[3], [128, T])
                for tg in TG:
                    kb.op("act", lambda e, tg=tg: e.activation(out=t1[:, cs(tg)], in_=t1[:, cs(tg)], func=AF.Sigmoid, scale=1.5957691216057308),
                          reads=[b_t1[tg]], writes=[b_t1[tg]])
                for tg in TG:
                    kb.op("dve", lambda e, tg=tg: e.tensor_tensor(t1[:, cs(tg)], t1[:, cs(tg)], ga[:, cs(tg)], ALU.mult), reads=[b_t1[tg], b_ga[tg]], writes=[b_t1[tg]])
                    kb.op("dve", lambda e, tg=tg: e.tensor_tensor(t1[:, cs(tg)], t1[:, cs(tg)], h[:, cs(tg)], ALU.mult), reads=[b_t1[tg], b_h[tg]], writes=[b_t1[tg]])
                    kb.op("dve", lambda e, tg=tg: e.tensor_tensor(self.yT[:, g, cs(tg)], t1[:, cs(tg)], t2[:, cs(tg)], ALU.mult),
                          reads=[b_t1[tg], b_t2[tg]], cw=[self.b_yT[g]])

    def phase_gla(self, es1):
        nc, kb = self.nc, self.kb
        cst = self.cstb
        TRI, UU, ONES = cst[:, 128:256], cst[:, 256:384], cst[:, 512:640]
        TRI32 = self.cst[:, 128:256]
        bc = self.b_cstb
        with ExitStack() as es:
            NW = 8
            self.wring = [self.sb(es, "gw%d" % i, [128, 1024], BF16) for i in range(NW)]
            self.wringb = [Buf("gw%d" % i) for i in range(NW)]
            self.w_rr = 0
            wglr = self.sb(es, "wglr", [128, 128], BF16)
            b_wglr = Buf("wglr")
            kb.dma("pool", lambda e: e.dma_start(out=wglr[:], in_=self.din["w_glr"][:, :]), writes=[b_wglr])
            wg = self.sb(es, "wg", [16, 512], BF16)
            bg = self.sb(es, "bg", [1, 512], BF16)
            ng = self.sb(es, "ng", [128, 2])
            b_wg, b_bg, b_ng = Buf("wg"), Buf("bg"), Buf("ng")
            kb.dma("pool", lambda e: e.dma_start(out=wg[:], in_=self.din["gla_wg"][:, :]), writes=[b_wg])
            kb.dma("pool", lambda e: e.dma_start(out=bg[:], in_=self.din["gla_bg"][:, :]), writes=[b_bg])
            kb.dma("sp", lambda e: e.dma_start(out=ng[:], in_=self.din["gla_ng"][:, :]), writes=[b_ng])
            glrT = self.sb(es, "glrT", [16, T], BF16)
            b_glrT = Buf("glrT")
            for tg in range(4):
                self.inproj_fm(wglr, b_wglr, 16, tg, lambda ps, pb, tg=tg: kb.op(
                    "act", lambda e: e.activation(out=glrT[:, tg * 512:(tg + 1) * 512], in_=ps[0:16, :], func=AF.Copy),
                    reads=[pb], cw=[b_glrT]))

            self.ck("glrT")

            def mk(name, shape, dt=F32):
                return self.sb(es, "G_" + name, shape, dt), Buf("G_" + name)
            QT, b_QT = mk("QT", [128, T], BF16)
            KT, b_KT = mk("KT", [128, T], BF16)
            KD0, b_KD0 = mk("KD0", [128, 16, 128], BF16)
            KD1, b_KD1 = mk("KD1", [128, 16, 128], BF16)
            V, b_V = mk("V", [128, 16, 256], BF16)
            OT, b_OT = mk("OT", [128, 2, T])
            EB, b_EB = mk("EB", [128, 32])
            Gsp, b_Gsp = mk("Gsp", [128, 4, 128], BF16)
            Gz, b_Gz = mk("Gz", [128, 4, 128])
            EQ, b_EQ = mk("EQ", [128, 512])
            EK, b_EK = mk("EK", [128, 512])
            ED, b_ED = mk("ED", [128, 4, 128])
            STs = [mk("ST%d" % i, [128, 128], BF16) for i in range(2)]
            Sb = [mk("S%d" % i, [128, 256]) for i in range(4)]
            Sbb = [mk("Sb%d" % i, [128, 256], BF16) for i in range(4)]
            SQ = [mk("SQ%d" % i, [128, 512], BF16) for i in range(2)]
            RIf, b_RIf = mk("RIf", [128, T])
            SG, b_SG = mk("SG", [128, 512])
            SM, b_SM = mk("SM", [128, 512])
            TT, b_TT = mk("TT", [128, 512])

            HW = {}

            def h1(hd, tg):
                if tg == 0:
                    HW[hd] = dict(q=self.load_w(16 + hd), k=self.load_w(20 + hd), v=[self.load_w(24 + 2 * hd + j) for j in range(2)])
                w_q, wb_q = HW[hd]['q']
                w_k, wb_k = HW[hd]['k']
                w_v = HW[hd]['v']
                ps, pb = self.bank()
                for j in range(4):
                    tt = tg * 4 + j
                    kb.op("pe", lambda e, j=j, tt=tt, ps=ps: e.matmul(ps[:, j * 128:(j + 1) * 128], glrT[0:16, tt * 128:(tt + 1) * 128],
                                                                  wg[0:16, hd * 128:(hd + 1) * 128], start=True, stop=False),
                          reads=[b_glrT, b_wg], writes=[pb], inc=False)
                    kb.op("pe", lambda e, j=j, ps=ps: e.matmul(ps[:, j * 128:(j + 1) * 128], cst[0:1, 512:640],
                                                           bg[0:1, hd * 128:(hd + 1) * 128], start=False, stop=True),
                          reads=[bc, b_bg], writes=[pb], inc=(j == 3))
                kb.op("act", lambda e, ps=ps: e.activation(out=Gz[:, :, :], in_=ps[:, :].rearrange("p (j k) -> p j k", k=128),
                                                          func=AF.Exp, scale=-1.0), reads=[pb], writes=[b_Gz])
                kb.op("act", lambda e: e.activation(out=Gsp[:, :, :], in_=Gz[:, :, :], func=AF.Ln, bias=1.0),
                      reads=[b_Gz], writes=[b_Gsp])
                self.ck("z/Gsp")
                ps_c, pb_c = self.bank()
                ps_r, pb_r = self.bank()
                for j in range(4):
                    kb.op("pe", lambda e, j=j, ps_c=ps_c: e.matmul(ps_c[:, j * 128:(j + 1) * 128], Gsp[:, j, :], TRI, start=True, stop=True),
                          reads=[b_Gsp, bc], writes=[pb_c], inc=False)
                    kb.op("pe", lambda e, j=j, ps_r=ps_r: e.matmul(ps_r[:, j * 128:(j + 1) * 128], UU, Gsp[:, j, :], start=True, stop=True),
                          reads=[b_Gsp, bc], writes=[pb_r], inc=(j == 3))
                kb.op("act", lambda e, ps_c=ps_c: e.activation(out=EQ[:, :], in_=ps_c[:, :], func=AF.Exp, scale=-1.0 / 16), reads=[pb_c], writes=[b_EQ])
                kb.op("act", lambda e, ps_c=ps_c: e.activation(out=EK[:, :], in_=ps_c[:, :], func=AF.Exp, scale=1.0 / 16), reads=[pb_c], writes=[b_EK])
                kb.op("act", lambda e, ps_r=ps_r: e.activation(out=ED[:, :, :], in_=ps_r[:, :].rearrange("p (j k) -> p j k", k=128),
                                                            func=AF.Exp, scale=-1.0 / 16), reads=[pb_r], writes=[b_ED])
                kb.op("dve", lambda e, tg=tg: e.tensor_copy(EB[:, tg * 8:(tg + 1) * 8], EQ[:, 63:512:64]), reads=[b_EQ], cw=[b_EB])
                self.ck("cs/rev/E")
                self.inproj_fm(w_q, wb_q, 128, tg, lambda ps, pb, tg=tg: kb.op(
                    "dve", lambda e: e.scalar_tensor_tensor(QT[:, tg * 512:(tg + 1) * 512], ps[:, :], 128.0 ** -0.5, EQ[:, :], ALU.mult, ALU.mult),
                    reads=[pb, b_EQ], cw=[b_QT]))
                self.inproj_fm(w_k, wb_k, 128, tg, lambda ps, pb, tg=tg: kb.op(
                    "dve", lambda e: e.tensor_tensor(KT[:, tg * 512:(tg + 1) * 512], ps[:, :], EK[:, :], ALU.mult),
                    reads=[pb, b_EK], cw=[b_KT]))
                self.ck("qk fm")
                ps, pb = self.bank()
                for j in range(4):
                    tt = tg * 4 + j
                    for kc in range(8):
                        kb.op("pe", lambda e, j=j, tt=tt, kc=kc, ps=ps: e.matmul(ps[:, j * 128:(j + 1) * 128], self.XT[:, kc, tt * 128:(tt + 1) * 128],
                                                                             w_k[:, kc * 128:(kc + 1) * 128], start=(kc == 0), stop=(kc == 7)),
                              reads=[self.b_XT, wb_k], writes=[pb], inc=(j == 3 and kc == 7))
                for KDm, b_KDm, mcol in ((KD0, b_KD0, 704), (KD1, b_KD1, 705)):
                    kb.op("dve", lambda e, ps=ps, tg=tg, KDm=KDm, mcol=mcol: e.scalar_tensor_tensor(
                        KDm[:, tg * 4:(tg + 1) * 4, :], ps[:, :].rearrange("p (j k) -> p j k", k=128), self.cst[:, mcol:mcol + 1],
                        ED[:, :, :], ALU.mult, ALU.mult), reads=[pb, b_ED, self.b_cst], cw=[b_KDm])
                self.ck("kd")
                for jj in range(2):
                    ps, pb = self.bank()
                    for j2 in range(2):
                        tt = tg * 4 + jj * 2 + j2
                        for half in range(2):
                            wv, wbv = w_v[half]
                            for kc in range(8):
                                kb.op("pe", lambda e, j2=j2, tt=tt, kc=kc, ps=ps, half=half, wv=wv: e.matmul(
                                    ps[:, j2 * 256 + half * 128:j2 * 256 + (half + 1) * 128], self.XT[:, kc, tt * 128:(tt + 1) * 128],
                                    wv[:, kc * 128:(kc + 1) * 128], start=(kc == 0), stop=(kc == 7)),
                                    reads=[self.b_XT, wbv], writes=[pb], inc=(j2 == 1 and half == 1 and kc == 7))
                    t0 = tg * 4 + jj * 2
                    kb.op("dve", lambda e, ps=ps, t0=t0: e.tensor_copy(V[:, t0:t0 + 2, :], ps[:, :].rearrange("p (j v) -> p j v", v=256)),
                          reads=[pb], cw=[b_V])
                self.ck("v")

            def h2(hd):
                kb.op("dve", lambda e: e.memset(Sb[0][0][:, :], 0.0), writes=[Sb[0][1]])
                kb.op("dve", lambda e: e.memset(Sbb[0][0][:, :], 0.0), writes=[Sbb[0][1]])
                for tt in range(16):
                    c0, c1 = 2 * tt, 2 * tt + 1
                    ps_st, pb_st = self.ps[tt % 2], self.psb[tt % 2]
                    st, b_st = STs[tt % 2]
                    kb.op("pe", lambda e: e.matmul(ps_st[:, 0:128], KT[:, tt * 128:(tt + 1) * 128], QT[:, tt * 128:(tt + 1) * 128], start=True, stop=True),
                          reads=[b_KT, b_QT], writes=[pb_st])
                    kb.op("dve", lambda e: e.tensor_tensor(st[:, :], ps_st[:, 0:128], TRI32, ALU.mult), reads=[pb_st, self.b_cst], writes=[b_st])
                    ps_kv, pb_kv = self.ps[2 + tt % 2], self.psb[2 + tt % 2]
                    kb.op("pe", lambda e: e.matmul(ps_kv[:, 0:256], KD0[:, tt, :], V[:, tt, :], start=True, stop=True),
                          reads=[b_KD0, b_V], writes=[pb_kv], inc=False)
                    kb.op("pe", lambda e: e.matmul(ps_kv[:, 256:512], KD1[:, tt, :], V[:, tt, :], start=True, stop=True),
                          reads=[b_KD1, b_V], writes=[pb_kv])
                    self.ck("st/kv")
                    grp = (tt // 4) % 2
                    col = (tt % 4) * 128
                    for vc in range(2):
                        pso, pbo = self.ps[4 + 2 * grp + vc], self.psb[4 + 2 * grp + vc]
                        kb.op("pe", lambda e, vc=vc, pso=pso: e.matmul(pso[:, col:col + 128], V[:, tt, vc * 128:(vc + 1) * 128], st[:, :], start=True, stop=False),
                              reads=[b_V, b_st], writes=[pbo], inc=False)
                        kb.op("pe", lambda e, vc=vc, pso=pso: e.matmul(pso[:, col:col + 64], Sbb[c0 % 4][0][:, vc * 128:(vc + 1) * 128], QT[:, c0 * 64:(c0 + 1) * 64],
                                                                    start=False, stop=False), reads=[Sbb[c0 % 4][1], b_QT], writes=[pbo], inc=False)
                        if vc == 0:
                            kb.op("dve", lambda e: e.scalar_tensor_tensor(Sb[c1 % 4][0][:, :], Sb[c0 % 4][0][:, :], EB[:, c0:c0 + 1], ps_kv[:, 0:256], ALU.mult, ALU.add),
                                  reads=[Sb[c0 % 4][1], b_EB, pb_kv], writes=[Sb[c1 % 4][1]])
                            kb.op("act", lambda e: e.activation(out=Sbb[c1 % 4][0][:, :], in_=Sb[c1 % 4][0][:, :], func=AF.Copy),
                                  reads=[Sb[c1 % 4][1]], writes=[Sbb[c1 % 4][1]])
                        kb.op("pe", lambda e, vc=vc, pso=pso: e.matmul(pso[:, col + 64:col + 128], Sbb[c1 % 4][0][:, vc * 128:(vc + 1) * 128], QT[:, c1 * 64:(c1 + 1) * 64],
                                                                    start=False, stop=True), reads=[Sbb[c1 % 4][1], b_QT], writes=[pbo], inc=True)
                    kb.op("dve", lambda e: e.scalar_tensor_tensor(Sb[(c1 + 1) % 4][0][:, :], Sb[c1 % 4][0][:, :], EB[:, c1:c1 + 1], ps_kv[:, 256:512], ALU.mult, ALU.add),
                          reads=[Sb[c1 % 4][1], b_EB, pb_kv], writes=[Sb[(c1 + 1) % 4][1]])
                    kb.op("act", lambda e: e.activation(out=Sbb[(c1 + 1) % 4][0][:, :], in_=Sb[(c1 + 1) % 4][0][:, :], func=AF.Copy),
                          reads=[Sb[(c1 + 1) % 4][1]], writes=[Sbb[(c1 + 1) % 4][1]])
                    self.ck("o tile")
                    if tt % 4 == 3:
                        tg = tt // 4
                        for vc in range(2):
                            pso, pbo = self.ps[4 + 2 * grp + vc], self.psb[4 + 2 * grp + vc]
                            kb.op("act", lambda e, vc=vc, pso=pso, tg=tg: e.activation(out=OT[:, vc, tg * 512:(tg + 1) * 512], in_=pso[:, :], func=AF.Copy),
                                  reads=[pbo], cw=[b_OT])
                if hd == 0:
                    self.dump("o_raw0", OT[:, 0, :], b_OT, [128, T])
                for tg in range(4):
                    sl = slice(tg * 512, (tg + 1) * 512)
                    for vc in range(2):
                        kb.op("act", lambda e, vc=vc, sl=sl: e.activation(out=SQ[vc][0][:, :], in_=OT[:, vc, sl], func=AF.Square), reads=[b_OT], writes=[SQ[vc][1]])
                    ps, pb = self.bank()
                    for vc in range(2):
                        kb.op("pe", lambda e, vc=vc, ps=ps: e.matmul(ps[:, :], ONES, SQ[vc][0][:, :], start=(vc == 0), stop=(vc == 1)),
                              reads=[bc, SQ[vc][1]], writes=[pb], inc=(vc == 1))
                    kb.op("act", lambda e, ps=ps, sl=sl: e.activation(out=RIf[:, sl], in_=ps[:, :], func=AF.Sqrt, scale=1.0 / 256, bias=1e-5), reads=[pb], cw=[b_RIf])
                kb.op("dve", lambda e: e.reciprocal(RIf[:, :], RIf[:, :]), reads=[b_RIf], writes=[b_RIf])

            def h3(hd, tg):
                if tg == 0:
                    HW[hd]['go'] = [self.load_w(32 + 2 * hd + j) for j in range(2)]
                    HW[hd]['mb'] = [self.load_w(48 + 2 * hd + j) for j in range(2)]
                w_go, w_mb = HW[hd]['go'], HW[hd]['mb']
                sl = slice(tg * 512, (tg + 1) * 512)
                for vc in range(2):
                    g = hd * 2 + vc
                    go_ps = {}
                    self.inproj_fm(w_go[vc][0], w_go[vc][1], 128, tg, lambda ps, pb: (go_ps.update(ps=ps, pb=pb), kb.op(
                        "act", lambda e: e.activation(out=SG[:, :], in_=ps[:, :], func=AF.Sigmoid), reads=[pb], writes=[b_SG])))
                    self.inproj_fm(w_mb[vc][0], w_mb[vc][1], 128, tg, lambda ps, pb: kb.op(
                        "act", lambda e: e.activation(out=SM[:, :], in_=ps[:, :], func=AF.Sigmoid), reads=[pb], writes=[b_SM]))
                    kb.op("dve", lambda e, vc=vc, sl=sl: e.scalar_tensor_tensor(TT[:, :], OT[:, vc, sl], ng[:, vc:vc + 1], RIf[:, sl], ALU.mult, ALU.mult),
                          reads=[b_OT, b_ng, b_RIf], writes=[b_TT])
                    kb.op("dve", lambda e: e.tensor_tensor(TT[:, :], TT[:, :], SG[:, :], ALU.mult), reads=[b_TT, b_SG], writes=[b_TT])
                    kb.op("dve", lambda e: e.tensor_tensor(TT[:, :], TT[:, :], go_ps["ps"][:, :], ALU.mult), reads=[b_TT, b_SG, go_ps["pb"]], writes=[b_TT])
                    kb.op("dve", lambda e: e.tensor_tensor(TT[:, :], TT[:, :], SM[:, :], ALU.mult), reads=[b_TT, b_SM], writes=[b_TT])
                    kb.op("dve", lambda e, g=g, sl=sl: e.tensor_tensor(self.yT[:, g, sl], TT[:, :], self.yT[:, g, sl], ALU.add),
                          reads=[b_TT, self.b_yT[g]], writes=[self.b_yT[g]])


            for hd in range(4):
                for tg in range(4):
                    h1(hd, tg)
                    if hd > 0:
                        h3(hd - 1, tg)
                h2(hd)
            for tg in range(4):
                h3(3, tg)

    def layer_norm(self, es_tmp, R, b_R, grow, brow, OUT, b_OUT, tag):
        kb = self.kb
        st = self.ln_st
        kb.op("dve", lambda e: e.bn_stats(st["stats"][:, 0, :], R[:, 0:512]), reads=[b_R], writes=[st["b"]])
        kb.op("dve", lambda e: e.bn_stats(st["stats"][:, 1, :], R[:, 512:1024]), reads=[b_R], writes=[st["b"]])
        kb.op("dve", lambda e: e.bn_aggr(st["mv"][:, :], st["stats"][:, :, :].rearrange("p a b -> p (a b)")), reads=[st["b"]], writes=[st["b"]])
        kb.op("act", lambda e: e.activation(out=st["rs"][:, 0:1], in_=st["mv"][:, 1:2], func=AF.Ln, bias=1e-5), reads=[st["b"]], writes=[st["b2"]])
        kb.op("act", lambda e: e.activation(out=st["rs"][:, 0:1], in_=st["rs"][:, 0:1], func=AF.Exp, scale=-0.5), reads=[st["b2"]], writes=[st["b2"]])
        kb.op("dve", lambda e: e.scalar_tensor_tensor(st["rs"][:, 1:2], st["mv"][:, 0:1], -1.0, st["rs"][:, 0:1], ALU.mult, ALU.mult),
              reads=[st["b"], st["b2"]], writes=[st["b2"]])
        kb.op("act", lambda e: e.activation(out=OUT[:, :], in_=R[:, :], func=AF.Identity, scale=st["rs"][:, 0:1], bias=st["rs"][:, 1:2]),
              reads=[b_R, st["b2"]], writes=[b_OUT])
        kb.op("dve", lambda e: e.tensor_tensor(OUT[:, :], OUT[:, :], self.rowp[:, grow, :], ALU.mult), reads=[b_OUT, self.b_rowp], writes=[b_OUT])
        kb.op("dve", lambda e: e.tensor_tensor(OUT[:, :], OUT[:, :], self.rowp[:, brow, :], ALU.add), reads=[b_OUT, self.b_rowp], writes=[b_OUT])

    def alloc_ln(self, es):
        g = self.ps_gen
        self.rowp = self.sb(es, "rowp_sb%d" % g, [128, 5, 1024])
        self.b_rowp = Buf("rowp")
        self.kb.dma("sp", lambda e: e.dma_start(out=self.rowp[:], in_=self.din["rowp"].rearrange("p (r d) -> p r d", d=1024)),
                    writes=[self.b_rowp])
        self.ln_st = {"stats": self.sb(es, "ln_stats%d" % g, [128, 2, 6]), "mv": self.sb(es, "ln_mv%d" % g, [128, 2]),
                      "rs": self.sb(es, "ln_rs%d" % g, [128, 2]), "b": Buf("ln_b"), "b2": Buf("ln_b2")}

    def phase_outproj(self):
        nc, kb = self.nc, self.kb
        cstb, bcb = self.cstb, self.b_cstb
        IDb, LTb, ONEb = cstb[:, 0:128], cstb[:, 384:512], cstb[:, 512:640]
        with ExitStack() as es:
            self.alloc_psum(es, 6, 2)
            self.alloc_ln(es)

            def mk(name, shape, dt=F32):
                return self.sb(es, "P2_" + name, shape, dt), Buf("P2_" + name)
            Wout, b_Wout = mk("Wout", [128, 8, 1024], BF16)
            kb.dma("pool", lambda e: e.dma_start(out=Wout[:, 0:4, :], in_=self.din["w_out"].rearrange("p (k n) -> p k n", n=1024)[:, 0:4, :]), cw=[b_Wout])
            kb.dma("pool", lambda e: e.dma_start(out=Wout[:, 4:8, :], in_=self.din["w_out"].rearrange("p (k n) -> p k n", n=1024)[:, 4:8, :]), cw=[b_Wout])
            wr32, b_wr32 = mk("wr32", [128, 8, 32])
            wrh, b_wrh = mk("wrh", [128, 8, 32], BF16)
            wrl, b_wrl = mk("wrl", [128, 8, 32], BF16)
            kb.dma("sp", lambda e: e.dma_start(out=wr32[:], in_=self.din["w_router"].rearrange("p (k n) -> p k n", n=32)), writes=[b_wr32])
            kb.op("dve", lambda e: e.tensor_copy(wrh[:], wr32[:]), reads=[b_wr32], writes=[b_wrh])
            kb.op("dve", lambda e: e.tensor_tensor(wrl[:], wr32[:], wrh[:], ALU.subtract), reads=[b_wr32, b_wrh], writes=[b_wrl])
            brt, b_brt = mk("brt", [128, 32])
            kb.dma("sp", lambda e: e.dma_start(out=brt[:], in_=self.din["b_router"][0:1, :].partition_broadcast(128)), writes=[b_brt])
            carry, b_carry = mk("carry", [128, 32])
            kb.op("dve", lambda e: e.memset(carry[:], 0.0), writes=[b_carry])
            Xt = [mk("x%d" % i, [128, 1024]) for i in range(2)]
            R, b_R = mk("R", [128, 1024])
            H1 = [mk("H1_%d" % i, [128, 1024]) for i in range(2)]
            H1b = [mk("H1b_%d" % i, [128, 1024], BF16) for i in range(2)]
            H1l = [mk("H1l_%d" % i, [128, 1024], BF16) for i in range(2)]
            HT = [mk("HT_%d" % i, [128, 8, 128], BF16) for i in range(2)]
            lg, b_lg = mk("lg", [128, 32])
            v8, b_v8 = mk("v8", [128, 8])
            i8, b_i8 = mk("i8", [128, 8], U32)
            i8f, b_i8f = mk("i8f", [128, 8])
            sm, b_sm = mk("sm", [128, 8])
            mask, b_mask = mk("mask", [128, 32], BF16)
            sc, b_sc = mk("sc", [128, 32])
            ov, b_ov = mk("ov", [128, 32])
            junk, b_junk = mk("junk", [128, 32])
            slf, b_slf = mk("slf", [128, 4])

            for tt in range(16):
                tsl = slice(tt * 128, (tt + 1) * 128)
                xt, b_xt = Xt[tt % 2]
                kb.dma("sp", lambda e: e.dma_start(out=xt[:], in_=self.din["x"][tsl, :]), writes=[b_xt])
                for half in range(2):
                    ps, pb = self.bank()
                    for kc in range(8):
                        kb.op("pe", lambda e, kc=kc, ps=ps, half=half: e.matmul(ps[:, :], self.yT[:, kc, tsl], Wout[:, kc, half * 512:(half + 1) * 512],
                                                                             start=(kc == 0), stop=(kc == 7)),
                              reads=[self.b_yT[kc], b_Wout], writes=[pb], inc=(kc == 7))
                    kb.op("dve", lambda e, ps=ps, half=half: e.scalar_tensor_tensor(R[:, half * 512:(half + 1) * 512], xt[:, half * 512:(half + 1) * 512], ALPHA,
                                                                                  ps[:, :], ALU.mult, ALU.add), reads=[b_xt, pb], cw=[b_R])
                h1, b_h1 = H1[tt % 2]
                self.layer_norm(es, R, b_R, 0, 1, h1, b_h1, "ln1")
                kb.dma("sp", lambda e: e.dma_start(out=self.H1d[tsl, :], in_=h1[:]), reads=[b_h1], cw=[self.b_H1d])
                if tt == 0:
                    self.dump("h1_0", h1[:], b_h1, [128, 1024])
                hb, b_hb = H1b[tt % 2]
                hl, b_hl = H1l[tt % 2]
                kb.op("act", lambda e: e.activation(out=hb[:, :], in_=h1[:, :], func=AF.Identity), reads=[b_h1], writes=[b_hb])
                kb.op("dve", lambda e: e.tensor_tensor(hl[:, :], h1[:, :], hb[:, :], ALU.subtract), reads=[b_h1, b_hb], writes=[b_hl])
                for (src, b_src, (dst, b_dst)) in ((hb, b_hb, HT[0]), (hl, b_hl, HT[1])):
                    pt, ptb = self.bank16()
                    for kc in range(8):
                        kb.op("pe", lambda e, kc=kc, pt=pt, src=src: e.transpose(pt[:, kc * 128:(kc + 1) * 128], src[:, kc * 128:(kc + 1) * 128], IDb),
                              reads=[b_src, bcb], writes=[ptb], inc=(kc == 7))
                    kb.op("dve", lambda e, pt=pt, dst=dst: e.tensor_copy(dst[:, :, :], pt[:, :].rearrange("p (k t) -> p k t", t=128)),
                          reads=[ptb], writes=[b_dst])
                ps, pb = self.bank()
                combos = [(HT[0], wrh, b_wrh), (HT[0], wrl, b_wrl), (HT[1], wrh, b_wrh)]
                n = 0
                for (ht, b_ht), w, b_w in combos:
                    for kc in range(8):
                        n += 1
                        kb.op("pe", lambda e, kc=kc, ps=ps, ht=ht, w=w, n=n: e.matmul(ps[:, 0:32], ht[:, kc, :], w[:, kc, :], start=(n == 1), stop=(n == 24)),
                              reads=[b_ht, b_w], writes=[pb], inc=(n == 24))
                kb.op("dve", lambda e, ps=ps: e.tensor_tensor(lg[:, :], ps[:, 0:32], brt[:, :], ALU.add), reads=[pb, b_brt], writes=[b_lg])
                if tt == 0:
                    self.dump("lg_0", lg[:], b_lg, [128, 32])
                kb.op("dve", lambda e: e.max(out=v8[:, :], in_=lg[:, :]), reads=[b_lg], writes=[b_v8])
                kb.op("dve", lambda e: e.max_index(out=i8[:, :], in_max=v8[:, :], in_values=lg[:, :]), reads=[b_lg, b_v8], writes=[b_i8])
                kb.op("dve", lambda e: e.tensor_copy(i8f[:, :], i8[:, :]), reads=[b_i8], writes=[b_i8f])
                kb.op("dve", lambda e: e.tensor_scalar_mul(sm[:, 0:1], v8[:, 0:1], -1.0), reads=[b_v8], writes=[b_sm])
                kb.op("act", lambda e: e.activation(out=sm[:, 4:8], in_=v8[:, 0:4], func=AF.Exp, bias=sm[:, 0:1], accum_out=sm[:, 1:2]),
                      reads=[b_v8, b_sm], writes=[b_sm])
                kb.op("dve", lambda e: e.reciprocal(sm[:, 2:3], sm[:, 1:2]), reads=[b_sm], writes=[b_sm])
                kb.op("dve", lambda e: e.tensor_scalar_mul(self.GATES[:, tt, :], sm[:, 4:8], sm[:, 2:3]), reads=[b_sm], writes=[self.b_route[tt]])
                kb.op("dve", lambda e: e.tensor_scalar(mask[:, :], lg[:, :], v8[:, 3:4], None, ALU.is_ge), reads=[b_lg, b_v8], writes=[b_mask])
                ps, pb = self.bank()
                kb.op("pe", lambda e, ps=ps: e.matmul(ps[:, 0:32], LTb, mask[:, :], start=True, stop=True), reads=[bcb, b_mask], writes=[pb], inc=False)
                kb.op("pe", lambda e, ps=ps: e.matmul(ps[:, 32:64], ONEb, mask[:, :], start=True, stop=True), reads=[bcb, b_mask], writes=[pb])
                kb.op("dve", lambda e, ps=ps: e.tensor_tensor(sc[:, :], ps[:, 0:32], carry[:, :], ALU.add), reads=[pb, b_carry], writes=[b_sc])
                kb.op("dve", lambda e, ps=ps: e.tensor_tensor(carry[:, :], ps[:, 32:64], carry[:, :], ALU.add), reads=[pb, b_carry, b_sc], writes=[b_carry])
                kb.op("dve", lambda e: e.tensor_scalar(ov[:, :], sc[:, :], float(CAP), float(4 * NSLOT), ALU.is_ge, ALU.mult), reads=[b_sc], writes=[b_ov])
                kb.op("dve", lambda e: e.tensor_tensor(sc[:, :], sc[:, :], self.cst[:, 672:704], ALU.add), reads=[b_sc, self.b_cst], writes=[b_sc])
                kb.op("dve", lambda e: e.tensor_tensor(sc[:, :], sc[:, :], ov[:, :], ALU.add), reads=[b_sc, b_ov], writes=[b_sc])
                for k in range(4):
                    kb.op("dve", lambda e, k=k: e.scalar_tensor_tensor(junk[:, :], self.cst[:, 640:672], i8f[:, k:k + 1], sc[:, :], ALU.is_equal, ALU.mult,
                                                                      accum_out=slf[:, k:k + 1]), reads=[self.b_cst, b_i8f, b_sc], writes=[b_junk, b_slf])
                kb.op("dve", lambda e: e.tensor_copy(self.SLOTS[:, tt, :], slf[:, :]), reads=[b_slf], writes=[self.b_route[tt]])
                for k in range(4):
                    kb.dma("pool", lambda e, k=k: e.indirect_dma_start(
                        out=self.Xg, out_offset=bass.IndirectOffsetOnAxis(ap=self.SLOTS[:, tt, k:k + 1], axis=0),
                        in_=hb[:, :], in_offset=None, bounds_check=self.bound_reg, oob_is_err=False),
                        reads=[b_hb, self.b_route[tt], self.b_Xgz], cw=[self.b_Xg])
            self.dump("gates", self.GATES[:].rearrange("p a b -> p (a b)"), self.b_route[15], [128, 64])
            if "slots" in self.dbg:
                sf, b_sf = mk("slots_f", [128, 64])
                kb.op("dve", lambda e: e.tensor_copy(sf[:, :], self.SLOTS[:].rearrange("p a b -> p (a b)")), reads=self.b_route, writes=[b_sf])
                self.dump("slots", sf[:], b_sf, [128, 64])

    def phase_moe(self):
        nc, kb = self.nc, self.kb
        IDb, bcb = self.cstb[:, 0:128], self.b_cstb
        NST = CAP // 128
        with ExitStack() as es:
            self.alloc_psum(es, 6, 2)

            def mk(name, shape, dt=F32):
                return self.sb(es, "M_" + name, shape, dt), Buf("M_" + name)
            WU = [mk("wu%d" % i, [128, 8, 2048], BF16) for i in range(2)]
            WD = [mk("wd%d" % i, [128, 8, 1024], BF16) for i in range(2)]
            BD = [mk("bd%d" % i, [128, 1024]) for i in range(2)]
            XG = [mk("xg%d" % i, [128, NST, 1024], BF16) for i in range(2)]
            XGT = [mk("xgt%d" % i, [128, 8, CAP], BF16) for i in range(2)]
            ACTT, b_ACTT = mk("actt", [128, 8, CAP], BF16)
            Gt = [mk("g%d" % i, [128, CAP]) for i in range(2)]
            St = [mk("s%d" % i, [128, CAP]) for i in range(2)]
            Ut = [mk("u%d" % i, [128, CAP]) for i in range(2)]
            Ysb = [mk("y%d" % i, [128, 1024]) for i in range(2)]
            bup, b_bup = mk("bup", [128, 32, 16])
            kb.dma("sp", lambda e: e.dma_start(out=bup[:], in_=self.din["b_up"].rearrange("p (e f) -> p e f", f=16)), writes=[b_bup])

            def load(ex):
                sl = ex % 2
                wu = self.din["w_up"][ex].rearrange("(kc p) f -> p kc f", p=128)
                wd = self.din["w_down"][ex].rearrange("(kc p) f -> p kc f", p=128)
                kb.dma("sp", lambda e: e.dma_start(out=XG[sl][0][:], in_=self.Xg[ex * CAP:(ex + 1) * CAP, :].rearrange("(st p) d -> p st d", p=128)),
                       reads=[self.b_Xg], writes=[XG[sl][1]])
                kb.dma("sp", lambda e: e.dma_start(out=BD[sl][0][:], in_=self.din["b_down"][ex:ex + 1, :].partition_broadcast(128)), writes=[BD[sl][1]])
                for q in range(4):
                    kb.dma("pool", lambda e, q=q: e.dma_start(out=WU[sl][0][:, 2 * q:2 * q + 2, :], in_=wu[:, 2 * q:2 * q + 2, :]), cw=[WU[sl][1]])
                for q in range(2):
                    kb.dma("pool", lambda e, q=q: e.dma_start(out=WD[sl][0][:, 4 * q:4 * q + 4, :], in_=wd[:, 4 * q:4 * q + 4, :]), cw=[WD[sl][1]])

            def tposes(ex):
                sl = ex % 2
                xg, b_xg = XG[sl]
                xgt, b_xgt = XGT[sl]
                for st in range(NST):
                    pt, ptb = self.bank16()
                    for kc in range(8):
                        kb.op("pe", lambda e, kc=kc, pt=pt, st=st: e.transpose(pt[:, kc * 128:(kc + 1) * 128], xg[:, st, kc * 128:(kc + 1) * 128], IDb),
                              reads=[b_xg, bcb], writes=[ptb], inc=(kc == 7))
                    kb.op("dve", lambda e, pt=pt, st=st: e.tensor_copy(xgt[:, :, st * 128:(st + 1) * 128], pt[:, :].rearrange("p (k s) -> p k s", s=128)),
                          reads=[ptb], cw=[b_xgt])

            def up(ex):
                sl = ex % 2
                wu, b_wu = WU[sl]
                xgt, b_xgt = XGT[sl]
                for c in range(8):
                    g, b_g = Gt[c % 2]
                    s_, b_s = St[c % 2]
                    u, b_u = Ut[c % 2]
                    ps_g, pb_g = self.bank()
                    for kc in range(8):
                        kb.op("pe", lambda e, kc=kc, ps_g=ps_g, c=c: e.matmul(ps_g[:, 0:CAP], wu[:, kc, c * 128:(c + 1) * 128], xgt[:, kc, :], start=(kc == 0), stop=(kc == 7)),
                              reads=[b_wu, b_xgt], writes=[pb_g], inc=(kc == 7))
                    ps_u, pb_u = self.bank()
                    for kc in range(8):
                        kb.op("pe", lambda e, kc=kc, ps_u=ps_u, c=c: e.matmul(ps_u[:, 0:CAP], wu[:, kc, 1024 + c * 128:1024 + (c + 1) * 128], xgt[:, kc, :],
                                                                           start=(kc == 0), stop=(kc == 7)),
                              reads=[b_wu, b_xgt], writes=[pb_u], inc=(kc == 7))
                    kb.op("dve", lambda e, ps_g=ps_g, c=c: e.tensor_scalar(g[:, :], ps_g[:, 0:CAP], bup[:, ex, c:c + 1], 7.0, ALU.add, ALU.min),
                          reads=[pb_g, b_bup], writes=[b_g])
                    kb.op("act", lambda e: e.activation(out=s_[:, :], in_=g[:, :], func=AF.Sigmoid, scale=1.702), reads=[b_g], writes=[b_s])
                    kb.op("dve", lambda e, ps_u=ps_u, c=c: e.tensor_scalar(u[:, :], ps_u[:, 0:CAP], bup[:, ex, 8 + c:9 + c], 7.0, ALU.add, ALU.min),
                          reads=[pb_u, b_bup], writes=[b_u])
                    kb.op("dve", lambda e: e.tensor_scalar(u[:, :], u[:, :], -7.0, 1.0, ALU.max, ALU.add), reads=[b_u], writes=[b_u])
                    kb.op("dve", lambda e: e.tensor_tensor(g[:, :], g[:, :], s_[:, :], ALU.mult), reads=[b_g, b_s], writes=[b_g])
                    kb.op("dve", lambda e, c=c: e.tensor_tensor(ACTT[:, c, :], g[:, :], u[:, :], ALU.mult), reads=[b_g, b_u], cw=[b_ACTT])

            def down(ex):
                sl = ex % 2
                wd, b_wd = WD[sl]
                bd, b_bd = BD[sl]
                for st in range(NST):
                    y, b_y = Ysb[st % 2]
                    for half in range(2):
                        ps, pb = self.bank()
                        for fc in range(8):
                            kb.op("pe", lambda e, fc=fc, ps=ps, half=half, st=st: e.matmul(ps[:, :], ACTT[:, fc, st * 128:(st + 1) * 128],
                                                                                        wd[:, fc, half * 512:(half + 1) * 512], start=(fc == 0), stop=(fc == 7)),
                                  reads=[b_ACTT, b_wd], writes=[pb], inc=(fc == 7))
                        kb.op("dve", lambda e, ps=ps, half=half: e.tensor_tensor(y[:, half * 512:(half + 1) * 512], ps[:, :], bd[:, half * 512:(half + 1) * 512], ALU.add),
                              reads=[pb, b_bd], cw=[b_y])
                    r0 = ex * CAP + st * 128
                    kb.dma("sp", lambda e, r0=r0: e.dma_start(out=self.Yg[r0:r0 + 128, :], in_=y[:]), reads=[b_y], cw=[self.b_Yg])

            ne = getattr(self, "n_experts", NE)
            load(0)
            if ne > 1:
                load(1)
            tposes(0)
            for ex in range(ne):
                up(ex)
                if ex + 1 < ne:
                    tposes(ex + 1)
                down(ex)
                if ex + 2 < ne:
                    load(ex + 2)

    def phase_final(self):
        nc, kb = self.nc, self.kb
        IDb, bcb = self.cstb[:, 0:128], self.b_cstb
        with ExitStack() as es:
            self.alloc_psum(es, 6, 2)
            self.alloc_ln(es)

            def mk(name, shape, dt=F32):
                return self.sb(es, "F_" + name, shape, dt), Buf("F_" + name)
            Wpg, b_Wpg = mk("Wpg", [128, 8, 1024], BF16)
            for q in range(2):
                kb.dma("pool", lambda e, q=q: e.dma_start(out=Wpg[:, 4 * q:4 * q + 4, :], in_=self.din["w_pg"].rearrange("p (k n) -> p k n", n=1024)[:, 4 * q:4 * q + 4, :]),
                       cw=[b_Wpg])
            Wple, b_Wple = mk("Wple", [128, 2, 1024], BF16)
            kb.dma("pool", lambda e: e.dma_start(out=Wple[:], in_=self.din["w_ple"].rearrange("p (k n) -> p k n", n=1024)), writes=[b_Wple])
            PT, b_PT = mk("PT", [128, 2, T], BF16)
            pT = self.din["pT"].rearrange("(kc p) t -> p kc t", p=128)
            for kc in range(2):
                kb.dma("pool", lambda e, kc=kc: e.dma_start(out=PT[:, kc, :], in_=pT[:, kc, :]), cw=[b_PT])
            YG = [mk("yg%d" % i, [128, 4, 1024]) for i in range(3)]
            H1t = [mk("h1_%d" % i, [128, 1024]) for i in range(3)]
            ACC, b_ACC = mk("acc", [128, 1024])
            H2T, b_H2T = mk("h2T", [128, 8, 128], BF16)
            SGT, b_SGT = mk("sgt", [128, 1024])
            OUT = [mk("out%d" % i, [128, 1024]) for i in range(2)]
            def prefetch(tt):
                tsl = slice(tt * 128, (tt + 1) * 128)
                yg, b_yg = YG[tt % 3]
                h1, b_h1 = H1t[tt % 3]
                kb.op("pool", lambda e: e.memset(yg[:], 0.0), writes=[b_yg])
                for k in range(4):
                    kb.dma("pool", lambda e, k=k: e.indirect_dma_start(
                        out=yg[:, k, :], out_offset=None, in_=self.Yg,
                        in_offset=bass.IndirectOffsetOnAxis(ap=self.SLOTS[:, tt, k:k + 1], axis=0),
                        bounds_check=self.bound_reg, oob_is_err=False), reads=[self.b_Yg, self.b_route[tt]], cw=[b_yg])
                kb.dma("sp", lambda e: e.dma_start(out=h1[:], in_=self.H1d[tsl, :]), reads=[self.b_H1d], writes=[b_h1])

            H2s = [mk("h2_%d" % i, [128, 1024]) for i in range(2)]
            H2bs = [mk("h2b_%d" % i, [128, 1024], BF16) for i in range(2)]

            def stage_a(tt):
                yg, b_yg = YG[tt % 3]
                h1, b_h1 = H1t[tt % 3]
                H2, b_H2 = H2s[tt % 2]
                H2b, b_H2b = H2bs[tt % 2]
                kb.op("act", lambda e: e.activation(out=ACC[:, :], in_=h1[:, :], func=AF.Identity, scale=ALPHA), reads=[b_h1], writes=[b_ACC])
                for k in range(4):
                    kb.op("dve", lambda e, k=k: e.scalar_tensor_tensor(ACC[:, :], yg[:, k, :], self.GATES[:, tt, k:k + 1], ACC[:, :], ALU.mult, ALU.add),
                          reads=[b_yg, self.b_route[tt], b_ACC], writes=[b_ACC])
                self.layer_norm(es, ACC, b_ACC, 2, 3, H2, b_H2, "ln2")
                if tt == 0:
                    self.dump("h2_0", H2[:], b_H2, [128, 1024])
                kb.op("act", lambda e: e.activation(out=H2b[:, :], in_=H2[:, :], func=AF.Identity), reads=[b_H2], writes=[b_H2b])

            def stage_b(tt):
                tsl = slice(tt * 128, (tt + 1) * 128)
                H2, b_H2 = H2s[tt % 2]
                H2b, b_H2b = H2bs[tt % 2]
                pt, ptb = self.bank16()
                for kc in range(8):
                    kb.op("pe", lambda e, kc=kc, pt=pt: e.transpose(pt[:, kc * 128:(kc + 1) * 128], H2b[:, kc * 128:(kc + 1) * 128], IDb),
                          reads=[b_H2b, bcb], writes=[ptb], inc=(kc == 7))
                kb.op("dve", lambda e, pt=pt: e.tensor_copy(H2T[:, :, :], pt[:, :].rearrange("p (k t) -> p k t", t=128)), reads=[ptb], writes=[b_H2T])
                o, b_o = OUT[tt % 2]
                for half in range(2):
                    hs = slice(half * 512, (half + 1) * 512)
                    ps, pb = self.bank()
                    for kc in range(8):
                        kb.op("pe", lambda e, kc=kc, ps=ps, hs=hs: e.matmul(ps[:, :], H2T[:, kc, :], Wpg[:, kc, hs], start=(kc == 0), stop=(kc == 7)),
                              reads=[b_H2T, b_Wpg], writes=[pb], inc=(kc == 7))
                    kb.op("dve", lambda e, ps=ps, hs=hs: e.tensor_tensor(SGT[:, hs], ps[:, :], self.rowp[:, 4, hs], ALU.add), reads=[pb, self.b_rowp], cw=[b_SGT])
                    kb.op("act", lambda e, hs=hs: e.activation(out=SGT[:, hs], in_=SGT[:, hs], func=AF.Sigmoid), reads=[b_SGT], cw=[b_SGT])
                    ps2, pb2 = self.bank()
                    for kc in range(2):
                        kb.op("pe", lambda e, kc=kc, ps2=ps2, hs=hs: e.matmul(ps2[:, :], PT[:, kc, tsl], Wple[:, kc, hs], start=(kc == 0), stop=(kc == 1)),
                              reads=[b_PT, b_Wple], writes=[pb2], inc=(kc == 1))
                    kb.op("dve", lambda e, ps2=ps2, hs=hs: e.tensor_tensor(o[:, hs], SGT[:, hs], ps2[:, :], ALU.mult), reads=[b_SGT, pb2], cw=[b_o])
                    kb.op("dve", lambda e, hs=hs: e.tensor_tensor(o[:, hs], o[:, hs], H2[:, hs], ALU.add), reads=[b_o, b_H2], cw=[b_o])
                kb.dma("sp", lambda e: e.dma_start(out=self.out[tsl, :], in_=o[:]), reads=[b_o])

            prefetch(0)
            prefetch(1)
            stage_a(0)
            for tt in range(16):
                if tt + 2 < 16:
                    prefetch(tt + 2)
                if tt + 1 < 16:
                    stage_a(tt + 1)
                stage_b(tt)


_PROG_CACHE = {}


def kernel(**inputs):
    inp = {k: np.asarray(v) for k, v in inputs.items()}
    sh = prep_shared(inp)
    in_maps = [dict(sh, **prep_core(inp, b)) for b in range(8)]
    if "nc" not in _PROG_CACHE:
        _PROG_CACHE["nc"] = Prog().build()
    nc = _PROG_CACHE["nc"]
    res = run_bass_kernel_spmd(nc, in_maps, core_ids=list(range(8)))
    out = np.stack([np.asarray(r["out"], dtype=np.float32) for r in res.results], axis=0)
    return out
```

```python
import numpy as np
from contextlib import ExitStack
import concourse.bass as bass
import concourse.mybir as mybir
from concourse.bass_utils import run_bass_kernel_spmd

F32 = mybir.dt.float32
BF16 = mybir.dt.bfloat16
U32 = mybir.dt.uint32
AF = mybir.ActivationFunctionType
ALU = mybir.AluOpType
AX = mybir.AxisListType

T = 2048
D = 1024
NE = 32
CAP = 384
NSLOT = NE * CAP
ALPHA = 2.0 ** 0.25
N_IN = 7184
O_XA, O_GA, O_Q, O_K, O_V, O_GO, O_GLR, O_MA, O_MB = 0, 1024, 2048, 2560, 3072, 4096, 5120, 5136, 6160


class Buf:
    __slots__ = ("name", "w", "r", "c")

    def __init__(self, name):
        self.name = name
        self.w = {}
        self.r = {}
        self.c = {}


class KB:
    def __init__(self, nc, es, n_dma_sems=24):
        self.nc = nc
        self.eng = dict(pe=nc.tensor, act=nc.scalar, dve=nc.vector, pool=nc.gpsimd, sp=nc.sync)
        self.esem = {}
        self.ecnt = {}
        self.seen = {}
        self.semobj = {}
        for n in self.eng:
            s = es.enter_context(nc.semaphore("es_" + n))
            self.esem[n] = s
            self.semobj[id(s)] = s
            self.ecnt[n] = 0
            self.seen[n] = {}
        self.dsem = []
        self.dcnt = []
        for i in range(2 * n_dma_sems):
            s = es.enter_context(nc.semaphore("ds_%d" % i))
            self.dsem.append(s)
            self.semobj[id(s)] = s
            self.dcnt.append(0)
        self.nds = n_dma_sems
        self.drr = {"pool": 0, "hw": 0}

    def _wait(self, en, toks):
        e = self.eng[en]
        seen = self.seen[en]
        for sid, val in toks.items():
            if en == "pe" and sid == id(self.esem["pe"]):
                continue
            if seen.get(sid, 0) >= val:
                continue
            e.wait_ge(self.semobj[sid], val)
            seen[sid] = val

    @staticmethod
    def _merge(dst, src):
        for k, v in src.items():
            if dst.get(k, 0) < v:
                dst[k] = v

    def _deps(self, reads, writes, cw=()):
        toks = {}
        for b in reads:
            self._merge(toks, b.w)
            self._merge(toks, b.c)
        for b in writes:
            self._merge(toks, b.w)
            self._merge(toks, b.c)
            self._merge(toks, b.r)
        for b in cw:
            self._merge(toks, b.w)
            self._merge(toks, b.r)
        return toks

    def _commit(self, tok, reads, writes, cw=()):
        for b in reads:
            self._merge(b.r, tok)
        for b in writes:
            b.w = dict(tok)
            b.r = {}
            b.c = {}
        for b in cw:
            self._merge(b.c, tok)

    disabled = False

    def op(self, en, fn, reads=(), writes=(), inc=True, cw=()):
        if self.disabled:
            return None
        self._wait(en, self._deps(reads, writes, cw))
        ins = fn(self.eng[en])
        s = self.esem[en]
        if inc:
            self.ecnt[en] += 1
            ins.then_inc(s, 1)
            tok = {id(s): self.ecnt[en]}
        else:
            tok = {id(s): self.ecnt[en] + 1}
        self._commit(tok, reads, writes, cw)
        return ins

    def dma(self, en, fn, reads=(), writes=(), cw=()):
        if self.disabled:
            return None
        kind = "pool" if en == "pool" else "hw"
        i = self.drr[kind] + (self.nds if kind == "pool" else 0)
        self.drr[kind] = (self.drr[kind] + 1) % self.nds
        s = self.dsem[i]
        toks = self._deps(reads, writes, cw)
        if self.dcnt[i] > 0:
            self._merge(toks, {id(s): self.dcnt[i]})
        self._wait(en, toks)
        ins = fn(self.eng[en])
        self.dcnt[i] += 16
        ins.then_inc(s, 16)
        tok = {id(s): self.dcnt[i]}
        self._commit(tok, reads, writes, cw)
        return ins

    def all_tokens(self):
        toks = {}
        for n in self.eng:
            if self.ecnt[n] > 0:
                toks[id(self.esem[n])] = self.ecnt[n]
        for i, s in enumerate(self.dsem):
            if self.dcnt[i] > 0:
                toks[id(s)] = self.dcnt[i]
        return toks

    def barrier(self, engines=None):
        toks = self.all_tokens()
        for n in (engines or list(self.eng)):
            own = id(self.esem[n])
            t = {k: v for k, v in toks.items() if not (n == "pe" and k == own)}
            self._wait(n, t)


def _consts():
    c = np.zeros((128, 5 * 128 + 64 + 4), np.float32)
    j = np.arange(128)
    same = (j[:, None] // 64) == (j[None, :] // 64)
    c[:, 0:128] = np.eye(128, dtype=np.float32)
    c[:, 128:256] = (same & (j[:, None] <= j[None, :]))
    c[:, 256:384] = (same & (j[:, None] > j[None, :]))
    c[:, 384:512] = (j[:, None] < j[None, :])
    c[:, 512:640] = 1.0
    c[:, 640:672] = np.arange(32)[None, :]
    c[:, 672:704] = (np.arange(32) * CAP)[None, :]
    c[:, 704] = (j < 64)
    c[:, 705] = (j >= 64)
    return c


def prep_shared(inp):
    f = lambda a: np.ascontiguousarray(a, dtype=np.float32)
    w_in = inp["w_in"][0]
    cols = np.concatenate([np.arange(0, O_GLR), np.arange(O_MA, N_IN)])
    wm = w_in[:, cols]
    sh = {}
    sh["w_in_t"] = f(wm.reshape(8, 128, 56, 128).transpose(2, 1, 0, 3).reshape(56, 128, 1024))
    sh["w_glr"] = f(w_in[:, O_GLR:O_GLR + 16].reshape(8, 128, 16).transpose(1, 0, 2).reshape(128, 128))
    chan = np.concatenate([inp["conv_w"][0], inp["conv_b"], inp["lru_b_r"], inp["lru_b_i"],
                           inp["lru_lambda"]], axis=0)
    sh["chanp"] = f(chan.reshape(8, 8, 128).transpose(2, 1, 0).reshape(128, 64))
    sh["lru_wr"] = f(inp["lru_w_r"][0].transpose(1, 0, 2).reshape(128, 1024))
    sh["lru_wi"] = f(inp["lru_w_i"][0].transpose(1, 0, 2).reshape(128, 1024))
    sh["gla_wg"] = f(inp["gla_w_gate"][0])
    sh["gla_bg"] = f(inp["gla_b_gate"])
    sh["gla_ng"] = f(inp["gla_norm_g"][0].reshape(2, 128).T)
    sh["w_out"] = f(inp["w_out"][0].reshape(8, 128, 1024).transpose(1, 0, 2).reshape(128, 8192))
    rows = np.concatenate([inp["ln1_g"], inp["ln1_b"], inp["ln2_g"], inp["ln2_b"],
                           inp["b_ple_gate"]], axis=0)
    sh["rowp"] = f(np.broadcast_to(rows.reshape(1, 5 * 1024), (128, 5 * 1024)))
    sh["w_router"] = f(inp["w_router"][0].reshape(8, 128, 32).transpose(1, 0, 2).reshape(128, 256))
    sh["b_router"] = f(inp["b_router"])
    sh["w_up"] = inp["w_up"][0]
    sh["b_up"] = f(inp["b_up"][0].reshape(32, 16, 128).transpose(2, 0, 1).reshape(128, 512))
    sh["w_down"] = inp["w_down"][0]
    sh["b_down"] = f(inp["b_down"][0])
    sh["w_ple"] = f(inp["w_ple"][0].reshape(2, 128, 1024).transpose(1, 0, 2).reshape(128, 2048))
    sh["w_pg"] = f(inp["w_ple_gate"][0].reshape(8, 128, 1024).transpose(1, 0, 2).reshape(128, 8192))
    sh["consts"] = _consts()
    return sh


def prep_core(inp, b):
    x = np.asarray(inp["x"][b], dtype=np.float32)
    p = np.asarray(inp["p"][0, b], dtype=np.float32)
    return {"x": np.ascontiguousarray(x), "xT": np.ascontiguousarray(x.T),
            "pT": np.ascontiguousarray(p.T)}


SHARED_SHAPES = {
    "w_in_t": [56, 128, 1024], "w_glr": [128, 128], "chanp": [128, 64], "lru_wr": [128, 1024],
    "lru_wi": [128, 1024], "gla_wg": [16, 512], "gla_bg": [1, 512], "gla_ng": [128, 2],
    "w_out": [128, 8192], "rowp": [128, 5120], "w_router": [128, 256], "b_router": [1, 32],
    "w_up": [32, 1024, 2048], "b_up": [128, 512], "w_down": [32, 1024, 1024], "b_down": [32, 1024],
    "w_ple": [128, 2048], "w_pg": [128, 8192], "consts": [128, 708],
}
CORE_SHAPES = {"x": [T, D], "xT": [D, T], "pT": [256, T]}


class StopBuild(Exception):
    pass


class Prog:
    ck_n = 0
    ck_stop = None

    def ck(self, label=""):
        self.ck_n += 1
        if self.ck_stop is not None and self.ck_n >= self.ck_stop:
            if not self.kb.disabled:
                print("STOP at checkpoint", self.ck_n, label)
            self.kb.disabled = True

    def __init__(self, dbg=(), stop_after=None):
        self.dbg = set(dbg)
        self.stop_after = stop_after
        self.nc = nc = bass.Bass("TRN2", target_bir_lowering=False)
        self.din = {}
        for n, s in list(SHARED_SHAPES.items()) + list(CORE_SHAPES.items()):
            self.din[n] = nc.dram_tensor(n, s, F32, kind="ExternalInput").ap()
        self.out = nc.dram_tensor("out", [T, D], F32, kind="ExternalOutput").ap()
        self.dbg_out = {}
        self.es = ExitStack()

    def dbg_tensor(self, name, shape):
        t = self.nc.dram_tensor("dbg_" + name, shape, F32, kind="ExternalOutput").ap()
        self.dbg_out[name] = t
        return t

    def sb(self, es, name, shape, dt=F32):
        return es.enter_context(self.nc.sbuf_tensor(name, shape, dt))

    def build(self):
        nc = self.nc
        with self.es as es:
            kb = self.kb = KB(nc, es)
            self.bound_reg = nc.gpsimd.to_reg(NSLOT - 1)
            self.cst = self.sb(es, "cst", [128, 708])
            self.b_cst = Buf("cst")
            kb.dma("sp", lambda e: e.dma_start(out=self.cst[:], in_=self.din["consts"][:, :]),
                   writes=[self.b_cst])
            self.cstb = self.sb(es, "cstb", [128, 708], BF16)
            self.b_cstb = Buf("cstb")
            kb.op("dve", lambda e: e.tensor_copy(self.cstb[:], self.cst[:]),
                  reads=[self.b_cst], writes=[self.b_cstb])
            self.GATES = self.sb(es, "GATES", [128, 16, 4])
            self.SLOTS = self.sb(es, "SLOTS", [128, 16, 4], U32)
            self.b_route = [Buf("route%d" % i) for i in range(16)]
            self.Xg = nc.dram_tensor("Xg", [NSLOT, D], BF16).ap()
            self.Yg = nc.dram_tensor("Yg", [NSLOT, D], F32).ap()
            self.H1d = nc.dram_tensor("H1d", [T, D], F32).ap()
            self.b_Xg, self.b_Yg, self.b_H1d = Buf("Xg"), Buf("Yg"), Buf("H1d")
            self.b_Xgz = Buf("Xgz")
            self.zero_xg(es)
            with ExitStack() as es_y:
                self.yT = self.sb(es_y, "yT", [128, 8, T], BF16)
                self.b_yT = [Buf("yT%d" % g) for g in range(8)]
                with ExitStack() as es1:
                    self.alloc_psum(es1, 8, 0)
                    self.XT = self.sb(es1, "XT", [128, 8, T], BF16)
                    self.b_XT = Buf("XT")
                    xT = self.din["xT"].rearrange("(kc p) t -> p kc t", p=128)
                    for kc in range(8):
                        kb.dma("pool", lambda e, kc=kc: e.dma_start(out=self.XT[:, kc, :], in_=xT[:, kc, :]),
                               cw=[self.b_XT])
                    if not getattr(self, "skip_lru", False):
                        self.phase_lru(es1)
                    else:
                        kb.op("dve", lambda e: e.memset(self.yT[:], 0.0), writes=self.b_yT)
                    if self.stop_after == "lru":
                        return self.finish()
                    kb.barrier()
                    self.phase_gla(es1)
                    if self.stop_after == "gla":
                        return self.dump_yT()
                kb.barrier()
                with ExitStack() as esw:
                    self.WU = [(self.sb(esw, "M_wu%d" % i, [128, 8, 2048], BF16), Buf("M_wu%d" % i)) for i in range(2)]
                    self.WD = [(self.sb(esw, "M_wd%d" % i, [128, 8, 1024], BF16), Buf("M_wd%d" % i)) for i in range(2)]
                    self.phase_outproj()
                    if self.stop_after == "outproj":
                        return self.finish()
                    kb.barrier()
                    self.phase_moe()
                    if self.stop_after == "moe":
                        return self.finish()
            kb.barrier()
            self.phase_final()
        return self.finish()

    def load_expert_w(self, ex):
        kb = self.kb
        sl = ex % 2
        WU, WD = self.WU, self.WD
        wu = self.din["w_up"][ex].rearrange("(kc p) f -> p kc f", p=128)
        wd = self.din["w_down"][ex].rearrange("(kc p) f -> p kc f", p=128)
        for q in range(4):
            kb.dma("pool", lambda e, q=q: e.dma_start(out=WU[sl][0][:, 2 * q:2 * q + 2, :], in_=wu[:, 2 * q:2 * q + 2, :]), cw=[WU[sl][1]])
        for q in range(2):
            kb.dma("pool", lambda e, q=q: e.dma_start(out=WD[sl][0][:, 4 * q:4 * q + 4, :], in_=wd[:, 4 * q:4 * q + 4, :]), cw=[WD[sl][1]])

    def alloc_psum(self, es, n32, n16):
        nc = self.nc
        self.ps = [es.enter_context(nc.psum_tensor("ps%d_%d" % (i, self.ps_gen), [128, 512], F32)) for i in range(n32)]
        self.psb = [Buf("ps%d" % i) for i in range(n32)]
        self.pst = [es.enter_context(nc.psum_tensor("pst%d_%d" % (i, self.ps_gen), [128, 1024], BF16)) for i in range(n16)]
        self.pstb = [Buf("pst%d" % i) for i in range(n16)]
        self.ps_rr = 0
        self.pst_rr = 0
        self.ps_gen += 1

    def zero_xg(self, es):
        kb = self.kb
        z = self.sb(es, "zeros", [128, 2, 1024], BF16)
        bz = Buf("zeros")
        kb.op("dve", lambda e: e.memset(z[:], 0.0), writes=[bz])
        xg = self.Xg.rearrange("(n p) d -> p n d", p=128)
        for i in range(NSLOT // 128 // 2):
            kb.dma("sp", lambda e, i=i: e.dma_start(out=xg[:, i * 2:(i + 1) * 2, :], in_=z[:]), reads=[bz], cw=[self.b_Xgz])

    ps_gen = 0

    def bank(self):
        i = self.ps_rr
        self.ps_rr = (self.ps_rr + 1) % len(self.ps)
        return self.ps[i], self.psb[i]

    def bank16(self):
        i = self.pst_rr
        self.pst_rr = (self.pst_rr + 1) % len(self.pst)
        return self.pst[i], self.pstb[i]

    def finish(self):
        kb = self.kb
        kb.disabled = False
        kb.barrier(["sp"])
        return self.nc

    def dump_yT(self):
        kb = self.kb
        kb.barrier()
        with ExitStack() as es:
            tmp = self.sb(es, "dump_tmp", [128, 8, T])
            b = Buf("dump_tmp")
            kb.op("dve", lambda e: e.tensor_copy(tmp[:], self.yT[:]), reads=self.b_yT, writes=[b])
            t = self.dbg_tensor("yT", [128, 8, T])
            kb.dma("sp", lambda e: e.dma_start(out=t, in_=tmp[:]), reads=[b])
            return self.finish()

    def dump(self, name, sb_ap, buf, shape):
        if name not in self.dbg:
            return
        t = self.dbg_tensor(name, shape)
        self.kb.dma("sp", lambda e: e.dma_start(out=t, in_=sb_ap), reads=[buf])

    def inproj_fm(self, wt, wb, ncols, tg, evac):
        kb = self.kb
        ps, pb = self.bank()
        for kc in range(8):
            kb.op("pe", lambda e, kc=kc: e.matmul(ps[0:ncols, :], wt[:, kc * ncols:(kc + 1) * ncols],
                                                   self.XT[:, kc, tg * 512:(tg + 1) * 512],
                                                   start=(kc == 0), stop=(kc == 7)),
                  reads=[wb, self.b_XT], writes=[pb], inc=(kc == 7))
        evac(ps, pb)

    def load_w(self, grp):
        i = self.w_rr
        self.w_rr = (self.w_rr + 1) % len(self.wring)
        wt, wb = self.wring[i], self.wringb[i]
        self.kb.dma("pool", lambda e: e.dma_start(out=wt[:], in_=self.din["w_in_t"][grp, :, :]), writes=[wb])
        return wt, wb

    def phase_lru(self, es1):
        nc, kb = self.nc, self.kb
        with ExitStack() as es:
            NW = 6
            self.wring = [self.sb(es, "wr%d" % i, [128, 1024], BF16) for i in range(NW)]
            self.wringb = [Buf("wr%d" % i) for i in range(NW)]
            self.w_rr = 0
            chan = self.sb(es, "chan", [128, 8, 8])
            b_chan = Buf("chan")
            kb.dma("sp", lambda e: e.dma_start(out=chan[:], in_=self.din["chanp"].rearrange("p (g k) -> p g k", k=8)),
                   writes=[b_chan])
            wr = self.sb(es, "lwr", [128, 8, 128], BF16)
            wi = self.sb(es, "lwi", [128, 8, 128], BF16)
            b_wr, b_wi = Buf("lwr"), Buf("lwi")
            kb.dma("pool", lambda e: e.dma_start(out=wr[:], in_=self.din["lru_wr"].rearrange("p (g d) -> p g d", d=128)), writes=[b_wr])
            kb.dma("pool", lambda e: e.dma_start(out=wi[:], in_=self.din["lru_wi"].rearrange("p (g d) -> p g d", d=128)), writes=[b_wi])
            sc = self.sb(es, "lsc", [128, 8, 4])
            b_sc = Buf("lsc")
            kb.op("act", lambda e: e.activation(out=sc[:, :, 0], in_=chan[:, :, 7], func=AF.Exp, scale=-1.0),
                  reads=[b_chan], writes=[b_sc])
            kb.op("act", lambda e: e.activation(out=sc[:, :, 1], in_=sc[:, :, 0], func=AF.Ln, bias=1.0),
                  reads=[b_sc], writes=[b_sc])
            kb.op("dve", lambda e: e.tensor_scalar_mul(sc[:, :, 2], sc[:, :, 1], -8.0), reads=[b_sc], writes=[b_sc])
            kb.op("dve", lambda e: e.tensor_scalar_mul(sc[:, :, 3], sc[:, :, 1], -16.0), reads=[b_sc], writes=[b_sc])

            def mk(name, shape, dt=F32):
                return self.sb(es, "L_" + name, shape, dt), [Buf("L_%s_%d" % (name, i)) for i in range(4)]
            xa, b_xa = mk("xa", [128, T + 3], BF16)
            DG = self.sb(es, "L_dg", [128, 8, 4, 128], BF16)
            b_DG = Buf("L_dg")
            for g_ in range(8):
                for k_ in range(4):
                    kb.op("dve", lambda e, g_=g_, k_=k_: e.tensor_scalar_mul(DG[:, g_, k_, :], self.cstb[:, 0:128], chan[:, g_, k_:k_ + 1]),
                          reads=[self.b_cstb, b_chan], cw=[b_DG])
            xcb, b_xcb = mk("xcb", [128, T], BF16)
            xc, b_xc = mk("xc", [128, T])
            r, b_r = mk("r", [128, T])
            ii, b_ii = mk("i", [128, T])
            aa, b_aa = mk("a", [128, T])
            mm, b_mm = mk("m", [128, T])
            h, b_h = mk("h", [128, T])
            ga, b_ga = mk("ga", [128, T])
            t1, b_t1 = mk("t1", [128, T])
            t2, b_t2 = mk("t2", [128, T])
            b_pad = Buf("xa_pad")
            kb.op("dve", lambda e: e.memset(xa[:, 0:3], 0.0), writes=[b_pad])
            TG = range(4)

            def cs(tg, off=0):
                return slice(off + tg * 512, off + (tg + 1) * 512)

            for g in range(8):
                w_xa, wb_xa = self.load_w(g)
                w_ga, wb_ga = self.load_w(8 + g)
                w_ma, wb_ma = self.load_w(40 + g)
                for tg in TG:
                    self.inproj_fm(w_xa, wb_xa, 128, tg, lambda ps, pb, tg=tg: kb.op(
                        "act", lambda e: e.activation(out=xa[:, cs(tg, 3)], in_=ps[:, :], func=AF.Copy), reads=[pb], writes=[b_xa[tg]]))
                for tg in TG:
                    self.inproj_fm(w_ga, wb_ga, 128, tg, lambda ps, pb, tg=tg: kb.op(
                        "act", lambda e: e.activation(out=ga[:, cs(tg)], in_=ps[:, :], func=AF.Copy), reads=[pb], writes=[b_ga[tg]]))
                for tg in TG:
                    self.inproj_fm(w_ma, wb_ma, 128, tg, lambda ps, pb, tg=tg: kb.op(
                        "act", lambda e: e.activation(out=t2[:, cs(tg)], in_=ps[:, :], func=AF.Sigmoid), reads=[pb], writes=[b_t2[tg]]))
                for tg in TG:
                    kb.op("dve", lambda e, tg=tg: e.tensor_tensor(t1[:, cs(tg)], ga[:, cs(tg)], ga[:, cs(tg)], ALU.mult), reads=[b_ga[tg]], writes=[b_t1[tg]])
                    kb.op("dve", lambda e, tg=tg: e.tensor_scalar(t1[:, cs(tg)], t1[:, cs(tg)], 0.044715, 1.0, ALU.mult, ALU.add), reads=[b_t1[tg]], writes=[b_t1[tg]])
                    kb.op("dve", lambda e, tg=tg: e.tensor_tensor(t1[:, cs(tg)], t1[:, cs(tg)], ga[:, cs(tg)], ALU.mult), reads=[b_t1[tg], b_ga[tg]], writes=[b_t1[tg]])
                for tg in TG:
                    prev = [b_xa[tg - 1]] if tg > 0 else [b_pad]
                    ps, pb = self.bank()
                    for k in range(4):
                        kb.op("pe", lambda e, k=k, tg=tg, ps=ps: e.matmul(ps[:, :], DG[:, g, k, :], xa[:, cs(tg, k)], start=(k == 0), stop=(k == 3)),
                              reads=[b_DG, b_xa[tg]] + prev, writes=[pb], inc=(k == 3))
                    kb.op("act", lambda e, tg=tg, ps=ps: e.activation(out=xc[:, cs(tg)], in_=ps[:, :], func=AF.Identity, bias=chan[:, g, 4:5]),
                          reads=[pb, b_chan], writes=[b_xc[tg]])
                    kb.op("dve", lambda e, tg=tg: e.tensor_copy(xcb[:, cs(tg)], xc[:, cs(tg)]), reads=[b_xc[tg]], writes=[b_xcb[tg]])
                if g == 0:
                    self.dump("xc0", xc[:, :], b_xc[3], [128, T])
                for (wg, bwg, dst, b_dst, bi) in ((wr, b_wr, r, b_r, 5), (wi, b_wi, ii, b_ii, 6)):
                    for tg in TG:
                        ps, pb = self.bank()
                        kb.op("pe", lambda e, tg=tg, ps=ps, wg=wg: e.matmul(ps[:, :], wg[:, g, :], xcb[:, cs(tg)], start=True, stop=True),
                              reads=[bwg, b_xcb[tg]], writes=[pb])
                        kb.op("act", lambda e, tg=tg, ps=ps, dst=dst, bi=bi: e.activation(
                            out=dst[:, cs(tg)], in_=ps[:, :], func=AF.Sigmoid, bias=chan[:, g, bi:bi + 1]),
                            reads=[pb, b_chan], writes=[b_dst[tg]])
                for tg in TG:
                    kb.op("act", lambda e, tg=tg: e.activation(out=aa[:, cs(tg)], in_=r[:, cs(tg)], func=AF.Exp, scale=sc[:, g, 2:3]),
                          reads=[b_r[tg], b_sc], writes=[b_aa[tg]])
                for tg in TG:
                    kb.op("act", lambda e, tg=tg: e.activation(out=mm[:, cs(tg)], in_=r[:, cs(tg)], func=AF.Exp, scale=sc[:, g, 3:4]),
                          reads=[b_r[tg], b_sc], writes=[b_mm[tg]])
                for tg in TG:
                    kb.op("act", lambda e, tg=tg: e.activation(out=mm[:, cs(tg)], in_=mm[:, cs(tg)], func=AF.Ln, scale=-1.0, bias=1.0),
                          reads=[b_mm[tg]], writes=[b_mm[tg]])
                for tg in TG:
                    kb.op("act", lambda e, tg=tg: e.activation(out=mm[:, cs(tg)], in_=mm[:, cs(tg)], func=AF.Exp, scale=0.5),
                          reads=[b_mm[tg]], writes=[b_mm[tg]])
                for tg in TG:
                    kb.op("dve", lambda e, tg=tg: e.tensor_tensor(mm[:, cs(tg)], mm[:, cs(tg)], ii[:, cs(tg)], ALU.mult), reads=[b_mm[tg], b_ii[tg]], writes=[b_mm[tg]])
                    kb.op("dve", lambda e, tg=tg: e.tensor_tensor(mm[:, cs(tg)], mm[:, cs(tg)], xc[:, cs(tg)], ALU.mult), reads=[b_mm[tg], b_xc[tg]], writes=[b_mm[tg]])
                for tg in TG:
                    init = 0.0 if tg == 0 else h[:, tg * 512 - 1:tg * 512]
                    kb.op("dve", lambda e, tg=tg, init=init: e.tensor_tensor_scan(h[:, cs(tg)], aa[:, cs(tg)], mm[:, cs(tg)], init, ALU.mult, ALU.add),
                          reads=[b_aa[tg], b_mm[tg]] + ([b_h[tg - 1]] if tg > 0 else []), writes=[b_h[tg]])
                if g == 0:
                    self.dump("h0", h[:, :], b_h[3], [128, T])
                for tg in TG:
                    kb.op("act", lambda e, tg=tg: e.activation(out=t1[:, cs(tg)], in_=t1[:, cs(tg)], func=AF.Sigmoid, scale=1.5957691216057308),
                          reads=[b_t1[tg]], writes=[b_t1[tg]])
                for tg in TG:
                    kb.op("dve", lambda e, tg=tg: e.tensor_tensor(t1[:, cs(tg)], t1[:, cs(tg)], ga[:, cs(tg)], ALU.mult), reads=[b_t1[tg], b_ga[tg]], writes=[b_t1[tg]])
                    kb.op("dve", lambda e, tg=tg: e.tensor_tensor(t1[:, cs(tg)], t1[:, cs(tg)], h[:, cs(tg)], ALU.mult), reads=[b_t1[tg], b_h[tg]], writes=[b_t1[tg]])
                    kb.op("dve", lambda e, tg=tg: e.tensor_tensor(self.yT[:, g, cs(tg)], t1[:, cs(tg)], t2[:, cs(tg)], ALU.mult),
                          reads=[b_t1[tg], b_t2[tg]], cw=[self.b_yT[g]])

    def phase_gla(self, es1):
        nc, kb = self.nc, self.kb
        cst = self.cstb
        TRI, UU, ONES = cst[:, 128:256], cst[:, 256:384], cst[:, 512:640]
        TRI32 = self.cst[:, 128:256]
        bc = self.b_cstb
        with ExitStack() as es:
            NW = 8
            self.wring = [self.sb(es, "gw%d" % i, [128, 1024], BF16) for i in range(NW)]
            self.wringb = [Buf("gw%d" % i) for i in range(NW)]
            self.w_rr = 0
            wglr = self.sb(es, "wglr", [128, 128], BF16)
            b_wglr = Buf("wglr")
            kb.dma("pool", lambda e: e.dma_start(out=wglr[:], in_=self.din["w_glr"][:, :]), writes=[b_wglr])
            wg = self.sb(es, "wg", [16, 512], BF16)
            bg = self.sb(es, "bg", [1, 512], BF16)
            ng = self.sb(es, "ng", [128, 2])
            b_wg, b_bg, b_ng = Buf("wg"), Buf("bg"), Buf("ng")
            kb.dma("pool", lambda e: e.dma_start(out=wg[:], in_=self.din["gla_wg"][:, :]), writes=[b_wg])
            kb.dma("pool", lambda e: e.dma_start(out=bg[:], in_=self.din["gla_bg"][:, :]), writes=[b_bg])
            kb.dma("sp", lambda e: e.dma_start(out=ng[:], in_=self.din["gla_ng"][:, :]), writes=[b_ng])
            glrT = self.sb(es, "glrT", [16, T], BF16)
            b_glrT = Buf("glrT")
            for tg in range(4):
                self.inproj_fm(wglr, b_wglr, 16, tg, lambda ps, pb, tg=tg: kb.op(
                    "act", lambda e: e.activation(out=glrT[:, tg * 512:(tg + 1) * 512], in_=ps[0:16, :], func=AF.Copy),
                    reads=[pb], cw=[b_glrT]))

            self.ck("glrT")

            def mk(name, shape, dt=F32):
                return self.sb(es, "G_" + name, shape, dt), Buf("G_" + name)
            QT, b_QT = mk("QT", [128, T], BF16)
            KT, b_KT = mk("KT", [128, T], BF16)
            KD0, b_KD0 = mk("KD0", [128, 16, 128], BF16)
            KD1, b_KD1 = mk("KD1", [128, 16, 128], BF16)
            V, b_V = mk("V", [128, 16, 256], BF16)
            OT, b_OT = mk("OT", [128, 2, T])
            EB, b_EB = mk("EB", [128, 32])
            Gsp, b_Gsp = mk("Gsp", [128, 4, 128], BF16)
            Gz, b_Gz = mk("Gz", [128, 4, 128])
            EQ, b_EQ = mk("EQ", [128, 512])
            EK, b_EK = mk("EK", [128, 512])
            ED, b_ED = mk("ED", [128, 4, 128])
            STs = [mk("ST%d" % i, [128, 128], BF16) for i in range(2)]
            Sb = [mk("S%d" % i, [128, 256]) for i in range(4)]
            Sbb = [mk("Sb%d" % i, [128, 256], BF16) for i in range(4)]
            SQ = [mk("SQ%d" % i, [128, 512], BF16) for i in range(2)]
            RIf, b_RIf = mk("RIf", [128, T])
            SG, b_SG = mk("SG", [128, 512])
            SM, b_SM = mk("SM", [128, 512])
            TT, b_TT = mk("TT", [128, 512])

            HW = {}

            def h1(hd, tg):
                if tg == 0:
                    HW[hd] = dict(q=self.load_w(16 + hd), k=self.load_w(20 + hd), v=[self.load_w(24 + 2 * hd + j) for j in range(2)])
                w_q, wb_q = HW[hd]['q']
                w_k, wb_k = HW[hd]['k']
                w_v = HW[hd]['v']
                ps, pb = self.bank()
                for j in range(4):
                    tt = tg * 4 + j
                    kb.op("pe", lambda e, j=j, tt=tt, ps=ps: e.matmul(ps[:, j * 128:(j + 1) * 128], glrT[0:16, tt * 128:(tt + 1) * 128],
                                                                  wg[0:16, hd * 128:(hd + 1) * 128], start=True, stop=False),
                          reads=[b_glrT, b_wg], writes=[pb], inc=False)
                    kb.op("pe", lambda e, j=j, ps=ps: e.matmul(ps[:, j * 128:(j + 1) * 128], cst[0:1, 512:640],
                                                           bg[0:1, hd * 128:(hd + 1) * 128], start=False, stop=True),
                          reads=[bc, b_bg], writes=[pb], inc=(j == 3))
                kb.op("act", lambda e, ps=ps: e.activation(out=Gz[:, :, :], in_=ps[:, :].rearrange("p (j k) -> p j k", k=128),
                                                          func=AF.Exp, scale=-1.0), reads=[pb], writes=[b_Gz])
                kb.op("act", lambda e: e.activation(out=Gsp[:, :, :], in_=Gz[:, :, :], func=AF.Ln, bias=1.0),
                      reads=[b_Gz], writes=[b_Gsp])
                self.ck("z/Gsp")
                ps_c, pb_c = self.bank()
                ps_r, pb_r = self.bank()
                for j in range(4):
                    kb.op("pe", lambda e, j=j, ps_c=ps_c: e.matmul(ps_c[:, j * 128:(j + 1) * 128], Gsp[:, j, :], TRI, start=True, stop=True),
                          reads=[b_Gsp, bc], writes=[pb_c], inc=False)
                    kb.op("pe", lambda e, j=j, ps_r=ps_r: e.matmul(ps_r[:, j * 128:(j + 1) * 128], UU, Gsp[:, j, :], start=True, stop=True),
                          reads=[b_Gsp, bc], writes=[pb_r], inc=(j == 3))
                kb.op("act", lambda e, ps_c=ps_c: e.activation(out=EQ[:, :], in_=ps_c[:, :], func=AF.Exp, scale=-1.0 / 16), reads=[pb_c], writes=[b_EQ])
                kb.op("act", lambda e, ps_c=ps_c: e.activation(out=EK[:, :], in_=ps_c[:, :], func=AF.Exp, scale=1.0 / 16), reads=[pb_c], writes=[b_EK])
                kb.op("act", lambda e, ps_r=ps_r: e.activation(out=ED[:, :, :], in_=ps_r[:, :].rearrange("p (j k) -> p j k", k=128),
                                                            func=AF.Exp, scale=-1.0 / 16), reads=[pb_r], writes=[b_ED])
                kb.op("dve", lambda e, tg=tg: e.tensor_copy(EB[:, tg * 8:(tg + 1) * 8], EQ[:, 63:512:64]), reads=[b_EQ], cw=[b_EB])
                self.ck("cs/rev/E")
                self.inproj_fm(w_q, wb_q, 128, tg, lambda ps, pb, tg=tg: kb.op(
                    "dve", lambda e: e.scalar_tensor_tensor(QT[:, tg * 512:(tg + 1) * 512], ps[:, :], 128.0 ** -0.5, EQ[:, :], ALU.mult, ALU.mult),
                    reads=[pb, b_EQ], cw=[b_QT]))
                self.inproj_fm(w_k, wb_k, 128, tg, lambda ps, pb, tg=tg: kb.op(
                    "dve", lambda e: e.tensor_tensor(KT[:, tg * 512:(tg + 1) * 512], ps[:, :], EK[:, :], ALU.mult),
                    reads=[pb, b_EK], cw=[b_KT]))
                self.ck("qk fm")
                ps, pb = self.bank()
                for j in range(4):
                    tt = tg * 4 + j
                    for kc in range(8):
                        kb.op("pe", lambda e, j=j, tt=tt, kc=kc, ps=ps: e.matmul(ps[:, j * 128:(j + 1) * 128], self.XT[:, kc, tt * 128:(tt + 1) * 128],
                                                                             w_k[:, kc * 128:(kc + 1) * 128], start=(kc == 0), stop=(kc == 7)),
                              reads=[self.b_XT, wb_k], writes=[pb], inc=(j == 3 and kc == 7))
                for KDm, b_KDm, mcol in ((KD0, b_KD0, 704), (KD1, b_KD1, 705)):
                    kb.op("dve", lambda e, ps=ps, tg=tg, KDm=KDm, mcol=mcol: e.scalar_tensor_tensor(
                        KDm[:, tg * 4:(tg + 1) * 4, :], ps[:, :].rearrange("p (j k) -> p j k", k=128), self.cst[:, mcol:mcol + 1],
                        ED[:, :, :], ALU.mult, ALU.mult), reads=[pb, b_ED, self.b_cst], cw=[b_KDm])
                self.ck("kd")
                for jj in range(2):
                    ps, pb = self.bank()
                    for j2 in range(2):
                        tt = tg * 4 + jj * 2 + j2
                        for half in range(2):
                            wv, wbv = w_v[half]
                            for kc in range(8):
                                kb.op("pe", lambda e, j2=j2, tt=tt, kc=kc, ps=ps, half=half, wv=wv: e.matmul(
                                    ps[:, j2 * 256 + half * 128:j2 * 256 + (half + 1) * 128], self.XT[:, kc, tt * 128:(tt + 1) * 128],
                                    wv[:, kc * 128:(kc + 1) * 128], start=(kc == 0), stop=(kc == 7)),
                                    reads=[self.b_XT, wbv], writes=[pb], inc=(j2 == 1 and half == 1 and kc == 7))
                    t0 = tg * 4 + jj * 2
                    kb.op("dve", lambda e, ps=ps, t0=t0: e.tensor_copy(V[:, t0:t0 + 2, :], ps[:, :].rearrange("p (j v) -> p j v", v=256)),
                          reads=[pb], cw=[b_V])
                self.ck("v")

            def h2(hd):
                kb.op("dve", lambda e: e.memset(Sb[0][0][:, :], 0.0), writes=[Sb[0][1]])
                kb.op("dve", lambda e: e.memset(Sbb[0][0][:, :], 0.0), writes=[Sbb[0][1]])
                for tt in range(16):
                    c0, c1 = 2 * tt, 2 * tt + 1
                    ps_st, pb_st = self.ps[tt % 2], self.psb[tt % 2]
                    st, b_st = STs[tt % 2]
                    kb.op("pe", lambda e: e.matmul(ps_st[:, 0:128], KT[:, tt * 128:(tt + 1) * 128], QT[:, tt * 128:(tt + 1) * 128], start=True, stop=True),
                          reads=[b_KT, b_QT], writes=[pb_st])
                    kb.op("dve", lambda e: e.tensor_tensor(st[:, :], ps_st[:, 0:128], TRI32, ALU.mult), reads=[pb_st, self.b_cst], writes=[b_st])
                    ps_kv, pb_kv = self.ps[2 + tt % 2], self.psb[2 + tt % 2]
                    kb.op("pe", lambda e: e.matmul(ps_kv[:, 0:256], KD0[:, tt, :], V[:, tt, :], start=True, stop=True),
                          reads=[b_KD0, b_V], writes=[pb_kv], inc=False)
                    kb.op("pe", lambda e: e.matmul(ps_kv[:, 256:512], KD1[:, tt, :], V[:, tt, :], start=True, stop=True),
                          reads=[b_KD1, b_V], writes=[pb_kv])
                    self.ck("st/kv")
                    grp = (tt // 4) % 2
                    col = (tt % 4) * 128
                    for vc in range(2):
                        pso, pbo = self.ps[4 + 2 * grp + vc], self.psb[4 + 2 * grp + vc]
                        kb.op("pe", lambda e, vc=vc, pso=pso: e.matmul(pso[:, col:col + 128], V[:, tt, vc * 128:(vc + 1) * 128], st[:, :], start=True, stop=False),
                              reads=[b_V, b_st], writes=[pbo], inc=False)
                        kb.op("pe", lambda e, vc=vc, pso=pso: e.matmul(pso[:, col:col + 64], Sbb[c0 % 4][0][:, vc * 128:(vc + 1) * 128], QT[:, c0 * 64:(c0 + 1) * 64],
                                                                    start=False, stop=False), reads=[Sbb[c0 % 4][1], b_QT], writes=[pbo], inc=False)
                        if vc == 0:
                            kb.op("dve", lambda e: e.scalar_tensor_tensor(Sb[c1 % 4][0][:, :], Sb[c0 % 4][0][:, :], EB[:, c0:c0 + 1], ps_kv[:, 0:256], ALU.mult, ALU.add),
                                  reads=[Sb[c0 % 4][1], b_EB, pb_kv], writes=[Sb[c1 % 4][1]])
                            kb.op("act", lambda e: e.activation(out=Sbb[c1 % 4][0][:, :], in_=Sb[c1 % 4][0][:, :], func=AF.Copy),
                                  reads=[Sb[c1 % 4][1]], writes=[Sbb[c1 % 4][1]])
                        kb.op("pe", lambda e, vc=vc, pso=pso: e.matmul(pso[:, col + 64:col + 128], Sbb[c1 % 4][0][:, vc * 128:(vc + 1) * 128], QT[:, c1 * 64:(c1 + 1) * 64],
                                                                    start=False, stop=True), reads=[Sbb[c1 % 4][1], b_QT], writes=[pbo], inc=True)
                    kb.op("dve", lambda e: e.scalar_tensor_tensor(Sb[(c1 + 1) % 4][0][:, :], Sb[c1 % 4][0][:, :], EB[:, c1:c1 + 1], ps_kv[:, 256:512], ALU.mult, ALU.add),
                          reads=[Sb[c1 % 4][1], b_EB, pb_kv], writes=[Sb[(c1 + 1) % 4][1]])
                    kb.op("act", lambda e: e.activation(out=Sbb[(c1 + 1) % 4][0][:, :], in_=Sb[(c1 + 1) % 4][0][:, :], func=AF.Copy),
                          reads=[Sb[(c1 + 1) % 4][1]], writes=[Sbb[(c1 + 1) % 4][1]])
                    self.ck("o tile")
                    if tt % 4 == 3:
                        tg = tt // 4
                        for vc in range(2):
                            pso, pbo = self.ps[4 + 2 * grp + vc], self.psb[4 + 2 * grp + vc]
                            kb.op("act", lambda e, vc=vc, pso=pso, tg=tg: e.activation(out=OT[:, vc, tg * 512:(tg + 1) * 512], in_=pso[:, :], func=AF.Copy),
                                  reads=[pbo], cw=[b_OT])
                if hd == 0:
                    self.dump("o_raw0", OT[:, 0, :], b_OT, [128, T])
                for tg in range(4):
                    sl = slice(tg * 512, (tg + 1) * 512)
                    for vc in range(2):
                        kb.op("act", lambda e, vc=vc, sl=sl: e.activation(out=SQ[vc][0][:, :], in_=OT[:, vc, sl], func=AF.Square), reads=[b_OT], writes=[SQ[vc][1]])
                    ps, pb = self.bank()
                    for vc in range(2):
                        kb.op("pe", lambda e, vc=vc, ps=ps: e.matmul(ps[:, :], ONES, SQ[vc][0][:, :], start=(vc == 0), stop=(vc == 1)),
                              reads=[bc, SQ[vc][1]], writes=[pb], inc=(vc == 1))
                    kb.op("act", lambda e, ps=ps, sl=sl: e.activation(out=RIf[:, sl], in_=ps[:, :], func=AF.Sqrt, scale=1.0 / 256, bias=1e-5), reads=[pb], cw=[b_RIf])
                kb.op("dve", lambda e: e.reciprocal(RIf[:, :], RIf[:, :]), reads=[b_RIf], writes=[b_RIf])

            def h3(hd, tg):
                if tg == 0:
                    HW[hd]['go'] = [self.load_w(32 + 2 * hd + j) for j in range(2)]
                    HW[hd]['mb'] = [self.load_w(48 + 2 * hd + j) for j in range(2)]
                w_go, w_mb = HW[hd]['go'], HW[hd]['mb']
                sl = slice(tg * 512, (tg + 1) * 512)
                for vc in range(2):
                    g = hd * 2 + vc
                    go_ps = {}
                    self.inproj_fm(w_go[vc][0], w_go[vc][1], 128, tg, lambda ps, pb: (go_ps.update(ps=ps, pb=pb), kb.op(
                        "act", lambda e: e.activation(out=SG[:, :], in_=ps[:, :], func=AF.Sigmoid), reads=[pb], writes=[b_SG])))
                    self.inproj_fm(w_mb[vc][0], w_mb[vc][1], 128, tg, lambda ps, pb: kb.op(
                        "act", lambda e: e.activation(out=SM[:, :], in_=ps[:, :], func=AF.Sigmoid), reads=[pb], writes=[b_SM]))
                    kb.op("dve", lambda e, vc=vc, sl=sl: e.scalar_tensor_tensor(TT[:, :], OT[:, vc, sl], ng[:, vc:vc + 1], RIf[:, sl], ALU.mult, ALU.mult),
                          reads=[b_OT, b_ng, b_RIf], writes=[b_TT])
                    kb.op("dve", lambda e: e.tensor_tensor(TT[:, :], TT[:, :], SG[:, :], ALU.mult), reads=[b_TT, b_SG], writes=[b_TT])
                    kb.op("dve", lambda e: e.tensor_tensor(TT[:, :], TT[:, :], go_ps["ps"][:, :], ALU.mult), reads=[b_TT, b_SG, go_ps["pb"]], writes=[b_TT])
                    kb.op("dve", lambda e: e.tensor_tensor(TT[:, :], TT[:, :], SM[:, :], ALU.mult), reads=[b_TT, b_SM], writes=[b_TT])
                    kb.op("dve", lambda e, g=g, sl=sl: e.tensor_tensor(self.yT[:, g, sl], TT[:, :], self.yT[:, g, sl], ALU.add),
                          reads=[b_TT, self.b_yT[g]], writes=[self.b_yT[g]])


            for hd in range(4):
                for tg in range(4):
                    h1(hd, tg)
                    if hd > 0:
                        h3(hd - 1, tg)
                h2(hd)
            for tg in range(4):
                h3(3, tg)

    def layer_norm(self, es_tmp, R, b_R, grow, brow, OUT, b_OUT, tag):
        kb = self.kb
        st = self.ln_st
        kb.op("dve", lambda e: e.bn_stats(st["stats"][:, 0, :], R[:, 0:512]), reads=[b_R], writes=[st["b"]])
        kb.op("dve", lambda e: e.bn_stats(st["stats"][:, 1, :], R[:, 512:1024]), reads=[b_R], writes=[st["b"]])
        kb.op("dve", lambda e: e.bn_aggr(st["mv"][:, :], st["stats"][:, :, :].rearrange("p a b -> p (a b)")), reads=[st["b"]], writes=[st["b"]])
        kb.op("act", lambda e: e.activation(out=st["rs"][:, 0:1], in_=st["mv"][:, 1:2], func=AF.Ln, bias=1e-5), reads=[st["b"]], writes=[st["b2"]])
        kb.op("act", lambda e: e.activation(out=st["rs"][:, 0:1], in_=st["rs"][:, 0:1], func=AF.Exp, scale=-0.5), reads=[st["b2"]], writes=[st["b2"]])
        kb.op("dve", lambda e: e.scalar_tensor_tensor(st["rs"][:, 1:2], st["mv"][:, 0:1], -1.0, st["rs"][:, 0:1], ALU.mult, ALU.mult),
              reads=[st["b"], st["b2"]], writes=[st["b2"]])
        kb.op("act", lambda e: e.activation(out=OUT[:, :], in_=R[:, :], func=AF.Identity, scale=st["rs"][:, 0:1], bias=st["rs"][:, 1:2]),
              reads=[b_R, st["b2"]], writes=[b_OUT])
        kb.op("dve", lambda e: e.tensor_tensor(OUT[:, :], OUT[:, :], self.rowp[:, grow, :], ALU.mult), reads=[b_OUT, self.b_rowp], writes=[b_OUT])
        kb.op("dve", lambda e: e.tensor_tensor(OUT[:, :], OUT[:, :], self.rowp[:, brow, :], ALU.add), reads=[b_OUT, self.b_rowp], writes=[b_OUT])

    def alloc_ln(self, es):
        g = self.ps_gen
        self.rowp = self.sb(es, "rowp_sb%d" % g, [128, 5, 1024])
        self.b_rowp = Buf("rowp")
        self.kb.dma("sp", lambda e: e.dma_start(out=self.rowp[:], in_=self.din["rowp"].rearrange("p (r d) -> p r d", d=1024)),
                    writes=[self.b_rowp])
        self.ln_st = {"stats": self.sb(es, "ln_stats%d" % g, [128, 2, 6]), "mv": self.sb(es, "ln_mv%d" % g, [128, 2]),
                      "rs": self.sb(es, "ln_rs%d" % g, [128, 2]), "b": Buf("ln_b"), "b2": Buf("ln_b2")}

    def phase_outproj(self):
        nc, kb = self.nc, self.kb
        cstb, bcb = self.cstb, self.b_cstb
        IDb, LTb, ONEb = cstb[:, 0:128], cstb[:, 384:512], cstb[:, 512:640]
        with ExitStack() as es:
            self.alloc_psum(es, 6, 2)
            self.alloc_ln(es)

            def mk(name, shape, dt=F32):
                return self.sb(es, "P2_" + name, shape, dt), Buf("P2_" + name)
            Wout, b_Wout = mk("Wout", [128, 8, 1024], BF16)
            kb.dma("pool", lambda e: e.dma_start(out=Wout[:, 0:4, :], in_=self.din["w_out"].rearrange("p (k n) -> p k n", n=1024)[:, 0:4, :]), cw=[b_Wout])
            kb.dma("pool", lambda e: e.dma_start(out=Wout[:, 4:8, :], in_=self.din["w_out"].rearrange("p (k n) -> p k n", n=1024)[:, 4:8, :]), cw=[b_Wout])
            wr32, b_wr32 = mk("wr32", [128, 8, 32])
            wrh, b_wrh = mk("wrh", [128, 8, 32], BF16)
            wrl, b_wrl = mk("wrl", [128, 8, 32], BF16)
            kb.dma("sp", lambda e: e.dma_start(out=wr32[:], in_=self.din["w_router"].rearrange("p (k n) -> p k n", n=32)), writes=[b_wr32])
            kb.op("dve", lambda e: e.tensor_copy(wrh[:], wr32[:]), reads=[b_wr32], writes=[b_wrh])
            kb.op("dve", lambda e: e.tensor_tensor(wrl[:], wr32[:], wrh[:], ALU.subtract), reads=[b_wr32, b_wrh], writes=[b_wrl])
            brt, b_brt = mk("brt", [128, 32])
            kb.dma("sp", lambda e: e.dma_start(out=brt[:], in_=self.din["b_router"][0:1, :].partition_broadcast(128)), writes=[b_brt])
            carry, b_carry = mk("carry", [128, 32])
            kb.op("dve", lambda e: e.memset(carry[:], 0.0), writes=[b_carry])
            Xt = [mk("x%d" % i, [128, 1024]) for i in range(2)]
            R, b_R = mk("R", [128, 1024])
            H1 = [mk("H1_%d" % i, [128, 1024]) for i in range(2)]
            H1b = [mk("H1b_%d" % i, [128, 1024], BF16) for i in range(2)]
            H1l = [mk("H1l_%d" % i, [128, 1024], BF16) for i in range(2)]
            HT = [mk("HT_%d" % i, [128, 8, 128], BF16) for i in range(2)]
            lg, b_lg = mk("lg", [128, 32])
            v8, b_v8 = mk("v8", [128, 8])
            i8, b_i8 = mk("i8", [128, 8], U32)
            i8f, b_i8f = mk("i8f", [128, 8])
            sm, b_sm = mk("sm", [128, 8])
            mask, b_mask = mk("mask", [128, 32], BF16)
            sc, b_sc = mk("sc", [128, 32])
            ov, b_ov = mk("ov", [128, 32])
            junk, b_junk = mk("junk", [128, 32])
            slf, b_slf = mk("slf", [128, 4])

            for tt in range(16):
                tsl = slice(tt * 128, (tt + 1) * 128)
                xt, b_xt = Xt[tt % 2]
                if tt in (2, 8):
                    self.load_expert_w(0 if tt == 2 else 1)
                kb.dma("sp", lambda e: e.dma_start(out=xt[:], in_=self.din["x"][tsl, :]), writes=[b_xt])
                for half in range(2):
                    ps, pb = self.bank()
                    for kc in range(8):
                        kb.op("pe", lambda e, kc=kc, ps=ps, half=half: e.matmul(ps[:, :], self.yT[:, kc, tsl], Wout[:, kc, half * 512:(half + 1) * 512],
                                                                             start=(kc == 0), stop=(kc == 7)),
                              reads=[self.b_yT[kc], b_Wout], writes=[pb], inc=(kc == 7))
                    kb.op("dve", lambda e, ps=ps, half=half: e.scalar_tensor_tensor(R[:, half * 512:(half + 1) * 512], xt[:, half * 512:(half + 1) * 512], ALPHA,
                                                                                  ps[:, :], ALU.mult, ALU.add), reads=[b_xt, pb], cw=[b_R])
                h1, b_h1 = H1[tt % 2]
                self.layer_norm(es, R, b_R, 0, 1, h1, b_h1, "ln1")
                kb.dma("sp", lambda e: e.dma_start(out=self.H1d[tsl, :], in_=h1[:]), reads=[b_h1], cw=[self.b_H1d])
                if tt == 0:
                    self.dump("h1_0", h1[:], b_h1, [128, 1024])
                hb, b_hb = H1b[tt % 2]
                hl, b_hl = H1l[tt % 2]
                kb.op("act", lambda e: e.activation(out=hb[:, :], in_=h1[:, :], func=AF.Identity), reads=[b_h1], writes=[b_hb])
                kb.op("dve", lambda e: e.tensor_tensor(hl[:, :], h1[:, :], hb[:, :], ALU.subtract), reads=[b_h1, b_hb], writes=[b_hl])
                for (src, b_src, (dst, b_dst)) in ((hb, b_hb, HT[0]), (hl, b_hl, HT[1])):
                    pt, ptb = self.bank16()
                    for kc in range(8):
                        kb.op("pe", lambda e, kc=kc, pt=pt, src=src: e.transpose(pt[:, kc * 128:(kc + 1) * 128], src[:, kc * 128:(kc + 1) * 128], IDb),
                              reads=[b_src, bcb], writes=[ptb], inc=(kc == 7))
                    kb.op("dve", lambda e, pt=pt, dst=dst: e.tensor_copy(dst[:, :, :], pt[:, :].rearrange("p (k t) -> p k t", t=128)),
                          reads=[ptb], writes=[b_dst])
                ps, pb = self.bank()
                combos = [(HT[0], wrh, b_wrh), (HT[0], wrl, b_wrl), (HT[1], wrh, b_wrh)]
                n = 0
                for (ht, b_ht), w, b_w in combos:
                    for kc in range(8):
                        n += 1
                        kb.op("pe", lambda e, kc=kc, ps=ps, ht=ht, w=w, n=n: e.matmul(ps[:, 0:32], ht[:, kc, :], w[:, kc, :], start=(n == 1), stop=(n == 24)),
                              reads=[b_ht, b_w], writes=[pb], inc=(n == 24))
                kb.op("dve", lambda e, ps=ps: e.tensor_tensor(lg[:, :], ps[:, 0:32], brt[:, :], ALU.add), reads=[pb, b_brt], writes=[b_lg])
                if tt == 0:
                    self.dump("lg_0", lg[:], b_lg, [128, 32])
                kb.op("dve", lambda e: e.max(out=v8[:, :], in_=lg[:, :]), reads=[b_lg], writes=[b_v8])
                kb.op("dve", lambda e: e.max_index(out=i8[:, :], in_max=v8[:, :], in_values=lg[:, :]), reads=[b_lg, b_v8], writes=[b_i8])
                kb.op("dve", lambda e: e.tensor_copy(i8f[:, :], i8[:, :]), reads=[b_i8], writes=[b_i8f])
                kb.op("dve", lambda e: e.tensor_scalar_mul(sm[:, 0:1], v8[:, 0:1], -1.0), reads=[b_v8], writes=[b_sm])
                kb.op("act", lambda e: e.activation(out=sm[:, 4:8], in_=v8[:, 0:4], func=AF.Exp, bias=sm[:, 0:1], accum_out=sm[:, 1:2]),
                      reads=[b_v8, b_sm], writes=[b_sm])
                kb.op("dve", lambda e: e.reciprocal(sm[:, 2:3], sm[:, 1:2]), reads=[b_sm], writes=[b_sm])
                kb.op("dve", lambda e: e.tensor_scalar_mul(self.GATES[:, tt, :], sm[:, 4:8], sm[:, 2:3]), reads=[b_sm], writes=[self.b_route[tt]])
                kb.op("dve", lambda e: e.tensor_scalar(mask[:, :], lg[:, :], v8[:, 3:4], None, ALU.is_ge), reads=[b_lg, b_v8], writes=[b_mask])
                ps, pb = self.bank()
                kb.op("pe", lambda e, ps=ps: e.matmul(ps[:, 0:32], LTb, mask[:, :], start=True, stop=True), reads=[bcb, b_mask], writes=[pb], inc=False)
                kb.op("pe", lambda e, ps=ps: e.matmul(ps[:, 32:64], ONEb, mask[:, :], start=True, stop=True), reads=[bcb, b_mask], writes=[pb])
                kb.op("dve", lambda e, ps=ps: e.tensor_tensor(sc[:, :], ps[:, 0:32], carry[:, :], ALU.add), reads=[pb, b_carry], writes=[b_sc])
                kb.op("dve", lambda e, ps=ps: e.tensor_tensor(carry[:, :], ps[:, 32:64], carry[:, :], ALU.add), reads=[pb, b_carry, b_sc], writes=[b_carry])
                kb.op("dve", lambda e: e.tensor_scalar(ov[:, :], sc[:, :], float(CAP), float(4 * NSLOT), ALU.is_ge, ALU.mult), reads=[b_sc], writes=[b_ov])
                kb.op("dve", lambda e: e.tensor_tensor(sc[:, :], sc[:, :], self.cst[:, 672:704], ALU.add), reads=[b_sc, self.b_cst], writes=[b_sc])
                kb.op("dve", lambda e: e.tensor_tensor(sc[:, :], sc[:, :], ov[:, :], ALU.add), reads=[b_sc, b_ov], writes=[b_sc])
                for k in range(4):
                    kb.op("dve", lambda e, k=k: e.scalar_tensor_tensor(junk[:, :], self.cst[:, 640:672], i8f[:, k:k + 1], sc[:, :], ALU.is_equal, ALU.mult,
                                                                      accum_out=slf[:, k:k + 1]), reads=[self.b_cst, b_i8f, b_sc], writes=[b_junk, b_slf])
                kb.op("dve", lambda e: e.tensor_copy(self.SLOTS[:, tt, :], slf[:, :]), reads=[b_slf], writes=[self.b_route[tt]])
                for k in range(4):
                    kb.dma("pool", lambda e, k=k: e.indirect_dma_start(
                        out=self.Xg, out_offset=bass.IndirectOffsetOnAxis(ap=self.SLOTS[:, tt, k:k + 1], axis=0),
                        in_=hb[:, :], in_offset=None, bounds_check=self.bound_reg, oob_is_err=False),
                        reads=[b_hb, self.b_route[tt], self.b_Xgz], cw=[self.b_Xg])
            self.dump("gates", self.GATES[:].rearrange("p a b -> p (a b)"), self.b_route[15], [128, 64])
            if "slots" in self.dbg:
                sf, b_sf = mk("slots_f", [128, 64])
                kb.op("dve", lambda e: e.tensor_copy(sf[:, :], self.SLOTS[:].rearrange("p a b -> p (a b)")), reads=self.b_route, writes=[b_sf])
                self.dump("slots", sf[:], b_sf, [128, 64])

    def phase_moe(self):
        nc, kb = self.nc, self.kb
        IDb, bcb = self.cstb[:, 0:128], self.b_cstb
        NST = CAP // 128
        with ExitStack() as es:
            self.alloc_psum(es, 6, 2)

            def mk(name, shape, dt=F32):
                return self.sb(es, "M_" + name, shape, dt), Buf("M_" + name)
            WU, WD = self.WU, self.WD
            BD = [mk("bd%d" % i, [128, 1024]) for i in range(2)]
            XG = [mk("xg%d" % i, [128, NST, 1024], BF16) for i in range(2)]
            XGT = [mk("xgt%d" % i, [128, 8, CAP], BF16) for i in range(2)]
            ACTT, b_ACTT = mk("actt", [128, 8, CAP], BF16)
            Gt = [mk("g%d" % i, [128, CAP]) for i in range(2)]
            St = [mk("s%d" % i, [128, CAP]) for i in range(2)]
            Ut = [mk("u%d" % i, [128, CAP]) for i in range(2)]
            Ysb = [mk("y%d" % i, [128, 1024]) for i in range(2)]
            bup, b_bup = mk("bup", [128, 32, 16])
            kb.dma("sp", lambda e: e.dma_start(out=bup[:], in_=self.din["b_up"].rearrange("p (e f) -> p e f", f=16)), writes=[b_bup])

            def load(ex):
                sl = ex % 2
                wu = self.din["w_up"][ex].rearrange("(kc p) f -> p kc f", p=128)
                wd = self.din["w_down"][ex].rearrange("(kc p) f -> p kc f", p=128)
                kb.dma("sp", lambda e: e.dma_start(out=XG[sl][0][:], in_=self.Xg[ex * CAP:(ex + 1) * CAP, :].rearrange("(st p) d -> p st d", p=128)),
                       reads=[self.b_Xg], writes=[XG[sl][1]])
                kb.dma("sp", lambda e: e.dma_start(out=BD[sl][0][:], in_=self.din["b_down"][ex:ex + 1, :].partition_broadcast(128)), writes=[BD[sl][1]])
                if ex >= 2:
                    self.load_expert_w(ex)

            def tposes(ex):
                sl = ex % 2
                xg, b_xg = XG[sl]
                xgt, b_xgt = XGT[sl]
                for st in range(NST):
                    pt, ptb = self.bank16()
                    for kc in range(8):
                        kb.op("pe", lambda e, kc=kc, pt=pt, st=st: e.transpose(pt[:, kc * 128:(kc + 1) * 128], xg[:, st, kc * 128:(kc + 1) * 128], IDb),
                              reads=[b_xg, bcb], writes=[ptb], inc=(kc == 7))
                    kb.op("dve", lambda e, pt=pt, st=st: e.tensor_copy(xgt[:, :, st * 128:(st + 1) * 128], pt[:, :].rearrange("p (k s) -> p k s", s=128)),
                          reads=[ptb], cw=[b_xgt])

            def up(ex):
                sl = ex % 2
                wu, b_wu = WU[sl]
                xgt, b_xgt = XGT[sl]
                for c in range(8):
                    g, b_g = Gt[c % 2]
                    s_, b_s = St[c % 2]
                    u, b_u = Ut[c % 2]
                    ps_g, pb_g = self.bank()
                    for kc in range(8):
                        kb.op("pe", lambda e, kc=kc, ps_g=ps_g, c=c: e.matmul(ps_g[:, 0:CAP], wu[:, kc, c * 128:(c + 1) * 128], xgt[:, kc, :], start=(kc == 0), stop=(kc == 7)),
                              reads=[b_wu, b_xgt], writes=[pb_g], inc=(kc == 7))
                    ps_u, pb_u = self.bank()
                    for kc in range(8):
                        kb.op("pe", lambda e, kc=kc, ps_u=ps_u, c=c: e.matmul(ps_u[:, 0:CAP], wu[:, kc, 1024 + c * 128:1024 + (c + 1) * 128], xgt[:, kc, :],
                                                                           start=(kc == 0), stop=(kc == 7)),
                              reads=[b_wu, b_xgt], writes=[pb_u], inc=(kc == 7))
                    kb.op("dve", lambda e, ps_g=ps_g, c=c: e.tensor_scalar(g[:, :], ps_g[:, 0:CAP], bup[:, ex, c:c + 1], 7.0, ALU.add, ALU.min),
                          reads=[pb_g, b_bup], writes=[b_g])
                    kb.op("act", lambda e: e.activation(out=s_[:, :], in_=g[:, :], func=AF.Sigmoid, scale=1.702), reads=[b_g], writes=[b_s])
                    kb.op("dve", lambda e, ps_u=ps_u, c=c: e.tensor_scalar(u[:, :], ps_u[:, 0:CAP], bup[:, ex, 8 + c:9 + c], 7.0, ALU.add, ALU.min),
                          reads=[pb_u, b_bup], writes=[b_u])
                    kb.op("dve", lambda e: e.tensor_scalar(u[:, :], u[:, :], -7.0, 1.0, ALU.max, ALU.add), reads=[b_u], writes=[b_u])
                    kb.op("dve", lambda e: e.tensor_tensor(g[:, :], g[:, :], s_[:, :], ALU.mult), reads=[b_g, b_s], writes=[b_g])
                    kb.op("dve", lambda e, c=c: e.tensor_tensor(ACTT[:, c, :], g[:, :], u[:, :], ALU.mult), reads=[b_g, b_u], cw=[b_ACTT])

            def down(ex):
                sl = ex % 2
                wd, b_wd = WD[sl]
                bd, b_bd = BD[sl]
                for st in range(NST):
                    y, b_y = Ysb[st % 2]
                    for half in range(2):
                        ps, pb = self.bank()
                        for fc in range(8):
                            kb.op("pe", lambda e, fc=fc, ps=ps, half=half, st=st: e.matmul(ps[:, :], ACTT[:, fc, st * 128:(st + 1) * 128],
                                                                                        wd[:, fc, half * 512:(half + 1) * 512], start=(fc == 0), stop=(fc == 7)),
                                  reads=[b_ACTT, b_wd], writes=[pb], inc=(fc == 7))
                        kb.op("dve", lambda e, ps=ps, half=half: e.tensor_tensor(y[:, half * 512:(half + 1) * 512], ps[:, :], bd[:, half * 512:(half + 1) * 512], ALU.add),
                              reads=[pb, b_bd], cw=[b_y])
                    r0 = ex * CAP + st * 128
                    kb.dma("sp", lambda e, r0=r0: e.dma_start(out=self.Yg[r0:r0 + 128, :], in_=y[:]), reads=[b_y], cw=[self.b_Yg])

            ne = getattr(self, "n_experts", NE)
            load(0)
            if ne > 1:
                load(1)
            tposes(0)
            for ex in range(ne):
                up(ex)
                if ex + 1 < ne:
                    tposes(ex + 1)
                down(ex)
                if ex + 2 < ne:
                    load(ex + 2)

    def phase_final(self):
        nc, kb = self.nc, self.kb
        IDb, bcb = self.cstb[:, 0:128], self.b_cstb
        with ExitStack() as es:
            self.alloc_psum(es, 6, 2)
            self.alloc_ln(es)

            def mk(name, shape, dt=F32):
                return self.sb(es, "F_" + name, shape, dt), Buf("F_" + name)
            Wpg, b_Wpg = mk("Wpg", [128, 8, 1024], BF16)
            for q in range(2):
                kb.dma("pool", lambda e, q=q: e.dma_start(out=Wpg[:, 4 * q:4 * q + 4, :], in_=self.din["w_pg"].rearrange("p (k n) -> p k n", n=1024)[:, 4 * q:4 * q + 4, :]),
                       cw=[b_Wpg])
            Wple, b_Wple = mk("Wple", [128, 2, 1024], BF16)
            kb.dma("pool", lambda e: e.dma_start(out=Wple[:], in_=self.din["w_ple"].rearrange("p (k n) -> p k n", n=1024)), writes=[b_Wple])
            PT, b_PT = mk("PT", [128, 2, T], BF16)
            pT = self.din["pT"].rearrange("(kc p) t -> p kc t", p=128)
            for kc in range(2):
                kb.dma("pool", lambda e, kc=kc: e.dma_start(out=PT[:, kc, :], in_=pT[:, kc, :]), cw=[b_PT])
            YG = [mk("yg%d" % i, [128, 4, 1024]) for i in range(3)]
            H1t = [mk("h1_%d" % i, [128, 1024]) for i in range(3)]
            ACC, b_ACC = mk("acc", [128, 1024])
            H2T, b_H2T = mk("h2T", [128, 8, 128], BF16)
            SGT, b_SGT = mk("sgt", [128, 1024])
            OUT = [mk("out%d" % i, [128, 1024]) for i in range(2)]
            def prefetch(tt):
                tsl = slice(tt * 128, (tt + 1) * 128)
                yg, b_yg = YG[tt % 3]
                h1, b_h1 = H1t[tt % 3]
                kb.op("pool", lambda e: e.memset(yg[:], 0.0), writes=[b_yg])
                for k in range(4):
                    kb.dma("pool", lambda e, k=k: e.indirect_dma_start(
                        out=yg[:, k, :], out_offset=None, in_=self.Yg,
                        in_offset=bass.IndirectOffsetOnAxis(ap=self.SLOTS[:, tt, k:k + 1], axis=0),
                        bounds_check=self.bound_reg, oob_is_err=False), reads=[self.b_Yg, self.b_route[tt]], cw=[b_yg])
                kb.dma("sp", lambda e: e.dma_start(out=h1[:], in_=self.H1d[tsl, :]), reads=[self.b_H1d], writes=[b_h1])

            H2s = [mk("h2_%d" % i, [128, 1024]) for i in range(2)]
            H2bs = [mk("h2b_%d" % i, [128, 1024], BF16) for i in range(2)]

            def stage_a(tt):
                yg, b_yg = YG[tt % 3]
                h1, b_h1 = H1t[tt % 3]
                H2, b_H2 = H2s[tt % 2]
                H2b, b_H2b = H2bs[tt % 2]
                kb.op("act", lambda e: e.activation(out=ACC[:, :], in_=h1[:, :], func=AF.Identity, scale=ALPHA), reads=[b_h1], writes=[b_ACC])
                for k in range(4):
                    kb.op("dve", lambda e, k=k: e.scalar_tensor_tensor(ACC[:, :], yg[:, k, :], self.GATES[:, tt, k:k + 1], ACC[:, :], ALU.mult, ALU.add),
                          reads=[b_yg, self.b_route[tt], b_ACC], writes=[b_ACC])
                self.layer_norm(es, ACC, b_ACC, 2, 3, H2, b_H2, "ln2")
                if tt == 0:
                    self.dump("h2_0", H2[:], b_H2, [128, 1024])
                kb.op("act", lambda e: e.activation(out=H2b[:, :], in_=H2[:, :], func=AF.Identity), reads=[b_H2], writes=[b_H2b])

            def stage_b(tt):
                tsl = slice(tt * 128, (tt + 1) * 128)
                H2, b_H2 = H2s[tt % 2]
                H2b, b_H2b = H2bs[tt % 2]
                pt, ptb = self.bank16()
                for kc in range(8):
                    kb.op("pe", lambda e, kc=kc, pt=pt: e.transpose(pt[:, kc * 128:(kc + 1) * 128], H2b[:, kc * 128:(kc + 1) * 128], IDb),
                          reads=[b_H2b, bcb], writes=[ptb], inc=(kc == 7))
                kb.op("dve", lambda e, pt=pt: e.tensor_copy(H2T[:, :, :], pt[:, :].rearrange("p (k t) -> p k t", t=128)), reads=[ptb], writes=[b_H2T])
                o, b_o = OUT[tt % 2]
                for half in range(2):
                    hs = slice(half * 512, (half + 1) * 512)
                    ps, pb = self.bank()
                    for kc in range(8):
                        kb.op("pe", lambda e, kc=kc, ps=ps, hs=hs: e.matmul(ps[:, :], H2T[:, kc, :], Wpg[:, kc, hs], start=(kc == 0), stop=(kc == 7)),
                              reads=[b_H2T, b_Wpg], writes=[pb], inc=(kc == 7))
                    kb.op("dve", lambda e, ps=ps, hs=hs: e.tensor_tensor(SGT[:, hs], ps[:, :], self.rowp[:, 4, hs], ALU.add), reads=[pb, self.b_rowp], cw=[b_SGT])
                    kb.op("act", lambda e, hs=hs: e.activation(out=SGT[:, hs], in_=SGT[:, hs], func=AF.Sigmoid), reads=[b_SGT], cw=[b_SGT])
                    ps2, pb2 = self.bank()
                    for kc in range(2):
                        kb.op("pe", lambda e, kc=kc, ps2=ps2, hs=hs: e.matmul(ps2[:, :], PT[:, kc, tsl], Wple[:, kc, hs], start=(kc == 0), stop=(kc == 1)),
                              reads=[b_PT, b_Wple], writes=[pb2], inc=(kc == 1))
                    kb.op("dve", lambda e, ps2=ps2, hs=hs: e.tensor_tensor(o[:, hs], SGT[:, hs], ps2[:, :], ALU.mult), reads=[b_SGT, pb2], cw=[b_o])
                    kb.op("dve", lambda e, hs=hs: e.tensor_tensor(o[:, hs], o[:, hs], H2[:, hs], ALU.add), reads=[b_o, b_H2], cw=[b_o])
                kb.dma("sp", lambda e: e.dma_start(out=self.out[tsl, :], in_=o[:]), reads=[b_o])

            prefetch(0)
            prefetch(1)
            stage_a(0)
            for tt in range(16):
                if tt + 2 < 16:
                    prefetch(tt + 2)
                if tt + 1 < 16:
                    stage_a(tt + 1)
                stage_b(tt)


_PROG_CACHE = {}


def kernel(**inputs):
    inp = {k: np.asarray(v) for k, v in inputs.items()}
    sh = prep_shared(inp)
    in_maps = [dict(sh, **prep_core(inp, b)) for b in range(8)]
    if "nc" not in _PROG_CACHE:
        _PROG_CACHE["nc"] = Prog().build()
    nc = _PROG_CACHE["nc"]
    res = run_bass_kernel_spmd(nc, in_maps, core_ids=list(range(8)))
    out = np.stack([np.asarray(r["out"], dtype=np.float32) for r in res.results], axis=0)
    return out
```

```python
import numpy as np
from contextlib import ExitStack
import concourse.bass as bass
import concourse.mybir as mybir
from concourse.bass_utils import run_bass_kernel_spmd

F32 = mybir.dt.float32
BF16 = mybir.dt.bfloat16
U32 = mybir.dt.uint32
AF = mybir.ActivationFunctionType
ALU = mybir.AluOpType
AX = mybir.AxisListType

T = 2048
D = 1024
NE = 32
CAP = 384
NSLOT = NE * CAP
ALPHA = 2.0 ** 0.25
N_IN = 7184
O_XA, O_GA, O_Q, O_K, O_V, O_GO, O_GLR, O_MA, O_MB = 0, 1024, 2048, 2560, 3072, 4096, 5120, 5136, 6160


class Buf:
    __slots__ = ("name", "w", "r", "c")

    def __init__(self, name):
        self.name = name
        self.w = {}
        self.r = {}
        self.c = {}


class KB:
    def __init__(self, nc, es, n_dma_sems=24):
        self.nc = nc
        self.eng = dict(pe=nc.tensor, act=nc.scalar, dve=nc.vector, pool=nc.gpsimd, sp=nc.sync)
        self.esem = {}
        self.ecnt = {}
        self.seen = {}
        self.semobj = {}
        for n in self.eng:
            s = es.enter_context(nc.semaphore("es_" + n))
            self.esem[n] = s
            self.semobj[id(s)] = s
            self.ecnt[n] = 0
            self.seen[n] = {}
        self.dsem = []
        self.dcnt = []
        for i in range(2 * n_dma_sems):
            s = es.enter_context(nc.semaphore("ds_%d" % i))
            self.dsem.append(s)
            self.semobj[id(s)] = s
            self.dcnt.append(0)
        self.nds = n_dma_sems
        self.drr = {"pool": 0, "hw": 0}

    def _wait(self, en, toks):
        e = self.eng[en]
        seen = self.seen[en]
        for sid, val in toks.items():
            if en == "pe" and sid == id(self.esem["pe"]):
                continue
            if seen.get(sid, 0) >= val:
                continue
            e.wait_ge(self.semobj[sid], val)
            seen[sid] = val

    @staticmethod
    def _merge(dst, src):
        for k, v in src.items():
            if dst.get(k, 0) < v:
                dst[k] = v

    def _deps(self, reads, writes, cw=()):
        toks = {}
        for b in reads:
            self._merge(toks, b.w)
            self._merge(toks, b.c)
        for b in writes:
            self._merge(toks, b.w)
            self._merge(toks, b.c)
            self._merge(toks, b.r)
        for b in cw:
            self._merge(toks, b.w)
            self._merge(toks, b.r)
        return toks

    def _commit(self, tok, reads, writes, cw=()):
        for b in reads:
            self._merge(b.r, tok)
        for b in writes:
            b.w = dict(tok)
            b.r = {}
            b.c = {}
        for b in cw:
            self._merge(b.c, tok)

    disabled = False

    def op(self, en, fn, reads=(), writes=(), inc=True, cw=()):
        if self.disabled:
            return None
        self._wait(en, self._deps(reads, writes, cw))
        ins = fn(self.eng[en])
        s = self.esem[en]
        if inc:
            self.ecnt[en] += 1
            ins.then_inc(s, 1)
            tok = {id(s): self.ecnt[en]}
        else:
            tok = {id(s): self.ecnt[en] + 1}
        self._commit(tok, reads, writes, cw)
        return ins

    def dma(self, en, fn, reads=(), writes=(), cw=()):
        if self.disabled:
            return None
        kind = "pool" if en == "pool" else "hw"
        i = self.drr[kind] + (self.nds if kind == "pool" else 0)
        self.drr[kind] = (self.drr[kind] + 1) % self.nds
        s = self.dsem[i]
        toks = self._deps(reads, writes, cw)
        if self.dcnt[i] > 0:
            self._merge(toks, {id(s): self.dcnt[i]})
        self._wait(en, toks)
        ins = fn(self.eng[en])
        self.dcnt[i] += 16
        ins.then_inc(s, 16)
        tok = {id(s): self.dcnt[i]}
        self._commit(tok, reads, writes, cw)
        return ins

    def all_tokens(self):
        toks = {}
        for n in self.eng:
            if self.ecnt[n] > 0:
                toks[id(self.esem[n])] = self.ecnt[n]
        for i, s in enumerate(self.dsem):
            if self.dcnt[i] > 0:
                toks[id(s)] = self.dcnt[i]
        return toks

    def barrier(self, engines=None):
        toks = self.all_tokens()
        for n in (engines or list(self.eng)):
            own = id(self.esem[n])
            t = {k: v for k, v in toks.items() if not (n == "pe" and k == own)}
            self._wait(n, t)


def _consts():
    c = np.zeros((128, 5 * 128 + 64 + 4), np.float32)
    j = np.arange(128)
    same = np.ones((128, 128), bool)
    c[:, 0:128] = np.eye(128, dtype=np.float32)
    c[:, 128:256] = (same & (j[:, None] <= j[None, :]))
    c[:, 256:384] = (same & (j[:, None] > j[None, :]))
    c[:, 384:512] = (j[:, None] < j[None, :])
    c[:, 512:640] = 1.0
    c[:, 640:672] = np.arange(32)[None, :]
    c[:, 672:704] = (np.arange(32) * CAP)[None, :]
    c[:, 704] = (j < 64)
    c[:, 705] = (j >= 64)
    return c


def prep_shared(inp):
    f = lambda a: np.ascontiguousarray(a, dtype=np.float32)
    w_in = inp["w_in"][0]
    cols = np.concatenate([np.arange(0, O_GLR), np.arange(O_MA, N_IN)])
    wm = w_in[:, cols]
    sh = {}
    sh["w_in_t"] = f(wm.reshape(8, 128, 56, 128).transpose(2, 1, 0, 3).reshape(56, 128, 1024))
    sh["w_glr"] = f(w_in[:, O_GLR:O_GLR + 16].reshape(8, 128, 16).transpose(1, 0, 2).reshape(128, 128))
    chan = np.concatenate([inp["conv_w"][0], inp["conv_b"], inp["lru_b_r"], inp["lru_b_i"],
                           inp["lru_lambda"]], axis=0)
    sh["chanp"] = f(chan.reshape(8, 8, 128).transpose(2, 1, 0).reshape(128, 64))
    sh["lru_wr"] = f(inp["lru_w_r"][0].transpose(1, 0, 2).reshape(128, 1024))
    sh["lru_wi"] = f(inp["lru_w_i"][0].transpose(1, 0, 2).reshape(128, 1024))
    sh["gla_wg"] = f(inp["gla_w_gate"][0])
    sh["gla_bg"] = f(inp["gla_b_gate"])
    sh["gla_ng"] = f(inp["gla_norm_g"][0].reshape(2, 128).T)
    sh["w_out"] = f(inp["w_out"][0].reshape(8, 128, 1024).transpose(1, 0, 2).reshape(128, 8192))
    rows = np.concatenate([inp["ln1_g"], inp["ln1_b"], inp["ln2_g"], inp["ln2_b"],
                           inp["b_ple_gate"]], axis=0)
    sh["rowp"] = f(np.broadcast_to(rows.reshape(1, 5 * 1024), (128, 5 * 1024)))
    sh["w_router"] = f(inp["w_router"][0].reshape(8, 128, 32).transpose(1, 0, 2).reshape(128, 256))
    sh["b_router"] = f(inp["b_router"])
    sh["w_up"] = inp["w_up"][0]
    sh["b_up"] = f(inp["b_up"][0].reshape(32, 16, 128).transpose(2, 0, 1).reshape(128, 512))
    sh["w_down"] = inp["w_down"][0]
    sh["b_down"] = f(inp["b_down"][0])
    sh["w_ple"] = f(inp["w_ple"][0].reshape(2, 128, 1024).transpose(1, 0, 2).reshape(128, 2048))
    sh["w_pg"] = f(inp["w_ple_gate"][0].reshape(8, 128, 1024).transpose(1, 0, 2).reshape(128, 8192))
    sh["consts"] = _consts()
    return sh


def prep_core(inp, b):
    x = np.asarray(inp["x"][b], dtype=np.float32)
    p = np.asarray(inp["p"][0, b], dtype=np.float32)
    return {"x": np.ascontiguousarray(x), "xT": np.ascontiguousarray(x.T),
            "pT": np.ascontiguousarray(p.T)}


SHARED_SHAPES = {
    "w_in_t": [56, 128, 1024], "w_glr": [128, 128], "chanp": [128, 64], "lru_wr": [128, 1024],
    "lru_wi": [128, 1024], "gla_wg": [16, 512], "gla_bg": [1, 512], "gla_ng": [128, 2],
    "w_out": [128, 8192], "rowp": [128, 5120], "w_router": [128, 256], "b_router": [1, 32],
    "w_up": [32, 1024, 2048], "b_up": [128, 512], "w_down": [32, 1024, 1024], "b_down": [32, 1024],
    "w_ple": [128, 2048], "w_pg": [128, 8192], "consts": [128, 708],
}
CORE_SHAPES = {"x": [T, D], "xT": [D, T], "pT": [256, T]}


class StopBuild(Exception):
    pass


class Prog:
    ck_n = 0
    ck_stop = None

    def ck(self, label=""):
        self.ck_n += 1
        if self.ck_stop is not None and self.ck_n >= self.ck_stop:
            if not self.kb.disabled:
                print("STOP at checkpoint", self.ck_n, label)
            self.kb.disabled = True

    def __init__(self, dbg=(), stop_after=None):
        self.dbg = set(dbg)
        self.stop_after = stop_after
        self.nc = nc = bass.Bass("TRN2", target_bir_lowering=False)
        self.din = {}
        for n, s in list(SHARED_SHAPES.items()) + list(CORE_SHAPES.items()):
            self.din[n] = nc.dram_tensor(n, s, F32, kind="ExternalInput").ap()
        self.out = nc.dram_tensor("out", [T, D], F32, kind="ExternalOutput").ap()
        self.dbg_out = {}
        self.es = ExitStack()

    def dbg_tensor(self, name, shape):
        t = self.nc.dram_tensor("dbg_" + name, shape, F32, kind="ExternalOutput").ap()
        self.dbg_out[name] = t
        return t

    def sb(self, es, name, shape, dt=F32):
        return es.enter_context(self.nc.sbuf_tensor(name, shape, dt))

    def build(self):
        nc = self.nc
        with self.es as es:
            kb = self.kb = KB(nc, es)
            self.bound_reg = nc.gpsimd.to_reg(NSLOT - 1)
            self.cst = self.sb(es, "cst", [128, 708])
            self.b_cst = Buf("cst")
            kb.dma("sp", lambda e: e.dma_start(out=self.cst[:], in_=self.din["consts"][:, :]),
                   writes=[self.b_cst])
            self.cstb = self.sb(es, "cstb", [128, 708], BF16)
            self.b_cstb = Buf("cstb")
            kb.op("dve", lambda e: e.tensor_copy(self.cstb[:], self.cst[:]),
                  reads=[self.b_cst], writes=[self.b_cstb])
            self.GATES = self.sb(es, "GATES", [128, 16, 4])
            self.SLOTS = self.sb(es, "SLOTS", [128, 16, 4], U32)
            self.b_route = [Buf("route%d" % i) for i in range(16)]
            self.Xg = nc.dram_tensor("Xg", [NSLOT, D], BF16).ap()
            self.Yg = nc.dram_tensor("Yg", [NSLOT, D], F32).ap()
            self.H1d = nc.dram_tensor("H1d", [T, D], F32).ap()
            self.b_Xg, self.b_Yg, self.b_H1d = Buf("Xg"), Buf("Yg"), Buf("H1d")
            self.b_Xgz = Buf("Xgz")
            self.zero_xg(es)
            with ExitStack() as es_y:
                self.yT = self.sb(es_y, "yT", [128, 8, T], BF16)
                self.b_yT = [Buf("yT%d" % g) for g in range(8)]
                with ExitStack() as es1:
                    self.alloc_psum(es1, 8, 0)
                    self.XT = self.sb(es1, "XT", [128, 8, T], BF16)
                    self.b_XT = Buf("XT")
                    xT = self.din["xT"].rearrange("(kc p) t -> p kc t", p=128)
                    for kc in range(8):
                        kb.dma("pool", lambda e, kc=kc: e.dma_start(out=self.XT[:, kc, :], in_=xT[:, kc, :]),
                               cw=[self.b_XT])
                    if not getattr(self, "skip_lru", False):
                        self.phase_lru(es1)
                    else:
                        kb.op("dve", lambda e: e.memset(self.yT[:], 0.0), writes=self.b_yT)
                    if self.stop_after == "lru":
                        return self.finish()
                    kb.barrier()
                    self.phase_gla(es1)
                    if self.stop_after == "gla":
                        return self.dump_yT()
                kb.barrier()
                with ExitStack() as esw:
                    self.WU = [(self.sb(esw, "M_wu%d" % i, [128, 8, 2048], BF16), Buf("M_wu%d" % i)) for i in range(2)]
                    self.WD = [(self.sb(esw, "M_wd%d" % i, [128, 8, 1024], BF16), Buf("M_wd%d" % i)) for i in range(2)]
                    self.phase_outproj()
                    if self.stop_after == "outproj":
                        return self.finish()
                    kb.barrier()
                    self.phase_moe()
                    if self.stop_after == "moe":
                        return self.finish()
            kb.barrier()
            self.phase_final()
        return self.finish()

    def load_expert_w(self, ex):
        kb = self.kb
        sl = ex % 2
        WU, WD = self.WU, self.WD
        wu = self.din["w_up"][ex].rearrange("(kc p) f -> p kc f", p=128)
        wd = self.din["w_down"][ex].rearrange("(kc p) f -> p kc f", p=128)
        for q in range(4):
            kb.dma("pool", lambda e, q=q: e.dma_start(out=WU[sl][0][:, 2 * q:2 * q + 2, :], in_=wu[:, 2 * q:2 * q + 2, :]), cw=[WU[sl][1]])
        for q in range(2):
            kb.dma("pool", lambda e, q=q: e.dma_start(out=WD[sl][0][:, 4 * q:4 * q + 4, :], in_=wd[:, 4 * q:4 * q + 4, :]), cw=[WD[sl][1]])

    def alloc_psum(self, es, n32, n16):
        nc = self.nc
        self.ps = [es.enter_context(nc.psum_tensor("ps%d_%d" % (i, self.ps_gen), [128, 512], F32)) for i in range(n32)]
        self.psb = [Buf("ps%d" % i) for i in range(n32)]
        self.pst = [es.enter_context(nc.psum_tensor("pst%d_%d" % (i, self.ps_gen), [128, 1024], BF16)) for i in range(n16)]
        self.pstb = [Buf("pst%d" % i) for i in range(n16)]
        self.ps_rr = 0
        self.pst_rr = 0
        self.ps_gen += 1

    def zero_xg(self, es):
        kb = self.kb
        z = self.sb(es, "zeros", [128, 2, 1024], BF16)
        bz = Buf("zeros")
        kb.op("dve", lambda e: e.memset(z[:], 0.0), writes=[bz])
        xg = self.Xg.rearrange("(n p) d -> p n d", p=128)
        for i in range(NSLOT // 128 // 2):
            kb.dma("sp", lambda e, i=i: e.dma_start(out=xg[:, i * 2:(i + 1) * 2, :], in_=z[:]), reads=[bz], cw=[self.b_Xgz])

    ps_gen = 0

    def bank(self):
        i = self.ps_rr
        self.ps_rr = (self.ps_rr + 1) % len(self.ps)
        return self.ps[i], self.psb[i]

    def bank16(self):
        i = self.pst_rr
        self.pst_rr = (self.pst_rr + 1) % len(self.pst)
        return self.pst[i], self.pstb[i]

    def finish(self):
        kb = self.kb
        kb.disabled = False
        kb.barrier(["sp"])
        return self.nc

    def dump_yT(self):
        kb = self.kb
        kb.barrier()
        with ExitStack() as es:
            tmp = self.sb(es, "dump_tmp", [128, 8, T])
            b = Buf("dump_tmp")
            kb.op("dve", lambda e: e.tensor_copy(tmp[:], self.yT[:]), reads=self.b_yT, writes=[b])
            t = self.dbg_tensor("yT", [128, 8, T])
            kb.dma("sp", lambda e: e.dma_start(out=t, in_=tmp[:]), reads=[b])
            return self.finish()

    def dump(self, name, sb_ap, buf, shape):
        if name not in self.dbg:
            return
        t = self.dbg_tensor(name, shape)
        self.kb.dma("sp", lambda e: e.dma_start(out=t, in_=sb_ap), reads=[buf])

    def inproj_fm(self, wt, wb, ncols, tg, evac):
        kb = self.kb
        ps, pb = self.bank()
        for kc in range(8):
            kb.op("pe", lambda e, kc=kc: e.matmul(ps[0:ncols, :], wt[:, kc * ncols:(kc + 1) * ncols],
                                                   self.XT[:, kc, tg * 512:(tg + 1) * 512],
                                                   start=(kc == 0), stop=(kc == 7)),
                  reads=[wb, self.b_XT], writes=[pb], inc=(kc == 7))
        evac(ps, pb)

    def load_w(self, grp):
        i = self.w_rr
        self.w_rr = (self.w_rr + 1) % len(self.wring)
        wt, wb = self.wring[i], self.wringb[i]
        self.kb.dma("pool", lambda e: e.dma_start(out=wt[:], in_=self.din["w_in_t"][grp, :, :]), writes=[wb])
        return wt, wb

    def phase_lru(self, es1):
        nc, kb = self.nc, self.kb
        with ExitStack() as es:
            NW = 6
            self.wring = [self.sb(es, "wr%d" % i, [128, 1024], BF16) for i in range(NW)]
            self.wringb = [Buf("wr%d" % i) for i in range(NW)]
            self.w_rr = 0
            chan = self.sb(es, "chan", [128, 8, 8])
            b_chan = Buf("chan")
            kb.dma("sp", lambda e: e.dma_start(out=chan[:], in_=self.din["chanp"].rearrange("p (g k) -> p g k", k=8)),
                   writes=[b_chan])
            wr = self.sb(es, "lwr", [128, 8, 128], BF16)
            wi = self.sb(es, "lwi", [128, 8, 128], BF16)
            b_wr, b_wi = Buf("lwr"), Buf("lwi")
            kb.dma("pool", lambda e: e.dma_start(out=wr[:], in_=self.din["lru_wr"].rearrange("p (g d) -> p g d", d=128)), writes=[b_wr])
            kb.dma("pool", lambda e: e.dma_start(out=wi[:], in_=self.din["lru_wi"].rearrange("p (g d) -> p g d", d=128)), writes=[b_wi])
            sc = self.sb(es, "lsc", [128, 8, 4])
            b_sc = Buf("lsc")
            kb.op("act", lambda e: e.activation(out=sc[:, :, 0], in_=chan[:, :, 7], func=AF.Exp, scale=-1.0),
                  reads=[b_chan], writes=[b_sc])
            kb.op("act", lambda e: e.activation(out=sc[:, :, 1], in_=sc[:, :, 0], func=AF.Ln, bias=1.0),
                  reads=[b_sc], writes=[b_sc])
            kb.op("dve", lambda e: e.tensor_scalar_mul(sc[:, :, 2], sc[:, :, 1], -8.0), reads=[b_sc], writes=[b_sc])
            kb.op("dve", lambda e: e.tensor_scalar_mul(sc[:, :, 3], sc[:, :, 1], -16.0), reads=[b_sc], writes=[b_sc])

            def mk(name, shape, dt=F32):
                return self.sb(es, "L_" + name, shape, dt), [Buf("L_%s_%d" % (name, i)) for i in range(4)]
            xa, b_xa = mk("xa", [128, T + 3], BF16)
            DG = self.sb(es, "L_dg", [128, 8, 4, 128], BF16)
            b_DG = Buf("L_dg")
            for g_ in range(8):
                for k_ in range(4):
                    kb.op("dve", lambda e, g_=g_, k_=k_: e.tensor_scalar_mul(DG[:, g_, k_, :], self.cstb[:, 0:128], chan[:, g_, k_:k_ + 1]),
                          reads=[self.b_cstb, b_chan], cw=[b_DG])
            xcb, b_xcb = mk("xcb", [128, T], BF16)
            xc, b_xc = mk("xc", [128, T])
            r, b_r = mk("r", [128, T])
            ii, b_ii = mk("i", [128, T])
            aa, b_aa = mk("a", [128, T])
            mm, b_mm = mk("m", [128, T])
            h, b_h = mk("h", [128, T])
            ga, b_ga = mk("ga", [128, T])
            t1, b_t1 = mk("t1", [128, T])
            t2, b_t2 = mk("t2", [128, T])
            b_pad = Buf("xa_pad")
            kb.op("dve", lambda e: e.memset(xa[:, 0:3], 0.0), writes=[b_pad])
            TG = range(4)

            def cs(tg, off=0):
                return slice(off + tg * 512, off + (tg + 1) * 512)

            for g in range(8):
                w_xa, wb_xa = self.load_w(g)
                w_ga, wb_ga = self.load_w(8 + g)
                w_ma, wb_ma = self.load_w(40 + g)
                for tg in TG:
                    self.inproj_fm(w_xa, wb_xa, 128, tg, lambda ps, pb, tg=tg: kb.op(
                        "act", lambda e: e.activation(out=xa[:, cs(tg, 3)], in_=ps[:, :], func=AF.Copy), reads=[pb], writes=[b_xa[tg]]))
                for tg in TG:
                    self.inproj_fm(w_ga, wb_ga, 128, tg, lambda ps, pb, tg=tg: kb.op(
                        "act", lambda e: e.activation(out=ga[:, cs(tg)], in_=ps[:, :], func=AF.Copy), reads=[pb], writes=[b_ga[tg]]))
                for tg in TG:
                    self.inproj_fm(w_ma, wb_ma, 128, tg, lambda ps, pb, tg=tg: kb.op(
                        "act", lambda e: e.activation(out=t2[:, cs(tg)], in_=ps[:, :], func=AF.Sigmoid), reads=[pb], writes=[b_t2[tg]]))
                for tg in TG:
                    kb.op("dve", lambda e, tg=tg: e.tensor_tensor(t1[:, cs(tg)], ga[:, cs(tg)], ga[:, cs(tg)], ALU.mult), reads=[b_ga[tg]], writes=[b_t1[tg]])
                    kb.op("dve", lambda e, tg=tg: e.tensor_scalar(t1[:, cs(tg)], t1[:, cs(tg)], 0.044715, 1.0, ALU.mult, ALU.add), reads=[b_t1[tg]], writes=[b_t1[tg]])
                    kb.op("dve", lambda e, tg=tg: e.tensor_tensor(t1[:, cs(tg)], t1[:, cs(tg)], ga[:, cs(tg)], ALU.mult), reads=[b_t1[tg], b_ga[tg]], writes=[b_t1[tg]])
                for tg in TG:
                    prev = [b_xa[tg - 1]] if tg > 0 else [b_pad]
                    ps, pb = self.bank()
                    for k in range(4):
                        kb.op("pe", lambda e, k=k, tg=tg, ps=ps: e.matmul(ps[:, :], DG[:, g, k, :], xa[:, cs(tg, k)], start=(k == 0), stop=(k == 3)),
                              reads=[b_DG, b_xa[tg]] + prev, writes=[pb], inc=(k == 3))
                    kb.op("act", lambda e, tg=tg, ps=ps: e.activation(out=xc[:, cs(tg)], in_=ps[:, :], func=AF.Identity, bias=chan[:, g, 4:5]),
                          reads=[pb, b_chan], writes=[b_xc[tg]])
                    kb.op("dve", lambda e, tg=tg: e.tensor_copy(xcb[:, cs(tg)], xc[:, cs(tg)]), reads=[b_xc[tg]], writes=[b_xcb[tg]])
                if g == 0:
                    self.dump("xc0", xc[:, :], b_xc[3], [128, T])
                for (wg, bwg, dst, b_dst, bi) in ((wr, b_wr, r, b_r, 5), (wi, b_wi, ii, b_ii, 6)):
                    for tg in TG:
                        ps, pb = self.bank()
                        kb.op("pe", lambda e, tg=tg, ps=ps, wg=wg: e.matmul(ps[:, :], wg[:, g, :], xcb[:, cs(tg)], start=True, stop=True),
                              reads=[bwg, b_xcb[tg]], writes=[pb])
                        kb.op("act", lambda e, tg=tg, ps=ps, dst=dst, bi=bi: e.activation(
                            out=dst[:, cs(tg)], in_=ps[:, :], func=AF.Sigmoid, bias=chan[:, g, bi:bi + 1]),
                            reads=[pb, b_chan], writes=[b_dst[tg]])
                for tg in TG:
                    kb.op("act", lambda e, tg=tg: e.activation(out=aa[:, cs(tg)], in_=r[:, cs(tg)], func=AF.Exp, scale=sc[:, g, 2:3]),
                          reads=[b_r[tg], b_sc], writes=[b_aa[tg]])
                for tg in TG:
                    kb.op("act", lambda e, tg=tg: e.activation(out=mm[:, cs(tg)], in_=r[:, cs(tg)], func=AF.Exp, scale=sc[:, g, 3:4]),
                          reads=[b_r[tg], b_sc], writes=[b_mm[tg]])
                for tg in TG:
                    kb.op("act", lambda e, tg=tg: e.activation(out=mm[:, cs(tg)], in_=mm[:, cs(tg)], func=AF.Ln, scale=-1.0, bias=1.0),
                          reads=[b_mm[tg]], writes=[b_mm[tg]])
                for tg in TG:
                    kb.op("act", lambda e, tg=tg: e.activation(out=mm[:, cs(tg)], in_=mm[:, cs(tg)], func=AF.Exp, scale=0.5),
                          reads=[b_mm[tg]], writes=[b_mm[tg]])
                for tg in TG:
                    kb.op("dve", lambda e, tg=tg: e.tensor_tensor(mm[:, cs(tg)], mm[:, cs(tg)], ii[:, cs(tg)], ALU.mult), reads=[b_mm[tg], b_ii[tg]], writes=[b_mm[tg]])
                    kb.op("dve", lambda e, tg=tg: e.tensor_tensor(mm[:, cs(tg)], mm[:, cs(tg)], xc[:, cs(tg)], ALU.mult), reads=[b_mm[tg], b_xc[tg]], writes=[b_mm[tg]])
                for tg in TG:
                    init = 0.0 if tg == 0 else h[:, tg * 512 - 1:tg * 512]
                    kb.op("dve", lambda e, tg=tg, init=init: e.tensor_tensor_scan(h[:, cs(tg)], aa[:, cs(tg)], mm[:, cs(tg)], init, ALU.mult, ALU.add),
                          reads=[b_aa[tg], b_mm[tg]] + ([b_h[tg - 1]] if tg > 0 else []), writes=[b_h[tg]])
                if g == 0:
                    self.dump("h0", h[:, :], b_h[3], [128, T])
                for tg in TG:
                    kb.op("act", lambda e, tg=tg: e.activation(out=t1[:, cs(tg)], in_=t1[:, cs(tg)], func=AF.Sigmoid, scale=1.5957691216057308),
                          reads=[b_t1[tg]], writes=[b_t1[tg]])
                for tg in TG:
                    kb.op("dve", lambda e, tg=tg: e.tensor_tensor(t1[:, cs(tg)], t1[:, cs(tg)], ga[:, cs(tg)], ALU.mult), reads=[b_t1[tg], b_ga[tg]], writes=[b_t1[tg]])
                    kb.op("dve", lambda e, tg=tg: e.tensor_tensor(t1[:, cs(tg)], t1[:, cs(tg)], h[:, cs(tg)], ALU.mult), reads=[b_t1[tg], b_h[tg]], writes=[b_t1[tg]])
                    kb.op("dve", lambda e, tg=tg: e.tensor_tensor(self.yT[:, g, cs(tg)], t1[:, cs(tg)], t2[:, cs(tg)], ALU.mult),
                          reads=[b_t1[tg], b_t2[tg]], cw=[self.b_yT[g]])

    def phase_gla(self, es1):
        nc, kb = self.nc, self.kb
        cst = self.cstb
        TRI, UU, ONES = cst[:, 128:256], cst[:, 256:384], cst[:, 512:640]
        TRI32 = self.cst[:, 128:256]
        bc = self.b_cstb
        with ExitStack() as es:
            NW = 8
            self.wring = [self.sb(es, "gw%d" % i, [128, 1024], BF16) for i in range(NW)]
            self.wringb = [Buf("gw%d" % i) for i in range(NW)]
            self.w_rr = 0
            wglr = self.sb(es, "wglr", [128, 128], BF16)
            b_wglr = Buf("wglr")
            kb.dma("pool", lambda e: e.dma_start(out=wglr[:], in_=self.din["w_glr"][:, :]), writes=[b_wglr])
            wg = self.sb(es, "wg", [16, 512], BF16)
            bg = self.sb(es, "bg", [1, 512], BF16)
            ng = self.sb(es, "ng", [128, 2])
            b_wg, b_bg, b_ng = Buf("wg"), Buf("bg"), Buf("ng")
            kb.dma("pool", lambda e: e.dma_start(out=wg[:], in_=self.din["gla_wg"][:, :]), writes=[b_wg])
            kb.dma("pool", lambda e: e.dma_start(out=bg[:], in_=self.din["gla_bg"][:, :]), writes=[b_bg])
            kb.dma("sp", lambda e: e.dma_start(out=ng[:], in_=self.din["gla_ng"][:, :]), writes=[b_ng])
            glrT = self.sb(es, "glrT", [16, T], BF16)
            b_glrT = Buf("glrT")
            for tg in range(4):
                self.inproj_fm(wglr, b_wglr, 16, tg, lambda ps, pb, tg=tg: kb.op(
                    "act", lambda e: e.activation(out=glrT[:, tg * 512:(tg + 1) * 512], in_=ps[0:16, :], func=AF.Copy),
                    reads=[pb], cw=[b_glrT]))

            self.ck("glrT")

            def mk(name, shape, dt=F32):
                return self.sb(es, "G_" + name, shape, dt), Buf("G_" + name)
            QT, b_QT = mk("QT", [128, T], BF16)
            KT, b_KT = mk("KT", [128, T], BF16)
            KD0, b_KD0 = mk("KD0", [128, 16, 128], BF16)
            KD1, b_KD1 = mk("KD1", [128, 16, 128], BF16)
            V, b_V = mk("V", [128, 16, 256], BF16)
            OT, b_OT = mk("OT", [128, 2, T])
            EB, b_EB = mk("EB", [128, 32])
            Gsp, b_Gsp = mk("Gsp", [128, 4, 128], BF16)
            Gz, b_Gz = mk("Gz", [128, 4, 128])
            EQ, b_EQ = mk("EQ", [128, 512])
            EK, b_EK = mk("EK", [128, 512])
            ED, b_ED = mk("ED", [128, 4, 128])
            STs = [mk("ST%d" % i, [128, 128], BF16) for i in range(2)]
            Sb = [mk("S%d" % i, [128, 256]) for i in range(4)]
            Sbb = [mk("Sb%d" % i, [128, 256], BF16) for i in range(4)]
            SQ = [mk("SQ%d" % i, [128, 512], BF16) for i in range(2)]
            RIf, b_RIf = mk("RIf", [128, T])
            SG, b_SG = mk("SG", [128, 512])
            SM, b_SM = mk("SM", [128, 512])
            TT, b_TT = mk("TT", [128, 512])

            HW = {}

            def h1(hd, tg):
                if tg == 0:
                    HW[hd] = dict(q=self.load_w(16 + hd), k=self.load_w(20 + hd), v=[self.load_w(24 + 2 * hd + j) for j in range(2)])
                w_q, wb_q = HW[hd]['q']
                w_k, wb_k = HW[hd]['k']
                w_v = HW[hd]['v']
                ps, pb = self.bank()
                for j in range(4):
                    tt = tg * 4 + j
                    kb.op("pe", lambda e, j=j, tt=tt, ps=ps: e.matmul(ps[:, j * 128:(j + 1) * 128], glrT[0:16, tt * 128:(tt + 1) * 128],
                                                                  wg[0:16, hd * 128:(hd + 1) * 128], start=True, stop=False),
                          reads=[b_glrT, b_wg], writes=[pb], inc=False)
                    kb.op("pe", lambda e, j=j, ps=ps: e.matmul(ps[:, j * 128:(j + 1) * 128], cst[0:1, 512:640],
                                                           bg[0:1, hd * 128:(hd + 1) * 128], start=False, stop=True),
                          reads=[bc, b_bg], writes=[pb], inc=(j == 3))
                kb.op("act", lambda e, ps=ps: e.activation(out=Gz[:, :, :], in_=ps[:, :].rearrange("p (j k) -> p j k", k=128),
                                                          func=AF.Exp, scale=-1.0), reads=[pb], writes=[b_Gz])
                kb.op("act", lambda e: e.activation(out=Gsp[:, :, :], in_=Gz[:, :, :], func=AF.Ln, bias=1.0),
                      reads=[b_Gz], writes=[b_Gsp])
                self.ck("z/Gsp")
                ps_c, pb_c = self.bank()
                ps_r, pb_r = self.bank()
                for j in range(4):
                    kb.op("pe", lambda e, j=j, ps_c=ps_c: e.matmul(ps_c[:, j * 128:(j + 1) * 128], Gsp[:, j, :], TRI, start=True, stop=True),
                          reads=[b_Gsp, bc], writes=[pb_c], inc=False)
                    kb.op("pe", lambda e, j=j, ps_r=ps_r: e.matmul(ps_r[:, j * 128:(j + 1) * 128], UU, Gsp[:, j, :], start=True, stop=True),
                          reads=[b_Gsp, bc], writes=[pb_r], inc=(j == 3))
                kb.op("act", lambda e, ps_c=ps_c: e.activation(out=EQ[:, :], in_=ps_c[:, :], func=AF.Exp, scale=-1.0 / 16), reads=[pb_c], writes=[b_EQ])
                kb.op("act", lambda e, ps_c=ps_c: e.activation(out=EK[:, :], in_=ps_c[:, :], func=AF.Exp, scale=1.0 / 16), reads=[pb_c], writes=[b_EK])
                kb.op("act", lambda e, ps_r=ps_r: e.activation(out=ED[:, :, :], in_=ps_r[:, :].rearrange("p (j k) -> p j k", k=128),
                                                            func=AF.Exp, scale=-1.0 / 16), reads=[pb_r], writes=[b_ED])
                kb.op("dve", lambda e, tg=tg: e.tensor_copy(EB[:, tg * 4:(tg + 1) * 4], EQ[:, 127:512:128]), reads=[b_EQ], cw=[b_EB])
                self.ck("cs/rev/E")
                self.inproj_fm(w_q, wb_q, 128, tg, lambda ps, pb, tg=tg: kb.op(
                    "dve", lambda e: e.scalar_tensor_tensor(QT[:, tg * 512:(tg + 1) * 512], ps[:, :], 128.0 ** -0.5, EQ[:, :], ALU.mult, ALU.mult),
                    reads=[pb, b_EQ], cw=[b_QT]))
                self.inproj_fm(w_k, wb_k, 128, tg, lambda ps, pb, tg=tg: kb.op(
                    "dve", lambda e: e.tensor_tensor(KT[:, tg * 512:(tg + 1) * 512], ps[:, :], EK[:, :], ALU.mult),
                    reads=[pb, b_EK], cw=[b_KT]))
                self.ck("qk fm")
                ps, pb = self.bank()
                for j in range(4):
                    tt = tg * 4 + j
                    for kc in range(8):
                        kb.op("pe", lambda e, j=j, tt=tt, kc=kc, ps=ps: e.matmul(ps[:, j * 128:(j + 1) * 128], self.XT[:, kc, tt * 128:(tt + 1) * 128],
                                                                             w_k[:, kc * 128:(kc + 1) * 128], start=(kc == 0), stop=(kc == 7)),
                              reads=[self.b_XT, wb_k], writes=[pb], inc=(j == 3 and kc == 7))
                kb.op("dve", lambda e, ps=ps, tg=tg: e.tensor_tensor(KD0[:, tg * 4:(tg + 1) * 4, :], ps[:, :].rearrange("p (j k) -> p j k", k=128),
                                                                   ED[:, :, :], ALU.mult), reads=[pb, b_ED], cw=[b_KD0])
                self.ck("kd")
                for jj in range(2):
                    ps, pb = self.bank()
                    for j2 in range(2):
                        tt = tg * 4 + jj * 2 + j2
                        for half in range(2):
                            wv, wbv = w_v[half]
                            for kc in range(8):
                                kb.op("pe", lambda e, j2=j2, tt=tt, kc=kc, ps=ps, half=half, wv=wv: e.matmul(
                                    ps[:, j2 * 256 + half * 128:j2 * 256 + (half + 1) * 128], self.XT[:, kc, tt * 128:(tt + 1) * 128],
                                    wv[:, kc * 128:(kc + 1) * 128], start=(kc == 0), stop=(kc == 7)),
                                    reads=[self.b_XT, wbv], writes=[pb], inc=(j2 == 1 and half == 1 and kc == 7))
                    t0 = tg * 4 + jj * 2
                    kb.op("dve", lambda e, ps=ps, t0=t0: e.tensor_copy(V[:, t0:t0 + 2, :], ps[:, :].rearrange("p (j v) -> p j v", v=256)),
                          reads=[pb], cw=[b_V])
                self.ck("v")

            def h2(hd):
                kb.op("dve", lambda e: e.memset(Sb[0][0][:, :], 0.0), writes=[Sb[0][1]])
                kb.op("dve", lambda e: e.memset(Sbb[0][0][:, :], 0.0), writes=[Sbb[0][1]])
                for tt in range(16):
                    c = tt
                    ps_st, pb_st = self.ps[tt % 2], self.psb[tt % 2]
                    st, b_st = STs[tt % 2]
                    kb.op("pe", lambda e: e.matmul(ps_st[:, 0:128], KT[:, tt * 128:(tt + 1) * 128], QT[:, tt * 128:(tt + 1) * 128], start=True, stop=True),
                          reads=[b_KT, b_QT], writes=[pb_st])
                    kb.op("dve", lambda e: e.tensor_tensor(st[:, :], ps_st[:, 0:128], TRI32, ALU.mult), reads=[pb_st, self.b_cst], writes=[b_st])
                    ps_kv, pb_kv = self.ps[2 + tt % 2], self.psb[2 + tt % 2]
                    kb.op("pe", lambda e: e.matmul(ps_kv[:, 0:256], KD0[:, tt, :], V[:, tt, :], start=True, stop=True),
                          reads=[b_KD0, b_V], writes=[pb_kv])
                    self.ck("st/kv")
                    grp = (tt // 4) % 2
                    col = (tt % 4) * 128
                    for vc in range(2):
                        pso, pbo = self.ps[4 + 2 * grp + vc], self.psb[4 + 2 * grp + vc]
                        kb.op("pe", lambda e, vc=vc, pso=pso: e.matmul(pso[:, col:col + 128], V[:, tt, vc * 128:(vc + 1) * 128], st[:, :], start=True, stop=False),
                              reads=[b_V, b_st], writes=[pbo], inc=False)
                        kb.op("pe", lambda e, vc=vc, pso=pso: e.matmul(pso[:, col:col + 128], Sbb[c % 4][0][:, vc * 128:(vc + 1) * 128], QT[:, tt * 128:(tt + 1) * 128],
                                                                    start=False, stop=True), reads=[Sbb[c % 4][1], b_QT], writes=[pbo], inc=True)
                    kb.op("dve", lambda e: e.scalar_tensor_tensor(Sb[(c + 1) % 4][0][:, :], Sb[c % 4][0][:, :], EB[:, c:c + 1], ps_kv[:, 0:256], ALU.mult, ALU.add),
                          reads=[Sb[c % 4][1], b_EB, pb_kv], writes=[Sb[(c + 1) % 4][1]])
                    kb.op("act", lambda e: e.activation(out=Sbb[(c + 1) % 4][0][:, :], in_=Sb[(c + 1) % 4][0][:, :], func=AF.Copy),
                          reads=[Sb[(c + 1) % 4][1]], writes=[Sbb[(c + 1) % 4][1]])
                    self.ck("o tile")
                    if tt % 4 == 3:
                        tg = tt // 4
                        for vc in range(2):
                            pso, pbo = self.ps[4 + 2 * grp + vc], self.psb[4 + 2 * grp + vc]
                            kb.op("act", lambda e, vc=vc, pso=pso, tg=tg: e.activation(out=OT[:, vc, tg * 512:(tg + 1) * 512], in_=pso[:, :], func=AF.Copy),
                                  reads=[pbo], cw=[b_OT])
                if hd == 0:
                    self.dump("o_raw0", OT[:, 0, :], b_OT, [128, T])
                for tg in range(4):
                    sl = slice(tg * 512, (tg + 1) * 512)
                    for vc in range(2):
                        kb.op("act", lambda e, vc=vc, sl=sl: e.activation(out=SQ[vc][0][:, :], in_=OT[:, vc, sl], func=AF.Square), reads=[b_OT], writes=[SQ[vc][1]])
                    ps, pb = self.bank()
                    for vc in range(2):
                        kb.op("pe", lambda e, vc=vc, ps=ps: e.matmul(ps[:, :], ONES, SQ[vc][0][:, :], start=(vc == 0), stop=(vc == 1)),
                              reads=[bc, SQ[vc][1]], writes=[pb], inc=(vc == 1))
                    kb.op("act", lambda e, ps=ps, sl=sl: e.activation(out=RIf[:, sl], in_=ps[:, :], func=AF.Sqrt, scale=1.0 / 256, bias=1e-5), reads=[pb], cw=[b_RIf])
                kb.op("dve", lambda e: e.reciprocal(RIf[:, :], RIf[:, :]), reads=[b_RIf], writes=[b_RIf])

            def h3(hd, tg):
                if tg == 0:
                    HW[hd]['go'] = [self.load_w(32 + 2 * hd + j) for j in range(2)]
                    HW[hd]['mb'] = [self.load_w(48 + 2 * hd + j) for j in range(2)]
                w_go, w_mb = HW[hd]['go'], HW[hd]['mb']
                sl = slice(tg * 512, (tg + 1) * 512)
                for vc in range(2):
                    g = hd * 2 + vc
                    go_ps = {}
                    self.inproj_fm(w_go[vc][0], w_go[vc][1], 128, tg, lambda ps, pb: (go_ps.update(ps=ps, pb=pb), kb.op(
                        "act", lambda e: e.activation(out=SG[:, :], in_=ps[:, :], func=AF.Sigmoid), reads=[pb], writes=[b_SG])))
                    self.inproj_fm(w_mb[vc][0], w_mb[vc][1], 128, tg, lambda ps, pb: kb.op(
                        "act", lambda e: e.activation(out=SM[:, :], in_=ps[:, :], func=AF.Sigmoid), reads=[pb], writes=[b_SM]))
                    kb.op("dve", lambda e, vc=vc, sl=sl: e.scalar_tensor_tensor(TT[:, :], OT[:, vc, sl], ng[:, vc:vc + 1], RIf[:, sl], ALU.mult, ALU.mult),
                          reads=[b_OT, b_ng, b_RIf], writes=[b_TT])
                    kb.op("dve", lambda e: e.tensor_tensor(TT[:, :], TT[:, :], SG[:, :], ALU.mult), reads=[b_TT, b_SG], writes=[b_TT])
                    kb.op("dve", lambda e: e.tensor_tensor(TT[:, :], TT[:, :], go_ps["ps"][:, :], ALU.mult), reads=[b_TT, b_SG, go_ps["pb"]], writes=[b_TT])
                    kb.op("dve", lambda e: e.tensor_tensor(TT[:, :], TT[:, :], SM[:, :], ALU.mult), reads=[b_TT, b_SM], writes=[b_TT])
                    kb.op("dve", lambda e, g=g, sl=sl: e.tensor_tensor(self.yT[:, g, sl], TT[:, :], self.yT[:, g, sl], ALU.add),
                          reads=[b_TT, self.b_yT[g]], writes=[self.b_yT[g]])


            for hd in range(4):
                for tg in range(4):
                    h1(hd, tg)
                    if hd > 0:
                        h3(hd - 1, tg)
                h2(hd)
            for tg in range(4):
                h3(3, tg)

    def layer_norm(self, es_tmp, R, b_R, grow, brow, OUT, b_OUT, tag):
        kb = self.kb
        st = self.ln_st
        kb.op("dve", lambda e: e.bn_stats(st["stats"][:, 0, :], R[:, 0:512]), reads=[b_R], writes=[st["b"]])
        kb.op("dve", lambda e: e.bn_stats(st["stats"][:, 1, :], R[:, 512:1024]), reads=[b_R], writes=[st["b"]])
        kb.op("dve", lambda e: e.bn_aggr(st["mv"][:, :], st["stats"][:, :, :].rearrange("p a b -> p (a b)")), reads=[st["b"]], writes=[st["b"]])
        kb.op("act", lambda e: e.activation(out=st["rs"][:, 0:1], in_=st["mv"][:, 1:2], func=AF.Ln, bias=1e-5), reads=[st["b"]], writes=[st["b2"]])
        kb.op("act", lambda e: e.activation(out=st["rs"][:, 0:1], in_=st["rs"][:, 0:1], func=AF.Exp, scale=-0.5), reads=[st["b2"]], writes=[st["b2"]])
        kb.op("dve", lambda e: e.scalar_tensor_tensor(st["rs"][:, 1:2], st["mv"][:, 0:1], -1.0, st["rs"][:, 0:1], ALU.mult, ALU.mult),
              reads=[st["b"], st["b2"]], writes=[st["b2"]])
        kb.op("act", lambda e: e.activation(out=OUT[:, :], in_=R[:, :], func=AF.Identity, scale=st["rs"][:, 0:1], bias=st["rs"][:, 1:2]),
              reads=[b_R, st["b2"]], writes=[b_OUT])
        kb.op("dve", lambda e: e.tensor_tensor(OUT[:, :], OUT[:, :], self.rowp[:, grow, :], ALU.mult), reads=[b_OUT, self.b_rowp], writes=[b_OUT])
        kb.op("dve", lambda e: e.tensor_tensor(OUT[:, :], OUT[:, :], self.rowp[:, brow, :], ALU.add), reads=[b_OUT, self.b_rowp], writes=[b_OUT])

    def alloc_ln(self, es):
        g = self.ps_gen
        self.rowp = self.sb(es, "rowp_sb%d" % g, [128, 5, 1024])
        self.b_rowp = Buf("rowp")
        self.kb.dma("sp", lambda e: e.dma_start(out=self.rowp[:], in_=self.din["rowp"].rearrange("p (r d) -> p r d", d=1024)),
                    writes=[self.b_rowp])
        self.ln_st = {"stats": self.sb(es, "ln_stats%d" % g, [128, 2, 6]), "mv": self.sb(es, "ln_mv%d" % g, [128, 2]),
                      "rs": self.sb(es, "ln_rs%d" % g, [128, 2]), "b": Buf("ln_b"), "b2": Buf("ln_b2")}

    def phase_outproj(self):
        nc, kb = self.nc, self.kb
        cstb, bcb = self.cstb, self.b_cstb
        IDb, LTb, ONEb = cstb[:, 0:128], cstb[:, 384:512], cstb[:, 512:640]
        with ExitStack() as es:
            self.alloc_psum(es, 6, 2)
            self.alloc_ln(es)

            def mk(name, shape, dt=F32):
                return self.sb(es, "P2_" + name, shape, dt), Buf("P2_" + name)
            Wout, b_Wout = mk("Wout", [128, 8, 1024], BF16)
            kb.dma("pool", lambda e: e.dma_start(out=Wout[:, 0:4, :], in_=self.din["w_out"].rearrange("p (k n) -> p k n", n=1024)[:, 0:4, :]), cw=[b_Wout])
            kb.dma("pool", lambda e: e.dma_start(out=Wout[:, 4:8, :], in_=self.din["w_out"].rearrange("p (k n) -> p k n", n=1024)[:, 4:8, :]), cw=[b_Wout])
            wr32, b_wr32 = mk("wr32", [128, 8, 32])
            wrh, b_wrh = mk("wrh", [128, 8, 32], BF16)
            wrl, b_wrl = mk("wrl", [128, 8, 32], BF16)
            kb.dma("sp", lambda e: e.dma_start(out=wr32[:], in_=self.din["w_router"].rearrange("p (k n) -> p k n", n=32)), writes=[b_wr32])
            kb.op("dve", lambda e: e.tensor_copy(wrh[:], wr32[:]), reads=[b_wr32], writes=[b_wrh])
            kb.op("dve", lambda e: e.tensor_tensor(wrl[:], wr32[:], wrh[:], ALU.subtract), reads=[b_wr32, b_wrh], writes=[b_wrl])
            brt, b_brt = mk("brt", [128, 32])
            kb.dma("sp", lambda e: e.dma_start(out=brt[:], in_=self.din["b_router"][0:1, :].partition_broadcast(128)), writes=[b_brt])
            carry, b_carry = mk("carry", [128, 32])
            kb.op("dve", lambda e: e.memset(carry[:], 0.0), writes=[b_carry])
            Xt = [mk("x%d" % i, [128, 1024]) for i in range(2)]
            R, b_R = mk("R", [128, 1024])
            H1 = [mk("H1_%d" % i, [128, 1024]) for i in range(2)]
            H1b = [mk("H1b_%d" % i, [128, 1024], BF16) for i in range(2)]
            H1l = [mk("H1l_%d" % i, [128, 1024], BF16) for i in range(2)]
            HT = [mk("HT_%d" % i, [128, 8, 128], BF16) for i in range(2)]
            lg, b_lg = mk("lg", [128, 32])
            v8, b_v8 = mk("v8", [128, 8])
            i8, b_i8 = mk("i8", [128, 8], U32)
            i8f, b_i8f = mk("i8f", [128, 8])
            sm, b_sm = mk("sm", [128, 8])
            mask, b_mask = mk("mask", [128, 32], BF16)
            sc, b_sc = mk("sc", [128, 32])
            ov, b_ov = mk("ov", [128, 32])
            junk, b_junk = mk("junk", [128, 32])
            slf, b_slf = mk("slf", [128, 4])

            for tt in range(16):
                tsl = slice(tt * 128, (tt + 1) * 128)
                xt, b_xt = Xt[tt % 2]
                if tt in (2, 8):
                    self.load_expert_w(0 if tt == 2 else 1)
                kb.dma("sp", lambda e: e.dma_start(out=xt[:], in_=self.din["x"][tsl, :]), writes=[b_xt])
                for half in range(2):
                    ps, pb = self.bank()
                    for kc in range(8):
                        kb.op("pe", lambda e, kc=kc, ps=ps, half=half: e.matmul(ps[:, :], self.yT[:, kc, tsl], Wout[:, kc, half * 512:(half + 1) * 512],
                                                                             start=(kc == 0), stop=(kc == 7)),
                              reads=[self.b_yT[kc], b_Wout], writes=[pb], inc=(kc == 7))
                    kb.op("dve", lambda e, ps=ps, half=half: e.scalar_tensor_tensor(R[:, half * 512:(half + 1) * 512], xt[:, half * 512:(half + 1) * 512], ALPHA,
                                                                                  ps[:, :], ALU.mult, ALU.add), reads=[b_xt, pb], cw=[b_R])
                h1, b_h1 = H1[tt % 2]
                self.layer_norm(es, R, b_R, 0, 1, h1, b_h1, "ln1")
                kb.dma("sp", lambda e: e.dma_start(out=self.H1d[tsl, :], in_=h1[:]), reads=[b_h1], cw=[self.b_H1d])
                if tt == 0:
                    self.dump("h1_0", h1[:], b_h1, [128, 1024])
                hb, b_hb = H1b[tt % 2]
                hl, b_hl = H1l[tt % 2]
                kb.op("act", lambda e: e.activation(out=hb[:, :], in_=h1[:, :], func=AF.Identity), reads=[b_h1], writes=[b_hb])
                kb.op("dve", lambda e: e.tensor_tensor(hl[:, :], h1[:, :], hb[:, :], ALU.subtract), reads=[b_h1, b_hb], writes=[b_hl])
                for (src, b_src, (dst, b_dst)) in ((hb, b_hb, HT[0]), (hl, b_hl, HT[1])):
                    pt, ptb = self.bank16()
                    for kc in range(8):
                        kb.op("pe", lambda e, kc=kc, pt=pt, src=src: e.transpose(pt[:, kc * 128:(kc + 1) * 128], src[:, kc * 128:(kc + 1) * 128], IDb),
                              reads=[b_src, bcb], writes=[ptb], inc=(kc == 7))
                    kb.op("dve", lambda e, pt=pt, dst=dst: e.tensor_copy(dst[:, :, :], pt[:, :].rearrange("p (k t) -> p k t", t=128)),
                          reads=[ptb], writes=[b_dst])
                ps, pb = self.bank()
                combos = [(HT[0], wrh, b_wrh), (HT[0], wrl, b_wrl), (HT[1], wrh, b_wrh)]
                n = 0
                for (ht, b_ht), w, b_w in combos:
                    for kc in range(8):
                        n += 1
                        kb.op("pe", lambda e, kc=kc, ps=ps, ht=ht, w=w, n=n: e.matmul(ps[:, 0:32], ht[:, kc, :], w[:, kc, :], start=(n == 1), stop=(n == 24)),
                              reads=[b_ht, b_w], writes=[pb], inc=(n == 24))
                kb.op("dve", lambda e, ps=ps: e.tensor_tensor(lg[:, :], ps[:, 0:32], brt[:, :], ALU.add), reads=[pb, b_brt], writes=[b_lg])
                if tt == 0:
                    self.dump("lg_0", lg[:], b_lg, [128, 32])
                kb.op("dve", lambda e: e.max(out=v8[:, :], in_=lg[:, :]), reads=[b_lg], writes=[b_v8])
                kb.op("dve", lambda e: e.max_index(out=i8[:, :], in_max=v8[:, :], in_values=lg[:, :]), reads=[b_lg, b_v8], writes=[b_i8])
                kb.op("dve", lambda e: e.tensor_copy(i8f[:, :], i8[:, :]), reads=[b_i8], writes=[b_i8f])
                kb.op("dve", lambda e: e.tensor_scalar_mul(sm[:, 0:1], v8[:, 0:1], -1.0), reads=[b_v8], writes=[b_sm])
                kb.op("act", lambda e: e.activation(out=sm[:, 4:8], in_=v8[:, 0:4], func=AF.Exp, bias=sm[:, 0:1], accum_out=sm[:, 1:2]),
                      reads=[b_v8, b_sm], writes=[b_sm])
                kb.op("dve", lambda e: e.reciprocal(sm[:, 2:3], sm[:, 1:2]), reads=[b_sm], writes=[b_sm])
                kb.op("dve", lambda e: e.tensor_scalar_mul(self.GATES[:, tt, :], sm[:, 4:8], sm[:, 2:3]), reads=[b_sm], writes=[self.b_route[tt]])
                kb.op("dve", lambda e: e.tensor_scalar(mask[:, :], lg[:, :], v8[:, 3:4], None, ALU.is_ge), reads=[b_lg, b_v8], writes=[b_mask])
                ps, pb = self.bank()
                kb.op("pe", lambda e, ps=ps: e.matmul(ps[:, 0:32], LTb, mask[:, :], start=True, stop=True), reads=[bcb, b_mask], writes=[pb], inc=False)
                kb.op("pe", lambda e, ps=ps: e.matmul(ps[:, 32:64], ONEb, mask[:, :], start=True, stop=True), reads=[bcb, b_mask], writes=[pb])
                kb.op("dve", lambda e, ps=ps: e.tensor_tensor(sc[:, :], ps[:, 0:32], carry[:, :], ALU.add), reads=[pb, b_carry], writes=[b_sc])
                kb.op("dve", lambda e, ps=ps: e.tensor_tensor(carry[:, :], ps[:, 32:64], carry[:, :], ALU.add), reads=[pb, b_carry, b_sc], writes=[b_carry])
                kb.op("dve", lambda e: e.tensor_scalar(ov[:, :], sc[:, :], float(CAP), float(4 * NSLOT), ALU.is_ge, ALU.mult), reads=[b_sc], writes=[b_ov])
                kb.op("dve", lambda e: e.tensor_tensor(sc[:, :], sc[:, :], self.cst[:, 672:704], ALU.add), reads=[b_sc, self.b_cst], writes=[b_sc])
                kb.op("dve", lambda e: e.tensor_tensor(sc[:, :], sc[:, :], ov[:, :], ALU.add), reads=[b_sc, b_ov], writes=[b_sc])
                for k in range(4):
                    kb.op("dve", lambda e, k=k: e.scalar_tensor_tensor(junk[:, :], self.cst[:, 640:672], i8f[:, k:k + 1], sc[:, :], ALU.is_equal, ALU.mult,
                                                                      accum_out=slf[:, k:k + 1]), reads=[self.b_cst, b_i8f, b_sc], writes=[b_junk, b_slf])
                kb.op("dve", lambda e: e.tensor_copy(self.SLOTS[:, tt, :], slf[:, :]), reads=[b_slf], writes=[self.b_route[tt]])
                for k in range(4):
                    kb.dma("pool", lambda e, k=k: e.indirect_dma_start(
                        out=self.Xg, out_offset=bass.IndirectOffsetOnAxis(ap=self.SLOTS[:, tt, k:k + 1], axis=0),
                        in_=hb[:, :], in_offset=None, bounds_check=self.bound_reg, oob_is_err=False),
                        reads=[b_hb, self.b_route[tt], self.b_Xgz], cw=[self.b_Xg])
            self.dump("gates", self.GATES[:].rearrange("p a b -> p (a b)"), self.b_route[15], [128, 64])
            if "slots" in self.dbg:
                sf, b_sf = mk("slots_f", [128, 64])
                kb.op("dve", lambda e: e.tensor_copy(sf[:, :], self.SLOTS[:].rearrange("p a b -> p (a b)")), reads=self.b_route, writes=[b_sf])
                self.dump("slots", sf[:], b_sf, [128, 64])

    def phase_moe(self):
        nc, kb = self.nc, self.kb
        IDb, bcb = self.cstb[:, 0:128], self.b_cstb
        NST = CAP // 128
        with ExitStack() as es:
            self.alloc_psum(es, 6, 2)

            def mk(name, shape, dt=F32):
                return self.sb(es, "M_" + name, shape, dt), Buf("M_" + name)
            WU, WD = self.WU, self.WD
            BD = [mk("bd%d" % i, [128, 1024]) for i in range(2)]
            XG = [mk("xg%d" % i, [128, NST, 1024], BF16) for i in range(2)]
            XGT = [mk("xgt%d" % i, [128, 8, CAP], BF16) for i in range(2)]
            ACTT, b_ACTT = mk("actt", [128, 8, CAP], BF16)
            Gt = [mk("g%d" % i, [128, CAP]) for i in range(2)]
            St = [mk("s%d" % i, [128, CAP]) for i in range(2)]
            Ut = [mk("u%d" % i, [128, CAP]) for i in range(2)]
            Ysb = [mk("y%d" % i, [128, 1024]) for i in range(2)]
            bup, b_bup = mk("bup", [128, 32, 16])
            kb.dma("sp", lambda e: e.dma_start(out=bup[:], in_=self.din["b_up"].rearrange("p (e f) -> p e f", f=16)), writes=[b_bup])

            def load(ex):
                sl = ex % 2
                wu = self.din["w_up"][ex].rearrange("(kc p) f -> p kc f", p=128)
                wd = self.din["w_down"][ex].rearrange("(kc p) f -> p kc f", p=128)
                kb.dma("sp", lambda e: e.dma_start(out=XG[sl][0][:], in_=self.Xg[ex * CAP:(ex + 1) * CAP, :].rearrange("(st p) d -> p st d", p=128)),
                       reads=[self.b_Xg], writes=[XG[sl][1]])
                kb.dma("sp", lambda e: e.dma_start(out=BD[sl][0][:], in_=self.din["b_down"][ex:ex + 1, :].partition_broadcast(128)), writes=[BD[sl][1]])
                if ex >= 2:
                    self.load_expert_w(ex)

            def tposes(ex):
                sl = ex % 2
                xg, b_xg = XG[sl]
                xgt, b_xgt = XGT[sl]
                for st in range(NST):
                    pt, ptb = self.bank16()
                    for kc in range(8):
                        kb.op("pe", lambda e, kc=kc, pt=pt, st=st: e.transpose(pt[:, kc * 128:(kc + 1) * 128], xg[:, st, kc * 128:(kc + 1) * 128], IDb),
                              reads=[b_xg, bcb], writes=[ptb], inc=(kc == 7))
                    kb.op("dve", lambda e, pt=pt, st=st: e.tensor_copy(xgt[:, :, st * 128:(st + 1) * 128], pt[:, :].rearrange("p (k s) -> p k s", s=128)),
                          reads=[ptb], cw=[b_xgt])

            def up(ex):
                sl = ex % 2
                wu, b_wu = WU[sl]
                xgt, b_xgt = XGT[sl]
                for c in range(8):
                    g, b_g = Gt[c % 2]
                    s_, b_s = St[c % 2]
                    u, b_u = Ut[c % 2]
                    ps_g, pb_g = self.bank()
                    for kc in range(8):
                        kb.op("pe", lambda e, kc=kc, ps_g=ps_g, c=c: e.matmul(ps_g[:, 0:CAP], wu[:, kc, c * 128:(c + 1) * 128], xgt[:, kc, :], start=(kc == 0), stop=(kc == 7)),
                              reads=[b_wu, b_xgt], writes=[pb_g], inc=(kc == 7))
                    ps_u, pb_u = self.bank()
                    for kc in range(8):
                        kb.op("pe", lambda e, kc=kc, ps_u=ps_u, c=c: e.matmul(ps_u[:, 0:CAP], wu[:, kc, 1024 + c * 128:1024 + (c + 1) * 128], xgt[:, kc, :],
                                                                           start=(kc == 0), stop=(kc == 7)),
                              reads=[b_wu, b_xgt], writes=[pb_u], inc=(kc == 7))
                    kb.op("dve", lambda e, ps_g=ps_g, c=c: e.tensor_scalar(g[:, :], ps_g[:, 0:CAP], bup[:, ex, c:c + 1], 7.0, ALU.add, ALU.min),
                          reads=[pb_g, b_bup], writes=[b_g])
                    kb.op("act", lambda e: e.activation(out=s_[:, :], in_=g[:, :], func=AF.Sigmoid, scale=1.702), reads=[b_g], writes=[b_s])
                    kb.op("dve", lambda e, ps_u=ps_u, c=c: e.tensor_scalar(u[:, :], ps_u[:, 0:CAP], bup[:, ex, 8 + c:9 + c], 7.0, ALU.add, ALU.min),
                          reads=[pb_u, b_bup], writes=[b_u])
                    kb.op("dve", lambda e: e.tensor_scalar(u[:, :], u[:, :], -7.0, 1.0, ALU.max, ALU.add), reads=[b_u], writes=[b_u])
                    kb.op("dve", lambda e: e.tensor_tensor(g[:, :], g[:, :], s_[:, :], ALU.mult), reads=[b_g, b_s], writes=[b_g])
                    kb.op("dve", lambda e, c=c: e.tensor_tensor(ACTT[:, c, :], g[:, :], u[:, :], ALU.mult), reads=[b_g, b_u], cw=[b_ACTT])

            def down(ex):
                sl = ex % 2
                wd, b_wd = WD[sl]
                bd, b_bd = BD[sl]
                for st in range(NST):
                    y, b_y = Ysb[st % 2]
                    for half in range(2):
                        ps, pb = self.bank()
                        for fc in range(8):
                            kb.op("pe", lambda e, fc=fc, ps=ps, half=half, st=st: e.matmul(ps[:, :], ACTT[:, fc, st * 128:(st + 1) * 128],
                                                                                        wd[:, fc, half * 512:(half + 1) * 512], start=(fc == 0), stop=(fc == 7)),
                                  reads=[b_ACTT, b_wd], writes=[pb], inc=(fc == 7))
                        kb.op("dve", lambda e, ps=ps, half=half: e.tensor_tensor(y[:, half * 512:(half + 1) * 512], ps[:, :], bd[:, half * 512:(half + 1) * 512], ALU.add),
                              reads=[pb, b_bd], cw=[b_y])
                    r0 = ex * CAP + st * 128
                    kb.dma("sp", lambda e, r0=r0: e.dma_start(out=self.Yg[r0:r0 + 128, :], in_=y[:]), reads=[b_y], cw=[self.b_Yg])

            ne = getattr(self, "n_experts", NE)
            load(0)
            if ne > 1:
                load(1)
            tposes(0)
            for ex in range(ne):
                up(ex)
                if ex + 1 < ne:
                    tposes(ex + 1)
                down(ex)
                if ex + 2 < ne:
                    load(ex + 2)

    def phase_final(self):
        nc, kb = self.nc, self.kb
        IDb, bcb = self.cstb[:, 0:128], self.b_cstb
        with ExitStack() as es:
            self.alloc_psum(es, 6, 2)
            self.alloc_ln(es)

            def mk(name, shape, dt=F32):
                return self.sb(es, "F_" + name, shape, dt), Buf("F_" + name)
            Wpg, b_Wpg = mk("Wpg", [128, 8, 1024], BF16)
            for q in range(2):
                kb.dma("pool", lambda e, q=q: e.dma_start(out=Wpg[:, 4 * q:4 * q + 4, :], in_=self.din["w_pg"].rearrange("p (k n) -> p k n", n=1024)[:, 4 * q:4 * q + 4, :]),
                       cw=[b_Wpg])
            Wple, b_Wple = mk("Wple", [128, 2, 1024], BF16)
            kb.dma("pool", lambda e: e.dma_start(out=Wple[:], in_=self.din["w_ple"].rearrange("p (k n) -> p k n", n=1024)), writes=[b_Wple])
            PT, b_PT = mk("PT", [128, 2, T], BF16)
            pT = self.din["pT"].rearrange("(kc p) t -> p kc t", p=128)
            for kc in range(2):
                kb.dma("pool", lambda e, kc=kc: e.dma_start(out=PT[:, kc, :], in_=pT[:, kc, :]), cw=[b_PT])
            YG = [mk("yg%d" % i, [128, 4, 1024]) for i in range(3)]
            H1t = [mk("h1_%d" % i, [128, 1024]) for i in range(3)]
            ACC, b_ACC = mk("acc", [128, 1024])
            H2T, b_H2T = mk("h2T", [128, 8, 128], BF16)
            SGT, b_SGT = mk("sgt", [128, 1024])
            OUT = [mk("out%d" % i, [128, 1024]) for i in range(2)]
            def prefetch(tt):
                tsl = slice(tt * 128, (tt + 1) * 128)
                yg, b_yg = YG[tt % 3]
                h1, b_h1 = H1t[tt % 3]
                kb.op("pool", lambda e: e.memset(yg[:], 0.0), writes=[b_yg])
                for k in range(4):
                    kb.dma("pool", lambda e, k=k: e.indirect_dma_start(
                        out=yg[:, k, :], out_offset=None, in_=self.Yg,
                        in_offset=bass.IndirectOffsetOnAxis(ap=self.SLOTS[:, tt, k:k + 1], axis=0),
                        bounds_check=self.bound_reg, oob_is_err=False), reads=[self.b_Yg, self.b_route[tt]], cw=[b_yg])
                kb.dma("sp", lambda e: e.dma_start(out=h1[:], in_=self.H1d[tsl, :]), reads=[self.b_H1d], writes=[b_h1])

            H2s = [mk("h2_%d" % i, [128, 1024]) for i in range(2)]
            H2bs = [mk("h2b_%d" % i, [128, 1024], BF16) for i in range(2)]

            def stage_a(tt):
                yg, b_yg = YG[tt % 3]
                h1, b_h1 = H1t[tt % 3]
                H2, b_H2 = H2s[tt % 2]
                H2b, b_H2b = H2bs[tt % 2]
                kb.op("act", lambda e: e.activation(out=ACC[:, :], in_=h1[:, :], func=AF.Identity, scale=ALPHA), reads=[b_h1], writes=[b_ACC])
                for k in range(4):
                    kb.op("dve", lambda e, k=k: e.scalar_tensor_tensor(ACC[:, :], yg[:, k, :], self.GATES[:, tt, k:k + 1], ACC[:, :], ALU.mult, ALU.add),
                          reads=[b_yg, self.b_route[tt], b_ACC], writes=[b_ACC])
                self.layer_norm(es, ACC, b_ACC, 2, 3, H2, b_H2, "ln2")
                if tt == 0:
                    self.dump("h2_0", H2[:], b_H2, [128, 1024])
                kb.op("act", lambda e: e.activation(out=H2b[:, :], in_=H2[:, :], func=AF.Identity), reads=[b_H2], writes=[b_H2b])

            def stage_b(tt):
                tsl = slice(tt * 128, (tt + 1) * 128)
                H2, b_H2 = H2s[tt % 2]
                H2b, b_H2b = H2bs[tt % 2]
                pt, ptb = self.bank16()
                for kc in range(8):
                    kb.op("pe", lambda e, kc=kc, pt=pt: e.transpose(pt[:, kc * 128:(kc + 1) * 128], H2b[:, kc * 128:(kc + 1) * 128], IDb),
                          reads=[b_H2b, bcb], writes=[ptb], inc=(kc == 7))
                kb.op("dve", lambda e, pt=pt: e.tensor_copy(H2T[:, :, :], pt[:, :].rearrange("p (k t) -> p k t", t=128)), reads=[ptb], writes=[b_H2T])
                o, b_o = OUT[tt % 2]
                for half in range(2):
                    hs = slice(half * 512, (half + 1) * 512)
                    ps, pb = self.bank()
                    for kc in range(8):
                        kb.op("pe", lambda e, kc=kc, ps=ps, hs=hs: e.matmul(ps[:, :], H2T[:, kc, :], Wpg[:, kc, hs], start=(kc == 0), stop=(kc == 7)),
                              reads=[b_H2T, b_Wpg], writes=[pb], inc=(kc == 7))
                    kb.op("dve", lambda e, ps=ps, hs=hs: e.tensor_tensor(SGT[:, hs], ps[:, :], self.rowp[:, 4, hs], ALU.add), reads=[pb, self.b_rowp], cw=[b_SGT])
                    kb.op("act", lambda e, hs=hs: e.activation(out=SGT[:, hs], in_=SGT[:, hs], func=AF.Sigmoid), reads=[b_SGT], cw=[b_SGT])
                    ps2, pb2 = self.bank()
                    for kc in range(2):
                        kb.op("pe", lambda e, kc=kc, ps2=ps2, hs=hs: e.matmul(ps2[:, :], PT[:, kc, tsl], Wple[:, kc, hs], start=(kc == 0), stop=(kc == 1)),
                              reads=[b_PT, b_Wple], writes=[pb2], inc=(kc == 1))
                    kb.op("dve", lambda e, ps2=ps2, hs=hs: e.tensor_tensor(o[:, hs], SGT[:, hs], ps2[:, :], ALU.mult), reads=[b_SGT, pb2], cw=[b_o])
                    kb.op("dve", lambda e, hs=hs: e.tensor_tensor(o[:, hs], o[:, hs], H2[:, hs], ALU.add), reads=[b_o, b_H2], cw=[b_o])
                kb.dma("sp", lambda e: e.dma_start(out=self.out[tsl, :], in_=o[:]), reads=[b_o])

            prefetch(0)
            prefetch(1)
            stage_a(0)
            for tt in range(16):
                if tt + 2 < 16:
                    prefetch(tt + 2)
                if tt + 1 < 16:
                    stage_a(tt + 1)
                stage_b(tt)


_PROG_CACHE = {}


def kernel(**inputs):
    inp = {k: np.asarray(v) for k, v in inputs.items()}
    sh = prep_shared(inp)
    in_maps = [dict(sh, **prep_core(inp, b)) for b in range(8)]
    if "nc" not in _PROG_CACHE:
        _PROG_CACHE["nc"] = Prog().build()
    nc = _PROG_CACHE["nc"]
    res = run_bass_kernel_spmd(nc, in_maps, core_ids=list(range(8)))
    out = np.stack([np.asarray(r["out"], dtype=np.float32) for r in res.results], axis=0)
    return out
```

```python
import numpy as np
from contextlib import ExitStack
import concourse.bass as bass
import concourse.mybir as mybir
from concourse.bass_utils import run_bass_kernel_spmd

F32 = mybir.dt.float32
BF16 = mybir.dt.bfloat16
U32 = mybir.dt.uint32
AF = mybir.ActivationFunctionType
ALU = mybir.AluOpType
AX = mybir.AxisListType

T = 2048
D = 1024
NE = 32
CAP = 384
NSLOT = NE * CAP
ALPHA = 2.0 ** 0.25
N_IN = 7184
O_XA, O_GA, O_Q, O_K, O_V, O_GO, O_GLR, O_MA, O_MB = 0, 1024, 2048, 2560, 3072, 4096, 5120, 5136, 6160


class Buf:
    __slots__ = ("name", "w", "r", "c")

    def __init__(self, name):
        self.name = name
        self.w = {}
        self.r = {}
        self.c = {}


class KB:
    def __init__(self, nc, es, n_dma_sems=24):
        self.nc = nc
        self.eng = dict(pe=nc.tensor, act=nc.scalar, dve=nc.vector, pool=nc.gpsimd, sp=nc.sync)
        self.esem = {}
        self.ecnt = {}
        self.seen = {}
        self.semobj = {}
        for n in self.eng:
            s = es.enter_context(nc.semaphore("es_" + n))
            self.esem[n] = s
            self.semobj[id(s)] = s
            self.ecnt[n] = 0
            self.seen[n] = {}
        self.dsem = []
        self.dcnt = []
        for i in range(2 * n_dma_sems):
            s = es.enter_context(nc.semaphore("ds_%d" % i))
            self.dsem.append(s)
            self.semobj[id(s)] = s
            self.dcnt.append(0)
        self.nds = n_dma_sems
        self.drr = {"pool": 0, "hw": 0}

    def _wait(self, en, toks):
        e = self.eng[en]
        seen = self.seen[en]
        for sid, val in toks.items():
            if en == "pe" and sid == id(self.esem["pe"]):
                continue
            if seen.get(sid, 0) >= val:
                continue
            e.wait_ge(self.semobj[sid], val)
            seen[sid] = val

    @staticmethod
    def _merge(dst, src):
        for k, v in src.items():
            if dst.get(k, 0) < v:
                dst[k] = v

    def _deps(self, reads, writes, cw=()):
        toks = {}
        for b in reads:
            self._merge(toks, b.w)
            self._merge(toks, b.c)
        for b in writes:
            self._merge(toks, b.w)
            self._merge(toks, b.c)
            self._merge(toks, b.r)
        for b in cw:
            self._merge(toks, b.w)
            self._merge(toks, b.r)
        return toks

    def _commit(self, tok, reads, writes, cw=()):
        for b in reads:
            self._merge(b.r, tok)
        for b in writes:
            b.w = dict(tok)
            b.r = {}
            b.c = {}
        for b in cw:
            self._merge(b.c, tok)

    disabled = False

    def op(self, en, fn, reads=(), writes=(), inc=True, cw=()):
        if self.disabled:
            return None
        self._wait(en, self._deps(reads, writes, cw))
        ins = fn(self.eng[en])
        s = self.esem[en]
        if inc:
            self.ecnt[en] += 1
            ins.then_inc(s, 1)
            tok = {id(s): self.ecnt[en]}
        else:
            tok = {id(s): self.ecnt[en] + 1}
        self._commit(tok, reads, writes, cw)
        return ins

    def dma(self, en, fn, reads=(), writes=(), cw=()):
        if self.disabled:
            return None
        kind = "pool" if en == "pool" else "hw"
        i = self.drr[kind] + (self.nds if kind == "pool" else 0)
        self.drr[kind] = (self.drr[kind] + 1) % self.nds
        s = self.dsem[i]
        toks = self._deps(reads, writes, cw)
        if self.dcnt[i] > 0:
            self._merge(toks, {id(s): self.dcnt[i]})
        self._wait(en, toks)
        ins = fn(self.eng[en])
        self.dcnt[i] += 16
        ins.then_inc(s, 16)
        tok = {id(s): self.dcnt[i]}
        self._commit(tok, reads, writes, cw)
        return ins

    def all_tokens(self):
        toks = {}
        for n in self.eng:
            if self.ecnt[n] > 0:
                toks[id(self.esem[n])] = self.ecnt[n]
        for i, s in enumerate(self.dsem):
            if self.dcnt[i] > 0:
                toks[id(s)] = self.dcnt[i]
        return toks

    def barrier(self, engines=None):
        toks = self.all_tokens()
        for n in (engines or list(self.eng)):
            own = id(self.esem[n])
            t = {k: v for k, v in toks.items() if not (n == "pe" and k == own)}
            self._wait(n, t)


def _consts():
    c = np.zeros((128, 5 * 128 + 64 + 4), np.float32)
    j = np.arange(128)
    same = np.ones((128, 128), bool)
    c[:, 0:128] = np.eye(128, dtype=np.float32)
    c[:, 128:256] = (same & (j[:, None] <= j[None, :]))
    c[:, 256:384] = (same & (j[:, None] > j[None, :]))
    c[:, 384:512] = (j[:, None] < j[None, :])
    c[:, 512:640] = 1.0
    c[:, 640:672] = np.arange(32)[None, :]
    c[:, 672:704] = (np.arange(32) * CAP)[None, :]
    c[:, 704] = (j < 64)
    c[:, 705] = (j >= 64)
    return c


def prep_shared(inp):
    f = lambda a: np.ascontiguousarray(a, dtype=np.float32)
    w_in = inp["w_in"][0]
    cols = np.concatenate([np.arange(0, O_GLR), np.arange(O_MA, N_IN)])
    wm = w_in[:, cols]
    sh = {}
    sh["w_in_t"] = f(wm.reshape(8, 128, 56, 128).transpose(2, 1, 0, 3).reshape(56, 128, 1024))
    sh["w_glr"] = f(w_in[:, O_GLR:O_GLR + 16].reshape(8, 128, 16).transpose(1, 0, 2).reshape(128, 128))
    chan = np.concatenate([inp["conv_w"][0], inp["conv_b"], inp["lru_b_r"], inp["lru_b_i"],
                           inp["lru_lambda"]], axis=0)
    sh["chanp"] = f(chan.reshape(8, 8, 128).transpose(2, 1, 0).reshape(128, 64))
    sh["lru_wr"] = f(inp["lru_w_r"][0].transpose(1, 0, 2).reshape(128, 1024))
    sh["lru_wi"] = f(inp["lru_w_i"][0].transpose(1, 0, 2).reshape(128, 1024))
    sh["gla_wg"] = f(inp["gla_w_gate"][0])
    sh["gla_bg"] = f(inp["gla_b_gate"])
    sh["gla_ng"] = f(inp["gla_norm_g"][0].reshape(2, 128).T)
    sh["w_out"] = f(inp["w_out"][0].reshape(8, 128, 1024).transpose(1, 0, 2).reshape(128, 8192))
    rows = np.concatenate([inp["ln1_g"], inp["ln1_b"], inp["ln2_g"], inp["ln2_b"],
                           inp["b_ple_gate"]], axis=0)
    sh["rowp"] = f(np.broadcast_to(rows.reshape(1, 5 * 1024), (128, 5 * 1024)))
    sh["w_router"] = f(inp["w_router"][0].reshape(8, 128, 32).transpose(1, 0, 2).reshape(128, 256))
    sh["b_router"] = f(inp["b_router"])
    sh["w_up"] = inp["w_up"][0]
    sh["b_up"] = f(inp["b_up"][0].reshape(32, 16, 128).transpose(2, 0, 1).reshape(128, 512))
    sh["w_down"] = inp["w_down"][0]
    sh["b_down"] = f(inp["b_down"][0])
    sh["w_ple"] = f(inp["w_ple"][0].reshape(2, 128, 1024).transpose(1, 0, 2).reshape(128, 2048))
    sh["w_pg"] = f(inp["w_ple_gate"][0].reshape(8, 128, 1024).transpose(1, 0, 2).reshape(128, 8192))
    sh["consts"] = _consts()
    return sh


def prep_core(inp, b):
    x = np.asarray(inp["x"][b], dtype=np.float32)
    p = np.asarray(inp["p"][0, b], dtype=np.float32)
    return {"x": np.ascontiguousarray(x), "xT": np.ascontiguousarray(x.T),
            "pT": np.ascontiguousarray(p.T)}


SHARED_SHAPES = {
    "w_in_t": [56, 128, 1024], "w_glr": [128, 128], "chanp": [128, 64], "lru_wr": [128, 1024],
    "lru_wi": [128, 1024], "gla_wg": [16, 512], "gla_bg": [1, 512], "gla_ng": [128, 2],
    "w_out": [128, 8192], "rowp": [128, 5120], "w_router": [128, 256], "b_router": [1, 32],
    "w_up": [32, 1024, 2048], "b_up": [128, 512], "w_down": [32, 1024, 1024], "b_down": [32, 1024],
    "w_ple": [128, 2048], "w_pg": [128, 8192], "consts": [128, 708],
}
CORE_SHAPES = {"x": [T, D], "xT": [D, T], "pT": [256, T]}


class StopBuild(Exception):
    pass


class Prog:
    ck_n = 0
    ck_stop = None

    def ck(self, label=""):
        self.ck_n += 1
        if self.ck_stop is not None and self.ck_n >= self.ck_stop:
            if not self.kb.disabled:
                print("STOP at checkpoint", self.ck_n, label)
            self.kb.disabled = True

    def __init__(self, dbg=(), stop_after=None):
        self.dbg = set(dbg)
        self.stop_after = stop_after
        self.nc = nc = bass.Bass("TRN2", target_bir_lowering=False)
        self.din = {}
        for n, s in list(SHARED_SHAPES.items()) + list(CORE_SHAPES.items()):
            self.din[n] = nc.dram_tensor(n, s, F32, kind="ExternalInput").ap()
        self.out = nc.dram_tensor("out", [T, D], F32, kind="ExternalOutput").ap()
        self.dbg_out = {}
        self.es = ExitStack()

    def dbg_tensor(self, name, shape):
        t = self.nc.dram_tensor("dbg_" + name, shape, F32, kind="ExternalOutput").ap()
        self.dbg_out[name] = t
        return t

    def sb(self, es, name, shape, dt=F32):
        return es.enter_context(self.nc.sbuf_tensor(name, shape, dt))

    def build(self):
        nc = self.nc
        with self.es as es:
            kb = self.kb = KB(nc, es)
            self.bound_reg = nc.gpsimd.to_reg(NSLOT - 1)
            self.cst = self.sb(es, "cst", [128, 708])
            self.b_cst = Buf("cst")
            kb.dma("sp", lambda e: e.dma_start(out=self.cst[:], in_=self.din["consts"][:, :]),
                   writes=[self.b_cst])
            self.cstb = self.sb(es, "cstb", [128, 708], BF16)
            self.b_cstb = Buf("cstb")
            kb.op("dve", lambda e: e.tensor_copy(self.cstb[:], self.cst[:]),
                  reads=[self.b_cst], writes=[self.b_cstb])
            self.GATES = self.sb(es, "GATES", [128, 16, 4])
            self.SLOTS = self.sb(es, "SLOTS", [128, 16, 4], U32)
            self.b_route = [Buf("route%d" % i) for i in range(16)]
            self.Xg = nc.dram_tensor("Xg", [NSLOT, D], BF16).ap()
            self.Yg = nc.dram_tensor("Yg", [NSLOT, D], F32).ap()
            self.H1d = nc.dram_tensor("H1d", [T, D], F32).ap()
            self.b_Xg, self.b_Yg, self.b_H1d = Buf("Xg"), Buf("Yg"), Buf("H1d")
            self.b_Xgz = Buf("Xgz")
            self.zero_xg(es)
            with ExitStack() as es_y:
                self.yT = self.sb(es_y, "yT", [128, 8, T], BF16)
                self.b_yT = [Buf("yT%d" % g) for g in range(8)]
                with ExitStack() as es1:
                    self.alloc_psum(es1, 8, 0)
                    self.XT = self.sb(es1, "XT", [128, 8, T], BF16)
                    self.b_XT = Buf("XT")
                    xT = self.din["xT"].rearrange("(kc p) t -> p kc t", p=128)
                    for kc in range(8):
                        kb.dma("pool", lambda e, kc=kc: e.dma_start(out=self.XT[:, kc, :], in_=xT[:, kc, :]),
                               cw=[self.b_XT])
                    if not getattr(self, "skip_lru", False):
                        self.phase_lru(es1)
                    else:
                        kb.op("dve", lambda e: e.memset(self.yT[:], 0.0), writes=self.b_yT)
                    if self.stop_after == "lru":
                        return self.finish()
                    kb.barrier()
                    self.phase_gla(es1)
                    if self.stop_after == "gla":
                        return self.dump_yT()
                kb.barrier()
                with ExitStack() as esw:
                    self.WU = [(self.sb(esw, "M_wu%d" % i, [128, 8, 2048], BF16), Buf("M_wu%d" % i)) for i in range(2)]
                    self.WD = [(self.sb(esw, "M_wd%d" % i, [128, 8, 1024], BF16), Buf("M_wd%d" % i)) for i in range(2)]
                    self.phase_outproj()
                    if self.stop_after == "outproj":
                        return self.finish()
                    kb.barrier()
                    self.phase_moe()
                    if self.stop_after == "moe":
                        return self.finish()
            kb.barrier()
            self.phase_final()
        return self.finish()

    def load_expert_w(self, ex):
        kb = self.kb
        sl = ex % 2
        WU, WD = self.WU, self.WD
        wu = self.din["w_up"][ex].rearrange("(kc p) f -> p kc f", p=128)
        wd = self.din["w_down"][ex].rearrange("(kc p) f -> p kc f", p=128)
        for q in range(4):
            kb.dma("pool", lambda e, q=q: e.dma_start(out=WU[sl][0][:, 2 * q:2 * q + 2, :], in_=wu[:, 2 * q:2 * q + 2, :]), cw=[WU[sl][1]])
        for q in range(2):
            kb.dma("pool", lambda e, q=q: e.dma_start(out=WD[sl][0][:, 4 * q:4 * q + 4, :], in_=wd[:, 4 * q:4 * q + 4, :]), cw=[WD[sl][1]])

    def alloc_psum(self, es, n32, n16):
        nc = self.nc
        self.ps = [es.enter_context(nc.psum_tensor("ps%d_%d" % (i, self.ps_gen), [128, 512], F32)) for i in range(n32)]
        self.psb = [Buf("ps%d" % i) for i in range(n32)]
        self.pst = [es.enter_context(nc.psum_tensor("pst%d_%d" % (i, self.ps_gen), [128, 1024], BF16)) for i in range(n16)]
        self.pstb = [Buf("pst%d" % i) for i in range(n16)]
        self.ps_rr = 0
        self.pst_rr = 0
        self.ps_gen += 1

    def zero_xg(self, es):
        kb = self.kb
        z = self.sb(es, "zeros", [128, 2, 1024], BF16)
        bz = Buf("zeros")
        kb.op("dve", lambda e: e.memset(z[:], 0.0), writes=[bz])
        xg = self.Xg.rearrange("(n p) d -> p n d", p=128)
        for i in range(NSLOT // 128 // 2):
            kb.dma("sp", lambda e, i=i: e.dma_start(out=xg[:, i * 2:(i + 1) * 2, :], in_=z[:]), reads=[bz], cw=[self.b_Xgz])

    ps_gen = 0

    def bank(self):
        i = self.ps_rr
        self.ps_rr = (self.ps_rr + 1) % len(self.ps)
        return self.ps[i], self.psb[i]

    def bank16(self):
        i = self.pst_rr
        self.pst_rr = (self.pst_rr + 1) % len(self.pst)
        return self.pst[i], self.pstb[i]

    def finish(self):
        kb = self.kb
        kb.disabled = False
        kb.barrier(["sp"])
        return self.nc

    def dump_yT(self):
        kb = self.kb
        kb.barrier()
        with ExitStack() as es:
            tmp = self.sb(es, "dump_tmp", [128, 8, T])
            b = Buf("dump_tmp")
            kb.op("dve", lambda e: e.tensor_copy(tmp[:], self.yT[:]), reads=self.b_yT, writes=[b])
            t = self.dbg_tensor("yT", [128, 8, T])
            kb.dma("sp", lambda e: e.dma_start(out=t, in_=tmp[:]), reads=[b])
            return self.finish()

    def dump(self, name, sb_ap, buf, shape):
        if name not in self.dbg:
            return
        t = self.dbg_tensor(name, shape)
        self.kb.dma("sp", lambda e: e.dma_start(out=t, in_=sb_ap), reads=[buf])

    def inproj_fm(self, wt, wb, ncols, tg, evac):
        kb = self.kb
        ps, pb = self.bank()
        for kc in range(8):
            kb.op("pe", lambda e, kc=kc: e.matmul(ps[0:ncols, :], wt[:, kc * ncols:(kc + 1) * ncols],
                                                   self.XT[:, kc, tg * 512:(tg + 1) * 512],
                                                   start=(kc == 0), stop=(kc == 7)),
                  reads=[wb, self.b_XT], writes=[pb], inc=(kc == 7))
        evac(ps, pb)

    def load_w(self, grp):
        i = self.w_rr
        self.w_rr = (self.w_rr + 1) % len(self.wring)
        wt, wb = self.wring[i], self.wringb[i]
        self.kb.dma("pool", lambda e: e.dma_start(out=wt[:], in_=self.din["w_in_t"][grp, :, :]), writes=[wb])
        return wt, wb

    def phase_lru(self, es1):
        nc, kb = self.nc, self.kb
        with ExitStack() as es:
            NW = 6
            self.wring = [self.sb(es, "wr%d" % i, [128, 1024], BF16) for i in range(NW)]
            self.wringb = [Buf("wr%d" % i) for i in range(NW)]
            self.w_rr = 0
            chan = self.sb(es, "chan", [128, 8, 8])
            b_chan = Buf("chan")
            kb.dma("sp", lambda e: e.dma_start(out=chan[:], in_=self.din["chanp"].rearrange("p (g k) -> p g k", k=8)),
                   writes=[b_chan])
            wr = self.sb(es, "lwr", [128, 8, 128], BF16)
            wi = self.sb(es, "lwi", [128, 8, 128], BF16)
            b_wr, b_wi = Buf("lwr"), Buf("lwi")
            kb.dma("pool", lambda e: e.dma_start(out=wr[:], in_=self.din["lru_wr"].rearrange("p (g d) -> p g d", d=128)), writes=[b_wr])
            kb.dma("pool", lambda e: e.dma_start(out=wi[:], in_=self.din["lru_wi"].rearrange("p (g d) -> p g d", d=128)), writes=[b_wi])
            sc = self.sb(es, "lsc", [128, 8, 4])
            b_sc = Buf("lsc")
            kb.op("act", lambda e: e.activation(out=sc[:, :, 0], in_=chan[:, :, 7], func=AF.Exp, scale=-1.0),
                  reads=[b_chan], writes=[b_sc])
            kb.op("act", lambda e: e.activation(out=sc[:, :, 1], in_=sc[:, :, 0], func=AF.Ln, bias=1.0),
                  reads=[b_sc], writes=[b_sc])
            kb.op("dve", lambda e: e.tensor_scalar_mul(sc[:, :, 2], sc[:, :, 1], -8.0), reads=[b_sc], writes=[b_sc])
            kb.op("dve", lambda e: e.tensor_scalar_mul(sc[:, :, 3], sc[:, :, 1], -16.0), reads=[b_sc], writes=[b_sc])

            def mk(name, shape, dt=F32):
                return self.sb(es, "L_" + name, shape, dt), [Buf("L_%s_%d" % (name, i)) for i in range(4)]
            xa, b_xa = mk("xa", [128, T + 3], BF16)
            DG = self.sb(es, "L_dg", [128, 8, 4, 128], BF16)
            b_DG = Buf("L_dg")
            for g_ in range(8):
                for k_ in range(4):
                    kb.op("dve", lambda e, g_=g_, k_=k_: e.tensor_scalar_mul(DG[:, g_, k_, :], self.cstb[:, 0:128], chan[:, g_, k_:k_ + 1]),
                          reads=[self.b_cstb, b_chan], cw=[b_DG])
            xcb, b_xcb = mk("xcb", [128, T], BF16)
            xc, b_xc = mk("xc", [128, T])
            r, b_r = mk("r", [128, T])
            ii, b_ii = mk("i", [128, T])
            aa, b_aa = mk("a", [128, T])
            mm, b_mm = mk("m", [128, T])
            h, b_h = mk("h", [128, T])
            ga, b_ga = mk("ga", [128, T])
            t1, b_t1 = mk("t1", [128, T])
            t2, b_t2 = mk("t2", [128, T])
            b_pad = Buf("xa_pad")
            kb.op("dve", lambda e: e.memset(xa[:, 0:3], 0.0), writes=[b_pad])
            TG = range(4)

            def cs(tg, off=0):
                return slice(off + tg * 512, off + (tg + 1) * 512)

            for g in range(8):
                w_xa, wb_xa = self.load_w(g)
                w_ga, wb_ga = self.load_w(8 + g)
                w_ma, wb_ma = self.load_w(40 + g)
                for tg in TG:
                    self.inproj_fm(w_xa, wb_xa, 128, tg, lambda ps, pb, tg=tg: kb.op(
                        "act", lambda e: e.activation(out=xa[:, cs(tg, 3)], in_=ps[:, :], func=AF.Copy), reads=[pb], writes=[b_xa[tg]]))
                for tg in TG:
                    self.inproj_fm(w_ga, wb_ga, 128, tg, lambda ps, pb, tg=tg: kb.op(
                        "act", lambda e: e.activation(out=ga[:, cs(tg)], in_=ps[:, :], func=AF.Copy), reads=[pb], writes=[b_ga[tg]]))
                for tg in TG:
                    self.inproj_fm(w_ma, wb_ma, 128, tg, lambda ps, pb, tg=tg: kb.op(
                        "act", lambda e: e.activation(out=t2[:, cs(tg)], in_=ps[:, :], func=AF.Sigmoid), reads=[pb], writes=[b_t2[tg]]))
                for tg in TG:
                    kb.op("dve", lambda e, tg=tg: e.tensor_tensor(t1[:, cs(tg)], ga[:, cs(tg)], ga[:, cs(tg)], ALU.mult), reads=[b_ga[tg]], writes=[b_t1[tg]])
                    kb.op("dve", lambda e, tg=tg: e.tensor_scalar(t1[:, cs(tg)], t1[:, cs(tg)], 0.044715, 1.0, ALU.mult, ALU.add), reads=[b_t1[tg]], writes=[b_t1[tg]])
                    kb.op("dve", lambda e, tg=tg: e.tensor_tensor(t1[:, cs(tg)], t1[:, cs(tg)], ga[:, cs(tg)], ALU.mult), reads=[b_t1[tg], b_ga[tg]], writes=[b_t1[tg]])
                for tg in TG:
                    prev = [b_xa[tg - 1]] if tg > 0 else [b_pad]
                    ps, pb = self.bank()
                    for k in range(4):
                        kb.op("pe", lambda e, k=k, tg=tg, ps=ps: e.matmul(ps[:, :], DG[:, g, k, :], xa[:, cs(tg, k)], start=(k == 0), stop=(k == 3)),
                              reads=[b_DG, b_xa[tg]] + prev, writes=[pb], inc=(k == 3))
                    kb.op("act", lambda e, tg=tg, ps=ps: e.activation(out=xc[:, cs(tg)], in_=ps[:, :], func=AF.Identity, bias=chan[:, g, 4:5]),
                          reads=[pb, b_chan], writes=[b_xc[tg]])
                    kb.op("dve", lambda e, tg=tg: e.tensor_copy(xcb[:, cs(tg)], xc[:, cs(tg)]), reads=[b_xc[tg]], writes=[b_xcb[tg]])
                if g == 0:
                    self.dump("xc0", xc[:, :], b_xc[3], [128, T])
                for (wg, bwg, dst, b_dst, bi) in ((wr, b_wr, r, b_r, 5), (wi, b_wi, ii, b_ii, 6)):
                    for tg in TG:
                        ps, pb = self.bank()
                        kb.op("pe", lambda e, tg=tg, ps=ps, wg=wg: e.matmul(ps[:, :], wg[:, g, :], xcb[:, cs(tg)], start=True, stop=True),
                              reads=[bwg, b_xcb[tg]], writes=[pb])
                        kb.op("act", lambda e, tg=tg, ps=ps, dst=dst, bi=bi: e.activation(
                            out=dst[:, cs(tg)], in_=ps[:, :], func=AF.Sigmoid, bias=chan[:, g, bi:bi + 1]),
                            reads=[pb, b_chan], writes=[b_dst[tg]])
                for tg in TG:
                    kb.op("act", lambda e, tg=tg: e.activation(out=aa[:, cs(tg)], in_=r[:, cs(tg)], func=AF.Exp, scale=sc[:, g, 2:3]),
                          reads=[b_r[tg], b_sc], writes=[b_aa[tg]])
                for tg in TG:
                    kb.op("act", lambda e, tg=tg: e.activation(out=mm[:, cs(tg)], in_=r[:, cs(tg)], func=AF.Exp, scale=sc[:, g, 3:4]),
                          reads=[b_r[tg], b_sc], writes=[b_mm[tg]])
                for tg in TG:
                    kb.op("act", lambda e, tg=tg: e.activation(out=mm[:, cs(tg)], in_=mm[:, cs(tg)], func=AF.Ln, scale=-1.0, bias=1.0),
                          reads=[b_mm[tg]], writes=[b_mm[tg]])
                for tg in TG:
                    kb.op("act", lambda e, tg=tg: e.activation(out=mm[:, cs(tg)], in_=mm[:, cs(tg)], func=AF.Exp, scale=0.5),
                          reads=[b_mm[tg]], writes=[b_mm[tg]])
                for tg in TG:
                    kb.op("dve", lambda e, tg=tg: e.tensor_tensor(mm[:, cs(tg)], mm[:, cs(tg)], ii[:, cs(tg)], ALU.mult), reads=[b_mm[tg], b_ii[tg]], writes=[b_mm[tg]])
                    kb.op("dve", lambda e, tg=tg: e.tensor_tensor(mm[:, cs(tg)], mm[:, cs(tg)], xc[:, cs(tg)], ALU.mult), reads=[b_mm[tg], b_xc[tg]], writes=[b_mm[tg]])
                for tg in TG:
                    init = 0.0 if tg == 0 else h[:, tg * 512 - 1:tg * 512]
                    kb.op("dve", lambda e, tg=tg, init=init: e.tensor_tensor_scan(h[:, cs(tg)], aa[:, cs(tg)], mm[:, cs(tg)], init, ALU.mult, ALU.add),
                          reads=[b_aa[tg], b_mm[tg]] + ([b_h[tg - 1]] if tg > 0 else []), writes=[b_h[tg]])
                if g == 0:
                    self.dump("h0", h[:, :], b_h[3], [128, T])
                for tg in TG:
                    kb.op("act", lambda e, tg=tg: e.activation(out=t1[:, cs(tg)], in_=t1[:, cs(tg)], func=AF.Sigmoid, scale=1.5957691216057308),
                          reads=[b_t1[tg]], writes=[b_t1[tg]])
                for tg in TG:
                    kb.op("dve", lambda e, tg=tg: e.tensor_tensor(t1[:, cs(tg)], t1[:, cs(tg)], ga[:, cs(tg)], ALU.mult), reads=[b_t1[tg], b_ga[tg]], writes=[b_t1[tg]])
                    kb.op("dve", lambda e, tg=tg: e.tensor_tensor(t1[:, cs(tg)], t1[:, cs(tg)], h[:, cs(tg)], ALU.mult), reads=[b_t1[tg], b_h[tg]], writes=[b_t1[tg]])
                    kb.op("dve", lambda e, tg=tg: e.tensor_tensor(self.yT[:, g, cs(tg)], t1[:, cs(tg)], t2[:, cs(tg)], ALU.mult),
                          reads=[b_t1[tg], b_t2[tg]], cw=[self.b_yT[g]])

    def phase_gla(self, es1):
        nc, kb = self.nc, self.kb
        cst = self.cstb
        TRI, UU, ONES = cst[:, 128:256], cst[:, 256:384], cst[:, 512:640]
        TRI32 = self.cst[:, 128:256]
        bc = self.b_cstb
        with ExitStack() as es:
            NW = 8
            self.wring = [self.sb(es, "gw%d" % i, [128, 1024], BF16) for i in range(NW)]
            self.wringb = [Buf("gw%d" % i) for i in range(NW)]
            self.w_rr = 0
            wglr = self.sb(es, "wglr", [128, 128], BF16)
            b_wglr = Buf("wglr")
            kb.dma("pool", lambda e: e.dma_start(out=wglr[:], in_=self.din["w_glr"][:, :]), writes=[b_wglr])
            wg = self.sb(es, "wg", [16, 512], BF16)
            bg = self.sb(es, "bg", [1, 512], BF16)
            ng = self.sb(es, "ng", [128, 2])
            b_wg, b_bg, b_ng = Buf("wg"), Buf("bg"), Buf("ng")
            kb.dma("pool", lambda e: e.dma_start(out=wg[:], in_=self.din["gla_wg"][:, :]), writes=[b_wg])
            kb.dma("pool", lambda e: e.dma_start(out=bg[:], in_=self.din["gla_bg"][:, :]), writes=[b_bg])
            kb.dma("sp", lambda e: e.dma_start(out=ng[:], in_=self.din["gla_ng"][:, :]), writes=[b_ng])
            glrT = self.sb(es, "glrT", [16, T], BF16)
            b_glrT = Buf("glrT")
            for tg in range(4):
                self.inproj_fm(wglr, b_wglr, 16, tg, lambda ps, pb, tg=tg: kb.op(
                    "act", lambda e: e.activation(out=glrT[:, tg * 512:(tg + 1) * 512], in_=ps[0:16, :], func=AF.Copy),
                    reads=[pb], cw=[b_glrT]))

            self.ck("glrT")

            def mk(name, shape, dt=F32):
                return self.sb(es, "G_" + name, shape, dt), Buf("G_" + name)
            QT, b_QT = mk("QT", [128, T], BF16)
            KT, b_KT = mk("KT", [128, T], BF16)
            KD0, b_KD0 = mk("KD0", [128, 16, 128], BF16)
            KD1, b_KD1 = mk("KD1", [128, 16, 128], BF16)
            V, b_V = mk("V", [128, 16, 256], BF16)
            OT, b_OT = mk("OT", [128, 2, T])
            EB, b_EB = mk("EB", [128, 32])
            Gsp, b_Gsp = mk("Gsp", [128, 4, 128], BF16)
            Gz, b_Gz = mk("Gz", [128, 4, 128])
            EQ, b_EQ = mk("EQ", [128, 512])
            EK, b_EK = mk("EK", [128, 512])
            ED, b_ED = mk("ED", [128, 4, 128])
            STs = [mk("ST%d" % i, [128, 128], BF16) for i in range(2)]
            Sb = [mk("S%d" % i, [128, 256]) for i in range(4)]
            Sbb = [mk("Sb%d" % i, [128, 256], BF16) for i in range(4)]
            SQ = [mk("SQ%d" % i, [128, 512], BF16) for i in range(2)]
            RIf, b_RIf = mk("RIf", [128, T])
            SG, b_SG = mk("SG", [128, 512])
            SM, b_SM = mk("SM", [128, 512])
            TT, b_TT = mk("TT", [128, 512])

            HW = {}

            def h1(hd, tg):
                if tg == 0:
                    HW[hd] = dict(q=self.load_w(16 + hd), k=self.load_w(20 + hd), v=[self.load_w(24 + 2 * hd + j) for j in range(2)])
                w_q, wb_q = HW[hd]['q']
                w_k, wb_k = HW[hd]['k']
                w_v = HW[hd]['v']
                ps, pb = self.bank()
                for j in range(4):
                    tt = tg * 4 + j
                    kb.op("pe", lambda e, j=j, tt=tt, ps=ps: e.matmul(ps[:, j * 128:(j + 1) * 128], glrT[0:16, tt * 128:(tt + 1) * 128],
                                                                  wg[0:16, hd * 128:(hd + 1) * 128], start=True, stop=False),
                          reads=[b_glrT, b_wg], writes=[pb], inc=False)
                    kb.op("pe", lambda e, j=j, ps=ps: e.matmul(ps[:, j * 128:(j + 1) * 128], cst[0:1, 512:640],
                                                           bg[0:1, hd * 128:(hd + 1) * 128], start=False, stop=True),
                          reads=[bc, b_bg], writes=[pb], inc=(j == 3))
                kb.op("act", lambda e, ps=ps: e.activation(out=Gz[:, :, :], in_=ps[:, :].rearrange("p (j k) -> p j k", k=128),
                                                          func=AF.Exp, scale=-1.0), reads=[pb], writes=[b_Gz])
                kb.op("act", lambda e: e.activation(out=Gsp[:, :, :], in_=Gz[:, :, :], func=AF.Ln, bias=1.0),
                      reads=[b_Gz], writes=[b_Gsp])
                self.ck("z/Gsp")
                ps_c, pb_c = self.bank()
                ps_r, pb_r = self.bank()
                for j in range(4):
                    kb.op("pe", lambda e, j=j, ps_c=ps_c: e.matmul(ps_c[:, j * 128:(j + 1) * 128], Gsp[:, j, :], TRI, start=True, stop=True),
                          reads=[b_Gsp, bc], writes=[pb_c], inc=False)
                    kb.op("pe", lambda e, j=j, ps_r=ps_r: e.matmul(ps_r[:, j * 128:(j + 1) * 128], UU, Gsp[:, j, :], start=True, stop=True),
                          reads=[b_Gsp, bc], writes=[pb_r], inc=(j == 3))
                kb.op("act", lambda e, ps_c=ps_c: e.activation(out=EQ[:, :], in_=ps_c[:, :], func=AF.Exp, scale=-1.0 / 16), reads=[pb_c], writes=[b_EQ])
                kb.op("act", lambda e, ps_c=ps_c: e.activation(out=EK[:, :], in_=ps_c[:, :], func=AF.Exp, scale=1.0 / 16), reads=[pb_c], writes=[b_EK])
                kb.op("act", lambda e, ps_r=ps_r: e.activation(out=ED[:, :, :], in_=ps_r[:, :].rearrange("p (j k) -> p j k", k=128),
                                                            func=AF.Exp, scale=-1.0 / 16), reads=[pb_r], writes=[b_ED])
                kb.op("dve", lambda e, tg=tg: e.tensor_copy(EB[:, tg * 4:(tg + 1) * 4], EQ[:, 127:512:128]), reads=[b_EQ], cw=[b_EB])
                self.ck("cs/rev/E")
                self.inproj_fm(w_q, wb_q, 128, tg, lambda ps, pb, tg=tg: kb.op(
                    "dve", lambda e: e.scalar_tensor_tensor(QT[:, tg * 512:(tg + 1) * 512], ps[:, :], 128.0 ** -0.5, EQ[:, :], ALU.mult, ALU.mult),
                    reads=[pb, b_EQ], cw=[b_QT]))
                self.inproj_fm(w_k, wb_k, 128, tg, lambda ps, pb, tg=tg: kb.op(
                    "dve", lambda e: e.tensor_tensor(KT[:, tg * 512:(tg + 1) * 512], ps[:, :], EK[:, :], ALU.mult),
                    reads=[pb, b_EK], cw=[b_KT]))
                self.ck("qk fm")
                ps, pb = self.bank()
                for j in range(4):
                    tt = tg * 4 + j
                    for kc in range(8):
                        kb.op("pe", lambda e, j=j, tt=tt, kc=kc, ps=ps: e.matmul(ps[:, j * 128:(j + 1) * 128], self.XT[:, kc, tt * 128:(tt + 1) * 128],
                                                                             w_k[:, kc * 128:(kc + 1) * 128], start=(kc == 0), stop=(kc == 7)),
                              reads=[self.b_XT, wb_k], writes=[pb], inc=(j == 3 and kc == 7))
                kb.op("dve", lambda e, ps=ps, tg=tg: e.tensor_tensor(KD0[:, tg * 4:(tg + 1) * 4, :], ps[:, :].rearrange("p (j k) -> p j k", k=128),
                                                                   ED[:, :, :], ALU.mult), reads=[pb, b_ED], cw=[b_KD0])
                self.ck("kd")
                for jj in range(2):
                    ps, pb = self.bank()
                    for j2 in range(2):
                        tt = tg * 4 + jj * 2 + j2
                        for half in range(2):
                            wv, wbv = w_v[half]
                            for kc in range(8):
                                kb.op("pe", lambda e, j2=j2, tt=tt, kc=kc, ps=ps, half=half, wv=wv: e.matmul(
                                    ps[:, j2 * 256 + half * 128:j2 * 256 + (half + 1) * 128], self.XT[:, kc, tt * 128:(tt + 1) * 128],
                                    wv[:, kc * 128:(kc + 1) * 128], start=(kc == 0), stop=(kc == 7)),
                                    reads=[self.b_XT, wbv], writes=[pb], inc=(j2 == 1 and half == 1 and kc == 7))
                    t0 = tg * 4 + jj * 2
                    kb.op("dve", lambda e, ps=ps, t0=t0: e.tensor_copy(V[:, t0:t0 + 2, :], ps[:, :].rearrange("p (j v) -> p j v", v=256)),
                          reads=[pb], cw=[b_V])
                self.ck("v")

            def h2(hd):
                kb.op("dve", lambda e: e.memset(Sb[0][0][:, :], 0.0), writes=[Sb[0][1]])
                kb.op("dve", lambda e: e.memset(Sbb[0][0][:, :], 0.0), writes=[Sbb[0][1]])
                for tt in range(16):
                    c = tt
                    ps_st, pb_st = self.ps[tt % 2], self.psb[tt % 2]
                    st, b_st = STs[tt % 2]
                    kb.op("pe", lambda e: e.matmul(ps_st[:, 0:128], KT[:, tt * 128:(tt + 1) * 128], QT[:, tt * 128:(tt + 1) * 128], start=True, stop=True),
                          reads=[b_KT, b_QT], writes=[pb_st])
                    kb.op("dve", lambda e: e.tensor_tensor(st[:, :], ps_st[:, 0:128], TRI32, ALU.mult), reads=[pb_st, self.b_cst], writes=[b_st])
                    ps_kv, pb_kv = self.ps[2 + tt % 2], self.psb[2 + tt % 2]
                    kb.op("pe", lambda e: e.matmul(ps_kv[:, 0:256], KD0[:, tt, :], V[:, tt, :], start=True, stop=True),
                          reads=[b_KD0, b_V], writes=[pb_kv])
                    self.ck("st/kv")
                    grp = (tt // 4) % 2
                    col = (tt % 4) * 128
                    for vc in range(2):
                        pso, pbo = self.ps[4 + 2 * grp + vc], self.psb[4 + 2 * grp + vc]
                        kb.op("pe", lambda e, vc=vc, pso=pso: e.matmul(pso[:, col:col + 128], V[:, tt, vc * 128:(vc + 1) * 128], st[:, :], start=True, stop=False),
                              reads=[b_V, b_st], writes=[pbo], inc=False)
                        kb.op("pe", lambda e, vc=vc, pso=pso: e.matmul(pso[:, col:col + 128], Sbb[c % 4][0][:, vc * 128:(vc + 1) * 128], QT[:, tt * 128:(tt + 1) * 128],
                                                                    start=False, stop=True), reads=[Sbb[c % 4][1], b_QT], writes=[pbo], inc=True)
                    kb.op("dve", lambda e: e.scalar_tensor_tensor(Sb[(c + 1) % 4][0][:, :], Sb[c % 4][0][:, :], EB[:, c:c + 1], ps_kv[:, 0:256], ALU.mult, ALU.add),
                          reads=[Sb[c % 4][1], b_EB, pb_kv], writes=[Sb[(c + 1) % 4][1]])
                    kb.op("act", lambda e: e.activation(out=Sbb[(c + 1) % 4][0][:, :], in_=Sb[(c + 1) % 4][0][:, :], func=AF.Copy),
                          reads=[Sb[(c + 1) % 4][1]], writes=[Sbb[(c + 1) % 4][1]])
                    self.ck("o tile")
                    if tt % 4 == 3:
                        tg = tt // 4
                        for vc in range(2):
                            pso, pbo = self.ps[4 + 2 * grp + vc], self.psb[4 + 2 * grp + vc]
                            kb.op("act", lambda e, vc=vc, pso=pso, tg=tg: e.activation(out=OT[:, vc, tg * 512:(tg + 1) * 512], in_=pso[:, :], func=AF.Copy),
                                  reads=[pbo], cw=[b_OT])
                if hd == 0:
                    self.dump("o_raw0", OT[:, 0, :], b_OT, [128, T])
                for tg in range(4):
                    sl = slice(tg * 512, (tg + 1) * 512)
                    for vc in range(2):
                        kb.op("act", lambda e, vc=vc, sl=sl: e.activation(out=SQ[vc][0][:, :], in_=OT[:, vc, sl], func=AF.Square), reads=[b_OT], writes=[SQ[vc][1]])
                    ps, pb = self.bank()
                    for vc in range(2):
                        kb.op("pe", lambda e, vc=vc, ps=ps: e.matmul(ps[:, :], ONES, SQ[vc][0][:, :], start=(vc == 0), stop=(vc == 1)),
                              reads=[bc, SQ[vc][1]], writes=[pb], inc=(vc == 1))
                    kb.op("act", lambda e, ps=ps, sl=sl: e.activation(out=RIf[:, sl], in_=ps[:, :], func=AF.Sqrt, scale=1.0 / 256, bias=1e-5), reads=[pb], cw=[b_RIf])
                kb.op("dve", lambda e: e.reciprocal(RIf[:, :], RIf[:, :]), reads=[b_RIf], writes=[b_RIf])

            def h3(hd, tg):
                if tg == 0:
                    HW[hd]['go'] = [self.load_w(32 + 2 * hd + j) for j in range(2)]
                    HW[hd]['mb'] = [self.load_w(48 + 2 * hd + j) for j in range(2)]
                w_go, w_mb = HW[hd]['go'], HW[hd]['mb']
                sl = slice(tg * 512, (tg + 1) * 512)
                for vc in range(2):
                    g = hd * 2 + vc
                    go_ps = {}
                    self.inproj_fm(w_go[vc][0], w_go[vc][1], 128, tg, lambda ps, pb: (go_ps.update(ps=ps, pb=pb), kb.op(
                        "act", lambda e: e.activation(out=SG[:, :], in_=ps[:, :], func=AF.Sigmoid), reads=[pb], writes=[b_SG])))
                    self.inproj_fm(w_mb[vc][0], w_mb[vc][1], 128, tg, lambda ps, pb: kb.op(
                        "act", lambda e: e.activation(out=SM[:, :], in_=ps[:, :], func=AF.Sigmoid), reads=[pb], writes=[b_SM]))
                    kb.op("dve", lambda e, vc=vc, sl=sl: e.scalar_tensor_tensor(TT[:, :], OT[:, vc, sl], ng[:, vc:vc + 1], RIf[:, sl], ALU.mult, ALU.mult),
                          reads=[b_OT, b_ng, b_RIf], writes=[b_TT])
                    kb.op("dve", lambda e: e.tensor_tensor(TT[:, :], TT[:, :], SG[:, :], ALU.mult), reads=[b_TT, b_SG], writes=[b_TT])
                    kb.op("dve", lambda e: e.tensor_tensor(TT[:, :], TT[:, :], go_ps["ps"][:, :], ALU.mult), reads=[b_TT, b_SG, go_ps["pb"]], writes=[b_TT])
                    kb.op("dve", lambda e: e.tensor_tensor(TT[:, :], TT[:, :], SM[:, :], ALU.mult), reads=[b_TT, b_SM], writes=[b_TT])
                    kb.op("dve", lambda e, g=g, sl=sl: e.tensor_tensor(self.yT[:, g, sl], TT[:, :], self.yT[:, g, sl], ALU.add),
                          reads=[b_TT, self.b_yT[g]], writes=[self.b_yT[g]])


            for hd in range(4):
                for tg in range(4):
                    h1(hd, tg)
                    if hd > 0:
                        h3(hd - 1, tg)
                h2(hd)
            for tg in range(4):
                h3(3, tg)

    def layer_norm(self, es_tmp, R, b_R, grow, brow, OUT, b_OUT, tag):
        kb = self.kb
        st = self.ln_st
        kb.op("dve", lambda e: e.bn_stats(st["stats"][:, 0, :], R[:, 0:512]), reads=[b_R], writes=[st["b"]])
        kb.op("dve", lambda e: e.bn_stats(st["stats"][:, 1, :], R[:, 512:1024]), reads=[b_R], writes=[st["b"]])
        kb.op("dve", lambda e: e.bn_aggr(st["mv"][:, :], st["stats"][:, :, :].rearrange("p a b -> p (a b)")), reads=[st["b"]], writes=[st["b"]])
        kb.op("act", lambda e: e.activation(out=st["rs"][:, 0:1], in_=st["mv"][:, 1:2], func=AF.Ln, bias=1e-5), reads=[st["b"]], writes=[st["b2"]])
        kb.op("act", lambda e: e.activation(out=st["rs"][:, 0:1], in_=st["rs"][:, 0:1], func=AF.Exp, scale=-0.5), reads=[st["b2"]], writes=[st["b2"]])
        kb.op("dve", lambda e: e.scalar_tensor_tensor(st["rs"][:, 1:2], st["mv"][:, 0:1], -1.0, st["rs"][:, 0:1], ALU.mult, ALU.mult),
              reads=[st["b"], st["b2"]], writes=[st["b2"]])
        kb.op("act", lambda e: e.activation(out=OUT[:, :], in_=R[:, :], func=AF.Identity, scale=st["rs"][:, 0:1], bias=st["rs"][:, 1:2]),
              reads=[b_R, st["b2"]], writes=[b_OUT])
        kb.op("dve", lambda e: e.tensor_tensor(OUT[:, :], OUT[:, :], self.rowp[:, grow, :], ALU.mult), reads=[b_OUT, self.b_rowp], writes=[b_OUT])
        kb.op("dve", lambda e: e.tensor_tensor(OUT[:, :], OUT[:, :], self.rowp[:, brow, :], ALU.add), reads=[b_OUT, self.b_rowp], writes=[b_OUT])

    def alloc_ln(self, es):
        g = self.ps_gen
        self.rowp = self.sb(es, "rowp_sb%d" % g, [128, 5, 1024])
        self.b_rowp = Buf("rowp")
        self.kb.dma("sp", lambda e: e.dma_start(out=self.rowp[:], in_=self.din["rowp"].rearrange("p (r d) -> p r d", d=1024)),
                    writes=[self.b_rowp])
        self.ln_st = {"stats": self.sb(es, "ln_stats%d" % g, [128, 2, 6]), "mv": self.sb(es, "ln_mv%d" % g, [128, 2]),
                      "rs": self.sb(es, "ln_rs%d" % g, [128, 2]), "b": Buf("ln_b"), "b2": Buf("ln_b2")}

    def phase_outproj(self):
        nc, kb = self.nc, self.kb
        cstb, bcb = self.cstb, self.b_cstb
        IDb, LTb, ONEb = cstb[:, 0:128], cstb[:, 384:512], cstb[:, 512:640]
        with ExitStack() as es:
            self.alloc_psum(es, 6, 2)
            self.alloc_ln(es)

            def mk(name, shape, dt=F32):
                return self.sb(es, "P2_" + name, shape, dt), Buf("P2_" + name)
            Wout, b_Wout = mk("Wout", [128, 8, 1024], BF16)
            kb.dma("pool", lambda e: e.dma_start(out=Wout[:, 0:4, :], in_=self.din["w_out"].rearrange("p (k n) -> p k n", n=1024)[:, 0:4, :]), cw=[b_Wout])
            kb.dma("pool", lambda e: e.dma_start(out=Wout[:, 4:8, :], in_=self.din["w_out"].rearrange("p (k n) -> p k n", n=1024)[:, 4:8, :]), cw=[b_Wout])
            wr32, b_wr32 = mk("wr32", [128, 8, 32])
            wrh, b_wrh = mk("wrh", [128, 8, 32], BF16)
            wrl, b_wrl = mk("wrl", [128, 8, 32], BF16)
            kb.dma("sp", lambda e: e.dma_start(out=wr32[:], in_=self.din["w_router"].rearrange("p (k n) -> p k n", n=32)), writes=[b_wr32])
            kb.op("dve", lambda e: e.tensor_copy(wrh[:], wr32[:]), reads=[b_wr32], writes=[b_wrh])
            kb.op("dve", lambda e: e.tensor_tensor(wrl[:], wr32[:], wrh[:], ALU.subtract), reads=[b_wr32, b_wrh], writes=[b_wrl])
            brt, b_brt = mk("brt", [128, 32])
            kb.dma("sp", lambda e: e.dma_start(out=brt[:], in_=self.din["b_router"][0:1, :].partition_broadcast(128)), writes=[b_brt])
            carry, b_carry = mk("carry", [128, 32])
            kb.op("dve", lambda e: e.memset(carry[:], 0.0), writes=[b_carry])
            Xt = [mk("x%d" % i, [128, 1024]) for i in range(2)]
            R, b_R = mk("R", [128, 1024])
            H1 = [mk("H1_%d" % i, [128, 1024]) for i in range(2)]
            H1b = [mk("H1b_%d" % i, [128, 1024], BF16) for i in range(2)]
            H1l = [mk("H1l_%d" % i, [128, 1024], BF16) for i in range(2)]
            HT = [mk("HT_%d" % i, [128, 8, 128], BF16) for i in range(2)]
            lg, b_lg = mk("lg", [128, 32])
            v8, b_v8 = mk("v8", [128, 8])
            i8, b_i8 = mk("i8", [128, 8], U32)
            i8f, b_i8f = mk("i8f", [128, 8])
            sm, b_sm = mk("sm", [128, 8])
            mask, b_mask = mk("mask", [128, 32], BF16)
            sc, b_sc = mk("sc", [128, 32])
            ov, b_ov = mk("ov", [128, 32])
            junk, b_junk = mk("junk", [128, 32])
            slf, b_slf = mk("slf", [128, 4])

            for tt in range(16):
                tsl = slice(tt * 128, (tt + 1) * 128)
                xt, b_xt = Xt[tt % 2]
                if tt in (2, 8):
                    self.load_expert_w(0 if tt == 2 else 1)
                kb.dma("sp", lambda e: e.dma_start(out=xt[:], in_=self.din["x"][tsl, :]), writes=[b_xt])
                for half in range(2):
                    ps, pb = self.bank()
                    for kc in range(8):
                        kb.op("pe", lambda e, kc=kc, ps=ps, half=half: e.matmul(ps[:, :], self.yT[:, kc, tsl], Wout[:, kc, half * 512:(half + 1) * 512],
                                                                             start=(kc == 0), stop=(kc == 7)),
                              reads=[self.b_yT[kc], b_Wout], writes=[pb], inc=(kc == 7))
                    kb.op("dve", lambda e, ps=ps, half=half: e.scalar_tensor_tensor(R[:, half * 512:(half + 1) * 512], xt[:, half * 512:(half + 1) * 512], ALPHA,
                                                                                  ps[:, :], ALU.mult, ALU.add), reads=[b_xt, pb], cw=[b_R])
                h1, b_h1 = H1[tt % 2]
                self.layer_norm(es, R, b_R, 0, 1, h1, b_h1, "ln1")
                kb.dma("sp", lambda e: e.dma_start(out=self.H1d[tsl, :], in_=h1[:]), reads=[b_h1], cw=[self.b_H1d])
                if tt == 0:
                    self.dump("h1_0", h1[:], b_h1, [128, 1024])
                hb, b_hb = H1b[tt % 2]
                hl, b_hl = H1l[tt % 2]
                kb.op("act", lambda e: e.activation(out=hb[:, :], in_=h1[:, :], func=AF.Identity), reads=[b_h1], writes=[b_hb])
                kb.op("dve", lambda e: e.tensor_tensor(hl[:, :], h1[:, :], hb[:, :], ALU.subtract), reads=[b_h1, b_hb], writes=[b_hl])
                for (src, b_src, (dst, b_dst)) in ((hb, b_hb, HT[0]), (hl, b_hl, HT[1])):
                    pt, ptb = self.bank16()
                    for kc in range(8):
                        kb.op("pe", lambda e, kc=kc, pt=pt, src=src: e.transpose(pt[:, kc * 128:(kc + 1) * 128], src[:, kc * 128:(kc + 1) * 128], IDb),
                              reads=[b_src, bcb], writes=[ptb], inc=(kc == 7))
                    kb.op("dve", lambda e, pt=pt, dst=dst: e.tensor_copy(dst[:, :, :], pt[:, :].rearrange("p (k t) -> p k t", t=128)),
                          reads=[ptb], writes=[b_dst])
                ps, pb = self.bank()
                combos = [(HT[0], wrh, b_wrh), (HT[0], wrl, b_wrl), (HT[1], wrh, b_wrh)]
                n = 0
                for (ht, b_ht), w, b_w in combos:
                    for kc in range(8):
                        n += 1
                        kb.op("pe", lambda e, kc=kc, ps=ps, ht=ht, w=w, n=n: e.matmul(ps[:, 0:32], ht[:, kc, :], w[:, kc, :], start=(n == 1), stop=(n == 24)),
                              reads=[b_ht, b_w], writes=[pb], inc=(n == 24))
                kb.op("dve", lambda e, ps=ps: e.tensor_tensor(lg[:, :], ps[:, 0:32], brt[:, :], ALU.add), reads=[pb, b_brt], writes=[b_lg])
                if tt == 0:
                    self.dump("lg_0", lg[:], b_lg, [128, 32])
                kb.op("dve", lambda e: e.max(out=v8[:, :], in_=lg[:, :]), reads=[b_lg], writes=[b_v8])
                kb.op("dve", lambda e: e.max_index(out=i8[:, :], in_max=v8[:, :], in_values=lg[:, :]), reads=[b_lg, b_v8], writes=[b_i8])
                kb.op("dve", lambda e: e.tensor_copy(i8f[:, :], i8[:, :]), reads=[b_i8], writes=[b_i8f])
                kb.op("dve", lambda e: e.tensor_scalar_mul(sm[:, 0:1], v8[:, 0:1], -1.0), reads=[b_v8], writes=[b_sm])
                kb.op("act", lambda e: e.activation(out=sm[:, 4:8], in_=v8[:, 0:4], func=AF.Exp, bias=sm[:, 0:1], accum_out=sm[:, 1:2]),
                      reads=[b_v8, b_sm], writes=[b_sm])
                kb.op("dve", lambda e: e.reciprocal(sm[:, 2:3], sm[:, 1:2]), reads=[b_sm], writes=[b_sm])
                kb.op("dve", lambda e: e.tensor_scalar_mul(self.GATES[:, tt, :], sm[:, 4:8], sm[:, 2:3]), reads=[b_sm], writes=[self.b_route[tt]])
                kb.op("dve", lambda e: e.tensor_scalar(mask[:, :], lg[:, :], v8[:, 3:4], None, ALU.is_ge), reads=[b_lg, b_v8], writes=[b_mask])
                ps, pb = self.bank()
                kb.op("pe", lambda e, ps=ps: e.matmul(ps[:, 0:32], LTb, mask[:, :], start=True, stop=True), reads=[bcb, b_mask], writes=[pb], inc=False)
                kb.op("pe", lambda e, ps=ps: e.matmul(ps[:, 32:64], ONEb, mask[:, :], start=True, stop=True), reads=[bcb, b_mask], writes=[pb])
                kb.op("dve", lambda e, ps=ps: e.tensor_tensor(sc[:, :], ps[:, 0:32], carry[:, :], ALU.add), reads=[pb, b_carry], writes=[b_sc])
                kb.op("dve", lambda e, ps=ps: e.tensor_tensor(carry[:, :], ps[:, 32:64], carry[:, :], ALU.add), reads=[pb, b_carry, b_sc], writes=[b_carry])
                kb.op("dve", lambda e: e.tensor_scalar(ov[:, :], sc[:, :], float(CAP), float(4 * NSLOT), ALU.is_ge, ALU.mult), reads=[b_sc], writes=[b_ov])
                kb.op("dve", lambda e: e.tensor_tensor(sc[:, :], sc[:, :], self.cst[:, 672:704], ALU.add), reads=[b_sc, self.b_cst], writes=[b_sc])
                kb.op("dve", lambda e: e.tensor_tensor(sc[:, :], sc[:, :], ov[:, :], ALU.add), reads=[b_sc, b_ov], writes=[b_sc])
                for k in range(4):
                    kb.op("dve", lambda e, k=k: e.scalar_tensor_tensor(junk[:, :], self.cst[:, 640:672], i8f[:, k:k + 1], sc[:, :], ALU.is_equal, ALU.mult,
                                                                      accum_out=slf[:, k:k + 1]), reads=[self.b_cst, b_i8f, b_sc], writes=[b_junk, b_slf])
                kb.op("dve", lambda e: e.tensor_copy(self.SLOTS[:, tt, :], slf[:, :]), reads=[b_slf], writes=[self.b_route[tt]])
                for k in range(4):
                    kb.dma("pool", lambda e, k=k: e.indirect_dma_start(
                        out=self.Xg, out_offset=bass.IndirectOffsetOnAxis(ap=self.SLOTS[:, tt, k:k + 1], axis=0),
                        in_=hb[:, :], in_offset=None, bounds_check=self.bound_reg, oob_is_err=False),
                        reads=[b_hb, self.b_route[tt], self.b_Xgz], cw=[self.b_Xg])
            self.dump("gates", self.GATES[:].rearrange("p a b -> p (a b)"), self.b_route[15], [128, 64])
            if "slots" in self.dbg:
                sf, b_sf = mk("slots_f", [128, 64])
                kb.op("dve", lambda e: e.tensor_copy(sf[:, :], self.SLOTS[:].rearrange("p a b -> p (a b)")), reads=self.b_route, writes=[b_sf])
                self.dump("slots", sf[:], b_sf, [128, 64])

    def phase_moe(self):
        nc, kb = self.nc, self.kb
        IDb, bcb = self.cstb[:, 0:128], self.b_cstb
        NST = CAP // 128
        with ExitStack() as es:
            self.alloc_psum(es, 6, 2)

            def mk(name, shape, dt=F32):
                return self.sb(es, "M_" + name, shape, dt), Buf("M_" + name)
            WU, WD = self.WU, self.WD
            BD = [mk("bd%d" % i, [128, 1024]) for i in range(2)]
            XG = [mk("xg%d" % i, [128, NST, 1024], BF16) for i in range(2)]
            XGT = [mk("xgt%d" % i, [128, 8, CAP], BF16) for i in range(2)]
            ACTT, b_ACTT = mk("actt", [128, 8, CAP], BF16)
            Gt = [mk("g%d" % i, [128, CAP]) for i in range(2)]
            St = [mk("s%d" % i, [128, CAP]) for i in range(2)]
            Ut = [mk("u%d" % i, [128, CAP]) for i in range(2)]
            Ysb = [mk("y%d" % i, [128, 1024]) for i in range(2)]
            bup, b_bup = mk("bup", [128, 32, 16])
            kb.dma("sp", lambda e: e.dma_start(out=bup[:], in_=self.din["b_up"].rearrange("p (e f) -> p e f", f=16)), writes=[b_bup])

            def load(ex):
                sl = ex % 2
                wu = self.din["w_up"][ex].rearrange("(kc p) f -> p kc f", p=128)
                wd = self.din["w_down"][ex].rearrange("(kc p) f -> p kc f", p=128)
                kb.dma("sp", lambda e: e.dma_start(out=XG[sl][0][:], in_=self.Xg[ex * CAP:(ex + 1) * CAP, :].rearrange("(st p) d -> p st d", p=128)),
                       reads=[self.b_Xg], writes=[XG[sl][1]])
                kb.dma("sp", lambda e: e.dma_start(out=BD[sl][0][:], in_=self.din["b_down"][ex:ex + 1, :].partition_broadcast(128)), writes=[BD[sl][1]])
                if ex >= 2:
                    self.load_expert_w(ex)

            def tposes(ex):
                sl = ex % 2
                xg, b_xg = XG[sl]
                xgt, b_xgt = XGT[sl]
                for st in range(NST):
                    pt, ptb = self.bank16()
                    for kc in range(8):
                        kb.op("pe", lambda e, kc=kc, pt=pt, st=st: e.transpose(pt[:, kc * 128:(kc + 1) * 128], xg[:, st, kc * 128:(kc + 1) * 128], IDb),
                              reads=[b_xg, bcb], writes=[ptb], inc=(kc == 7))
                    kb.op("dve", lambda e, pt=pt, st=st: e.tensor_copy(xgt[:, :, st * 128:(st + 1) * 128], pt[:, :].rearrange("p (k s) -> p k s", s=128)),
                          reads=[ptb], cw=[b_xgt])

            def up(ex):
                sl = ex % 2
                wu, b_wu = WU[sl]
                xgt, b_xgt = XGT[sl]
                for c in range(8):
                    g, b_g = Gt[c % 2]
                    s_, b_s = St[c % 2]
                    u, b_u = Ut[c % 2]
                    ps_g, pb_g = self.bank()
                    for kc in range(8):
                        kb.op("pe", lambda e, kc=kc, ps_g=ps_g, c=c: e.matmul(ps_g[:, 0:CAP], wu[:, kc, c * 128:(c + 1) * 128], xgt[:, kc, :], start=(kc == 0), stop=(kc == 7)),
                              reads=[b_wu, b_xgt], writes=[pb_g], inc=(kc == 7))
                    ps_u, pb_u = self.bank()
                    for kc in range(8):
                        kb.op("pe", lambda e, kc=kc, ps_u=ps_u, c=c: e.matmul(ps_u[:, 0:CAP], wu[:, kc, 1024 + c * 128:1024 + (c + 1) * 128], xgt[:, kc, :],
                                                                           start=(kc == 0), stop=(kc == 7)),
                              reads=[b_wu, b_xgt], writes=[pb_u], inc=(kc == 7))
                    kb.op("dve", lambda e, ps_g=ps_g, c=c: e.tensor_scalar(g[:, :], ps_g[:, 0:CAP], bup[:, ex, c:c + 1], 7.0, ALU.add, ALU.min),
                          reads=[pb_g, b_bup], writes=[b_g])
                    kb.op("act", lambda e: e.activation(out=s_[:, :], in_=g[:, :], func=AF.Sigmoid, scale=1.702), reads=[b_g], writes=[b_s])
                    kb.op("dve", lambda e, ps_u=ps_u, c=c: e.tensor_scalar(u[:, :], ps_u[:, 0:CAP], bup[:, ex, 8 + c:9 + c], 7.0, ALU.add, ALU.min),
                          reads=[pb_u, b_bup], writes=[b_u])
                    kb.op("dve", lambda e: e.tensor_scalar(u[:, :], u[:, :], -7.0, 1.0, ALU.max, ALU.add), reads=[b_u], writes=[b_u])
                    kb.op("dve", lambda e: e.tensor_tensor(g[:, :], g[:, :], s_[:, :], ALU.mult), reads=[b_g, b_s], writes=[b_g])
                    kb.op("dve", lambda e, c=c: e.tensor_tensor(ACTT[:, c, :], g[:, :], u[:, :], ALU.mult), reads=[b_g, b_u], cw=[b_ACTT])

            def down(ex):
                sl = ex % 2
                wd, b_wd = WD[sl]
                bd, b_bd = BD[sl]
                for st in range(NST):
                    y, b_y = Ysb[st % 2]
                    for half in range(2):
                        ps, pb = self.bank()
                        for fc in range(8):
                            kb.op("pe", lambda e, fc=fc, ps=ps, half=half, st=st: e.matmul(ps[:, :], ACTT[:, fc, st * 128:(st + 1) * 128],
                                                                                        wd[:, fc, half * 512:(half + 1) * 512], start=(fc == 0), stop=(fc == 7)),
                                  reads=[b_ACTT, b_wd], writes=[pb], inc=(fc == 7))
                        kb.op("dve", lambda e, ps=ps, half=half: e.tensor_tensor(y[:, half * 512:(half + 1) * 512], ps[:, :], bd[:, half * 512:(half + 1) * 512], ALU.add),
                              reads=[pb, b_bd], cw=[b_y])
                    r0 = ex * CAP + st * 128
                    kb.dma("sp", lambda e, r0=r0: e.dma_start(out=self.Yg[r0:r0 + 128, :], in_=y[:]), reads=[b_y], cw=[self.b_Yg])

            ne = getattr(self, "n_experts", NE)
            load(0)
            if ne > 1:
                load(1)
            tposes(0)
            for ex in range(ne):
                up(ex)
                if ex + 1 < ne:
                    tposes(ex + 1)
                down(ex)
                if ex + 2 < ne:
                    load(ex + 2)

    def phase_final(self):
        nc, kb = self.nc, self.kb
        IDb, bcb = self.cstb[:, 0:128], self.b_cstb
        with ExitStack() as es:
            self.alloc_psum(es, 6, 2)
            self.alloc_ln(es)

            def mk(name, shape, dt=F32):
                return self.sb(es, "F_" + name, shape, dt), Buf("F_" + name)
            Wpg, b_Wpg = mk("Wpg", [128, 8, 1024], BF16)
            for q in range(2):
                kb.dma("pool", lambda e, q=q: e.dma_start(out=Wpg[:, 4 * q:4 * q + 4, :], in_=self.din["w_pg"].rearrange("p (k n) -> p k n", n=1024)[:, 4 * q:4 * q + 4, :]),
                       cw=[b_Wpg])
            Wple, b_Wple = mk("Wple", [128, 2, 1024], BF16)
            kb.dma("pool", lambda e: e.dma_start(out=Wple[:], in_=self.din["w_ple"].rearrange("p (k n) -> p k n", n=1024)), writes=[b_Wple])
            PT, b_PT = mk("PT", [128, 2, T], BF16)
            pT = self.din["pT"].rearrange("(kc p) t -> p kc t", p=128)
            for kc in range(2):
                kb.dma("pool", lambda e, kc=kc: e.dma_start(out=PT[:, kc, :], in_=pT[:, kc, :]), cw=[b_PT])
            YG = [mk("yg%d" % i, [128, 4, 1024]) for i in range(3)]
            H1t = [mk("h1_%d" % i, [128, 1024]) for i in range(3)]
            ACC, b_ACC = mk("acc", [128, 1024])
            H2T, b_H2T = mk("h2T", [128, 8, 128], BF16)
            SGT, b_SGT = mk("sgt", [128, 1024])
            OUT = [mk("out%d" % i, [128, 1024]) for i in range(2)]
            def prefetch(tt):
                tsl = slice(tt * 128, (tt + 1) * 128)
                yg, b_yg = YG[tt % 3]
                h1, b_h1 = H1t[tt % 3]
                kb.op("pool", lambda e: e.memset(yg[:], 0.0), writes=[b_yg])
                for k in range(4):
                    kb.dma("pool", lambda e, k=k: e.indirect_dma_start(
                        out=yg[:, k, :], out_offset=None, in_=self.Yg,
                        in_offset=bass.IndirectOffsetOnAxis(ap=self.SLOTS[:, tt, k:k + 1], axis=0),
                        bounds_check=self.bound_reg, oob_is_err=False), reads=[self.b_Yg, self.b_route[tt]], cw=[b_yg])
                kb.dma("sp", lambda e: e.dma_start(out=h1[:], in_=self.H1d[tsl, :]), reads=[self.b_H1d], writes=[b_h1])

            H2s = [mk("h2_%d" % i, [128, 1024]) for i in range(2)]
            H2bs = [mk("h2b_%d" % i, [128, 1024], BF16) for i in range(2)]

            st = self.ln_st
            PSB = {}

            def a1(tt):
                yg, b_yg = YG[tt % 3]
                h1, b_h1 = H1t[tt % 3]
                kb.op("act", lambda e: e.activation(out=ACC[:, :], in_=h1[:, :], func=AF.Identity, scale=ALPHA), reads=[b_h1], writes=[b_ACC])
                for k in range(4):
                    kb.op("dve", lambda e, k=k: e.scalar_tensor_tensor(ACC[:, :], yg[:, k, :], self.GATES[:, tt, k:k + 1], ACC[:, :], ALU.mult, ALU.add),
                          reads=[b_yg, self.b_route[tt], b_ACC], writes=[b_ACC])
                kb.op("dve", lambda e: e.bn_stats(st["stats"][:, 0, :], ACC[:, 0:512]), reads=[b_ACC], writes=[st["b"]])
                kb.op("dve", lambda e: e.bn_stats(st["stats"][:, 1, :], ACC[:, 512:1024]), reads=[b_ACC], writes=[st["b"]])
                kb.op("dve", lambda e: e.bn_aggr(st["mv"][:, :], st["stats"][:, :, :].rearrange("p a b -> p (a b)")), reads=[st["b"]], writes=[st["b"]])
                kb.op("act", lambda e: e.activation(out=st["rs"][:, 0:1], in_=st["mv"][:, 1:2], func=AF.Ln, bias=1e-5), reads=[st["b"]], writes=[st["b2"]])
                kb.op("act", lambda e: e.activation(out=st["rs"][:, 0:1], in_=st["rs"][:, 0:1], func=AF.Exp, scale=-0.5), reads=[st["b2"]], writes=[st["b2"]])

            def a2(tt):
                H2, b_H2 = H2s[tt % 2]
                kb.op("dve", lambda e: e.scalar_tensor_tensor(st["rs"][:, 1:2], st["mv"][:, 0:1], -1.0, st["rs"][:, 0:1], ALU.mult, ALU.mult),
                      reads=[st["b"], st["b2"]], writes=[st["b2"]])
                kb.op("act", lambda e: e.activation(out=H2[:, :], in_=ACC[:, :], func=AF.Identity, scale=st["rs"][:, 0:1], bias=st["rs"][:, 1:2]),
                      reads=[b_ACC, st["b2"]], writes=[b_H2])

            def a3(tt):
                H2, b_H2 = H2s[tt % 2]
                H2b, b_H2b = H2bs[tt % 2]
                kb.op("dve", lambda e: e.tensor_tensor(H2[:, :], H2[:, :], self.rowp[:, 2, :], ALU.mult), reads=[b_H2, self.b_rowp], writes=[b_H2])
                kb.op("dve", lambda e: e.tensor_tensor(H2[:, :], H2[:, :], self.rowp[:, 3, :], ALU.add), reads=[b_H2, self.b_rowp], writes=[b_H2])
                if tt == 0:
                    self.dump("h2_0", H2[:], b_H2, [128, 1024])
                kb.op("act", lambda e: e.activation(out=H2b[:, :], in_=H2[:, :], func=AF.Identity), reads=[b_H2], writes=[b_H2b])

            def b1(tt):
                tsl = slice(tt * 128, (tt + 1) * 128)
                H2b, b_H2b = H2bs[tt % 2]
                pt, ptb = self.bank16()
                for kc in range(8):
                    kb.op("pe", lambda e, kc=kc, pt=pt: e.transpose(pt[:, kc * 128:(kc + 1) * 128], H2b[:, kc * 128:(kc + 1) * 128], IDb),
                          reads=[b_H2b, bcb], writes=[ptb], inc=(kc == 7))
                kb.op("dve", lambda e, pt=pt: e.tensor_copy(H2T[:, :, :], pt[:, :].rearrange("p (k t) -> p k t", t=128)), reads=[ptb], writes=[b_H2T])
                PSB[tt] = []
                for half in range(2):
                    hs = slice(half * 512, (half + 1) * 512)
                    ps, pb = self.bank()
                    for kc in range(8):
                        kb.op("pe", lambda e, kc=kc, ps=ps, hs=hs: e.matmul(ps[:, :], H2T[:, kc, :], Wpg[:, kc, hs], start=(kc == 0), stop=(kc == 7)),
                              reads=[b_H2T, b_Wpg], writes=[pb], inc=(kc == 7))
                    ps2, pb2 = self.bank()
                    for kc in range(2):
                        kb.op("pe", lambda e, kc=kc, ps2=ps2, hs=hs: e.matmul(ps2[:, :], PT[:, kc, tsl], Wple[:, kc, hs], start=(kc == 0), stop=(kc == 1)),
                              reads=[b_PT, b_Wple], writes=[pb2], inc=(kc == 1))
                    PSB[tt].append((hs, ps, pb, ps2, pb2))

            def b2(tt):
                for hs, ps, pb, ps2, pb2 in PSB[tt]:
                    kb.op("dve", lambda e, ps=ps, hs=hs: e.tensor_tensor(SGT[:, hs], ps[:, :], self.rowp[:, 4, hs], ALU.add), reads=[pb, self.b_rowp], cw=[b_SGT])
                    kb.op("act", lambda e, hs=hs: e.activation(out=SGT[:, hs], in_=SGT[:, hs], func=AF.Sigmoid), reads=[b_SGT], cw=[b_SGT])

            def b3(tt):
                tsl = slice(tt * 128, (tt + 1) * 128)
                H2, b_H2 = H2s[tt % 2]
                o, b_o = OUT[tt % 2]
                for hs, ps, pb, ps2, pb2 in PSB.pop(tt):
                    kb.op("dve", lambda e, ps2=ps2, hs=hs: e.tensor_tensor(o[:, hs], SGT[:, hs], ps2[:, :], ALU.mult), reads=[b_SGT, pb2], cw=[b_o])
                    kb.op("dve", lambda e, hs=hs: e.tensor_tensor(o[:, hs], o[:, hs], H2[:, hs], ALU.add), reads=[b_o, b_H2], cw=[b_o])
                kb.dma("sp", lambda e: e.dma_start(out=self.out[tsl, :], in_=o[:]), reads=[b_o])

            prefetch(0)
            prefetch(1)
            a1(0)
            a2(0)
            a3(0)
            for tt in range(16):
                if tt + 2 < 16:
                    prefetch(tt + 2)
                nxt = tt + 1 < 16
                if nxt:
                    a1(tt + 1)
                b1(tt)
                if nxt:
                    a2(tt + 1)
                b2(tt)
                if nxt:
                    a3(tt + 1)
                b3(tt)


_PROG_CACHE = {}


def kernel(**inputs):
    inp = {k: np.asarray(v) for k, v in inputs.items()}
    sh = prep_shared(inp)
    in_maps = [dict(sh, **prep_core(inp, b)) for b in range(8)]
    if "nc" not in _PROG_CACHE:
        _PROG_CACHE["nc"] = Prog().build()
    nc = _PROG_CACHE["nc"]
    res = run_bass_kernel_spmd(nc, in_maps, core_ids=list(range(8)))
    out = np.stack([np.asarray(r["out"], dtype=np.float32) for r in res.results], axis=0)
    return out
```
